# Optimizing a Trainium2 kernel written in Bass

```python
import jax, jax.numpy as jnp
from jax import lax
import numpy as np

D_MODEL = 2048
BATCH = 4
SEQ = 4096
DEPTH = 2

FOURIER_GROUPS = 4
FOURIER_GROUP_DIM = 256
FOURIER_WIDTH = FOURIER_GROUPS * FOURIER_GROUP_DIM
DN_HEADS = 16
DN_HEAD_DIM = 128
DN_WIDTH = DN_HEADS * DN_HEAD_DIM
CONV_K = 5
CHUNK = 64
D_FF_DENSE = 5632
N_EXPERTS = 8
TOP_K = 2
D_FF_EXPERT = 7168
PLE_DIM = 256
DEEPNORM_ALPHA = (2 * DEPTH) ** 0.25
DEEPNORM_BETA = (8 * DEPTH) ** -0.25
LN_EPS = 1e-5
RMS_EPS = 1e-6
L2_EPS = 1e-6
N_DENSE = (DEPTH + 1) // 2
N_MOE = DEPTH // 2
IN_SPLITS = (FOURIER_WIDTH, 3 * DN_WIDTH, DN_WIDTH, 2 * DN_HEADS, 2 * DN_HEADS, D_MODEL, D_MODEL)
IN_WIDTH = sum(IN_SPLITS)
IN_SPLIT_IDX = tuple(sum(IN_SPLITS[:j]) for j in range(1, len(IN_SPLITS)))

kernel_name = "hybrid_fnet_gdn_moe_deepnorm_encoder"


def layer_norm(x, g, b):
    xf = x.astype(jnp.float32)
    mu = jnp.mean(xf, -1, keepdims=True)
    var = jnp.mean(jnp.square(xf - mu), -1, keepdims=True)
    return ((xf - mu) * lax.rsqrt(var + LN_EPS) * g.astype(jnp.float32) + b.astype(jnp.float32)).astype(x.dtype)


def l2norm(t):
    return t * lax.rsqrt(jnp.sum(t * t, -1, keepdims=True) + L2_EPS)


def centred_dwconv(u, w):
    pad = (CONV_K - 1) // 2
    return lax.conv_general_dilated(
        u, w[:, None, :].astype(u.dtype), window_strides=(1,), padding=((pad, pad),),
        dimension_numbers=("NWC", "WIO", "NWC"), feature_group_count=u.shape[-1])


def fourier_mix(u):
    b, s, _ = u.shape
    ug = u.astype(jnp.float32).reshape(b, s, FOURIER_GROUPS, FOURIER_GROUP_DIM)
    f = jnp.fft.fft2(ug, axes=(1, 3), norm="ortho")
    return jnp.real(f).reshape(b, s, FOURIER_WIDTH).astype(u.dtype)


def gated_delta_chunked(q, k, v, g, beta):
    b, h, s, dk = q.shape
    dv = v.shape[-1]
    n = s // CHUNK
    qc = q.reshape(b, h, n, CHUNK, dk)
    kc = k.reshape(b, h, n, CHUNK, dk)
    vc = v.reshape(b, h, n, CHUNK, dv)
    bc = beta.reshape(b, h, n, CHUNK)
    gcum = jnp.cumsum(g.reshape(b, h, n, CHUNK), -1)
    causal = jnp.tril(jnp.ones((CHUNK, CHUNK), bool))
    strict = jnp.tril(jnp.ones((CHUNK, CHUNK), bool), -1)
    decay = jnp.exp(jnp.where(causal, gcum[..., :, None] - gcum[..., None, :], -jnp.inf))
    kk = jnp.einsum("bhnid,bhnjd->bhnij", kc, kc)
    a_mat = jnp.where(strict, bc[..., :, None] * kk * decay, 0.0) + jnp.eye(CHUNK, dtype=jnp.float32)
    rhs = jnp.concatenate([vc * bc[..., None], kc * (bc * jnp.exp(gcum))[..., None]], -1)
    sol = lax.linalg.triangular_solve(a_mat, rhs, left_side=True, lower=True, unit_diagonal=True)
    u_c, w_c = sol[..., :dv], sol[..., dv:]
    qk = jnp.where(causal, jnp.einsum("bhnid,bhnjd->bhnij", qc, kc) * decay, 0.0)
    q_dec = qc * jnp.exp(gcum)[..., None]
    k_dec = kc * jnp.exp(gcum[..., -1:] - gcum)[..., None]
    g_last = jnp.exp(gcum[..., -1])
    xs = tuple(jnp.moveaxis(t, 2, 0) for t in (u_c, w_c, qk, q_dec, k_dec, g_last))

    def step(state, inp):
        u_i, w_i, qk_i, qd_i, kd_i, gl_i = inp
        v_new = u_i - jnp.einsum("bhck,bhkv->bhcv", w_i, state)
        o_i = jnp.einsum("bhck,bhkv->bhcv", qd_i, state) + jnp.einsum("bhij,bhjv->bhiv", qk_i, v_new)
        state = state * gl_i[..., None, None] + jnp.einsum("bhck,bhcv->bhkv", kd_i, v_new)
        return state, o_i

    s0 = jnp.zeros((b, h, dk, dv), jnp.float32)
    _, o = lax.scan(step, s0, xs)
    return jnp.moveaxis(o, 0, 2).reshape(b, h, s, dv)


def deltanet_branch(qkv, z, beta_raw, a_raw, conv_w, a_log, dt_bias, o_norm_w):
    b, s, _ = z.shape
    qkv = jax.nn.silu(centred_dwconv(qkv, conv_w))
    q, k, v = jnp.split(qkv, 3, axis=-1)

    def heads(t):
        return t.reshape(b, s, DN_HEADS, DN_HEAD_DIM).transpose(0, 2, 1, 3).astype(jnp.float32)

    q = l2norm(heads(q)) * (DN_HEAD_DIM ** -0.5)
    k = l2norm(heads(k))
    v = heads(v)
    beta = jax.nn.sigmoid(beta_raw.astype(jnp.float32)).reshape(b, s, 2, DN_HEADS).transpose(2, 0, 3, 1)
    a_in = a_raw.astype(jnp.float32).reshape(b, s, 2, DN_HEADS).transpose(2, 0, 3, 1)
    g = -jnp.exp(a_log.astype(jnp.float32))[:, None, :, None] * jax.nn.softplus(
        a_in + dt_bias.astype(jnp.float32)[:, None, :, None])
    o_fwd = gated_delta_chunked(q, k, v, g[0], beta[0])
    rev = lambda t: jnp.flip(t, axis=2)
    o_bwd = rev(gated_delta_chunked(rev(q), rev(k), rev(v), rev(g[1]), rev(beta[1])))
    o = (o_fwd + o_bwd).transpose(0, 2, 1, 3)
    zh = z.astype(jnp.float32).reshape(b, s, DN_HEADS, DN_HEAD_DIM)
    o = o * lax.rsqrt(jnp.mean(o * o, -1, keepdims=True) + RMS_EPS) * o_norm_w.astype(jnp.float32) * jax.nn.silu(zh)
    return o.reshape(b, s, DN_WIDTH).astype(z.dtype)


def swiglu(t, w_gate_up, w_down):
    gu = t @ w_gate_up
    gate, up = jnp.split(gu, 2, axis=-1)
    return (jax.nn.silu(gate) * up) @ w_down


def moe_swiglu(x, router_w, e_gate_up, e_down):
    b, s, d = x.shape
    t = x.reshape(b * s, d)
    logits = (t @ router_w).astype(jnp.float32)
    top_v, top_i = lax.top_k(logits, TOP_K)
    top_w = jax.nn.softmax(top_v, axis=-1)
    combine = jnp.sum(jax.nn.one_hot(top_i, N_EXPERTS, dtype=jnp.float32) * top_w[..., None], axis=1)
    out = jnp.zeros_like(t)
    for e in range(N_EXPERTS):
        out = out + combine[:, e:e + 1].astype(t.dtype) * swiglu(t, e_gate_up[e], e_down[e])
    return out.reshape(b, s, d)


def setup_inputs(seed: int = 0) -> dict:
    key = jax.random.key(seed)
    ks = jax.random.split(key, 26)
    f32 = jnp.float32
    nrm = lambda k, shape, scale: jax.random.normal(k, shape, f32) * scale
    dt = jnp.exp(jax.random.uniform(ks[5], (DEPTH, 2, DN_HEADS), f32, np.log(1e-3), np.log(1e-1)))
    return {
        "x": nrm(ks[0], (BATCH, SEQ, D_MODEL), 1.0),
        "p": nrm(ks[1], (DEPTH, BATCH, SEQ, PLE_DIM), 1.0),
        "emb_ln_g": 1.0 + nrm(ks[2], (D_MODEL,), 0.01),
        "emb_ln_b": nrm(ks[3], (D_MODEL,), 0.01),
        "w_in": nrm(ks[4], (DEPTH, D_MODEL, IN_WIDTH), D_MODEL ** -0.5),
        "conv_w": nrm(ks[6], (DEPTH, CONV_K, 3 * DN_WIDTH), CONV_K ** -0.5),
        "a_log": jnp.log(jax.random.uniform(ks[7], (DEPTH, 2, DN_HEADS), f32, 1.0, 16.0)),
        "dt_bias": dt + jnp.log(-jnp.expm1(-dt)),
        "o_norm_w": 1.0 + nrm(ks[8], (DEPTH, DN_HEAD_DIM), 0.01),
        "w_fourier": nrm(ks[9], (DEPTH, FOURIER_WIDTH, D_MODEL), DEEPNORM_BETA * FOURIER_WIDTH ** -0.5),
        "w_delta": nrm(ks[10], (DEPTH, DN_WIDTH, D_MODEL), DEEPNORM_BETA * DN_WIDTH ** -0.5),
        "w_out": nrm(ks[11], (DEPTH, D_MODEL, D_MODEL), DEEPNORM_BETA * D_MODEL ** -0.5),
        "ln1_g": 1.0 + nrm(ks[12], (DEPTH, D_MODEL), 0.01),
        "ln1_b": nrm(ks[13], (DEPTH, D_MODEL), 0.01),
        "ffn_gate_up": nrm(ks[14], (N_DENSE, D_MODEL, 2 * D_FF_DENSE), D_MODEL ** -0.5),
        "ffn_down": nrm(ks[15], (N_DENSE, D_FF_DENSE, D_MODEL), DEEPNORM_BETA * D_FF_DENSE ** -0.5),
        "router_w": nrm(ks[16], (N_MOE, D_MODEL, N_EXPERTS), D_MODEL ** -0.5),
        "exp_gate_up": nrm(ks[17], (N_MOE, N_EXPERTS, D_MODEL, 2 * D_FF_EXPERT), D_MODEL ** -0.5),
        "exp_down": nrm(ks[18], (N_MOE, N_EXPERTS, D_FF_EXPERT, D_MODEL), DEEPNORM_BETA * D_FF_EXPERT ** -0.5),
        "ple_gate": nrm(ks[19], (DEPTH, D_MODEL, D_MODEL), D_MODEL ** -0.5),
        "ple_proj": nrm(ks[20], (DEPTH, PLE_DIM, D_MODEL), DEEPNORM_BETA * PLE_DIM ** -0.5),
        "ln2_g": 1.0 + nrm(ks[21], (DEPTH, D_MODEL), 0.01),
        "ln2_b": nrm(ks[22], (DEPTH, D_MODEL), 0.01),
    }


def reference(x, p, emb_ln_g, emb_ln_b, w_in, conv_w, a_log, dt_bias, o_norm_w, w_fourier, w_delta,
              w_out, ln1_g, ln1_b, ffn_gate_up, ffn_down, router_w, exp_gate_up, exp_down,
              ple_gate, ple_proj, ln2_g, ln2_b):
    x = layer_norm(x, emb_ln_g, emb_ln_b)
    for i in range(DEPTH):
        proj = x @ w_in[i]
        u_f, qkv, z, beta_raw, a_raw, gate_f, gate_d = jnp.split(proj, IN_SPLIT_IDX, axis=-1)
        y_f = fourier_mix(u_f) @ w_fourier[i]
        y_d = deltanet_branch(qkv, z, beta_raw, a_raw, conv_w[i], a_log[i], dt_bias[i], o_norm_w[i]) @ w_delta[i]
        merged = jax.nn.sigmoid(gate_f) * y_f + jax.nn.sigmoid(gate_d) * y_d
        mix = merged @ w_out[i]
        x = layer_norm(DEEPNORM_ALPHA * x + mix, ln1_g[i], ln1_b[i])
        if i % 2 == 0:
            ff = swiglu(x, ffn_gate_up[i // 2], ffn_down[i // 2])
        else:
            ff = moe_swiglu(x, router_w[i // 2], exp_gate_up[i // 2], exp_down[i // 2])
        ple = jax.nn.sigmoid(x @ ple_gate[i]) * (p[i] @ ple_proj[i])
        x = layer_norm(DEEPNORM_ALPHA * x + ff + ple, ln2_g[i], ln2_b[i])
    return x
```

```python
import contextlib
import numpy as np
import concourse.bass as bass
import concourse.mybir as mybir
from concourse.bass_utils import run_bass_kernel_spmd

F32 = mybir.dt.float32
BF16 = mybir.dt.bfloat16
AF = mybir.ActivationFunctionType
ALU = mybir.AluOpType

D = 2048
KT = 16
NCORES = 8
IN_W = 13376
LN_EPS = 1e-5


class Stream:
    def __init__(self, key, sem):
        self.key = key
        self.sem = sem
        self.cnt = 0


class Sched:
    def __init__(self, nc, stack, same_sync=True):
        self.nc = nc
        self.stack = stack
        self.same_sync = same_sync
        self.eng = {"pe": nc.tensor, "act": nc.scalar, "dve": nc.vector, "pool": nc.gpsimd, "sp": nc.sync}
        self.semh = {}
        self.cnt = {}
        self.waited = {}
        for e in self.eng:
            self.semh[e] = stack.enter_context(nc.semaphore("sem_" + e))
            self.cnt[e] = 0
            self.waited[e] = {}
        self.keys = {}
        self.excl = set()
        self.streams = []
        self.nstream = 0

    def stream(self, name=None):
        self.nstream += 1
        key = "dma%d_%s" % (self.nstream, name or "")
        sem = self.stack.enter_context(self.nc.semaphore(key))
        st = Stream(key, sem)
        self.semh[key] = sem
        self.streams.append(st)
        return st

    def _deps(self, e, reads, writes):
        deps = {}

        def add(ev):
            if ev is None:
                return
            k, v, pe = ev
            if pe == e and not self.same_sync:
                return
            if deps.get(k, 0) < v:
                deps[k] = v

        for k in reads:
            st = self.keys.get(k)
            if st:
                add(st["w"])
                if k in self.excl:
                    for ev in st["r"].values():
                        if ev[2] != e:
                            add(ev)
        for k in writes:
            st = self.keys.get(k)
            if st:
                add(st["w"])
                for ev in st["r"].values():
                    add(ev)
        for k, v in deps.items():
            if self.waited[e].get(k, 0) >= v:
                continue
            self.eng[e].wait_ge(self.semh[k], v)
            self.waited[e][k] = v

    def _record(self, ev, reads, writes):
        for k in reads:
            st = self.keys.setdefault(k, {"w": None, "r": {}})
            st["r"][ev[0]] = ev
        for k in writes:
            self.keys[k] = {"w": ev, "r": {}}

    def op(self, e, fn, reads=(), writes=()):
        self._deps(e, reads, writes)
        ins = fn(self.eng[e])
        self.cnt[e] += 1
        ins.then_inc(self.semh[e], 1)
        self._record((e, self.cnt[e], e), reads, writes)

    def group(self, e, fns, reads=(), writes=()):
        self._deps(e, reads, writes)
        ins = None
        for fn in fns:
            ins = fn(self.eng[e])
        self.cnt[e] += 1
        ins.then_inc(self.semh[e], 1)
        self._record((e, self.cnt[e], e), reads, writes)

    def dma(self, q, stream, out, in_, reads=(), writes=(), **kw):
        self._deps(q, reads, writes)
        ins = self.eng[q].dma_start(out=out, in_=in_, **kw)
        stream.cnt += 16
        ins.then_inc(stream.sem, 16)
        self._record((stream.key, stream.cnt, None), reads, writes)

    def merge_into(self, dst, srcs, reset=False):
        if reset or dst not in self.keys:
            self.keys[dst] = {"w": None, "r": {}}
        d = self.keys[dst]
        for k in srcs:
            st = self.keys.get(k)
            if not st:
                continue
            for ev in [st["w"]] + list(st["r"].values()):
                if ev is None:
                    continue
                cur = d["r"].get(ev[0])
                if cur is None or cur[1] < ev[1]:
                    d["r"][ev[0]] = ev

    def finish(self, q="sp"):
        for st in self.streams:
            if st.cnt and self.waited[q].get(st.key, 0) < st.cnt:
                self.eng[q].wait_ge(st.sem, st.cnt)
                self.waited[q][st.key] = st.cnt


class Ctx:
    def __init__(self):
        self.nc = bass.Bass("TRN2", target_bir_lowering=False)
        self.stack = contextlib.ExitStack()
        self.s = Sched(self.nc, self.stack)
        self.nps = 0

    def sb(self, name, shape, dt):
        return self.stack.enter_context(self.nc.sbuf_tensor("s_" + name, shape, dt))

    def ps(self, name, shape=(128, 512), dt=F32):
        return self.stack.enter_context(self.nc.psum_tensor(name, list(shape), dt))

    def din(self, name, shape, dt=F32):
        return self.nc.dram_tensor(name, list(shape), dt, kind="ExternalInput").ap()

    def dout(self, name, shape, dt=F32):
        return self.nc.dram_tensor(name, list(shape), dt, kind="ExternalOutput").ap()

    def close(self):
        self.s.finish("sp")
        self.stack.close()
        return self.nc


class PsumRing:
    def __init__(self, c, n, prefix="ps"):
        self.t = [c.ps("%s%d" % (prefix, i)) for i in range(n)]
        self.keys = ["%s%d" % (prefix, i) for i in range(n)]
        c.s.excl.update(self.keys)
        self.i = 0

    def next(self):
        i = self.i % len(self.t)
        self.i += 1
        return self.t[i], self.keys[i]


def load_small(c, q, stream, name, dram_ap, shape, dt=F32):
    t = c.sb(name, list(shape), dt)
    c.s.dma(q, stream, t[:], dram_ap, writes=[name])
    return t


def gemm_fm(c, W, n0, n1, xTb, xkey, kt_n, TT, wslots, wstreams, psr, epilogue, chunk=512, wq="pool"):
    s = c.s
    Wv = W.rearrange("(kt p) n -> p kt n", p=128)
    chunks = []
    a = n0
    while a < n1:
        cw = min(chunk, n1 - a)
        chunks.append((a, cw))
        a += cw
    nsl = len(wslots)

    def issue(ci):
        a, cw = chunks[ci]
        sl = ci % nsl
        s.dma(wq, wstreams[sl], wslots[sl][0][:, :kt_n, :cw], Wv[:, :, a:a + cw], writes=[wslots[sl][1]])

    for ci in range(min(nsl, len(chunks))):
        issue(ci)
    for ci, (a, cw) in enumerate(chunks):
        sl = ci % nsl
        wt, wkey = wslots[sl]
        j = 0
        while j < cw:
            rows = min(128, cw - j)
            for tb in range(TT // 512):
                pt, pkey = psr.next()
                fns = []
                for kt in range(kt_n):
                    fns.append(lambda pe, kt=kt, pt=pt, j=j, rows=rows, tb=tb, wt=wt: pe.matmul(
                        pt[:rows, :], lhsT=wt[:, kt, j:j + rows], rhs=xTb[:, kt, tb * 512:(tb + 1) * 512],
                        start=(kt == 0), stop=(kt == kt_n - 1)))
                s.group("pe", fns, reads=[wkey, xkey], writes=[pkey])
                epilogue(a + j, rows, tb, pt, pkey)
            j += rows
        if ci + nsl < len(chunks):
            issue(ci + nsl)


def ln_block(c, srcf, skey, kt_n, gT, bT, ones, psr, scr, out_bf=None, out_f32=None, okeys=(None, None), eps=LN_EPS):
    s = c.s
    sq, mean, rstd, tmp = scr
    p1, k1 = psr.next()
    p2, k2 = psr.next()
    s.group("pe", [lambda pe, kt=kt: pe.matmul(p1[:], lhsT=ones[:], rhs=srcf(kt), start=(kt == 0),
                                               stop=(kt == kt_n - 1)) for kt in range(kt_n)],
            reads=[skey, "ones"], writes=[k1])
    for kt in range(kt_n):
        q = sq[kt % 2]
        qk = "lnsq%d" % (kt % 2)
        s.op("act", lambda a, kt=kt, q=q: a.activation(out=q[:], in_=srcf(kt), func=AF.Square),
             reads=[skey], writes=[qk])
        s.op("pe", lambda pe, kt=kt, q=q: pe.matmul(p2[:], lhsT=ones[:], rhs=q[:], start=(kt == 0),
                                                    stop=(kt == kt_n - 1)),
             reads=[qk, "ones"], writes=[k2])
    s.op("act", lambda a: a.activation(out=mean[:], in_=p1[:], func=AF.Copy), reads=[k1], writes=["lnmean"])
    s.op("dve", lambda v: v.tensor_tensor(out=tmp[:], in0=mean[:], in1=mean[:], op=ALU.mult),
         reads=["lnmean"], writes=["lntmp"])
    s.op("dve", lambda v: v.tensor_tensor(out=rstd[:], in0=p2[:], in1=tmp[:], op=ALU.subtract),
         reads=[k2, "lntmp"], writes=["lnrstd"])
    s.op("dve", lambda v: v.tensor_scalar(out=rstd[:], in0=rstd[:], scalar1=float(eps), scalar2=None,
                                          op0=ALU.add), reads=["lnrstd"], writes=["lnrstd"])
    s.op("act", lambda a: a.activation(out=rstd[:], in_=rstd[:], func=AF.Ln), reads=["lnrstd"], writes=["lnrstd"])
    s.op("act", lambda a: a.activation(out=rstd[:], in_=rstd[:], func=AF.Exp, scale=-0.5),
         reads=["lnrstd"], writes=["lnrstd"])
    for kt in range(kt_n):
        s.op("dve", lambda v, kt=kt: v.tensor_tensor(out=tmp[:], in0=srcf(kt), in1=mean[:], op=ALU.subtract),
             reads=[skey, "lnmean"], writes=["lntmp"])
        s.op("dve", lambda v: v.tensor_tensor(out=tmp[:], in0=tmp[:], in1=rstd[:], op=ALU.mult),
             reads=["lntmp", "lnrstd"], writes=["lntmp"])
        if out_f32 is not None:
            s.op("act", lambda a, kt=kt: a.activation(out=out_f32(kt), in_=tmp[:], func=AF.Identity,
                                                      bias=bT[:, kt:kt + 1], scale=gT[:, kt:kt + 1]),
                 reads=["lntmp", "lngb"], writes=[okeys[1]])
        if out_bf is not None:
            s.op("act", lambda a, kt=kt: a.activation(out=out_bf(kt), in_=tmp[:], func=AF.Identity,
                                                      bias=bT[:, kt:kt + 1], scale=gT[:, kt:kt + 1]),
                 reads=["lntmp", "lngb"], writes=[okeys[0]])


def ln_scratch(c):
    return ([c.sb("lnsq0", [128, 512], F32), c.sb("lnsq1", [128, 512], F32)], c.sb("lnmean", [128, 512], F32),
            c.sb("lnrstd", [128, 512], F32), c.sb("lntmp", [128, 512], F32))


def build_k1(TT, do_ln):
    c = Ctx()
    nc, s = c.nc, c.s
    xT_d = c.din("xT", [D, TT])
    w_d = c.din("w", [D, IN_W])
    g_d = c.din("g", [128, KT])
    b_d = c.din("b", [128, KT])
    out_d = c.dout("projT", [IN_W, TT])
    xn_d = c.dout("xnT", [D, TT]) if do_ln else None

    st_misc = s.stream("misc")
    st_x = s.stream("x")
    st_xn = s.stream("xn")
    xf = c.sb("xf", [128, KT, 512], F32)
    xb = c.sb("xb", [128, KT, TT], BF16)
    gT = load_small(c, "sp", st_misc, "gT", g_d, [128, KT])
    bT = load_small(c, "sp", st_misc, "bT", b_d, [128, KT])
    s.keys["lngb"] = {"w": (st_misc.key, st_misc.cnt, None), "r": {}}
    ones = c.sb("ones", [128, 128], F32)
    s.op("dve", lambda v: v.memset(ones[:], 1.0 / D), writes=["ones"])
    psr = PsumRing(c, 8)
    scr = ln_scratch(c) if do_ln else None
    xTv = xT_d.rearrange("(kt p) t -> p kt t", p=128)
    for tb in range(TT // 512):
        sl = slice(tb * 512, (tb + 1) * 512)
        s.dma("sp", st_x, xf[:], xTv[:, :, sl], writes=["xf"])
        if do_ln:
            ln_block(c, lambda kt: xf[:, kt, :], "xf", KT, gT, bT, ones, psr, scr,
                     out_bf=lambda kt, sl=sl: xb[:, kt, sl], out_f32=lambda kt: xf[:, kt, :], okeys=("xb", "xf"))
            s.dma("sp", st_xn, xn_d.rearrange("(kt p) t -> p kt t", p=128)[:, :, sl], xf[:], reads=["xf"])
        else:
            for kt in range(KT):
                s.op("dve", lambda v, kt=kt, sl=sl: v.tensor_copy(out=xb[:, kt, sl], in_=xf[:, kt, :]),
                     reads=["xf"], writes=["xb"])

    NSL = 3
    wslots = [(c.sb("w%d" % i, [128, KT, 512], BF16), "w%d" % i) for i in range(NSL)]
    wstreams = [s.stream("w%d" % i) for i in range(NSL)]
    NST = 2
    stg = [c.sb("stg%d" % i, [128, TT], F32) for i in range(NST)]
    ststreams = [s.stream("st%d" % i) for i in range(NST)]
    state = {"n": 0}
    ntb = TT // 512

    def epi(row0, rows, tb, pt, pkey):
        i = state["n"] % NST
        sk = "stg%d" % i
        s.op("act", lambda a: a.activation(out=stg[i][:rows, tb * 512:(tb + 1) * 512], in_=pt[:rows, :], func=AF.Copy),
             reads=[pkey], writes=[sk])
        if tb == ntb - 1:
            s.dma("sp", ststreams[i], out_d[row0:row0 + rows, :], stg[i][:rows, :], reads=[sk])
            state["n"] += 1

    gemm_fm(c, w_d, 0, IN_W, xb, "xb", KT, TT, wslots, wstreams, psr, epi)
    return c.close()


def _vecT(v):
    return np.ascontiguousarray(v.reshape(-1, 128).T)


def run_k1(xT_cores, w, g, b, do_ln):
    TT = xT_cores[0].shape[1]
    nc = build_k1(TT, do_ln)
    in_maps = [{"xT": xT_cores[i], "w": w, "g": _vecT(g), "b": _vecT(b)} for i in range(NCORES)]
    res = run_bass_kernel_spmd(nc, in_maps, core_ids=list(range(NCORES)))
    return [r["projT"] for r in res.results], ([r["xnT"] for r in res.results] if do_ln else None)


S_LEN = 4096
FG = 256


def build_k2(NP=2, SBLK=256):
    c = Ctx()
    nc, s = c.nc, c.s
    u_d = c.din("uT", [NP, FG, S_LEN])
    cc_d = c.din("ccsc", [FG, 2 * FG], BF16)
    cs_d = c.din("csm", [S_LEN, S_LEN], BF16)
    ns_d = c.din("nsm", [S_LEN, S_LEN], BF16)
    out_d = c.dout("aT", [NP, FG, S_LEN])
    NST = S_LEN // 128
    st_misc = s.stream("misc")
    ccsc = c.sb("ccsc", [128, 2, 2 * FG], BF16)
    s.dma("sp", st_misc, ccsc[:], cc_d.rearrange("(ct p) n -> p ct n", p=128), writes=["ccsc"])
    psr = PsumRing(c, 8)
    ub = [c.sb("ub%d" % i, [128, 2, S_LEN], BF16) for i in range(NP)]
    pq = [c.sb("pq%d" % i, [128, NST, 2 * FG], BF16) for i in range(NP)]
    for pi in range(NP):
        s.dma("pool", st_misc, ub[pi][:], u_d[pi].rearrange("(ct p) t -> p ct t", p=128), writes=["ub%d" % pi])
        for st in range(NST):
            pt, pk = psr.next()
            s.group("pe", [lambda pe, ct=ct, pt=pt, st=st, pi=pi: pe.matmul(
                pt[:], lhsT=ub[pi][:, ct, st * 128:(st + 1) * 128], rhs=ccsc[:, ct, :], start=(ct == 0), stop=(ct == 1))
                for ct in range(2)], reads=["ub%d" % pi, "ccsc"], writes=[pk])
            eng = "act" if st % 2 == 0 else "dve"
            if eng == "act":
                s.op("act", lambda a, pt=pt, st=st, pi=pi: a.activation(out=pq[pi][:, st, :], in_=pt[:], func=AF.Copy),
                     reads=[pk], writes=["pq%d" % pi])
            else:
                s.op("dve", lambda v, pt=pt, st=st, pi=pi: v.tensor_copy(out=pq[pi][:, st, :], in_=pt[:]),
                     reads=[pk], writes=["pq%d" % pi])
    NSL = 2
    csl = [(c.sb("cs%d" % i, [128, NST, SBLK], BF16), c.sb("ns%d" % i, [128, NST, SBLK], BF16)) for i in range(NSL)]
    cstr = [s.stream("cs%d" % i) for i in range(NSL)]
    nblk = S_LEN // SBLK
    csv = cs_d.rearrange("(st p) s -> p st s", p=128)
    nsv = ns_d.rearrange("(st p) s -> p st s", p=128)

    def issue(bi):
        sl = bi % NSL
        s.dma("sp", cstr[sl], csl[sl][0][:], csv[:, :, bi * SBLK:(bi + 1) * SBLK], writes=["csl%d" % sl])
        s.dma("sp", cstr[sl], csl[sl][1][:], nsv[:, :, bi * SBLK:(bi + 1) * SBLK], writes=["csl%d" % sl])

    NSTG = 2
    stg = [c.sb("stg%d" % i, [128, SBLK], F32) for i in range(NSTG)]
    sstr = [s.stream("st%d" % i) for i in range(NSTG)]
    n = 0
    for bi in range(min(NSL, nblk)):
        issue(bi)
    for bi in range(nblk):
        sl = bi % NSL
        for pi in range(NP):
            for ct in range(2):
                pt, pk = psr.next()
                fns = []
                for half in range(2):
                    for st in range(NST):
                        fns.append(lambda pe, half=half, st=st, pt=pt, pi=pi, ct=ct, sl=sl: pe.matmul(
                            pt[:, :SBLK], lhsT=pq[pi][:, st, half * FG + ct * 128: half * FG + (ct + 1) * 128],
                            rhs=csl[sl][half][:, st, :], start=(half == 0 and st == 0),
                            stop=(half == 1 and st == NST - 1)))
                s.group("pe", fns, reads=["pq%d" % pi, "csl%d" % sl], writes=[pk])
                i = n % NSTG
                n += 1
                s.op("act", lambda a, pt=pt, i=i: a.activation(out=stg[i][:], in_=pt[:, :SBLK], func=AF.Copy),
                     reads=[pk], writes=["stg%d" % i])
                s.dma("sp", sstr[i], out_d[pi, ct * 128:(ct + 1) * 128, bi * SBLK:(bi + 1) * SBLK], stg[i][:],
                      reads=["stg%d" % i])
        if bi + NSL < nblk:
            issue(bi + NSL)
    return c.close()


def dft_consts():
    import ml_dtypes
    n = np.arange(S_LEN, dtype=np.int64)
    ang = 2.0 * np.pi * ((n[:, None] * n[None, :]) % S_LEN).astype(np.float64) / S_LEN
    csm = (np.cos(ang) / 64.0).astype(np.float32).astype(ml_dtypes.bfloat16)
    nsm = (-np.sin(ang) / 64.0).astype(np.float32).astype(ml_dtypes.bfloat16)
    m = np.arange(FG, dtype=np.int64)
    angc = 2.0 * np.pi * ((m[:, None] * m[None, :]) % FG).astype(np.float64) / FG
    ccsc = np.concatenate([np.cos(angc) / 16.0, np.sin(angc) / 16.0], axis=1).astype(np.float32).astype(ml_dtypes.bfloat16)
    return ccsc, csm, nsm


def run_k2(uT_cores):
    ccsc, csm, nsm = dft_consts()
    nc = build_k2(uT_cores[0].shape[0])
    in_maps = [{"uT": uT_cores[i], "ccsc": ccsc, "csm": csm, "nsm": nsm} for i in range(NCORES)]
    res = run_bass_kernel_spmd(nc, in_maps, core_ids=list(range(NCORES)))
    return [r["aT"] for r in res.results]


NEG = -30000.0
C_ID, C_UF, C_UB, C_BD, C_H0, C_H1, C_NTF, C_NTB, C_NSF, C_NSB, C_ONE, C_MISC = range(12)
NCONST = 12
RMS_EPS = 1e-6
L2_EPS = 1e-6


def delta_consts():
    p = np.arange(128)
    same = (p[:, None] // 64) == (p[None, :] // 64)
    cst = np.zeros((128, NCONST, 128), np.float32)
    cst[:, C_ID] = np.eye(128)
    cst[:, C_UF] = same & (p[:, None] <= p[None, :])
    cst[:, C_UB] = same & (p[:, None] >= p[None, :])
    cst[:, C_BD] = same
    cst[:, C_H0] = (p[:, None] < 64) & np.ones((1, 128), bool)
    cst[:, C_H1] = (p[:, None] >= 64) & np.ones((1, 128), bool)
    cst[:, C_NTF] = np.where(same & (p[None, :] >= p[:, None]), 0.0, NEG)
    cst[:, C_NTB] = np.where(same & (p[None, :] <= p[:, None]), 0.0, NEG)
    cst[:, C_NSF] = np.where(same & (p[:, None] > p[None, :]), 0.0, NEG)
    cst[:, C_NSB] = np.where(same & (p[:, None] < p[None, :]), 0.0, NEG)
    cst[:, C_ONE] = 1.0
    cst[:, C_MISC, 0] = (p < 64)
    cst[:, C_MISC, 1] = (p >= 64)
    return cst


def build_k3(NH=8, SEQ=4096, SEG=8):
    c = Ctx()
    nc, s = c.nc, c.s
    NSB = SEQ // 128
    NTB = SEQ // 512
    qkv_d = c.din("qkvT", [NH, 3, 128, SEQ])
    z_d = c.din("zT", [NH, 128, SEQ])
    gates_d = c.din("gates", [NH, 2, 2, 128, NSB])
    hp_d = c.din("hp", [NH, 128, 24])
    cst_d = c.din("consts", [128, NCONST, 128])
    out_d = c.dout("oT", [NH, 128, SEQ])

    st_c = s.stream("const")
    cst = c.sb("cst", [128, NCONST, 128], F32)
    s.dma("sp", st_c, cst[:], cst_d, writes=["cst"])
    ident = cst[:, C_ID, :]
    ones = cst[:, C_ONE, :]
    psr = PsumRing(c, 8)

    st_in = s.stream("in")
    st_hp = s.stream("hp")
    st_out = s.stream("out")
    upad = c.sb("upad", [128, SEQ + 4], F32)
    ybuf = c.sb("ybuf", [128, SEQ], F32)
    qT = c.sb("qT", [128, SEQ], F32)
    kT = c.sb("kT", [128, SEQ], F32)
    k_tm = c.sb("k_tm", [128, NSB, 128], F32)
    v_tm = c.sb("v_tm", [128, NSB, 128], F32)
    oT = c.sb("oT", [128, SEQ], F32)
    hp = c.sb("hp", [128, 24], F32)
    hq = c.sb("hq", [128, 8], F32)
    gt = c.sb("gt", [128, 2, NSB], F32)
    g_tm = c.sb("g_tm", [128, NSB], F32)
    beta_tm = c.sb("beta_tm", [128, NSB], F32)
    nbeta_tm = c.sb("nbeta_tm", [128, NSB], F32)
    gc_tm = c.sb("gc_tm", [128, NSB], F32)
    ngc_tm = c.sb("ngc_tm", [128, NSB], F32)
    bexp_tm = c.sb("bexp_tm", [128, NSB], F32)
    ekd_tm = c.sb("ekd_tm", [128, 2, NSB], F32)
    glast = c.sb("glast", [128, 2, NSB], F32)
    tsm = c.sb("tsm", [128, NSB], F32)
    S = c.sb("S", [128, 128], F32)
    vnew = c.sb("vnew", [128, 128], F32)
    sm = c.sb("sm", [128, 512], F32)
    sm2 = c.sb("sm2", [128, 512], F32)
    gbc = c.sb("gbc", [128, 128], F32)
    erow = c.sb("erow", [128, 128], F32)
    dT = c.sb("dT", [128, 128], F32)
    dS = c.sb("dS", [128, 128], F32)
    Pm = [c.sb("Pm%d" % i, [128, 128], F32) for i in range(2)]
    PmT = [c.sb("PmT%d" % i, [128, 128], F32) for i in range(2)]
    X = c.sb("X", [128, 128], F32)
    vb = c.sb("vb", [128, 128], F32)
    kbg = c.sb("kbg", [128, 128], F32)
    u_sg = c.sb("u_sg", [128, SEG, 128], F32)
    wT_sg = c.sb("wT_sg", [128, SEG, 128], F32)
    qd_sg = c.sb("qd_sg", [128, SEG, 128], F32)
    qk_sg = c.sb("qk_sg", [128, SEG, 128], F32)
    kd_sg = c.sb("kd_sg", [128, SEG, 2, 128], F32)

    s.op("dve", lambda v: v.memset(upad[:, 0:2], 0.0), writes=["upad"])
    s.op("dve", lambda v: v.memset(upad[:, SEQ + 2:SEQ + 4], 0.0), writes=["upad"])
    s.op("dve", lambda v: v.memset(vnew[:], 0.0), writes=["vnew"])

    def l2norm_inplace(buf, key, scale):
        for tb in range(NTB):
            sl = slice(tb * 512, (tb + 1) * 512)
            s.op("act", lambda a: a.activation(out=sm[:], in_=buf[:, sl], func=AF.Square), reads=[key], writes=["sm"])
            pt, pk = psr.next()
            s.op("pe", lambda pe: pe.matmul(pt[:], lhsT=ones, rhs=sm[:], start=True, stop=True),
                 reads=["sm", "cst"], writes=[pk])
            s.op("dve", lambda v: v.tensor_scalar(out=sm2[:], in0=pt[:], scalar1=float(L2_EPS), scalar2=None,
                                                  op0=ALU.add), reads=[pk], writes=["sm2"])
            s.op("act", lambda a: a.activation(out=sm2[:], in_=sm2[:], func=AF.Ln), reads=["sm2"], writes=["sm2"])
            s.op("act", lambda a: a.activation(out=sm2[:], in_=sm2[:], func=AF.Exp, scale=-0.5),
                 reads=["sm2"], writes=["sm2"])
            s.op("dve", lambda v: v.scalar_tensor_tensor(out=buf[:, sl], in0=buf[:, sl], scalar=float(scale),
                                                        in1=sm2[:], op0=ALU.mult, op1=ALU.mult),
                 reads=[key, "sm2"], writes=[key])

    def conv_silu(src_ap, col0, dst, dkey):
        s.dma("sp", st_in, upad[:, 2:SEQ + 2], src_ap, writes=["upad"])
        s.op("dve", lambda v: v.tensor_scalar(out=ybuf[:], in0=upad[:, 0:SEQ], scalar1=hp[:, col0:col0 + 1],
                                              scalar2=None, op0=ALU.mult), reads=["upad", "hp"], writes=["ybuf"])
        for j in range(1, 5):
            s.op("dve", lambda v, j=j: v.scalar_tensor_tensor(out=ybuf[:], in0=upad[:, j:SEQ + j],
                                                             scalar=hp[:, col0 + j:col0 + j + 1], in1=ybuf[:],
                                                             op0=ALU.mult, op1=ALU.add),
                 reads=["upad", "hp", "ybuf"], writes=["ybuf"])
        s.op("act", lambda a: a.activation(out=dst[:], in_=ybuf[:], func=AF.Silu), reads=["ybuf"], writes=[dkey])

    def to_tm(src, skey, dst, dkey):
        for g4 in range(NSB // 4):
            pt, pk = psr.next()
            fns = [lambda pe, i=i: pe.transpose(pt[:, i * 128:(i + 1) * 128],
                                                src[:, (g4 * 4 + i) * 128:(g4 * 4 + i + 1) * 128], ident)
                   for i in range(4)]
            s.group("pe", fns, reads=[skey, "cst"], writes=[pk])
            s.op("act", lambda a: a.activation(out=dst[:, g4 * 4:(g4 + 1) * 4, :],
                                               in_=pt[:].rearrange("p (a b) -> p a b", a=4), func=AF.Copy),
                 reads=[pk], writes=[dkey])

    def mm(out_pt, lhsT, rhs, reads, pk):
        s.op("pe", lambda pe: pe.matmul(out_pt, lhsT=lhsT, rhs=rhs, start=True, stop=True), reads=reads, writes=[pk])

    for h in range(NH):
        s.dma("sp", st_hp, hp[:], hp_d[h], writes=["hp"])
        s.op("act", lambda a: a.activation(out=hq[:, 0:2], in_=hp[:, 16:18], func=AF.Exp), reads=["hp"], writes=["hq"])
        s.op("dve", lambda v: v.tensor_scalar(out=hq[:, 0:2], in0=hq[:, 0:2], scalar1=-1.0, scalar2=None,
                                              op0=ALU.mult), reads=["hq"], writes=["hq"])
        conv_silu(qkv_d[h, 0], 0, qT, "qT")
        l2norm_inplace(qT, "qT", 128.0 ** -0.5)
        conv_silu(qkv_d[h, 1], 5, kT, "kT")
        l2norm_inplace(kT, "kT", 1.0)
        to_tm(kT, "kT", k_tm, "k_tm")
        conv_silu(qkv_d[h, 2], 10, oT, "oT")
        to_tm(oT, "oT", v_tm, "v_tm")

        for d in range(2):
            U = cst[:, C_UF + d, :]
            NT = cst[:, C_NTF + d, :]
            NS = cst[:, C_NSF + d, :]
            s.dma("sp", st_hp, gt[:], gates_d[h, d].rearrange("g p n -> p g n"), writes=["gt"])
            s.op("act", lambda a: a.activation(out=beta_tm[:], in_=gt[:, 0, :], func=AF.Sigmoid),
                 reads=["gt"], writes=["beta_tm"])
            s.op("dve", lambda v: v.tensor_scalar(out=nbeta_tm[:], in0=beta_tm[:], scalar1=-1.0, scalar2=None,
                                                  op0=ALU.mult), reads=["beta_tm"], writes=["nbeta_tm"])
            s.op("act", lambda a, d=d: a.activation(out=tsm[:], in_=gt[:, 1, :], func=AF.Exp,
                                                    bias=hp[:, 18 + d:19 + d]), reads=["gt", "hp"], writes=["tsm"])
            s.op("dve", lambda v: v.tensor_scalar(out=tsm[:], in0=tsm[:], scalar1=1.0, scalar2=None, op0=ALU.add),
                 reads=["tsm"], writes=["tsm"])
            s.op("act", lambda a: a.activation(out=tsm[:], in_=tsm[:], func=AF.Ln), reads=["tsm"], writes=["tsm"])
            s.op("dve", lambda v, d=d: v.tensor_scalar(out=g_tm[:], in0=tsm[:], scalar1=hq[:, d:d + 1], scalar2=None,
                                                       op0=ALU.mult), reads=["tsm", "hq"], writes=["g_tm"])
            pt, pk = psr.next()
            mm(pt[:, 0:NSB], U, g_tm[:], ["cst", "g_tm"], pk)
            s.op("act", lambda a: a.activation(out=gc_tm[:], in_=pt[:, 0:NSB], func=AF.Copy), reads=[pk], writes=["gc_tm"])
            s.op("dve", lambda v: v.tensor_scalar(out=ngc_tm[:], in0=pt[:, 0:NSB], scalar1=-1.0, scalar2=None,
                                                  op0=ALU.mult), reads=[pk], writes=["ngc_tm"])
            pt2, pk2 = psr.next()
            mm(pt2[:, 0:NSB], cst[:, C_BD, :], g_tm[:], ["cst", "g_tm"], pk2)
            s.op("dve", lambda v: v.tensor_tensor(out=tsm[:], in0=pt2[:, 0:NSB], in1=gc_tm[:], op=ALU.subtract),
                 reads=[pk2, "gc_tm"], writes=["tsm"])
            s.op("act", lambda a: a.activation(out=tsm[:], in_=tsm[:], func=AF.Exp), reads=["tsm"], writes=["tsm"])
            for hf in range(2):
                s.op("dve", lambda v, hf=hf: v.tensor_scalar(out=ekd_tm[:, hf, :], in0=tsm[:],
                                                             scalar1=cst[:, C_MISC, hf:hf + 1], scalar2=None,
                                                             op0=ALU.mult), reads=["tsm", "cst"], writes=["ekd_tm"])
            s.op("act", lambda a: a.activation(out=bexp_tm[:], in_=gc_tm[:], func=AF.Exp), reads=["gc_tm"], writes=["bexp_tm"])
            s.op("dve", lambda v: v.tensor_tensor(out=bexp_tm[:], in0=bexp_tm[:], in1=beta_tm[:], op=ALU.mult),
                 reads=["bexp_tm", "beta_tm"], writes=["bexp_tm"])
            for hf in range(2):
                pt3, pk3 = psr.next()
                mm(pt3[:, 0:NSB], cst[:, C_H0 + hf, :], g_tm[:], ["cst", "g_tm"], pk3)
                s.op("act", lambda a, hf=hf, pt3=pt3: a.activation(out=glast[:, hf, :], in_=pt3[:, 0:NSB], func=AF.Exp),
                     reads=[pk3], writes=["glast"])
            s.op("dve", lambda v: v.memset(S[:], 0.0), writes=["S"])

            nseg = NSB // SEG
            seg_order = range(nseg) if d == 0 else range(nseg - 1, -1, -1)
            for sg in seg_order:
                for si in range(SEG):
                    sb = sg * SEG + si
                    tsl = slice(sb * 128, (sb + 1) * 128)
                    kx = "_%d" % si
                    s.op("dve", lambda v, sb=sb: v.tensor_scalar(out=gbc[:], in0=ones, scalar1=g_tm[:, sb:sb + 1],
                                                                 scalar2=None, op0=ALU.mult),
                         reads=["cst", "g_tm"], writes=["gbc"])
                    pg, kg = psr.next()
                    mm(pg[:, 0:128], gbc[:], U, ["gbc", "cst"], kg)
                    s.op("act", lambda a, pg=pg: a.activation(out=erow[:], in_=pg[:, 0:128], func=AF.Exp),
                         reads=[kg], writes=["erow"])
                    s.op("dve", lambda v, pg=pg, sb=sb: v.scalar_tensor_tensor(
                        out=dT[:], in0=pg[:, 0:128], scalar=gc_tm[:, sb:sb + 1], in1=NT, op0=ALU.subtract, op1=ALU.add),
                         reads=[kg, "gc_tm", "cst"], writes=["dT"])
                    s.op("act", lambda a: a.activation(out=dT[:], in_=dT[:], func=AF.Exp), reads=["dT"], writes=["dT"])
                    s.op("dve", lambda v, pg=pg: v.scalar_tensor_tensor(
                        out=dS[:], in0=pg[:, 0:128], scalar=-1.0, in1=NS, op0=ALU.mult, op1=ALU.add),
                         reads=[kg, "cst"], writes=["dS"])
                    s.op("act", lambda a, sb=sb: a.activation(out=dS[:], in_=dS[:], func=AF.Exp,
                                                              bias=gc_tm[:, sb:sb + 1]),
                         reads=["dS", "gc_tm"], writes=["dS"])
                    s.op("dve", lambda v, si=si, tsl=tsl: v.tensor_tensor(out=qd_sg[:, si, :], in0=qT[:, tsl], in1=erow[:],
                                                                         op=ALU.mult),
                         reads=["qT", "erow"], writes=["qd" + kx])
                    pk_, kk_ = psr.next()
                    mm(pk_[:, 0:128], kT[:, tsl], kT[:, tsl], ["kT"], kk_)
                    s.op("dve", lambda v, pk_=pk_, sb=sb: v.scalar_tensor_tensor(
                        out=PmT[0][:], in0=pk_[:, 0:128], scalar=nbeta_tm[:, sb:sb + 1], in1=dS[:],
                        op0=ALU.mult, op1=ALU.mult), reads=[kk_, "nbeta_tm", "dS"], writes=["PmT0"])
                    pr, kr = psr.next()
                    s.op("pe", lambda pe, pr=pr: pe.transpose(pr[:, 0:128], PmT[0][:], ident), reads=["PmT0", "cst"],
                         writes=[kr])
                    s.op("act", lambda a, pr=pr: a.activation(out=Pm[0][:], in_=pr[:, 0:128], func=AF.Copy),
                         reads=[kr], writes=["Pm0"])
                    s.op("dve", lambda v, pr=pr: v.tensor_tensor(out=X[:], in0=pr[:, 0:128], in1=ident, op=ALU.add),
                         reads=[kr, "cst"], writes=["X"])
                    pq_, kq_ = psr.next()
                    mm(pq_[:, 0:128], kT[:, tsl], qT[:, tsl], ["kT", "qT"], kq_)
                    s.op("dve", lambda v, pq_=pq_, si=si: v.tensor_tensor(out=qk_sg[:, si, :], in0=pq_[:, 0:128], in1=dT[:],
                                                                         op=ALU.mult),
                         reads=[kq_, "dT"], writes=["qk" + kx])
                    cur = 0
                    for lvl in range(1, 6):
                        nxt = 1 - cur
                        pa, ka = psr.next()
                        mm(pa[:, 0:128], Pm[cur][:], PmT[cur][:], ["Pm%d" % cur, "PmT%d" % cur], ka)
                        if lvl < 5:
                            pb, kb = psr.next()
                            mm(pb[:, 0:128], PmT[cur][:], Pm[cur][:], ["Pm%d" % cur, "PmT%d" % cur], kb)
                        s.op("act", lambda a, pa=pa, nxt=nxt: a.activation(out=PmT[nxt][:], in_=pa[:, 0:128], func=AF.Copy),
                             reads=[ka], writes=["PmT%d" % nxt])
                        if lvl < 5:
                            s.op("dve", lambda v, pb=pb, nxt=nxt: v.tensor_copy(out=Pm[nxt][:], in_=pb[:, 0:128]),
                                 reads=[kb], writes=["Pm%d" % nxt])
                        px, kxp = psr.next()
                        mm(px[:, 0:128], PmT[nxt][:], X[:], ["PmT%d" % nxt, "X"], kxp)
                        s.op("dve", lambda v, px=px: v.tensor_tensor(out=X[:], in0=X[:], in1=px[:, 0:128], op=ALU.add),
                             reads=[kxp, "X"], writes=["X"])
                        cur = nxt
                    s.op("dve", lambda v, sb=sb: v.tensor_scalar(out=vb[:], in0=v_tm[:, sb, :], scalar1=beta_tm[:, sb:sb + 1],
                                                                 scalar2=None, op0=ALU.mult),
                         reads=["v_tm", "beta_tm"], writes=["vb"])
                    s.op("dve", lambda v, sb=sb: v.tensor_scalar(out=kbg[:], in0=k_tm[:, sb, :], scalar1=bexp_tm[:, sb:sb + 1],
                                                                 scalar2=None, op0=ALU.mult),
                         reads=["k_tm", "bexp_tm"], writes=["kbg"])
                    pu, ku = psr.next()
                    mm(pu[:, 0:128], X[:], vb[:], ["X", "vb"], ku)
                    s.op("act", lambda a, pu=pu, si=si: a.activation(out=u_sg[:, si, :], in_=pu[:, 0:128], func=AF.Copy),
                         reads=[ku], writes=["u" + kx])
                    pw, kw = psr.next()
                    mm(pw[:, 0:128], kbg[:], X[:], ["X", "kbg"], kw)
                    s.op("act", lambda a, pw=pw, si=si: a.activation(out=wT_sg[:, si, :], in_=pw[:, 0:128], func=AF.Copy),
                         reads=[kw], writes=["wT" + kx])
                    for hf in range(2):
                        s.op("dve", lambda v, sb=sb, si=si, hf=hf: v.tensor_scalar(
                            out=kd_sg[:, si, hf, :], in0=k_tm[:, sb, :], scalar1=ekd_tm[:, hf, sb:sb + 1], scalar2=None,
                            op0=ALU.mult), reads=["k_tm", "ekd_tm"], writes=["kd" + kx])
                si_order = range(SEG) if d == 0 else range(SEG - 1, -1, -1)
                for si in si_order:
                    sb = sg * SEG + si
                    kx = "_%d" % si
                    for hf in ((0, 1) if d == 0 else (1, 0)):
                        r = slice(hf * 64, (hf + 1) * 64)
                        tok = slice(sb * 128 + hf * 64, sb * 128 + (hf + 1) * 64)
                        p1, k1 = psr.next()
                        mm(p1[:, 0:128], wT_sg[:, si, :], S[:], ["wT" + kx, "S"], k1)
                        s.op("dve", lambda v, p1=p1, r=r, si=si: v.tensor_tensor(out=vnew[r, :], in0=u_sg[r, si, :],
                                                                                in1=p1[r, 0:128], op=ALU.subtract),
                             reads=[k1, "u" + kx], writes=["vnew"])
                        po, ko = psr.next()
                        s.group("pe", [
                            lambda pe, po=po, si=si, r=r: pe.matmul(po[:, 0:64], lhsT=S[:], rhs=qd_sg[:, si, r],
                                                                    start=True, stop=False),
                            lambda pe, po=po, si=si, r=r: pe.matmul(po[:, 0:64], lhsT=vnew[:], rhs=qk_sg[:, si, r],
                                                                    start=False, stop=True)],
                            reads=["S", "qd" + kx, "vnew", "qk" + kx], writes=[ko])
                        p2, k2 = psr.next()
                        mm(p2[:, 0:128], kd_sg[:, si, hf, :], vnew[:], ["kd" + kx, "vnew"], k2)
                        s.op("dve", lambda v, p2=p2, hf=hf, sb=sb: v.scalar_tensor_tensor(
                            out=S[:], in0=S[:], scalar=glast[:, hf, sb:sb + 1], in1=p2[:, 0:128],
                            op0=ALU.mult, op1=ALU.add), reads=[k2, "S", "glast"], writes=["S"])
                        if d == 0:
                            s.op("act", lambda a, po=po, tok=tok: a.activation(out=oT[:, tok], in_=po[:, 0:64], func=AF.Copy),
                                 reads=[ko], writes=["oT"])
                        else:
                            s.op("dve", lambda v, po=po, tok=tok: v.tensor_tensor(out=oT[:, tok], in0=oT[:, tok],
                                                                                in1=po[:, 0:64], op=ALU.add),
                                 reads=[ko, "oT"], writes=["oT"])
        s.dma("sp", st_in, upad[:, 2:SEQ + 2], z_d[h], writes=["upad"])
        s.op("act", lambda a: a.activation(out=ybuf[:], in_=upad[:, 2:SEQ + 2], func=AF.Silu), reads=["upad"], writes=["ybuf"])
        for tb in range(NTB):
            sl = slice(tb * 512, (tb + 1) * 512)
            s.op("act", lambda a, sl=sl: a.activation(out=sm[:], in_=oT[:, sl], func=AF.Square), reads=["oT"], writes=["sm"])
            pt, pk = psr.next()
            mm(pt[:], ones, sm[:], ["sm", "cst"], pk)
            s.op("dve", lambda v, pt=pt: v.tensor_scalar(out=sm2[:], in0=pt[:], scalar1=1.0 / 128.0, scalar2=float(RMS_EPS),
                                                         op0=ALU.mult, op1=ALU.add), reads=[pk], writes=["sm2"])
            s.op("act", lambda a: a.activation(out=sm2[:], in_=sm2[:], func=AF.Ln), reads=["sm2"], writes=["sm2"])
            s.op("act", lambda a: a.activation(out=sm2[:], in_=sm2[:], func=AF.Exp, scale=-0.5), reads=["sm2"], writes=["sm2"])
            s.op("dve", lambda v, sl=sl: v.tensor_tensor(out=sm2[:], in0=sm2[:], in1=oT[:, sl], op=ALU.mult),
                 reads=["sm2", "oT"], writes=["sm2"])
            s.op("dve", lambda v, sl=sl: v.scalar_tensor_tensor(out=ybuf[:, sl], in0=sm2[:], scalar=hp[:, 15:16],
                                                               in1=ybuf[:, sl], op0=ALU.mult, op1=ALU.mult),
                 reads=["sm2", "hp", "ybuf"], writes=["ybuf"])
        s.dma("sp", st_out, out_d[h], ybuf[:], reads=["ybuf"])
    return c.close()


def run_k3(ins_cores):
    cst = delta_consts()
    NH = ins_cores[0]["qkvT"].shape[0]
    SEQ = ins_cores[0]["qkvT"].shape[3]
    nc = build_k3(NH, SEQ)
    in_maps = [dict(m, consts=cst) for m in ins_cores]
    res = run_bass_kernel_spmd(nc, in_maps, core_ids=list(range(len(ins_cores))))
    return [r["oT"] for r in res.results]


def pack_k3(qkv, z, beta_raw, a_raw, conv_w, a_log, dt_bias, onw, heads, n_heads_total):
    S_ = qkv.shape[0]
    W = n_heads_total * 128
    NSB = S_ // 128
    NH = len(heads)
    qkvT = np.empty((NH, 3, 128, S_), np.float32)
    zT = np.empty((NH, 128, S_), np.float32)
    gates = np.empty((NH, 2, 2, 128, NSB), np.float32)
    hp = np.zeros((NH, 128, 24), np.float32)
    for i, h in enumerate(heads):
        for j in range(3):
            cols = slice(j * W + h * 128, j * W + (h + 1) * 128)
            qkvT[i, j] = qkv[:, cols].T
            hp[i, :, 5 * j:5 * j + 5] = conv_w[:, cols].T
        zT[i] = z[:, h * 128:(h + 1) * 128].T
        for d in range(2):
            gates[i, d, 0] = beta_raw[:, d * n_heads_total + h].reshape(NSB, 128).T
            gates[i, d, 1] = a_raw[:, d * n_heads_total + h].reshape(NSB, 128).T
            hp[i, :, 16 + d] = a_log[d, h]
            hp[i, :, 18 + d] = dt_bias[d, h]
        hp[i, :, 15] = onw
    return {"qkvT": qkvT, "zT": zT, "gates": gates, "hp": hp}


ALPHA = 4.0 ** 0.25
NE = 8


def build_k4(TT, moe, NHALF=1):
    c = Ctx()
    nc, s = c.nc, c.s
    NTB = TT // 512
    NTT = TT // 128
    FF = 7168 if moe else 5632
    xT_d = c.din("xT", [D, TT * NHALF])
    aT_d = c.din("aT", [1024, TT * NHALF])
    bT_d = c.din("bT", [D, TT * NHALF])
    gfT_d = c.din("gfT", [D, TT * NHALF])
    gdT_d = c.din("gdT", [D, TT * NHALF])
    pT_d = c.din("pT", [256, TT * NHALF])
    wf_d = c.din("wf", [1024, D])
    wdl_d = c.din("wdl", [D, D])
    wo_d = c.din("wo", [D, D])
    pg_d = c.din("pg", [D, D])
    pp_d = c.din("pp", [256, D])
    lnp_d = c.din("lnp", [128, 4, KT])
    id_d = c.din("ident", [128, 128])
    if moe:
        rw_d = c.din("rw", [128, KT, NE])
        gu_d = c.din("egu", [NE, D, 2 * FF])
        dn_d = c.din("edn", [NE, FF, D])
        experts = [(gu_d[e], dn_d[e]) for e in range(NE)]
    else:
        gu_d = c.din("gu", [D, 2 * FF])
        dn_d = c.din("dn", [FF, D])
        experts = [(gu_d, dn_d)]
    out_d = c.dout("x2T", [D, TT * NHALF])

    st_misc = s.stream("misc")
    lnp = load_small(c, "sp", st_misc, "lnp", lnp_d, [128, 4, KT])
    ident = load_small(c, "sp", st_misc, "ident", id_d, [128, 128])
    s.keys["lngb"] = {"w": (st_misc.key, st_misc.cnt, None), "r": {}}
    ones = c.sb("ones", [128, 128], F32)
    s.op("dve", lambda v: v.memset(ones[:], 1.0 / D), writes=["ones"])
    ones1 = c.sb("ones1", [128, 128], F32)
    s.op("dve", lambda v: v.memset(ones1[:], 1.0), writes=["ones1"])
    psr = PsumRing(c, 8)
    scr = ln_scratch(c)

    acc = c.sb("acc", [128, KT, TT], F32)
    bufA = c.sb("bufA", [128, KT, TT], BF16)
    bufB = c.sb("bufB", [128, max(KT * TT, 8192 + 4 * TT)], BF16)
    mb = bufB[:, 0:KT * TT].rearrange("p (a b) -> p a b", a=KT)
    ab = bufB[:, 0:8 * TT].rearrange("p (a b) -> p a b", a=8)
    NSL = 2
    wslots = [(c.sb("w%d" % i, [128, KT, 512], BF16), "w%d" % i) for i in range(NSL)]
    wstreams = [s.stream("w%d" % i) for i in range(NSL)]
    gts = [c.sb("gt%d" % i, [128, TT], F32) for i in range(2)]
    gstr = [s.stream("gt%d" % i) for i in range(2)]
    sgb = [c.sb("sg%d" % i, [128, 512], F32) for i in range(2)]
    tmpb = c.sb("tmpb", [128, 512], F32)
    pb = c.sb("pb", [128, 2, TT], BF16)
    wpp = c.sb("wpp", [128, 2, D], BF16)
    st_act = s.stream("actin")
    cnt = {"gt": 0, "sg": 0}

    wdstr = [s.stream("wd%d" % i) for i in range(2)]
    st_out = s.stream("out")
    if moe:
        rw = load_small(c, "sp", st_misc, "rw", rw_d, [128, KT, NE])
        comb_tm = c.sb("comb_tm", [128, NTT, NE], F32)
        rs = [c.sb("rs%d" % i, [128, NE], F32) for i in range(4)]
        r1 = c.sb("r1", [128, 8], F32)
        comb_e = c.sb("comb_e", [128, TT], F32)
        lbs = [c.sb("lb%d" % i, [128, 128], F32) for i in range(2)]
    s.dma("pool", st_act, wpp[:], pp_d.rearrange("(kt p) n -> p kt n", p=128), writes=["wpp"])
    for hf in range(NHALF):
        _k4_half(locals(), hf)
    return c.close()


def _k4_half(L, hf):
    g = globals()
    (c, s, TT, NTB, NTT, FF, moe, experts, psr, scr, acc, bufA, bufB, mb, ab, NSL, wslots, wstreams, gts, gstr, sgb, tmpb, pb,
     wpp, st_act, cnt, lnp, ident, ones, ones1, wdstr, st_out) = [L[k] for k in (
        "c", "s", "TT", "NTB", "NTT", "FF", "moe", "experts", "psr", "scr", "acc", "bufA", "bufB", "mb", "ab", "NSL", "wslots",
        "wstreams", "gts", "gstr", "sgb", "tmpb", "pb", "wpp", "st_act", "cnt", "lnp", "ident", "ones", "ones1", "wdstr", "st_out")]
    xT_d, aT_d, bT_d, gfT_d, gdT_d, pT_d, wf_d, wdl_d, wo_d, pg_d, out_d = [L[k] for k in (
        "xT_d", "aT_d", "bT_d", "gfT_d", "gdT_d", "pT_d", "wf_d", "wdl_d", "wo_d", "pg_d", "out_d")]
    if moe:
        rw, comb_tm, rs, r1, comb_e, lbs = [L[k] for k in ("rw", "comb_tm", "rs", "r1", "comb_e", "lbs")]
    c0 = hf * TT
    cs = slice(c0, c0 + TT)
    s.merge_into("bufB", ["wd0", "wd1", "hT0", "hT1"])
    s.dma("pool", st_act, ab, aT_d.rearrange("(kt p) t -> p kt t", p=128)[:, :, cs], writes=["bufB"])
    s.dma("pool", st_act, bufA[:], bT_d.rearrange("(kt p) t -> p kt t", p=128)[:, :, cs], writes=["bufA"])

    def load_tile(src_d, j, func):
        i = cnt["gt"] % 2
        cnt["gt"] += 1
        s.dma("sp", gstr[i], gts[i][:], src_d[j * 128:(j + 1) * 128, cs], writes=["gt%d" % i])
        if func is not None:
            s.op("act", lambda a: a.activation(out=gts[i][:], in_=gts[i][:], func=func), reads=["gt%d" % i],
                 writes=["gt%d" % i])
        return gts[i], "gt%d" % i

    cur = {}

    def epi1(row0, rows, tb, pt, pkey):
        j = row0 // 128
        sl = slice(tb * 512, (tb + 1) * 512)
        if tb == 0:
            cur["t"] = load_tile(gfT_d, j, AF.Sigmoid)
        g, gk = cur["t"]
        s.op("dve", lambda v: v.tensor_tensor(out=acc[:, j, sl], in0=g[:, sl], in1=pt[:], op=ALU.mult),
             reads=[gk, pkey], writes=["acc"])

    gemm_fm(c, wf_d, 0, D, ab, "bufB", 8, TT, wslots, wstreams, psr, epi1)

    def epi2(row0, rows, tb, pt, pkey):
        j = row0 // 128
        sl = slice(tb * 512, (tb + 1) * 512)
        if tb == 0:
            cur["t"] = load_tile(gdT_d, j, AF.Sigmoid)
        g, gk = cur["t"]
        s.op("dve", lambda v: v.tensor_tensor(out=tmpb[:], in0=g[:, sl], in1=pt[:], op=ALU.mult),
             reads=[gk, pkey], writes=["tmpb"])
        s.op("dve", lambda v: v.tensor_tensor(out=mb[:, j, sl], in0=tmpb[:], in1=acc[:, j, sl], op=ALU.add),
             reads=["tmpb", "acc"], writes=["bufB"])

    gemm_fm(c, wdl_d, 0, D, bufA, "bufA", KT, TT, wslots, wstreams, psr, epi2)

    def epi3(row0, rows, tb, pt, pkey):
        j = row0 // 128
        sl = slice(tb * 512, (tb + 1) * 512)
        if tb == 0:
            cur["t"] = load_tile(xT_d, j, None)
        g, gk = cur["t"]
        s.op("dve", lambda v: v.scalar_tensor_tensor(out=acc[:, j, sl], in0=g[:, sl], scalar=float(ALPHA), in1=pt[:],
                                                    op0=ALU.mult, op1=ALU.add), reads=[gk, pkey], writes=["acc"])

    gemm_fm(c, wo_d, 0, D, mb, "bufB", KT, TT, wslots, wstreams, psr, epi3)

    for tb in range(NTB):
        sl = slice(tb * 512, (tb + 1) * 512)
        ln_block(c, lambda kt, sl=sl: acc[:, kt, sl], "acc", KT, lnp[:, 0, :], lnp[:, 1, :], ones, psr, scr,
                 out_bf=lambda kt, sl=sl: bufA[:, kt, sl], out_f32=lambda kt, sl=sl: acc[:, kt, sl],
                 okeys=("bufA", "acc"))

    if moe:
        for tt in range(NTT):
            pt, pk = psr.next()
            s.group("pe", [lambda pe, kt=kt, tt=tt, pt=pt: pe.matmul(pt[:, 0:NE], lhsT=acc[:, kt, tt * 128:(tt + 1) * 128],
                                                                    rhs=rw[:, kt, :], start=(kt == 0), stop=(kt == KT - 1))
                           for kt in range(KT)], reads=["acc", "rw"], writes=[pk])
            s.op("act", lambda a, pt=pt: a.activation(out=rs[0][:], in_=pt[:, 0:NE], func=AF.Copy), reads=[pk], writes=["rs0"])
            s.op("dve", lambda v: v.reduce_max(out=r1[:, 0:1], in_=rs[0][:], axis=mybir.AxisListType.X),
                 reads=["rs0"], writes=["r1"])
            s.op("dve", lambda v: v.tensor_scalar(out=rs[1][:], in0=rs[0][:], scalar1=r1[:, 0:1], scalar2=None,
                                                  op0=ALU.is_equal), reads=["rs0", "r1"], writes=["rs1"])
            s.op("dve", lambda v: v.scalar_tensor_tensor(out=rs[2][:], in0=rs[1][:], scalar=-1e30, in1=rs[0][:],
                                                        op0=ALU.mult, op1=ALU.add), reads=["rs1", "rs0"], writes=["rs2"])
            s.op("dve", lambda v: v.reduce_max(out=r1[:, 1:2], in_=rs[2][:], axis=mybir.AxisListType.X),
                 reads=["rs2"], writes=["r1"])
            s.op("dve", lambda v: v.tensor_scalar(out=rs[3][:], in0=rs[2][:], scalar1=r1[:, 1:2], scalar2=None,
                                                  op0=ALU.is_equal), reads=["rs2", "r1"], writes=["rs3"])
            s.op("dve", lambda v: v.tensor_tensor(out=r1[:, 2:3], in0=r1[:, 1:2], in1=r1[:, 0:1], op=ALU.subtract),
                 reads=["r1"], writes=["r1"])
            s.op("act", lambda a: a.activation(out=r1[:, 3:4], in_=r1[:, 2:3], func=AF.Exp), reads=["r1"], writes=["r1"])
            s.op("dve", lambda v: v.tensor_scalar(out=r1[:, 4:5], in0=r1[:, 3:4], scalar1=1.0, scalar2=None, op0=ALU.add),
                 reads=["r1"], writes=["r1"])
            s.op("dve", lambda v: v.reciprocal(out=r1[:, 5:6], in_=r1[:, 4:5]), reads=["r1"], writes=["r1"])
            s.op("dve", lambda v: v.tensor_tensor(out=r1[:, 6:7], in0=r1[:, 3:4], in1=r1[:, 5:6], op=ALU.mult),
                 reads=["r1"], writes=["r1"])
            s.op("dve", lambda v: v.tensor_scalar(out=rs[0][:], in0=rs[1][:], scalar1=r1[:, 5:6], scalar2=None,
                                                  op0=ALU.mult), reads=["rs1", "r1"], writes=["rs0"])
            s.op("dve", lambda v, tt=tt: v.scalar_tensor_tensor(out=comb_tm[:, tt, :], in0=rs[3][:], scalar=r1[:, 6:7],
                                                               in1=rs[0][:], op0=ALU.mult, op1=ALU.add),
                 reads=["rs3", "rs0", "r1"], writes=["comb_tm"])
    for kt in range(KT):
        s.op("dve", lambda v, kt=kt: v.tensor_scalar(out=acc[:, kt, :], in0=acc[:, kt, :], scalar1=float(ALPHA), scalar2=None,
                                                     op0=ALU.mult), reads=["acc"], writes=["acc"])
    wds = [bufB[:, i * 4096:(i + 1) * 4096].rearrange("p (a b) -> p a b", a=2) for i in range(2)]
    hTs = [bufB[:, 8192 + i * 2 * TT: 8192 + (i + 1) * 2 * TT].rearrange("p (a b) -> p a b", a=2) for i in range(2)]
    for k in ("wd0", "wd1", "hT0", "hT1"):
        s.merge_into(k, ["bufB"], reset=True)
    nch = FF // 256
    work = [(e, ch) for e in range(len(experts)) for ch in range(nch)]

    def issue(wi):
        e, ch = work[wi]
        gu, dn = experts[e]
        guv = gu.rearrange("(kt p) n -> p kt n", p=128)
        c0 = ch * 256
        sl = wi % NSL
        s.dma("pool", wstreams[sl], wslots[sl][0][:, :, 0:256], guv[:, :, c0:c0 + 256], writes=[wslots[sl][1]])
        s.dma("pool", wstreams[sl], wslots[sl][0][:, :, 256:512], guv[:, :, FF + c0:FF + c0 + 256], writes=[wslots[sl][1]])
        s.dma("pool", wdstr[wi % 2], wds[wi % 2], dn[c0:c0 + 256, :].rearrange("(kt p) n -> p kt n", p=128),
              writes=["wd%d" % (wi % 2)])

    for wi in range(min(NSL, len(work))):
        issue(wi)
    for wi, (e, ch) in enumerate(work):
        if moe and ch == 0:
            for tt in range(NTT):
                lb = lbs[tt % 2]
                lk = "lb%d" % (tt % 2)
                s.op("dve", lambda v, tt=tt, lb=lb, e=e: v.tensor_scalar(out=lb[:], in0=ones1[:], scalar1=comb_tm[:, tt, e:e + 1],
                                                                       scalar2=None, op0=ALU.mult),
                     reads=["ones1", "comb_tm"], writes=[lk])
                if tt % 4 == 0:
                    pc, pck = psr.next()
                s.op("pe", lambda pe, pc=pc, tt=tt, lb=lb: pe.matmul(pc[:, (tt % 4) * 128:(tt % 4 + 1) * 128], lhsT=lb[:],
                                                                   rhs=ident[:], start=True, stop=True),
                     reads=[lk, "ident"], writes=[pck])
                if tt % 4 == 3:
                    s.op("act", lambda a, pc=pc, tt=tt: a.activation(out=comb_e[:, (tt // 4) * 512:(tt // 4 + 1) * 512],
                                                                     in_=pc[:], func=AF.Copy), reads=[pck], writes=["comb_e"])
        sl = wi % NSL
        wt, wkey = wslots[sl]
        hT = hTs[wi % 2]
        hk = "hT%d" % (wi % 2)
        wd = wds[wi % 2]
        wdk = "wd%d" % (wi % 2)
        for jt in range(2):
            for tb in range(NTB):
                tsl = slice(tb * 512, (tb + 1) * 512)
                pg_, pgk = psr.next()
                pu_, puk = psr.next()
                s.group("pe", [lambda pe, kt=kt, pg_=pg_, jt=jt, tsl=tsl, wt=wt: pe.matmul(
                    pg_[:], lhsT=wt[:, kt, jt * 128:(jt + 1) * 128], rhs=bufA[:, kt, tsl], start=(kt == 0), stop=(kt == KT - 1))
                    for kt in range(KT)], reads=[wkey, "bufA"], writes=[pgk])
                s.group("pe", [lambda pe, kt=kt, pu_=pu_, jt=jt, tsl=tsl, wt=wt: pe.matmul(
                    pu_[:], lhsT=wt[:, kt, 256 + jt * 128:256 + (jt + 1) * 128], rhs=bufA[:, kt, tsl], start=(kt == 0),
                    stop=(kt == KT - 1)) for kt in range(KT)], reads=[wkey, "bufA"], writes=[puk])
                i = cnt["sg"] % 2
                cnt["sg"] += 1
                s.op("act", lambda a, i=i, pg_=pg_: a.activation(out=sgb[i][:], in_=pg_[:], func=AF.Silu),
                     reads=[pgk], writes=["sg%d" % i])
                if moe:
                    s.op("dve", lambda v, i=i, pu_=pu_: v.tensor_tensor(out=tmpb[:], in0=sgb[i][:], in1=pu_[:], op=ALU.mult),
                         reads=["sg%d" % i, puk], writes=["tmpb"])
                    s.op("dve", lambda v, jt=jt, tsl=tsl, hT=hT: v.tensor_tensor(out=hT[:, jt, tsl], in0=tmpb[:],
                                                                                in1=comb_e[:, tsl], op=ALU.mult),
                         reads=["tmpb", "comb_e"], writes=[hk])
                else:
                    s.op("dve", lambda v, i=i, pu_=pu_, jt=jt, tsl=tsl, hT=hT: v.tensor_tensor(
                        out=hT[:, jt, tsl], in0=sgb[i][:], in1=pu_[:], op=ALU.mult),
                         reads=["sg%d" % i, puk], writes=[hk])
        for j in range(KT):
            for tb in range(NTB):
                tsl = slice(tb * 512, (tb + 1) * 512)
                pd_, pdk = psr.next()
                s.group("pe", [lambda pe, jt=jt, pd_=pd_, j=j, tsl=tsl, wd=wd, hT=hT: pe.matmul(
                    pd_[:], lhsT=wd[:, jt, j * 128:(j + 1) * 128], rhs=hT[:, jt, tsl], start=(jt == 0), stop=(jt == 1))
                    for jt in range(2)], reads=[wdk, hk], writes=[pdk])
                s.op("dve", lambda v, j=j, tsl=tsl, pd_=pd_: v.tensor_tensor(out=acc[:, j, tsl], in0=acc[:, j, tsl],
                                                                            in1=pd_[:], op=ALU.add),
                     reads=[pdk, "acc"], writes=["acc"])
        if wi + NSL < len(work):
            issue(wi + NSL)

    s.dma("pool", st_act, pb[:], pT_d.rearrange("(kt p) t -> p kt t", p=128)[:, :, cs], writes=["pb"])

    def epi6(row0, rows, tb, pt, pkey):
        j = row0 // 128
        sl = slice(tb * 512, (tb + 1) * 512)
        p2, p2k = psr.next()
        s.group("pe", [lambda pe, kt=kt: pe.matmul(p2[:], lhsT=wpp[:, kt, j * 128:(j + 1) * 128], rhs=pb[:, kt, sl],
                                                   start=(kt == 0), stop=(kt == 1)) for kt in range(2)],
                reads=["wpp", "pb"], writes=[p2k])
        i = cnt["sg"] % 2
        cnt["sg"] += 1
        s.op("act", lambda a: a.activation(out=sgb[i][:], in_=pt[:], func=AF.Sigmoid), reads=[pkey], writes=["sg%d" % i])
        s.op("dve", lambda v: v.tensor_tensor(out=tmpb[:], in0=sgb[i][:], in1=p2[:], op=ALU.mult),
             reads=["sg%d" % i, p2k], writes=["tmpb"])
        s.op("dve", lambda v: v.tensor_tensor(out=acc[:, j, sl], in0=acc[:, j, sl], in1=tmpb[:], op=ALU.add),
             reads=["tmpb", "acc"], writes=["acc"])

    gemm_fm(c, pg_d, 0, D, bufA, "bufA", KT, TT, wslots, wstreams, psr, epi6)

    for tb in range(NTB):
        sl = slice(tb * 512, (tb + 1) * 512)
        ln_block(c, lambda kt, sl=sl: acc[:, kt, sl], "acc", KT, lnp[:, 2, :], lnp[:, 3, :], ones, psr, scr,
                 out_f32=lambda kt, sl=sl: acc[:, kt, sl], okeys=(None, "acc"))
    s.dma("sp", st_out, out_d.rearrange("(kt p) t -> p kt t", p=128)[:, :, cs], acc[:], reads=["acc"])


def k4_weights(inp, layer):
    moe = (layer % 2 == 1)
    w = {
        "wf": inp["w_fourier"][layer], "wdl": inp["w_delta"][layer], "wo": inp["w_out"][layer],
        "pg": inp["ple_gate"][layer], "pp": inp["ple_proj"][layer],
        "lnp": np.ascontiguousarray(np.stack([_vecT(inp["ln1_g"][layer]), _vecT(inp["ln1_b"][layer]),
                                              _vecT(inp["ln2_g"][layer]), _vecT(inp["ln2_b"][layer])], axis=1)),
        "ident": np.eye(128, dtype=np.float32),
    }
    if moe:
        w["rw"] = np.ascontiguousarray(inp["router_w"][layer // 2].reshape(KT, 128, NE).transpose(1, 0, 2))
        w["egu"] = inp["exp_gate_up"][layer // 2]
        w["edn"] = inp["exp_down"][layer // 2]
    else:
        w["gu"] = inp["ffn_gate_up"][layer // 2]
        w["dn"] = inp["ffn_down"][layer // 2]
    return w


def run_k4(acts_cores, weights, moe, NHALF=1):
    TT = acts_cores[0]["xT"].shape[1] // NHALF
    nc = build_k4(TT, moe, NHALF)
    in_maps = [dict(a, **weights) for a in acts_cores]
    res = run_bass_kernel_spmd(nc, in_maps, core_ids=list(range(len(acts_cores))))
    return [r["x2T"] for r in res.results]


def pack_k3_fm(P, b, heads, conv_w, a_log, dt_bias, onw):
    cols = slice(b * S_LEN, (b + 1) * S_LEN)
    NSB = S_LEN // 128
    NH = len(heads)
    qkvT = np.empty((NH, 3, 128, S_LEN), np.float32)
    zT = np.empty((NH, 128, S_LEN), np.float32)
    gates = np.empty((NH, 2, 2, 128, NSB), np.float32)
    hp = np.zeros((NH, 128, 24), np.float32)
    for i, h in enumerate(heads):
        for j in range(3):
            r0 = 1024 + j * 2048 + h * 128
            qkvT[i, j] = P[r0:r0 + 128, cols]
            hp[i, :, 5 * j:5 * j + 5] = conv_w[:, j * 2048 + h * 128: j * 2048 + (h + 1) * 128].T
        zT[i] = P[7168 + h * 128: 7168 + (h + 1) * 128, cols]
        for d in range(2):
            gates[i, d, 0] = P[9216 + d * 16 + h, cols].reshape(NSB, 128).T
            gates[i, d, 1] = P[9248 + d * 16 + h, cols].reshape(NSB, 128).T
            hp[i, :, 16 + d] = a_log[d, h]
            hp[i, :, 18 + d] = dt_bias[d, h]
        hp[i, :, 15] = onw
    return {"qkvT": qkvT, "zT": zT, "gates": gates, "hp": hp}


def kernel(x, p, emb_ln_g, emb_ln_b, w_in, conv_w, a_log, dt_bias, o_norm_w, w_fourier, w_delta,
           w_out, ln1_g, ln1_b, ffn_gate_up, ffn_down, router_w, exp_gate_up, exp_down,
           ple_gate, ple_proj, ln2_g, ln2_b):
    inp = dict(w_fourier=w_fourier, w_delta=w_delta, w_out=w_out, ln1_g=ln1_g, ln1_b=ln1_b, ffn_gate_up=ffn_gate_up,
               ffn_down=ffn_down, router_w=router_w, exp_gate_up=exp_gate_up, exp_down=exp_down, ple_gate=ple_gate,
               ple_proj=ple_proj, ln2_g=ln2_g, ln2_b=ln2_b)
    inp = {k: np.asarray(v, np.float32) for k, v in inp.items()}
    x = np.asarray(x, np.float32)
    B_, S_, _ = x.shape
    NTOK = B_ * S_
    TC = NTOK // NCORES
    xf = x.reshape(NTOK, D)
    pf = np.asarray(p, np.float32).reshape(2, NTOK, 256)
    xT_c = [np.ascontiguousarray(xf[c * TC:(c + 1) * TC].T) for c in range(NCORES)]
    for layer in range(2):
        do_ln = layer == 0
        projT, xn = run_k1(xT_c, np.asarray(w_in[layer], np.float32), np.asarray(emb_ln_g, np.float32),
                           np.asarray(emb_ln_b, np.float32), do_ln)
        xres_c = xn if do_ln else xT_c
        P = np.concatenate(projT, axis=1)
        del projT
        pairs = [[((2 * c + i) // 4, (2 * c + i) % 4) for i in range(2)] for c in range(NCORES)]
        uT_cores = [np.stack([P[g * 256:(g + 1) * 256, b * S_LEN:(b + 1) * S_LEN] for (b, g) in pairs[c]])
                    for c in range(NCORES)]
        aT = run_k2(uT_cores)
        A_T = np.empty((1024, NTOK), np.float32)
        for c in range(NCORES):
            for i, (b, g) in enumerate(pairs[c]):
                A_T[g * 256:(g + 1) * 256, b * S_LEN:(b + 1) * S_LEN] = aT[c][i]
        del uT_cores, aT
        cw = np.asarray(conv_w[layer], np.float32)
        ins_cores = [pack_k3_fm(P, c // 2, list(range((c % 2) * 8, (c % 2) * 8 + 8)), cw,
                                np.asarray(a_log[layer], np.float32), np.asarray(dt_bias[layer], np.float32),
                                np.asarray(o_norm_w[layer], np.float32)) for c in range(NCORES)]
        oT = run_k3(ins_cores)
        B_T = np.empty((D, NTOK), np.float32)
        for c in range(NCORES):
            b = c // 2
            for i in range(8):
                h = (c % 2) * 8 + i
                B_T[h * 128:(h + 1) * 128, b * S_LEN:(b + 1) * S_LEN] = oT[c][i]
        del ins_cores, oT
        acts = []
        for c in range(NCORES):
            cols = slice(c * TC, (c + 1) * TC)
            acts.append({"xT": xres_c[c], "aT": np.ascontiguousarray(A_T[:, cols]), "bT": np.ascontiguousarray(B_T[:, cols]),
                         "gfT": np.ascontiguousarray(P[9280:11328, cols]), "gdT": np.ascontiguousarray(P[11328:13376, cols]),
                         "pT": np.ascontiguousarray(pf[layer, cols].T)})
        del P, A_T, B_T
        xT_c = run_k4(acts, k4_weights(inp, layer), layer % 2 == 1, NHALF=2)
        del acts
    out = np.concatenate([t.T for t in xT_c], axis=0).reshape(B_, S_, D)
    return np.ascontiguousarray(out.astype(np.float32))
```

```python
import contextlib
import numpy as np
import concourse.bass as bass
import concourse.mybir as mybir
from concourse.bass_utils import run_bass_kernel_spmd

F32 = mybir.dt.float32
BF16 = mybir.dt.bfloat16
AF = mybir.ActivationFunctionType
ALU = mybir.AluOpType

D = 2048
KT = 16
NCORES = 8
IN_W = 13376
LN_EPS = 1e-5


class Stream:
    def __init__(self, key, sem):
        self.key = key
        self.sem = sem
        self.cnt = 0


class Sched:
    def __init__(self, nc, stack, same_sync=True):
        self.nc = nc
        self.stack = stack
        self.same_sync = same_sync
        self.eng = {"pe": nc.tensor, "act": nc.scalar, "dve": nc.vector, "pool": nc.gpsimd, "sp": nc.sync}
        self.semh = {}
        self.cnt = {}
        self.waited = {}
        for e in self.eng:
            self.semh[e] = stack.enter_context(nc.semaphore("sem_" + e))
            self.cnt[e] = 0
            self.waited[e] = {}
        self.keys = {}
        self.chain = None
        self.excl = set()
        self.streams = []
        self.stream_by_name = {}
        self.nstream = 0

    def stream(self, name=None):
        if name is not None and name in self.stream_by_name:
            return self.stream_by_name[name]
        st = self._new_stream(name)
        if name is not None:
            self.stream_by_name[name] = st
        return st

    def barrier(self):
        for e in self.eng:
            for e2 in self.eng:
                if e2 != e and self.cnt[e2] and self.waited[e].get(e2, 0) < self.cnt[e2]:
                    self.eng[e].wait_ge(self.semh[e2], self.cnt[e2])
                    self.waited[e][e2] = self.cnt[e2]
            for st in self.streams:
                if st.cnt and self.waited[e].get(st.key, 0) < st.cnt:
                    self.eng[e].wait_ge(st.sem, st.cnt)
                    self.waited[e][st.key] = st.cnt
        self.keys = {}

    def _new_stream(self, name=None):
        self.nstream += 1
        key = "dma%d_%s" % (self.nstream, name or "")
        sem = self.stack.enter_context(self.nc.semaphore(key))
        st = Stream(key, sem)
        self.semh[key] = sem
        self.streams.append(st)
        return st

    def _deps(self, e, reads, writes):
        deps = {}

        def add(ev):
            if ev is None:
                return
            k, v, pe = ev
            if pe == e and not self.same_sync:
                return
            if deps.get(k, 0) < v:
                deps[k] = v

        for k in reads:
            st = self.keys.get(k)
            if st:
                add(st["w"])
                if k in self.excl:
                    for ev in st["r"].values():
                        if ev[2] != e:
                            add(ev)
        for k in writes:
            st = self.keys.get(k)
            if st:
                add(st["w"])
                for ev in st["r"].values():
                    add(ev)
        for k, v in deps.items():
            if self.waited[e].get(k, 0) >= v:
                continue
            self.eng[e].wait_ge(self.semh[k], v)
            self.waited[e][k] = v

    def _record(self, ev, reads, writes):
        for k in reads:
            st = self.keys.setdefault(k, {"w": None, "r": {}})
            st["r"][ev[0]] = ev
        for k in writes:
            self.keys[k] = {"w": ev, "r": {}}

    def run_chains(self, chains):
        its = [iter(ch) for ch in chains if ch]
        while its:
            for it in list(its):
                try:
                    kind, args = next(it)
                except StopIteration:
                    its.remove(it)
                    continue
                getattr(self, kind)(*args)

    def op(self, e, fn, reads=(), writes=()):
        if self.chain is not None:
            self.chain.append(("op", (e, fn, tuple(reads), tuple(writes))))
            return
        self._deps(e, reads, writes)
        ins = fn(self.eng[e])
        self.cnt[e] += 1
        ins.then_inc(self.semh[e], 1)
        self._record((e, self.cnt[e], e), reads, writes)

    def group(self, e, fns, reads=(), writes=()):
        if self.chain is not None:
            self.chain.append(("group", (e, list(fns), tuple(reads), tuple(writes))))
            return
        self._deps(e, reads, writes)
        ins = None
        for fn in fns:
            ins = fn(self.eng[e])
        self.cnt[e] += 1
        ins.then_inc(self.semh[e], 1)
        self._record((e, self.cnt[e], e), reads, writes)

    def dma(self, q, stream, out, in_, reads=(), writes=(), **kw):
        if self.chain is not None:
            assert not kw
            self.chain.append(("dma", (q, stream, out, in_, tuple(reads), tuple(writes))))
            return
        if not hasattr(stream, "subs"):
            stream.subs = {}
            stream.q0 = q
        if q != stream.q0:
            if q not in stream.subs:
                stream.subs[q] = self._new_stream((stream.key.split("_", 1)[1] or "x") + "_" + q)
            stream = stream.subs[q]
        kset = frozenset(reads) | frozenset(writes)
        if stream.cnt and getattr(stream, "last_keys", None) != kset and self.waited[q].get(stream.key, 0) < stream.cnt:
            self.eng[q].wait_ge(stream.sem, stream.cnt)
            self.waited[q][stream.key] = stream.cnt
        stream.last_keys = kset
        self._deps(q, reads, writes)
        ins = self.eng[q].dma_start(out=out, in_=in_, **kw)
        stream.cnt += 16
        ins.then_inc(stream.sem, 16)
        self._record((stream.key, stream.cnt, None), reads, writes)

    def merge_into(self, dst, srcs, reset=False):
        if reset or dst not in self.keys:
            self.keys[dst] = {"w": None, "r": {}}
        d = self.keys[dst]
        for k in srcs:
            st = self.keys.get(k)
            if not st:
                continue
            for ev in [st["w"]] + list(st["r"].values()):
                if ev is None:
                    continue
                cur = d["r"].get(ev[0])
                if cur is None or cur[1] < ev[1]:
                    d["r"][ev[0]] = ev

    def finish(self, q="sp"):
        for st in self.streams:
            if st.cnt and self.waited[q].get(st.key, 0) < st.cnt:
                self.eng[q].wait_ge(st.sem, st.cnt)
                self.waited[q][st.key] = st.cnt


class Ctx:
    def __init__(self):
        self.nc = bass.Bass("TRN2", target_bir_lowering=False)
        self.stack = contextlib.ExitStack()
        self.s = Sched(self.nc, self.stack)
        self.io = {}
        self.fused = False
        self.stage_stack = None
        self.stage_id = 0
        self._psr = None

    def begin_stage(self, io):
        self.fused = True
        self.io = dict(io)
        self.stage_id += 1
        self.stage_stack = contextlib.ExitStack()

    def sb(self, name, shape, dt):
        st = self.stage_stack if self.stage_stack is not None else self.stack
        return st.enter_context(self.nc.sbuf_tensor("s%d_%s" % (self.stage_id, name), shape, dt))

    def ps(self, name, shape=(128, 512), dt=F32):
        return self.stack.enter_context(self.nc.psum_tensor(name, list(shape), dt))

    def psum_ring(self):
        if self._psr is None:
            self._psr = PsumRing(self, 8)
        return self._psr

    def din(self, name, shape, dt=F32):
        if name in self.io:
            ap = self.io[name]
            assert list(ap.shape) == list(shape), (name, list(ap.shape), list(shape))
            return ap
        assert not self.fused, name
        return self.nc.dram_tensor(name, list(shape), dt, kind="ExternalInput").ap()

    def dout(self, name, shape, dt=F32):
        if name in self.io:
            ap = self.io[name]
            assert list(ap.shape) == list(shape), (name, list(ap.shape), list(shape))
            return ap
        assert not self.fused, name
        return self.nc.dram_tensor(name, list(shape), dt, kind="ExternalOutput").ap()

    def close(self):
        if self.fused:
            self.s.barrier()
            self.stage_stack.close()
            self.stage_stack = None
            return None
        self.s.finish("sp")
        self.stack.close()
        return self.nc

    def finish_all(self):
        self.s.finish("sp")
        self.stack.close()
        return self.nc


class PsumRing:
    def __init__(self, c, n, prefix="ps"):
        self.t = [c.ps("%s%d" % (prefix, i)) for i in range(n)]
        self.keys = ["%s%d" % (prefix, i) for i in range(n)]
        c.s.excl.update(self.keys)
        self.i = 0

    def next(self):
        i = self.i % len(self.t)
        self.i += 1
        return self.t[i], self.keys[i]

    def sub(self, idxs):
        r = object.__new__(PsumRing)
        r.t = [self.t[i] for i in idxs]
        r.keys = [self.keys[i] for i in idxs]
        r.i = 0
        return r


def load_small(c, q, stream, name, dram_ap, shape, dt=F32):
    t = c.sb(name, list(shape), dt)
    c.s.dma(q, stream, t[:], dram_ap, writes=[name])
    return t


def gemm_fm(c, W, n0, n1, xTb, xkey, kt_n, TT, wslots, wstreams, psr, epilogue, chunk=512, wq="pool"):
    s = c.s
    Wv = W.rearrange("(kt p) n -> p kt n", p=128)
    chunks = []
    a = n0
    while a < n1:
        cw = min(chunk, n1 - a)
        chunks.append((a, cw))
        a += cw
    nsl = len(wslots)

    def issue(ci):
        a, cw = chunks[ci]
        sl = ci % nsl
        s.dma(wq, wstreams[sl], wslots[sl][0][:, :kt_n, :cw], Wv[:, :, a:a + cw], writes=[wslots[sl][1]])

    for ci in range(min(nsl, len(chunks))):
        issue(ci)
    for ci, (a, cw) in enumerate(chunks):
        sl = ci % nsl
        wt, wkey = wslots[sl]
        j = 0
        while j < cw:
            rows = min(128, cw - j)
            for tb in range(TT // 512):
                pt, pkey = psr.next()
                fns = []
                for kt in range(kt_n):
                    fns.append(lambda pe, kt=kt, pt=pt, j=j, rows=rows, tb=tb, wt=wt: pe.matmul(
                        pt[:rows, :], lhsT=wt[:, kt, j:j + rows], rhs=xTb[:, kt, tb * 512:(tb + 1) * 512],
                        start=(kt == 0), stop=(kt == kt_n - 1)))
                s.group("pe", fns, reads=[wkey, xkey], writes=[pkey])
                epilogue(a + j, rows, tb, pt, pkey)
            j += rows
        if ci + nsl < len(chunks):
            issue(ci + nsl)


def ln_block(c, srcf, skey, kt_n, gT, bT, ones, psr, scr, out_bf=None, out_f32=None, okeys=(None, None), eps=LN_EPS):
    s = c.s
    sq, mean, rstd, tmp = scr
    p1, k1 = psr.next()
    p2, k2 = psr.next()
    s.group("pe", [lambda pe, kt=kt: pe.matmul(p1[:], lhsT=ones[:], rhs=srcf(kt), start=(kt == 0),
                                               stop=(kt == kt_n - 1)) for kt in range(kt_n)],
            reads=[skey, "ones"], writes=[k1])
    for kt in range(kt_n):
        q = sq[kt % 2]
        qk = "lnsq%d" % (kt % 2)
        s.op("act", lambda a, kt=kt, q=q: a.activation(out=q[:], in_=srcf(kt), func=AF.Square),
             reads=[skey], writes=[qk])
        s.op("pe", lambda pe, kt=kt, q=q: pe.matmul(p2[:], lhsT=ones[:], rhs=q[:], start=(kt == 0),
                                                    stop=(kt == kt_n - 1)),
             reads=[qk, "ones"], writes=[k2])
    s.op("act", lambda a: a.activation(out=mean[:], in_=p1[:], func=AF.Copy), reads=[k1], writes=["lnmean"])
    s.op("dve", lambda v: v.tensor_tensor(out=tmp[:], in0=mean[:], in1=mean[:], op=ALU.mult),
         reads=["lnmean"], writes=["lntmp"])
    s.op("dve", lambda v: v.tensor_tensor(out=rstd[:], in0=p2[:], in1=tmp[:], op=ALU.subtract),
         reads=[k2, "lntmp"], writes=["lnrstd"])
    s.op("dve", lambda v: v.tensor_scalar(out=rstd[:], in0=rstd[:], scalar1=float(eps), scalar2=None,
                                          op0=ALU.add), reads=["lnrstd"], writes=["lnrstd"])
    s.op("act", lambda a: a.activation(out=rstd[:], in_=rstd[:], func=AF.Ln), reads=["lnrstd"], writes=["lnrstd"])
    s.op("act", lambda a: a.activation(out=rstd[:], in_=rstd[:], func=AF.Exp, scale=-0.5),
         reads=["lnrstd"], writes=["lnrstd"])
    for kt in range(kt_n):
        s.op("dve", lambda v, kt=kt: v.tensor_tensor(out=tmp[:], in0=srcf(kt), in1=mean[:], op=ALU.subtract),
             reads=[skey, "lnmean"], writes=["lntmp"])
        s.op("dve", lambda v: v.tensor_tensor(out=tmp[:], in0=tmp[:], in1=rstd[:], op=ALU.mult),
             reads=["lntmp", "lnrstd"], writes=["lntmp"])
        if out_f32 is not None:
            s.op("act", lambda a, kt=kt: a.activation(out=out_f32(kt), in_=tmp[:], func=AF.Identity,
                                                      bias=bT[:, kt:kt + 1], scale=gT[:, kt:kt + 1]),
                 reads=["lntmp", "lngb"], writes=[okeys[1]])
        if out_bf is not None:
            s.op("act", lambda a, kt=kt: a.activation(out=out_bf(kt), in_=tmp[:], func=AF.Identity,
                                                      bias=bT[:, kt:kt + 1], scale=gT[:, kt:kt + 1]),
                 reads=["lntmp", "lngb"], writes=[okeys[0]])


def ln_scratch(c):
    return ([c.sb("lnsq0", [128, 512], F32), c.sb("lnsq1", [128, 512], F32)], c.sb("lnmean", [128, 512], F32),
            c.sb("lnrstd", [128, 512], F32), c.sb("lntmp", [128, 512], F32))


def build_k1(TT, do_ln, c=None):
    c = c or Ctx()
    nc, s = c.nc, c.s
    xT_d = c.din("xT", [D, TT])
    w_d = c.din("w", [D, IN_W])
    g_d = c.din("g", [128, KT])
    b_d = c.din("b", [128, KT])
    out_d = c.dout("projT", [IN_W, TT])
    xn_d = c.dout("xnT", [D, TT]) if do_ln else None

    st_misc = s.stream("misc")
    st_x = s.stream("x")
    st_xn = s.stream("xn")
    xf = c.sb("xf", [128, KT, 512], F32)
    xb = c.sb("xb", [128, KT, TT], BF16)
    gT = load_small(c, "sp", st_misc, "gT", g_d, [128, KT])
    bT = load_small(c, "sp", st_misc, "bT", b_d, [128, KT])
    s.keys["lngb"] = {"w": (st_misc.key, st_misc.cnt, None), "r": {}}
    ones = c.sb("ones", [128, 128], F32)
    s.op("dve", lambda v: v.memset(ones[:], 1.0 / D), writes=["ones"])
    psr = c.psum_ring()
    scr = ln_scratch(c) if do_ln else None
    xTv = xT_d.rearrange("(kt p) t -> p kt t", p=128)
    for tb in range(TT // 512):
        sl = slice(tb * 512, (tb + 1) * 512)
        s.dma("sp", st_x, xf[:], xTv[:, :, sl], writes=["xf"])
        if do_ln:
            ln_block(c, lambda kt: xf[:, kt, :], "xf", KT, gT, bT, ones, psr, scr,
                     out_bf=lambda kt, sl=sl: xb[:, kt, sl], out_f32=lambda kt: xf[:, kt, :], okeys=("xb", "xf"))
            s.dma("sp", st_xn, xn_d.rearrange("(kt p) t -> p kt t", p=128)[:, :, sl], xf[:], reads=["xf"])
        else:
            for kt in range(KT):
                s.op("dve", lambda v, kt=kt, sl=sl: v.tensor_copy(out=xb[:, kt, sl], in_=xf[:, kt, :]),
                     reads=["xf"], writes=["xb"])

    NSL = 3
    wslots = [(c.sb("w%d" % i, [128, KT, 512], BF16), "w%d" % i) for i in range(NSL)]
    wstreams = [s.stream("w%d" % i) for i in range(NSL)]
    NST = 2
    stg = [c.sb("stg%d" % i, [128, TT], F32) for i in range(NST)]
    ststreams = [s.stream("st%d" % i) for i in range(NST)]
    state = {"n": 0}
    ntb = TT // 512

    def epi(row0, rows, tb, pt, pkey):
        i = state["n"] % NST
        sk = "stg%d" % i
        s.op("act", lambda a: a.activation(out=stg[i][:rows, tb * 512:(tb + 1) * 512], in_=pt[:rows, :], func=AF.Copy),
             reads=[pkey], writes=[sk])
        if tb == ntb - 1:
            s.dma("sp", ststreams[i], out_d[row0:row0 + rows, :], stg[i][:rows, :], reads=[sk])
            state["n"] += 1

    gemm_fm(c, w_d, 0, IN_W, xb, "xb", KT, TT, wslots, wstreams, psr, epi)
    return c.close()


def _vecT(v):
    return np.ascontiguousarray(v.reshape(-1, 128).T)


def run_k1(xT_cores, w, g, b, do_ln):
    TT = xT_cores[0].shape[1]
    nc = build_k1(TT, do_ln)
    in_maps = [{"xT": xT_cores[i], "w": w, "g": _vecT(g), "b": _vecT(b)} for i in range(NCORES)]
    res = run_bass_kernel_spmd(nc, in_maps, core_ids=list(range(NCORES)))
    return [r["projT"] for r in res.results], ([r["xnT"] for r in res.results] if do_ln else None)


S_LEN = 4096
FG = 256


def build_k2(NP=2, SBLK=256, c=None, S_LEN=S_LEN):
    c = c or Ctx()
    nc, s = c.nc, c.s
    u_d = c.din("uT", [NP, FG, S_LEN])
    cc_d = c.din("ccsc", [FG, 2 * FG], BF16)
    cs_d = c.din("csm", [S_LEN, S_LEN], BF16)
    ns_d = c.din("nsm", [S_LEN, S_LEN], BF16)
    out_d = c.dout("aT", [NP, FG, S_LEN])
    NST = S_LEN // 128
    st_misc = s.stream("misc")
    ccsc = c.sb("ccsc", [128, 2, 2 * FG], BF16)
    s.dma("sp", st_misc, ccsc[:], cc_d.rearrange("(ct p) n -> p ct n", p=128), writes=["ccsc"])
    psr = c.psum_ring()
    ub = [c.sb("ub%d" % i, [128, 2, S_LEN], BF16) for i in range(NP)]
    pq = [c.sb("pq%d" % i, [128, NST, 2 * FG], BF16) for i in range(NP)]
    for pi in range(NP):
        s.dma("pool", st_misc, ub[pi][:], u_d[pi].rearrange("(ct p) t -> p ct t", p=128), writes=["ub%d" % pi])
        for st in range(NST):
            pt, pk = psr.next()
            s.group("pe", [lambda pe, ct=ct, pt=pt, st=st, pi=pi: pe.matmul(
                pt[:], lhsT=ub[pi][:, ct, st * 128:(st + 1) * 128], rhs=ccsc[:, ct, :], start=(ct == 0), stop=(ct == 1))
                for ct in range(2)], reads=["ub%d" % pi, "ccsc"], writes=[pk])
            eng = "act" if st % 2 == 0 else "dve"
            if eng == "act":
                s.op("act", lambda a, pt=pt, st=st, pi=pi: a.activation(out=pq[pi][:, st, :], in_=pt[:], func=AF.Copy),
                     reads=[pk], writes=["pq%d" % pi])
            else:
                s.op("dve", lambda v, pt=pt, st=st, pi=pi: v.tensor_copy(out=pq[pi][:, st, :], in_=pt[:]),
                     reads=[pk], writes=["pq%d" % pi])
    NSL = 2
    csl = [(c.sb("cs%d" % i, [128, NST, SBLK], BF16), c.sb("ns%d" % i, [128, NST, SBLK], BF16)) for i in range(NSL)]
    cstr = [s.stream("cs%d" % i) for i in range(NSL)]
    nblk = S_LEN // SBLK
    csv = cs_d.rearrange("(st p) s -> p st s", p=128)
    nsv = ns_d.rearrange("(st p) s -> p st s", p=128)

    def issue(bi):
        sl = bi % NSL
        s.dma("sp", cstr[sl], csl[sl][0][:], csv[:, :, bi * SBLK:(bi + 1) * SBLK], writes=["csl%d" % sl])
        s.dma("sp", cstr[sl], csl[sl][1][:], nsv[:, :, bi * SBLK:(bi + 1) * SBLK], writes=["csl%d" % sl])

    NSTG = 2
    stg = [c.sb("stg%d" % i, [128, SBLK], F32) for i in range(NSTG)]
    sstr = [s.stream("st%d" % i) for i in range(NSTG)]
    n = 0
    for bi in range(min(NSL, nblk)):
        issue(bi)
    for bi in range(nblk):
        sl = bi % NSL
        for pi in range(NP):
            for ct in range(2):
                pt, pk = psr.next()
                fns = []
                for half in range(2):
                    for st in range(NST):
                        fns.append(lambda pe, half=half, st=st, pt=pt, pi=pi, ct=ct, sl=sl: pe.matmul(
                            pt[:, :SBLK], lhsT=pq[pi][:, st, half * FG + ct * 128: half * FG + (ct + 1) * 128],
                            rhs=csl[sl][half][:, st, :], start=(half == 0 and st == 0),
                            stop=(half == 1 and st == NST - 1)))
                s.group("pe", fns, reads=["pq%d" % pi, "csl%d" % sl], writes=[pk])
                i = n % NSTG
                n += 1
                s.op("act", lambda a, pt=pt, i=i: a.activation(out=stg[i][:], in_=pt[:, :SBLK], func=AF.Copy),
                     reads=[pk], writes=["stg%d" % i])
                s.dma("sp", sstr[i], out_d[pi, ct * 128:(ct + 1) * 128, bi * SBLK:(bi + 1) * SBLK], stg[i][:],
                      reads=["stg%d" % i])
        if bi + NSL < nblk:
            issue(bi + NSL)
    return c.close()


def dft_consts(S_LEN=S_LEN, flip=False):
    import ml_dtypes
    n = np.arange(S_LEN, dtype=np.int64)
    if flip:
        n = n[::-1].copy()
    ang = 2.0 * np.pi * ((n[:, None] * n[None, :]) % S_LEN).astype(np.float64) / S_LEN
    csm = (np.cos(ang) / 64.0).astype(np.float32).astype(ml_dtypes.bfloat16)
    nsm = (-np.sin(ang) / 64.0).astype(np.float32).astype(ml_dtypes.bfloat16)
    m = np.arange(FG, dtype=np.int64)
    angc = 2.0 * np.pi * ((m[:, None] * m[None, :]) % FG).astype(np.float64) / FG
    ccsc = np.concatenate([np.cos(angc) / 16.0, np.sin(angc) / 16.0], axis=1).astype(np.float32).astype(ml_dtypes.bfloat16)
    return ccsc, csm, nsm


def run_k2(uT_cores):
    ccsc, csm, nsm = dft_consts()
    nc = build_k2(uT_cores[0].shape[0])
    in_maps = [{"uT": uT_cores[i], "ccsc": ccsc, "csm": csm, "nsm": nsm} for i in range(NCORES)]
    res = run_bass_kernel_spmd(nc, in_maps, core_ids=list(range(NCORES)))
    return [r["aT"] for r in res.results]


NEG = -30000.0
C_ID, C_UF, C_UB, C_BD, C_H0, C_H1, C_NTF, C_NTB, C_NSF, C_NSB, C_ONE, C_MISC = range(12)
NCONST = 12
RMS_EPS = 1e-6
L2_EPS = 1e-6


def delta_consts():
    p = np.arange(128)
    same = (p[:, None] // 64) == (p[None, :] // 64)
    cst = np.zeros((128, NCONST, 128), np.float32)
    cst[:, C_ID] = np.eye(128)
    cst[:, C_UF] = same & (p[:, None] <= p[None, :])
    cst[:, C_UB] = same & (p[:, None] >= p[None, :])
    cst[:, C_BD] = same
    cst[:, C_H0] = (p[:, None] < 64) & np.ones((1, 128), bool)
    cst[:, C_H1] = (p[:, None] >= 64) & np.ones((1, 128), bool)
    cst[:, C_NTF] = np.where(same & (p[None, :] >= p[:, None]), 0.0, NEG)
    cst[:, C_NTB] = np.where(same & (p[None, :] <= p[:, None]), 0.0, NEG)
    cst[:, C_NSF] = np.where(same & (p[:, None] > p[None, :]), 0.0, NEG)
    cst[:, C_NSB] = np.where(same & (p[:, None] < p[None, :]), 0.0, NEG)
    cst[:, C_ONE] = 1.0
    cst[:, C_MISC, 0] = (p < 64)
    cst[:, C_MISC, 1] = (p >= 64)
    return cst


def build_k3(NH=8, SEQ=4096, SEG=8, c=None):
    c = c or Ctx()
    nc, s = c.nc, c.s
    NSB = SEQ // 128
    NTB = SEQ // 512
    qkv_d = c.din("qkvT", [NH, 3, 128, SEQ])
    z_d = c.din("zT", [NH, 128, SEQ])
    gr_d = c.io.get("gate_rows")
    gates_d = None if gr_d is not None else c.din("gates", [NH, 2, 2, 128, NSB])
    hp_d = c.din("hp", [NH, 128, 24])
    cst_d = c.din("consts", [128, NCONST, 128])
    out_d = c.dout("oT", [NH, 128, SEQ])

    st_c = s.stream("const")
    cst = c.sb("cst", [128, NCONST, 128], F32)
    s.dma("sp", st_c, cst[:], cst_d, writes=["cst"])
    ident = cst[:, C_ID, :]
    ones = cst[:, C_ONE, :]
    psr = c.psum_ring()

    st_in = s.stream("in")
    st_hp = s.stream("hp")
    st_out = s.stream("out")
    upad = c.sb("upad", [128, SEQ + 4], F32)
    ybuf = c.sb("ybuf", [128, SEQ], F32)
    qT = c.sb("qT", [128, SEQ], F32)
    kT = c.sb("kT", [128, SEQ], F32)
    k_tm = c.sb("k_tm", [128, NSB, 128], F32)
    v_tm = c.sb("v_tm", [128, NSB, 128], F32)
    oT = c.sb("oT", [128, SEQ], F32)
    hp = c.sb("hp", [128, 24], F32)
    hq = c.sb("hq", [128, 8], F32)
    gt = c.sb("gt", [128, 2, NSB], F32)
    grow = c.sb("grow", [NSB, 2, 128], F32)
    g_tm = c.sb("g_tm", [128, NSB], F32)
    beta_tm = c.sb("beta_tm", [128, NSB], F32)
    nbeta_tm = c.sb("nbeta_tm", [128, NSB], F32)
    gc_tm = c.sb("gc_tm", [128, NSB], F32)
    ngc_tm = c.sb("ngc_tm", [128, NSB], F32)
    bexp_tm = c.sb("bexp_tm", [128, NSB], F32)
    ekd_tm = c.sb("ekd_tm", [128, 2, NSB], F32)
    glast = c.sb("glast", [128, 2, NSB], F32)
    tsm = c.sb("tsm", [128, NSB], F32)
    S = c.sb("S", [128, 128], F32)
    vnew = c.sb("vnew", [128, 128], F32)
    sm = c.sb("sm", [128, 512], F32)
    sm2 = c.sb("sm2", [128, 512], F32)
    gbc = c.sb("gbc", [128, 128], F32)
    erow = c.sb("erow", [128, 128], F32)
    dT = c.sb("dT", [128, 128], F32)
    dS = c.sb("dS", [128, 128], F32)
    Pm = [c.sb("Pm%d" % i, [128, 128], F32) for i in range(2)]
    PmT = [c.sb("PmT%d" % i, [128, 128], F32) for i in range(2)]
    X = c.sb("X", [128, 128], F32)
    vb = c.sb("vb", [128, 128], F32)
    kbg = c.sb("kbg", [128, 128], F32)
    u_sg = c.sb("u_sg", [128, SEG, 128], F32)
    wT_sg = c.sb("wT_sg", [128, SEG, 128], F32)
    qd_sg = c.sb("qd_sg", [128, SEG, 128], F32)
    qk_sg = c.sb("qk_sg", [128, SEG, 128], F32)
    kd_sg = c.sb("kd_sg", [128, SEG, 2, 128], F32)

    s.op("dve", lambda v: v.memset(upad[:, 0:2], 0.0), writes=["upad"])
    s.op("dve", lambda v: v.memset(upad[:, SEQ + 2:SEQ + 4], 0.0), writes=["upad"])
    s.op("dve", lambda v: v.memset(vnew[:], 0.0), writes=["vnew"])

    def l2norm_inplace(buf, key, scale):
        for tb in range(NTB):
            sl = slice(tb * 512, (tb + 1) * 512)
            s.op("act", lambda a: a.activation(out=sm[:], in_=buf[:, sl], func=AF.Square), reads=[key], writes=["sm"])
            pt, pk = psr.next()
            s.op("pe", lambda pe: pe.matmul(pt[:], lhsT=ones, rhs=sm[:], start=True, stop=True),
                 reads=["sm", "cst"], writes=[pk])
            s.op("dve", lambda v: v.tensor_scalar(out=sm2[:], in0=pt[:], scalar1=float(L2_EPS), scalar2=None,
                                                  op0=ALU.add), reads=[pk], writes=["sm2"])
            s.op("act", lambda a: a.activation(out=sm2[:], in_=sm2[:], func=AF.Ln), reads=["sm2"], writes=["sm2"])
            s.op("act", lambda a: a.activation(out=sm2[:], in_=sm2[:], func=AF.Exp, scale=-0.5),
                 reads=["sm2"], writes=["sm2"])
            s.op("dve", lambda v: v.scalar_tensor_tensor(out=buf[:, sl], in0=buf[:, sl], scalar=float(scale),
                                                        in1=sm2[:], op0=ALU.mult, op1=ALU.mult),
                 reads=[key, "sm2"], writes=[key])

    def conv_silu(src_ap, col0, dst, dkey):
        s.dma("sp", st_in, upad[:, 2:SEQ + 2], src_ap, writes=["upad"])
        s.op("dve", lambda v: v.tensor_scalar(out=ybuf[:], in0=upad[:, 0:SEQ], scalar1=hp[:, col0:col0 + 1],
                                              scalar2=None, op0=ALU.mult), reads=["upad", "hp"], writes=["ybuf"])
        for j in range(1, 5):
            s.op("dve", lambda v, j=j: v.scalar_tensor_tensor(out=ybuf[:], in0=upad[:, j:SEQ + j],
                                                             scalar=hp[:, col0 + j:col0 + j + 1], in1=ybuf[:],
                                                             op0=ALU.mult, op1=ALU.add),
                 reads=["upad", "hp", "ybuf"], writes=["ybuf"])
        s.op("act", lambda a: a.activation(out=dst[:], in_=ybuf[:], func=AF.Silu), reads=["ybuf"], writes=[dkey])

    def to_tm(src, skey, dst, dkey):
        for g4 in range(NSB // 4):
            pt, pk = psr.next()
            fns = [lambda pe, i=i: pe.transpose(pt[:, i * 128:(i + 1) * 128],
                                                src[:, (g4 * 4 + i) * 128:(g4 * 4 + i + 1) * 128], ident)
                   for i in range(4)]
            s.group("pe", fns, reads=[skey, "cst"], writes=[pk])
            s.op("act", lambda a: a.activation(out=dst[:, g4 * 4:(g4 + 1) * 4, :],
                                               in_=pt[:].rearrange("p (a b) -> p a b", a=4), func=AF.Copy),
                 reads=[pk], writes=[dkey])

    def mm(out_pt, lhsT, rhs, reads, pk):
        s.op("pe", lambda pe: pe.matmul(out_pt, lhsT=lhsT, rhs=rhs, start=True, stop=True), reads=reads, writes=[pk])

    for h in range(NH):
        s.dma("sp", st_hp, hp[:], hp_d[h], writes=["hp"])
        s.op("act", lambda a: a.activation(out=hq[:, 0:2], in_=hp[:, 16:18], func=AF.Exp), reads=["hp"], writes=["hq"])
        s.op("dve", lambda v: v.tensor_scalar(out=hq[:, 0:2], in0=hq[:, 0:2], scalar1=-1.0, scalar2=None,
                                              op0=ALU.mult), reads=["hq"], writes=["hq"])
        conv_silu(qkv_d[h, 0], 0, qT, "qT")
        l2norm_inplace(qT, "qT", 128.0 ** -0.5)
        conv_silu(qkv_d[h, 1], 5, kT, "kT")
        l2norm_inplace(kT, "kT", 1.0)
        to_tm(kT, "kT", k_tm, "k_tm")
        conv_silu(qkv_d[h, 2], 10, oT, "oT")
        to_tm(oT, "oT", v_tm, "v_tm")

        for d in range(2):
            U = cst[:, C_UF + d, :]
            NT = cst[:, C_NTF + d, :]
            NS = cst[:, C_NSF + d, :]
            if gates_d is not None:
                s.dma("sp", st_hp, gt[:], gates_d[h, d].rearrange("g p n -> p g n"), writes=["gt"])
            else:
                for gi in range(2):
                    s.dma("sp", st_hp, grow[:, gi, :], gr_d[gi * 32 + d * 16 + h].rearrange("(n p) -> n p", p=128),
                          writes=["grow"])
                ptg, pkg = psr.next()
                s.group("pe", [lambda pe, gi=gi, ptg=ptg: pe.transpose(ptg[:, gi * NSB:(gi + 1) * NSB], grow[:, gi, :],
                                                                       cst[0:NSB, C_ID, 0:NSB]) for gi in range(2)],
                        reads=["grow", "cst"], writes=[pkg])
                s.op("act", lambda a, ptg=ptg: a.activation(out=gt[:], in_=ptg[:, 0:2 * NSB].rearrange("p (a b) -> p a b", a=2),
                                                            func=AF.Copy), reads=[pkg], writes=["gt"])
            s.op("act", lambda a: a.activation(out=beta_tm[:], in_=gt[:, 0, :], func=AF.Sigmoid),
                 reads=["gt"], writes=["beta_tm"])
            s.op("dve", lambda v: v.tensor_scalar(out=nbeta_tm[:], in0=beta_tm[:], scalar1=-1.0, scalar2=None,
                                                  op0=ALU.mult), reads=["beta_tm"], writes=["nbeta_tm"])
            s.op("act", lambda a, d=d: a.activation(out=tsm[:], in_=gt[:, 1, :], func=AF.Exp,
                                                    bias=hp[:, 18 + d:19 + d]), reads=["gt", "hp"], writes=["tsm"])
            s.op("dve", lambda v: v.tensor_scalar(out=tsm[:], in0=tsm[:], scalar1=1.0, scalar2=None, op0=ALU.add),
                 reads=["tsm"], writes=["tsm"])
            s.op("act", lambda a: a.activation(out=tsm[:], in_=tsm[:], func=AF.Ln), reads=["tsm"], writes=["tsm"])
            s.op("dve", lambda v, d=d: v.tensor_scalar(out=g_tm[:], in0=tsm[:], scalar1=hq[:, d:d + 1], scalar2=None,
                                                       op0=ALU.mult), reads=["tsm", "hq"], writes=["g_tm"])
            pt, pk = psr.next()
            mm(pt[:, 0:NSB], U, g_tm[:], ["cst", "g_tm"], pk)
            s.op("act", lambda a: a.activation(out=gc_tm[:], in_=pt[:, 0:NSB], func=AF.Copy), reads=[pk], writes=["gc_tm"])
            s.op("dve", lambda v: v.tensor_scalar(out=ngc_tm[:], in0=pt[:, 0:NSB], scalar1=-1.0, scalar2=None,
                                                  op0=ALU.mult), reads=[pk], writes=["ngc_tm"])
            pt2, pk2 = psr.next()
            mm(pt2[:, 0:NSB], cst[:, C_BD, :], g_tm[:], ["cst", "g_tm"], pk2)
            s.op("dve", lambda v: v.tensor_tensor(out=tsm[:], in0=pt2[:, 0:NSB], in1=gc_tm[:], op=ALU.subtract),
                 reads=[pk2, "gc_tm"], writes=["tsm"])
            s.op("act", lambda a: a.activation(out=tsm[:], in_=tsm[:], func=AF.Exp), reads=["tsm"], writes=["tsm"])
            for hf in range(2):
                s.op("dve", lambda v, hf=hf: v.tensor_scalar(out=ekd_tm[:, hf, :], in0=tsm[:],
                                                             scalar1=cst[:, C_MISC, hf:hf + 1], scalar2=None,
                                                             op0=ALU.mult), reads=["tsm", "cst"], writes=["ekd_tm"])
            s.op("act", lambda a: a.activation(out=bexp_tm[:], in_=gc_tm[:], func=AF.Exp), reads=["gc_tm"], writes=["bexp_tm"])
            s.op("dve", lambda v: v.tensor_tensor(out=bexp_tm[:], in0=bexp_tm[:], in1=beta_tm[:], op=ALU.mult),
                 reads=["bexp_tm", "beta_tm"], writes=["bexp_tm"])
            for hf in range(2):
                pt3, pk3 = psr.next()
                mm(pt3[:, 0:NSB], cst[:, C_H0 + hf, :], g_tm[:], ["cst", "g_tm"], pk3)
                s.op("act", lambda a, hf=hf, pt3=pt3: a.activation(out=glast[:, hf, :], in_=pt3[:, 0:NSB], func=AF.Exp),
                     reads=[pk3], writes=["glast"])
            s.op("dve", lambda v: v.memset(S[:], 0.0), writes=["S"])

            nseg = NSB // SEG
            seg_order = range(nseg) if d == 0 else range(nseg - 1, -1, -1)
            for sg in seg_order:
                for si in range(SEG):
                    sb = sg * SEG + si
                    tsl = slice(sb * 128, (sb + 1) * 128)
                    kx = "_%d" % si
                    s.op("dve", lambda v, sb=sb: v.tensor_scalar(out=gbc[:], in0=ones, scalar1=g_tm[:, sb:sb + 1],
                                                                 scalar2=None, op0=ALU.mult),
                         reads=["cst", "g_tm"], writes=["gbc"])
                    pg, kg = psr.next()
                    mm(pg[:, 0:128], gbc[:], U, ["gbc", "cst"], kg)
                    s.op("act", lambda a, pg=pg: a.activation(out=erow[:], in_=pg[:, 0:128], func=AF.Exp),
                         reads=[kg], writes=["erow"])
                    s.op("dve", lambda v, pg=pg, sb=sb: v.scalar_tensor_tensor(
                        out=dT[:], in0=pg[:, 0:128], scalar=gc_tm[:, sb:sb + 1], in1=NT, op0=ALU.subtract, op1=ALU.add),
                         reads=[kg, "gc_tm", "cst"], writes=["dT"])
                    s.op("act", lambda a: a.activation(out=dT[:], in_=dT[:], func=AF.Exp), reads=["dT"], writes=["dT"])
                    s.op("dve", lambda v, pg=pg: v.scalar_tensor_tensor(
                        out=dS[:], in0=pg[:, 0:128], scalar=-1.0, in1=NS, op0=ALU.mult, op1=ALU.add),
                         reads=[kg, "cst"], writes=["dS"])
                    s.op("act", lambda a, sb=sb: a.activation(out=dS[:], in_=dS[:], func=AF.Exp,
                                                              bias=gc_tm[:, sb:sb + 1]),
                         reads=["dS", "gc_tm"], writes=["dS"])
                    s.op("dve", lambda v, si=si, tsl=tsl: v.tensor_tensor(out=qd_sg[:, si, :], in0=qT[:, tsl], in1=erow[:],
                                                                         op=ALU.mult),
                         reads=["qT", "erow"], writes=["qd" + kx])
                    pk_, kk_ = psr.next()
                    mm(pk_[:, 0:128], kT[:, tsl], kT[:, tsl], ["kT"], kk_)
                    s.op("dve", lambda v, pk_=pk_, sb=sb: v.scalar_tensor_tensor(
                        out=PmT[0][:], in0=pk_[:, 0:128], scalar=nbeta_tm[:, sb:sb + 1], in1=dS[:],
                        op0=ALU.mult, op1=ALU.mult), reads=[kk_, "nbeta_tm", "dS"], writes=["PmT0"])
                    pr, kr = psr.next()
                    s.op("pe", lambda pe, pr=pr: pe.transpose(pr[:, 0:128], PmT[0][:], ident), reads=["PmT0", "cst"],
                         writes=[kr])
                    s.op("act", lambda a, pr=pr: a.activation(out=Pm[0][:], in_=pr[:, 0:128], func=AF.Copy),
                         reads=[kr], writes=["Pm0"])
                    s.op("dve", lambda v, pr=pr: v.tensor_tensor(out=X[:], in0=pr[:, 0:128], in1=ident, op=ALU.add),
                         reads=[kr, "cst"], writes=["X"])
                    pq_, kq_ = psr.next()
                    mm(pq_[:, 0:128], kT[:, tsl], qT[:, tsl], ["kT", "qT"], kq_)
                    s.op("dve", lambda v, pq_=pq_, si=si: v.tensor_tensor(out=qk_sg[:, si, :], in0=pq_[:, 0:128], in1=dT[:],
                                                                         op=ALU.mult),
                         reads=[kq_, "dT"], writes=["qk" + kx])
                    cur = 0
                    for lvl in range(1, 6):
                        nxt = 1 - cur
                        pa, ka = psr.next()
                        mm(pa[:, 0:128], Pm[cur][:], PmT[cur][:], ["Pm%d" % cur, "PmT%d" % cur], ka)
                        if lvl < 5:
                            pb, kb = psr.next()
                            mm(pb[:, 0:128], PmT[cur][:], Pm[cur][:], ["Pm%d" % cur, "PmT%d" % cur], kb)
                        s.op("act", lambda a, pa=pa, nxt=nxt: a.activation(out=PmT[nxt][:], in_=pa[:, 0:128], func=AF.Copy),
                             reads=[ka], writes=["PmT%d" % nxt])
                        if lvl < 5:
                            s.op("dve", lambda v, pb=pb, nxt=nxt: v.tensor_copy(out=Pm[nxt][:], in_=pb[:, 0:128]),
                                 reads=[kb], writes=["Pm%d" % nxt])
                        px, kxp = psr.next()
                        mm(px[:, 0:128], PmT[nxt][:], X[:], ["PmT%d" % nxt, "X"], kxp)
                        s.op("dve", lambda v, px=px: v.tensor_tensor(out=X[:], in0=X[:], in1=px[:, 0:128], op=ALU.add),
                             reads=[kxp, "X"], writes=["X"])
                        cur = nxt
                    s.op("dve", lambda v, sb=sb: v.tensor_scalar(out=vb[:], in0=v_tm[:, sb, :], scalar1=beta_tm[:, sb:sb + 1],
                                                                 scalar2=None, op0=ALU.mult),
                         reads=["v_tm", "beta_tm"], writes=["vb"])
                    s.op("dve", lambda v, sb=sb: v.tensor_scalar(out=kbg[:], in0=k_tm[:, sb, :], scalar1=bexp_tm[:, sb:sb + 1],
                                                                 scalar2=None, op0=ALU.mult),
                         reads=["k_tm", "bexp_tm"], writes=["kbg"])
                    pu, ku = psr.next()
                    mm(pu[:, 0:128], X[:], vb[:], ["X", "vb"], ku)
                    s.op("act", lambda a, pu=pu, si=si: a.activation(out=u_sg[:, si, :], in_=pu[:, 0:128], func=AF.Copy),
                         reads=[ku], writes=["u" + kx])
                    pw, kw = psr.next()
                    mm(pw[:, 0:128], kbg[:], X[:], ["X", "kbg"], kw)
                    s.op("act", lambda a, pw=pw, si=si: a.activation(out=wT_sg[:, si, :], in_=pw[:, 0:128], func=AF.Copy),
                         reads=[kw], writes=["wT" + kx])
                    for hf in range(2):
                        s.op("dve", lambda v, sb=sb, si=si, hf=hf: v.tensor_scalar(
                            out=kd_sg[:, si, hf, :], in0=k_tm[:, sb, :], scalar1=ekd_tm[:, hf, sb:sb + 1], scalar2=None,
                            op0=ALU.mult), reads=["k_tm", "ekd_tm"], writes=["kd" + kx])
                si_order = range(SEG) if d == 0 else range(SEG - 1, -1, -1)
                for si in si_order:
                    sb = sg * SEG + si
                    kx = "_%d" % si
                    for hf in ((0, 1) if d == 0 else (1, 0)):
                        r = slice(hf * 64, (hf + 1) * 64)
                        tok = slice(sb * 128 + hf * 64, sb * 128 + (hf + 1) * 64)
                        p1, k1 = psr.next()
                        mm(p1[:, 0:128], wT_sg[:, si, :], S[:], ["wT" + kx, "S"], k1)
                        s.op("dve", lambda v, p1=p1, r=r, si=si: v.tensor_tensor(out=vnew[r, :], in0=u_sg[r, si, :],
                                                                                in1=p1[r, 0:128], op=ALU.subtract),
                             reads=[k1, "u" + kx], writes=["vnew"])
                        po, ko = psr.next()
                        s.group("pe", [
                            lambda pe, po=po, si=si, r=r: pe.matmul(po[:, 0:64], lhsT=S[:], rhs=qd_sg[:, si, r],
                                                                    start=True, stop=False),
                            lambda pe, po=po, si=si, r=r: pe.matmul(po[:, 0:64], lhsT=vnew[:], rhs=qk_sg[:, si, r],
                                                                    start=False, stop=True)],
                            reads=["S", "qd" + kx, "vnew", "qk" + kx], writes=[ko])
                        p2, k2 = psr.next()
                        mm(p2[:, 0:128], kd_sg[:, si, hf, :], vnew[:], ["kd" + kx, "vnew"], k2)
                        s.op("dve", lambda v, p2=p2, hf=hf, sb=sb: v.scalar_tensor_tensor(
                            out=S[:], in0=S[:], scalar=glast[:, hf, sb:sb + 1], in1=p2[:, 0:128],
                            op0=ALU.mult, op1=ALU.add), reads=[k2, "S", "glast"], writes=["S"])
                        if d == 0:
                            s.op("act", lambda a, po=po, tok=tok: a.activation(out=oT[:, tok], in_=po[:, 0:64], func=AF.Copy),
                                 reads=[ko], writes=["oT"])
                        else:
                            s.op("dve", lambda v, po=po, tok=tok: v.tensor_tensor(out=oT[:, tok], in0=oT[:, tok],
                                                                                in1=po[:, 0:64], op=ALU.add),
                                 reads=[ko, "oT"], writes=["oT"])
        s.dma("sp", st_in, upad[:, 2:SEQ + 2], z_d[h], writes=["upad"])
        s.op("act", lambda a: a.activation(out=ybuf[:], in_=upad[:, 2:SEQ + 2], func=AF.Silu), reads=["upad"], writes=["ybuf"])
        for tb in range(NTB):
            sl = slice(tb * 512, (tb + 1) * 512)
            s.op("act", lambda a, sl=sl: a.activation(out=sm[:], in_=oT[:, sl], func=AF.Square), reads=["oT"], writes=["sm"])
            pt, pk = psr.next()
            mm(pt[:], ones, sm[:], ["sm", "cst"], pk)
            s.op("dve", lambda v, pt=pt: v.tensor_scalar(out=sm2[:], in0=pt[:], scalar1=1.0 / 128.0, scalar2=float(RMS_EPS),
                                                         op0=ALU.mult, op1=ALU.add), reads=[pk], writes=["sm2"])
            s.op("act", lambda a: a.activation(out=sm2[:], in_=sm2[:], func=AF.Ln), reads=["sm2"], writes=["sm2"])
            s.op("act", lambda a: a.activation(out=sm2[:], in_=sm2[:], func=AF.Exp, scale=-0.5), reads=["sm2"], writes=["sm2"])
            s.op("dve", lambda v, sl=sl: v.tensor_tensor(out=sm2[:], in0=sm2[:], in1=oT[:, sl], op=ALU.mult),
                 reads=["sm2", "oT"], writes=["sm2"])
            s.op("dve", lambda v, sl=sl: v.scalar_tensor_tensor(out=ybuf[:, sl], in0=sm2[:], scalar=hp[:, 15:16],
                                                               in1=ybuf[:, sl], op0=ALU.mult, op1=ALU.mult),
                 reads=["sm2", "hp", "ybuf"], writes=["ybuf"])
        s.dma("sp", st_out, out_d[h], ybuf[:], reads=["ybuf"])
    return c.close()


def run_k3(ins_cores):
    cst = delta_consts()
    NH = ins_cores[0]["qkvT"].shape[0]
    SEQ = ins_cores[0]["qkvT"].shape[3]
    nc = build_k3(NH, SEQ)
    in_maps = [dict(m, consts=cst) for m in ins_cores]
    res = run_bass_kernel_spmd(nc, in_maps, core_ids=list(range(len(ins_cores))))
    return [r["oT"] for r in res.results]


def pack_k3(qkv, z, beta_raw, a_raw, conv_w, a_log, dt_bias, onw, heads, n_heads_total):
    S_ = qkv.shape[0]
    W = n_heads_total * 128
    NSB = S_ // 128
    NH = len(heads)
    qkvT = np.empty((NH, 3, 128, S_), np.float32)
    zT = np.empty((NH, 128, S_), np.float32)
    gates = np.empty((NH, 2, 2, 128, NSB), np.float32)
    hp = np.zeros((NH, 128, 24), np.float32)
    for i, h in enumerate(heads):
        for j in range(3):
            cols = slice(j * W + h * 128, j * W + (h + 1) * 128)
            qkvT[i, j] = qkv[:, cols].T
            hp[i, :, 5 * j:5 * j + 5] = conv_w[:, cols].T
        zT[i] = z[:, h * 128:(h + 1) * 128].T
        for d in range(2):
            gates[i, d, 0] = beta_raw[:, d * n_heads_total + h].reshape(NSB, 128).T
            gates[i, d, 1] = a_raw[:, d * n_heads_total + h].reshape(NSB, 128).T
            hp[i, :, 16 + d] = a_log[d, h]
            hp[i, :, 18 + d] = dt_bias[d, h]
        hp[i, :, 15] = onw
    return {"qkvT": qkvT, "zT": zT, "gates": gates, "hp": hp}


ALPHA = 4.0 ** 0.25
NE = 8


def build_k4(TT, moe, NHALF=1, c=None):
    c = c or Ctx()
    nc, s = c.nc, c.s
    NTB = TT // 512
    NTT = TT // 128
    FF = 7168 if moe else 5632
    xT_d = c.din("xT", [D, TT * NHALF])
    aT_d = c.din("aT", [1024, TT * NHALF])
    bT_d = c.din("bT", [D, TT * NHALF])
    gfT_d = c.din("gfT", [D, TT * NHALF])
    gdT_d = c.din("gdT", [D, TT * NHALF])
    pT_d = c.din("pT", [256, TT * NHALF])
    wf_d = c.din("wf", [1024, D])
    wdl_d = c.din("wdl", [D, D])
    wo_d = c.din("wo", [D, D])
    pg_d = c.din("pg", [D, D])
    pp_d = c.din("pp", [256, D])
    lnp_d = c.din("lnp", [128, 4, KT])
    id_d = c.din("ident", [128, 128])
    if moe:
        rw_d = c.din("rw", [128, KT, NE])
        gu_d = c.din("egu", [NE, D, 2 * FF])
        dn_d = c.din("edn", [NE, FF, D])
        experts = [(gu_d[e], dn_d[e]) for e in range(NE)]
    else:
        gu_d = c.din("gu", [D, 2 * FF])
        dn_d = c.din("dn", [FF, D])
        experts = [(gu_d, dn_d)]
    out_d = c.dout("x2T", [D, TT * NHALF])

    st_misc = s.stream("misc")
    lnp = load_small(c, "sp", st_misc, "lnp", lnp_d, [128, 4, KT])
    ident = load_small(c, "sp", st_misc, "ident", id_d, [128, 128])
    s.keys["lngb"] = {"w": (st_misc.key, st_misc.cnt, None), "r": {}}
    ones = c.sb("ones", [128, 128], F32)
    s.op("dve", lambda v: v.memset(ones[:], 1.0 / D), writes=["ones"])
    ones1 = c.sb("ones1", [128, 128], F32)
    s.op("dve", lambda v: v.memset(ones1[:], 1.0), writes=["ones1"])
    psr = c.psum_ring()
    scr = ln_scratch(c)

    acc = c.sb("acc", [128, KT, TT], F32)
    bufA = c.sb("bufA", [128, KT, TT], BF16)
    bufB = c.sb("bufB", [128, max(KT * TT, 8192 + 4 * TT)], BF16)
    mb = bufB[:, 0:KT * TT].rearrange("p (a b) -> p a b", a=KT)
    ab = bufB[:, 0:8 * TT].rearrange("p (a b) -> p a b", a=8)
    NSL = 2
    wslots = [(c.sb("w%d" % i, [128, KT, 512], BF16), "w%d" % i) for i in range(NSL)]
    wstreams = [s.stream("w%d" % i) for i in range(NSL)]
    gts = [c.sb("gt%d" % i, [128, TT], F32) for i in range(2)]
    gstr = [s.stream("gt%d" % i) for i in range(2)]
    sgb = [c.sb("sg%d" % i, [128, 512], F32) for i in range(2)]
    tmpb = c.sb("tmpb", [128, 512], F32)
    pb = c.sb("pb", [128, 2, TT], BF16)
    wpp = c.sb("wpp", [128, 2, D], BF16)
    st_act = s.stream("actin")
    cnt = {"gt": 0, "sg": 0}

    wdstr = [s.stream("wd%d" % i) for i in range(2)]
    st_out = s.stream("out")
    if moe:
        rw = load_small(c, "sp", st_misc, "rw", rw_d, [128, KT, NE])
        comb_tm = c.sb("comb_tm", [128, NTT, NE], F32)
        rs = [c.sb("rs%d" % i, [128, NE], F32) for i in range(4)]
        r1 = c.sb("r1", [128, 8], F32)
        comb_e = c.sb("comb_e", [128, TT], F32)
        lbs = [c.sb("lb%d" % i, [128, 128], F32) for i in range(2)]
    s.dma("pool", st_act, wpp[:], pp_d.rearrange("(kt p) n -> p kt n", p=128), writes=["wpp"])
    for hf in range(NHALF):
        _k4_half(locals(), hf)
    return c.close()


def _k4_half(L, hf):
    g = globals()
    (c, s, TT, NTB, NTT, FF, moe, experts, psr, scr, acc, bufA, bufB, mb, ab, NSL, wslots, wstreams, gts, gstr, sgb, tmpb, pb,
     wpp, st_act, cnt, lnp, ident, ones, ones1, wdstr, st_out) = [L[k] for k in (
        "c", "s", "TT", "NTB", "NTT", "FF", "moe", "experts", "psr", "scr", "acc", "bufA", "bufB", "mb", "ab", "NSL", "wslots",
        "wstreams", "gts", "gstr", "sgb", "tmpb", "pb", "wpp", "st_act", "cnt", "lnp", "ident", "ones", "ones1", "wdstr", "st_out")]
    xT_d, aT_d, bT_d, gfT_d, gdT_d, pT_d, wf_d, wdl_d, wo_d, pg_d, out_d = [L[k] for k in (
        "xT_d", "aT_d", "bT_d", "gfT_d", "gdT_d", "pT_d", "wf_d", "wdl_d", "wo_d", "pg_d", "out_d")]
    if moe:
        rw, comb_tm, rs, r1, comb_e, lbs = [L[k] for k in ("rw", "comb_tm", "rs", "r1", "comb_e", "lbs")]
    c0 = hf * TT
    cs = slice(c0, c0 + TT)
    s.merge_into("bufB", ["wd0", "wd1", "hT0", "hT1"])
    s.dma("pool", st_act, ab, aT_d.rearrange("(kt p) t -> p kt t", p=128)[:, :, cs], writes=["bufB"])
    s.dma("pool", st_act, bufA[:], bT_d.rearrange("(kt p) t -> p kt t", p=128)[:, :, cs], writes=["bufA"])

    def load_tile(src_d, j, func):
        i = cnt["gt"] % 2
        cnt["gt"] += 1
        s.dma("sp", gstr[i], gts[i][:], src_d[j * 128:(j + 1) * 128, cs], writes=["gt%d" % i])
        if func is not None:
            s.op("act", lambda a: a.activation(out=gts[i][:], in_=gts[i][:], func=func), reads=["gt%d" % i],
                 writes=["gt%d" % i])
        return gts[i], "gt%d" % i

    cur = {}

    def epi1(row0, rows, tb, pt, pkey):
        j = row0 // 128
        sl = slice(tb * 512, (tb + 1) * 512)
        if tb == 0:
            cur["t"] = load_tile(gfT_d, j, AF.Sigmoid)
        g, gk = cur["t"]
        s.op("dve", lambda v: v.tensor_tensor(out=acc[:, j, sl], in0=g[:, sl], in1=pt[:], op=ALU.mult),
             reads=[gk, pkey], writes=["acc"])

    gemm_fm(c, wf_d, 0, D, ab, "bufB", 8, TT, wslots, wstreams, psr, epi1)

    def epi2(row0, rows, tb, pt, pkey):
        j = row0 // 128
        sl = slice(tb * 512, (tb + 1) * 512)
        if tb == 0:
            cur["t"] = load_tile(gdT_d, j, AF.Sigmoid)
        g, gk = cur["t"]
        s.op("dve", lambda v: v.tensor_tensor(out=tmpb[:], in0=g[:, sl], in1=pt[:], op=ALU.mult),
             reads=[gk, pkey], writes=["tmpb"])
        s.op("dve", lambda v: v.tensor_tensor(out=mb[:, j, sl], in0=tmpb[:], in1=acc[:, j, sl], op=ALU.add),
             reads=["tmpb", "acc"], writes=["bufB"])

    gemm_fm(c, wdl_d, 0, D, bufA, "bufA", KT, TT, wslots, wstreams, psr, epi2)

    def epi3(row0, rows, tb, pt, pkey):
        j = row0 // 128
        sl = slice(tb * 512, (tb + 1) * 512)
        if tb == 0:
            cur["t"] = load_tile(xT_d, j, None)
        g, gk = cur["t"]
        s.op("dve", lambda v: v.scalar_tensor_tensor(out=acc[:, j, sl], in0=g[:, sl], scalar=float(ALPHA), in1=pt[:],
                                                    op0=ALU.mult, op1=ALU.add), reads=[gk, pkey], writes=["acc"])

    gemm_fm(c, wo_d, 0, D, mb, "bufB", KT, TT, wslots, wstreams, psr, epi3)

    for tb in range(NTB):
        sl = slice(tb * 512, (tb + 1) * 512)
        ln_block(c, lambda kt, sl=sl: acc[:, kt, sl], "acc", KT, lnp[:, 0, :], lnp[:, 1, :], ones, psr, scr,
                 out_bf=lambda kt, sl=sl: bufA[:, kt, sl], out_f32=lambda kt, sl=sl: acc[:, kt, sl],
                 okeys=("bufA", "acc"))

    if moe:
        for tt in range(NTT):
            pt, pk = psr.next()
            s.group("pe", [lambda pe, kt=kt, tt=tt, pt=pt: pe.matmul(pt[:, 0:NE], lhsT=acc[:, kt, tt * 128:(tt + 1) * 128],
                                                                    rhs=rw[:, kt, :], start=(kt == 0), stop=(kt == KT - 1))
                           for kt in range(KT)], reads=["acc", "rw"], writes=[pk])
            s.op("act", lambda a, pt=pt: a.activation(out=rs[0][:], in_=pt[:, 0:NE], func=AF.Copy), reads=[pk], writes=["rs0"])
            s.op("dve", lambda v: v.reduce_max(out=r1[:, 0:1], in_=rs[0][:], axis=mybir.AxisListType.X),
                 reads=["rs0"], writes=["r1"])
            s.op("dve", lambda v: v.tensor_scalar(out=rs[1][:], in0=rs[0][:], scalar1=r1[:, 0:1], scalar2=None,
                                                  op0=ALU.is_equal), reads=["rs0", "r1"], writes=["rs1"])
            s.op("dve", lambda v: v.scalar_tensor_tensor(out=rs[2][:], in0=rs[1][:], scalar=-1e30, in1=rs[0][:],
                                                        op0=ALU.mult, op1=ALU.add), reads=["rs1", "rs0"], writes=["rs2"])
            s.op("dve", lambda v: v.reduce_max(out=r1[:, 1:2], in_=rs[2][:], axis=mybir.AxisListType.X),
                 reads=["rs2"], writes=["r1"])
            s.op("dve", lambda v: v.tensor_scalar(out=rs[3][:], in0=rs[2][:], scalar1=r1[:, 1:2], scalar2=None,
                                                  op0=ALU.is_equal), reads=["rs2", "r1"], writes=["rs3"])
            s.op("dve", lambda v: v.tensor_tensor(out=r1[:, 2:3], in0=r1[:, 1:2], in1=r1[:, 0:1], op=ALU.subtract),
                 reads=["r1"], writes=["r1"])
            s.op("act", lambda a: a.activation(out=r1[:, 3:4], in_=r1[:, 2:3], func=AF.Exp), reads=["r1"], writes=["r1"])
            s.op("dve", lambda v: v.tensor_scalar(out=r1[:, 4:5], in0=r1[:, 3:4], scalar1=1.0, scalar2=None, op0=ALU.add),
                 reads=["r1"], writes=["r1"])
            s.op("dve", lambda v: v.reciprocal(out=r1[:, 5:6], in_=r1[:, 4:5]), reads=["r1"], writes=["r1"])
            s.op("dve", lambda v: v.tensor_tensor(out=r1[:, 6:7], in0=r1[:, 3:4], in1=r1[:, 5:6], op=ALU.mult),
                 reads=["r1"], writes=["r1"])
            s.op("dve", lambda v: v.tensor_scalar(out=rs[0][:], in0=rs[1][:], scalar1=r1[:, 5:6], scalar2=None,
                                                  op0=ALU.mult), reads=["rs1", "r1"], writes=["rs0"])
            s.op("dve", lambda v, tt=tt: v.scalar_tensor_tensor(out=comb_tm[:, tt, :], in0=rs[3][:], scalar=r1[:, 6:7],
                                                               in1=rs[0][:], op0=ALU.mult, op1=ALU.add),
                 reads=["rs3", "rs0", "r1"], writes=["comb_tm"])
    for kt in range(KT):
        s.op("dve", lambda v, kt=kt: v.tensor_scalar(out=acc[:, kt, :], in0=acc[:, kt, :], scalar1=float(ALPHA), scalar2=None,
                                                     op0=ALU.mult), reads=["acc"], writes=["acc"])
    wds = [bufB[:, i * 4096:(i + 1) * 4096].rearrange("p (a b) -> p a b", a=2) for i in range(2)]
    hTs = [bufB[:, 8192 + i * 2 * TT: 8192 + (i + 1) * 2 * TT].rearrange("p (a b) -> p a b", a=2) for i in range(2)]
    for k in ("wd0", "wd1", "hT0", "hT1"):
        s.merge_into(k, ["bufB"], reset=True)
    nch = FF // 256
    work = [(e, ch) for e in range(len(experts)) for ch in range(nch)]

    def issue(wi):
        e, ch = work[wi]
        gu, dn = experts[e]
        guv = gu.rearrange("(kt p) n -> p kt n", p=128)
        c0 = ch * 256
        sl = wi % NSL
        s.dma("pool", wstreams[sl], wslots[sl][0][:, :, 0:256], guv[:, :, c0:c0 + 256], writes=[wslots[sl][1]])
        s.dma("pool", wstreams[sl], wslots[sl][0][:, :, 256:512], guv[:, :, FF + c0:FF + c0 + 256], writes=[wslots[sl][1]])
        s.dma("pool", wdstr[wi % 2], wds[wi % 2], dn[c0:c0 + 256, :].rearrange("(kt p) n -> p kt n", p=128),
              writes=["wd%d" % (wi % 2)])

    for wi in range(min(NSL, len(work))):
        issue(wi)
    for wi, (e, ch) in enumerate(work):
        if moe and ch == 0:
            for tt in range(NTT):
                lb = lbs[tt % 2]
                lk = "lb%d" % (tt % 2)
                s.op("dve", lambda v, tt=tt, lb=lb, e=e: v.tensor_scalar(out=lb[:], in0=ones1[:], scalar1=comb_tm[:, tt, e:e + 1],
                                                                       scalar2=None, op0=ALU.mult),
                     reads=["ones1", "comb_tm"], writes=[lk])
                if tt % 4 == 0:
                    pc, pck = psr.next()
                s.op("pe", lambda pe, pc=pc, tt=tt, lb=lb: pe.matmul(pc[:, (tt % 4) * 128:(tt % 4 + 1) * 128], lhsT=lb[:],
                                                                   rhs=ident[:], start=True, stop=True),
                     reads=[lk, "ident"], writes=[pck])
                if tt % 4 == 3:
                    s.op("act", lambda a, pc=pc, tt=tt: a.activation(out=comb_e[:, (tt // 4) * 512:(tt // 4 + 1) * 512],
                                                                     in_=pc[:], func=AF.Copy), reads=[pck], writes=["comb_e"])
        sl = wi % NSL
        wt, wkey = wslots[sl]
        hT = hTs[wi % 2]
        hk = "hT%d" % (wi % 2)
        wd = wds[wi % 2]
        wdk = "wd%d" % (wi % 2)
        for jt in range(2):
            for tb in range(NTB):
                tsl = slice(tb * 512, (tb + 1) * 512)
                pg_, pgk = psr.next()
                pu_, puk = psr.next()
                s.group("pe", [lambda pe, kt=kt, pg_=pg_, jt=jt, tsl=tsl, wt=wt: pe.matmul(
                    pg_[:], lhsT=wt[:, kt, jt * 128:(jt + 1) * 128], rhs=bufA[:, kt, tsl], start=(kt == 0), stop=(kt == KT - 1))
                    for kt in range(KT)], reads=[wkey, "bufA"], writes=[pgk])
                s.group("pe", [lambda pe, kt=kt, pu_=pu_, jt=jt, tsl=tsl, wt=wt: pe.matmul(
                    pu_[:], lhsT=wt[:, kt, 256 + jt * 128:256 + (jt + 1) * 128], rhs=bufA[:, kt, tsl], start=(kt == 0),
                    stop=(kt == KT - 1)) for kt in range(KT)], reads=[wkey, "bufA"], writes=[puk])
                i = cnt["sg"] % 2
                cnt["sg"] += 1
                s.op("act", lambda a, i=i, pg_=pg_: a.activation(out=sgb[i][:], in_=pg_[:], func=AF.Silu),
                     reads=[pgk], writes=["sg%d" % i])
                if moe:
                    s.op("dve", lambda v, i=i, pu_=pu_: v.tensor_tensor(out=tmpb[:], in0=sgb[i][:], in1=pu_[:], op=ALU.mult),
                         reads=["sg%d" % i, puk], writes=["tmpb"])
                    s.op("dve", lambda v, jt=jt, tsl=tsl, hT=hT: v.tensor_tensor(out=hT[:, jt, tsl], in0=tmpb[:],
                                                                                in1=comb_e[:, tsl], op=ALU.mult),
                         reads=["tmpb", "comb_e"], writes=[hk])
                else:
                    s.op("dve", lambda v, i=i, pu_=pu_, jt=jt, tsl=tsl, hT=hT: v.tensor_tensor(
                        out=hT[:, jt, tsl], in0=sgb[i][:], in1=pu_[:], op=ALU.mult),
                         reads=["sg%d" % i, puk], writes=[hk])
        for j in range(KT):
            for tb in range(NTB):
                tsl = slice(tb * 512, (tb + 1) * 512)
                pd_, pdk = psr.next()
                s.group("pe", [lambda pe, jt=jt, pd_=pd_, j=j, tsl=tsl, wd=wd, hT=hT: pe.matmul(
                    pd_[:], lhsT=wd[:, jt, j * 128:(j + 1) * 128], rhs=hT[:, jt, tsl], start=(jt == 0), stop=(jt == 1))
                    for jt in range(2)], reads=[wdk, hk], writes=[pdk])
                s.op("dve", lambda v, j=j, tsl=tsl, pd_=pd_: v.tensor_tensor(out=acc[:, j, tsl], in0=acc[:, j, tsl],
                                                                            in1=pd_[:], op=ALU.add),
                     reads=[pdk, "acc"], writes=["acc"])
        if wi + NSL < len(work):
            issue(wi + NSL)

    s.dma("pool", st_act, pb[:], pT_d.rearrange("(kt p) t -> p kt t", p=128)[:, :, cs], writes=["pb"])

    def epi6(row0, rows, tb, pt, pkey):
        j = row0 // 128
        sl = slice(tb * 512, (tb + 1) * 512)
        p2, p2k = psr.next()
        s.group("pe", [lambda pe, kt=kt: pe.matmul(p2[:], lhsT=wpp[:, kt, j * 128:(j + 1) * 128], rhs=pb[:, kt, sl],
                                                   start=(kt == 0), stop=(kt == 1)) for kt in range(2)],
                reads=["wpp", "pb"], writes=[p2k])
        i = cnt["sg"] % 2
        cnt["sg"] += 1
        s.op("act", lambda a: a.activation(out=sgb[i][:], in_=pt[:], func=AF.Sigmoid), reads=[pkey], writes=["sg%d" % i])
        s.op("dve", lambda v: v.tensor_tensor(out=tmpb[:], in0=sgb[i][:], in1=p2[:], op=ALU.mult),
             reads=["sg%d" % i, p2k], writes=["tmpb"])
        s.op("dve", lambda v: v.tensor_tensor(out=acc[:, j, sl], in0=acc[:, j, sl], in1=tmpb[:], op=ALU.add),
             reads=["tmpb", "acc"], writes=["acc"])

    gemm_fm(c, pg_d, 0, D, bufA, "bufA", KT, TT, wslots, wstreams, psr, epi6)

    for tb in range(NTB):
        sl = slice(tb * 512, (tb + 1) * 512)
        ln_block(c, lambda kt, sl=sl: acc[:, kt, sl], "acc", KT, lnp[:, 2, :], lnp[:, 3, :], ones, psr, scr,
                 out_f32=lambda kt, sl=sl: acc[:, kt, sl], okeys=(None, "acc"))
    s.dma("sp", st_out, out_d.rearrange("(kt p) t -> p kt t", p=128)[:, :, cs], acc[:], reads=["acc"])


def k4_weights(inp, layer):
    moe = (layer % 2 == 1)
    w = {
        "wf": inp["w_fourier"][layer], "wdl": inp["w_delta"][layer], "wo": inp["w_out"][layer],
        "pg": inp["ple_gate"][layer], "pp": inp["ple_proj"][layer],
        "lnp": np.ascontiguousarray(np.stack([_vecT(inp["ln1_g"][layer]), _vecT(inp["ln1_b"][layer]),
                                              _vecT(inp["ln2_g"][layer]), _vecT(inp["ln2_b"][layer])], axis=1)),
        "ident": np.eye(128, dtype=np.float32),
    }
    if moe:
        w["rw"] = np.ascontiguousarray(inp["router_w"][layer // 2].reshape(KT, 128, NE).transpose(1, 0, 2))
        w["egu"] = inp["exp_gate_up"][layer // 2]
        w["edn"] = inp["exp_down"][layer // 2]
    else:
        w["gu"] = inp["ffn_gate_up"][layer // 2]
        w["dn"] = inp["ffn_down"][layer // 2]
    return w


def run_k4(acts_cores, weights, moe, NHALF=1):
    TT = acts_cores[0]["xT"].shape[1] // NHALF
    nc = build_k4(TT, moe, NHALF)
    in_maps = [dict(a, **weights) for a in acts_cores]
    res = run_bass_kernel_spmd(nc, in_maps, core_ids=list(range(len(acts_cores))))
    return [r["x2T"] for r in res.results]


def pack_k3_fm(P, b, heads, conv_w, a_log, dt_bias, onw):
    cols = slice(b * S_LEN, (b + 1) * S_LEN)
    NSB = S_LEN // 128
    NH = len(heads)
    qkvT = np.empty((NH, 3, 128, S_LEN), np.float32)
    zT = np.empty((NH, 128, S_LEN), np.float32)
    gates = np.empty((NH, 2, 2, 128, NSB), np.float32)
    hp = np.zeros((NH, 128, 24), np.float32)
    for i, h in enumerate(heads):
        for j in range(3):
            r0 = 1024 + j * 2048 + h * 128
            qkvT[i, j] = P[r0:r0 + 128, cols]
            hp[i, :, 5 * j:5 * j + 5] = conv_w[:, j * 2048 + h * 128: j * 2048 + (h + 1) * 128].T
        zT[i] = P[7168 + h * 128: 7168 + (h + 1) * 128, cols]
        for d in range(2):
            gates[i, d, 0] = P[9216 + d * 16 + h, cols].reshape(NSB, 128).T
            gates[i, d, 1] = P[9248 + d * 16 + h, cols].reshape(NSB, 128).T
            hp[i, :, 16 + d] = a_log[d, h]
            hp[i, :, 18 + d] = dt_bias[d, h]
        hp[i, :, 15] = onw
    return {"qkvT": qkvT, "zT": zT, "gates": gates, "hp": hp}


def build_fused(S=4096):
    c = Ctx()
    nc = c.nc
    HALF = S // 2
    ein = lambda name, shape, dt=F32: nc.dram_tensor(name, list(shape), dt, kind="ExternalInput").ap()
    xT = ein("xT", [D, S])
    p0T = ein("p0T", [256, S])
    p1T = ein("p1T", [256, HALF])
    embg = ein("embg", [128, KT])
    embb = ein("embb", [128, KT])
    w_in = ein("w_in", [2, D, IN_W])
    hp = ein("hp", [2, 16, 128, 24])
    dcst = ein("dconsts", [128, NCONST, 128])
    ccsc = ein("ccsc", [FG, 2 * FG], BF16)
    csm = ein("csm", [S, S], BF16)
    nsm = ein("nsm", [S, S], BF16)
    wf = ein("wf", [2, 1024, D])
    wdl = ein("wdl", [2, D, D])
    wo = ein("wo", [2, D, D])
    pg = ein("pg", [2, D, D])
    pp = ein("pp", [2, 256, D])
    lnp = ein("lnp", [2, 128, 4, KT])
    ident = ein("ident", [128, 128])
    gu = ein("gu", [D, 2 * 5632])
    dn = ein("dn", [5632, D])
    rw = ein("rw", [128, KT, NE])
    egu = ein("egu", [NE, D, 2 * 7168])
    edn = ein("edn", [NE, 7168, D])
    out = nc.dram_tensor("x2T", [D, HALF], F32, kind="ExternalOutput").ap()
    scr = lambda name, shape: nc.dram_tensor(name, list(shape), F32).ap()
    projT = scr("projT_s", [IN_W, S])
    xn = scr("xn_s", [D, S])
    A_T = scr("A_s", [1024, S])
    B_T = scr("B_s", [D, S])
    x1s = scr("x1_s", [D, S])
    TT1 = min(2048, S)
    TT4 = min(1024, HALF)
    for layer in range(2):
        xin = xT if layer == 0 else x1s
        for blk in range(S // TT1):
            cols = slice(blk * TT1, (blk + 1) * TT1)
            io = {"xT": xin[:, cols], "w": w_in[layer], "g": embg, "b": embb, "projT": projT[:, cols]}
            if layer == 0:
                io["xnT"] = xn[:, cols]
            c.begin_stage(io)
            build_k1(TT1, layer == 0, c=c)
        uview = projT[0:1024, :].rearrange("(g c) t -> g c t", g=4)
        aview = A_T.rearrange("(g c) t -> g c t", g=4)
        for gp in range(2):
            c.begin_stage({"uT": uview[2 * gp:2 * gp + 2], "ccsc": ccsc, "csm": csm, "nsm": nsm,
                           "aT": aview[2 * gp:2 * gp + 2]})
            build_k2(2, 256, c=c, S_LEN=S)
        c.begin_stage({"qkvT": projT[1024:7168, :].rearrange("(j h d) t -> h j d t", j=3, h=16),
                       "zT": projT[7168:9216, :].rearrange("(h d) t -> h d t", h=16),
                       "gate_rows": projT[9216:9280, :], "hp": hp[layer], "consts": dcst,
                       "oT": B_T.rearrange("(h d) t -> h d t", h=16)})
        build_k3v2(16, S, 4, c=c)
        ntok = S if layer == 0 else HALF
        xres = xn if layer == 0 else x1s
        io = {"xT": xres[:, 0:ntok], "aT": A_T[:, 0:ntok], "bT": B_T[:, 0:ntok], "gfT": projT[9280:11328, 0:ntok],
              "gdT": projT[11328:13376, 0:ntok], "pT": (p0T if layer == 0 else p1T),
              "wf": wf[layer], "wdl": wdl[layer], "wo": wo[layer], "pg": pg[layer], "pp": pp[layer], "lnp": lnp[layer],
              "ident": ident, "x2T": (x1s if layer == 0 else out)}
        if layer == 0:
            io.update({"gu": gu, "dn": dn})
        else:
            io.update({"rw": rw, "egu": egu, "edn": edn})
        c.begin_stage(io)
        build_k4(TT4, layer == 1, ntok // TT4, c=c)
    return c.finish_all()


def fused_inputs(inputs, ncores, S):
    f32 = lambda v: np.asarray(v, np.float32)
    x = f32(inputs["x"])
    p = f32(inputs["p"])
    HALF = S // 2
    w_in = f32(inputs["w_in"])
    w_in_sw = w_in.copy()
    w_in_sw[:, :, 9216:9232], w_in_sw[:, :, 9232:9248] = w_in[:, :, 9232:9248], w_in[:, :, 9216:9232]
    w_in_sw[:, :, 9248:9264], w_in_sw[:, :, 9264:9280] = w_in[:, :, 9264:9280], w_in[:, :, 9248:9264]
    conv_w, a_log, dt_bias, onw = f32(inputs["conv_w"]), f32(inputs["a_log"]), f32(inputs["dt_bias"]), f32(inputs["o_norm_w"])
    hps = []
    for r in range(2):
        hp = np.zeros((2, 16, 128, 24), np.float32)
        for layer in range(2):
            cw = conv_w[layer][::-1] if r else conv_w[layer]
            for h in range(16):
                for j in range(3):
                    hp[layer, h, :, 5 * j:5 * j + 5] = cw[:, j * 2048 + h * 128: j * 2048 + (h + 1) * 128].T
                hp[layer, h, :, 15] = onw[layer]
                for d in range(2):
                    ds = 1 - d if r else d
                    hp[layer, h, :, 16 + d] = a_log[layer, ds, h]
                    hp[layer, h, :, 18 + d] = dt_bias[layer, ds, h]
        hps.append(hp)
    dfts = [dft_consts(S, flip=False), dft_consts(S, flip=True)]
    shared = {
        "embg": _vecT(f32(inputs["emb_ln_g"])), "embb": _vecT(f32(inputs["emb_ln_b"])),
        "dconsts": delta_consts(),
        "wf": f32(inputs["w_fourier"]), "wdl": f32(inputs["w_delta"]), "wo": f32(inputs["w_out"]),
        "pg": f32(inputs["ple_gate"]), "pp": f32(inputs["ple_proj"]),
        "lnp": np.ascontiguousarray(np.stack([np.stack([_vecT(f32(inputs[k])[layer]) for k in ("ln1_g", "ln1_b", "ln2_g", "ln2_b")],
                                                       axis=1) for layer in range(2)])),
        "ident": np.eye(128, dtype=np.float32),
        "gu": f32(inputs["ffn_gate_up"])[0], "dn": f32(inputs["ffn_down"])[0],
        "rw": np.ascontiguousarray(f32(inputs["router_w"])[0].reshape(KT, 128, NE).transpose(1, 0, 2)),
        "egu": f32(inputs["exp_gate_up"])[0], "edn": f32(inputs["exp_down"])[0],
    }
    in_maps = []
    for cidx in range(ncores):
        b, r = cidx // 2, cidx % 2
        xb = x[b][::-1] if r else x[b]
        p0 = p[0, b][::-1] if r else p[0, b]
        p1 = p[1, b][::-1] if r else p[1, b]
        m = dict(shared)
        m.update({"xT": np.ascontiguousarray(xb.T), "p0T": np.ascontiguousarray(p0.T),
                  "p1T": np.ascontiguousarray(p1[:HALF].T), "w_in": (w_in_sw if r else w_in), "hp": hps[r],
                  "ccsc": dfts[r][0], "csm": dfts[r][1], "nsm": dfts[r][2]})
        in_maps.append(m)
    return in_maps


def fused_gather(results, B_, S):
    HALF = S // 2
    out = np.empty((B_, S, D), np.float32)
    for cidx, r_ in enumerate(results):
        b, r = cidx // 2, cidx % 2
        y = r_["x2T"].T
        if r:
            out[b, S - 1 - np.arange(HALF)] = y
        else:
            out[b, 0:HALF] = y
    return out


def kernel(**inputs):
    B_, S, _ = np.asarray(inputs["x"]).shape
    ncores = 2 * B_
    nc = build_fused(S)
    in_maps = fused_inputs(inputs, ncores, S)
    res = run_bass_kernel_spmd(nc, in_maps, core_ids=list(range(ncores)))
    return fused_gather(res.results, B_, S)


def build_k3v2(NH=8, SEQ=4096, SEG=4, c=None):
    c = c or Ctx()
    nc, s = c.nc, c.s
    NSB = SEQ // 128
    NTB = SEQ // 512
    qkv_d = c.din("qkvT", [NH, 3, 128, SEQ])
    z_d = c.din("zT", [NH, 128, SEQ])
    gr_d = c.io.get("gate_rows")
    gates_d = None if gr_d is not None else c.din("gates", [NH, 2, 2, 128, NSB])
    hp_d = c.din("hp", [NH, 128, 24])
    cst_d = c.din("consts", [128, NCONST, 128])
    out_d = c.dout("oT", [NH, 128, SEQ])

    st_c = s.stream("const")
    cst = c.sb("cst", [128, NCONST, 128], F32)
    s.dma("sp", st_c, cst[:], cst_d, writes=["cst"])
    ident = cst[:, C_ID, :]
    ones = cst[:, C_ONE, :]
    psr = c.psum_ring()
    st_in = s.stream("in")
    st_hp = s.stream("hp")
    st_g = [s.stream("g0"), s.stream("g1")]
    st_out = s.stream("out")
    upad = c.sb("upad", [128, SEQ + 4], F32)
    qT = c.sb("qT", [128, SEQ], F32)
    kT = c.sb("kT", [128, SEQ], F32)
    k_tm = c.sb("k_tm", [128, NSB, 128], F32)
    v_tm = c.sb("v_tm", [128, NSB, 128], F32)
    oTd = [c.sb("oT%d" % d, [128, SEQ], F32) for d in range(2)]
    ybuf = oTd[1]
    hp = c.sb("hp", [128, 24], F32)
    hq = c.sb("hq", [128, 8], F32)
    sm = c.sb("sm", [128, 512], F32)
    sm2 = c.sb("sm2", [128, 512], F32)

    class DirBuf:
        pass

    DB = []
    for d in range(2):
        b = DirBuf()
        t = lambda name, shape: c.sb("%s_%d" % (name, d), shape, F32)
        b.gt = t("gt", [128, 2, NSB])
        b.grow = t("grow", [NSB, 2, 128])
        b.g_tm = t("g_tm", [128, NSB])
        b.beta_tm = t("beta_tm", [128, NSB])
        b.nbeta_tm = t("nbeta_tm", [128, NSB])
        b.gc_tm = t("gc_tm", [128, NSB])
        b.bexp_tm = t("bexp_tm", [128, NSB])
        b.ekd_tm = t("ekd_tm", [128, 2, NSB])
        b.glast = t("glast", [128, 2, NSB])
        b.tsm = t("tsm", [128, NSB])
        b.S = t("S", [128, 128])
        b.vnew = t("vnew", [128, 128])
        b.gbc = t("gbc", [128, 128])
        b.erow = t("erow", [128, 128])
        b.dT = t("dT", [128, 128])
        b.dS = t("dS", [128, 128])
        b.Pm = [t("Pm%d" % i, [128, 128]) for i in range(2)]
        b.PmT = [t("PmT%d" % i, [128, 128]) for i in range(2)]
        b.X = t("X", [128, 128])
        b.vb = t("vb", [128, 128])
        b.kbg = t("kbg", [128, 128])
        b.u_sg = [t("u_sg%d" % p, [128, SEG, 128]) for p in range(2)]
        b.wT_sg = [t("wT_sg%d" % p, [128, SEG, 128]) for p in range(2)]
        b.qd_sg = [t("qd_sg%d" % p, [128, SEG, 128]) for p in range(2)]
        b.qk_sg = [t("qk_sg%d" % p, [128, SEG, 128]) for p in range(2)]
        b.kd_sg = [t("kd_sg%d" % p, [128, SEG, 2, 128]) for p in range(2)]
        b.pre_ps = psr.sub([4 * d, 4 * d + 1])
        b.scan_ps = psr.sub([4 * d + 2, 4 * d + 3])
        DB.append(b)

    s.op("dve", lambda v: v.memset(upad[:, 0:2], 0.0), writes=["upad"])
    s.op("dve", lambda v: v.memset(upad[:, SEQ + 2:SEQ + 4], 0.0), writes=["upad"])
    for d in range(2):
        s.op("dve", lambda v, d=d: v.memset(DB[d].vnew[:], 0.0), writes=["vnew_%d" % d])

    def l2norm_inplace(buf, key, scale):
        for tb in range(NTB):
            sl = slice(tb * 512, (tb + 1) * 512)
            s.op("act", lambda a, sl=sl: a.activation(out=sm[:], in_=buf[:, sl], func=AF.Square), reads=[key], writes=["sm"])
            pt, pk = psr.next()
            s.op("pe", lambda pe, pt=pt: pe.matmul(pt[:], lhsT=ones, rhs=sm[:], start=True, stop=True),
                 reads=["sm", "cst"], writes=[pk])
            s.op("dve", lambda v, pt=pt: v.tensor_scalar(out=sm2[:], in0=pt[:], scalar1=float(L2_EPS), scalar2=None,
                                                         op0=ALU.add), reads=[pk], writes=["sm2"])
            s.op("act", lambda a: a.activation(out=sm2[:], in_=sm2[:], func=AF.Ln), reads=["sm2"], writes=["sm2"])
            s.op("act", lambda a: a.activation(out=sm2[:], in_=sm2[:], func=AF.Exp, scale=-0.5),
                 reads=["sm2"], writes=["sm2"])
            s.op("dve", lambda v, sl=sl: v.scalar_tensor_tensor(out=buf[:, sl], in0=buf[:, sl], scalar=float(scale),
                                                               in1=sm2[:], op0=ALU.mult, op1=ALU.mult),
                 reads=[key, "sm2"], writes=[key])

    def conv_silu(src_ap, col0, dst, dkey):
        s.dma("sp", st_in, upad[:, 2:SEQ + 2], src_ap, writes=["upad"])
        s.op("dve", lambda v: v.tensor_scalar(out=ybuf[:], in0=upad[:, 0:SEQ], scalar1=hp[:, col0:col0 + 1],
                                              scalar2=None, op0=ALU.mult), reads=["upad", "hp"], writes=["oT1"])
        for j in range(1, 5):
            s.op("dve", lambda v, j=j: v.scalar_tensor_tensor(out=ybuf[:], in0=upad[:, j:SEQ + j],
                                                             scalar=hp[:, col0 + j:col0 + j + 1], in1=ybuf[:],
                                                             op0=ALU.mult, op1=ALU.add),
                 reads=["upad", "hp", "oT1"], writes=["oT1"])
        s.op("act", lambda a: a.activation(out=dst[:], in_=ybuf[:], func=AF.Silu), reads=["oT1"], writes=[dkey])

    def to_tm(src, skey, dst, dkey):
        for g4 in range(NSB // 4):
            pt, pk = psr.next()
            fns = [lambda pe, i=i, pt=pt, g4=g4: pe.transpose(pt[:, i * 128:(i + 1) * 128],
                                                              src[:, (g4 * 4 + i) * 128:(g4 * 4 + i + 1) * 128], ident)
                   for i in range(4)]
            s.group("pe", fns, reads=[skey, "cst"], writes=[pk])
            s.op("act", lambda a, pt=pt, g4=g4: a.activation(out=dst[:, g4 * 4:(g4 + 1) * 4, :],
                                                             in_=pt[:].rearrange("p (a b) -> p a b", a=4), func=AF.Copy),
                 reads=[pk], writes=[dkey])

    def mm(out_pt, lhsT, rhs, reads, pk):
        s.op("pe", lambda pe: pe.matmul(out_pt, lhsT=lhsT, rhs=rhs, start=True, stop=True), reads=reads, writes=[pk])

    def dir_setup(h, d):
        B = DB[d]
        K = lambda n: "%s_%d" % (n, d)
        U = cst[:, C_UF + d, :]
        ps_ = B.pre_ps
        if gates_d is not None:
            s.dma("sp", st_g[d], B.gt[:], gates_d[h, d].rearrange("g p n -> p g n"), writes=[K("gt")])
        else:
            for gi in range(2):
                s.dma("sp", st_g[d], B.grow[:, gi, :], gr_d[gi * 32 + d * 16 + h].rearrange("(n p) -> n p", p=128),
                      writes=[K("grow")])
            ptg, pkg = ps_.next()
            s.group("pe", [lambda pe, gi=gi, ptg=ptg: pe.transpose(ptg[:, gi * NSB:(gi + 1) * NSB], B.grow[:, gi, :],
                                                                   cst[0:NSB, C_ID, 0:NSB]) for gi in range(2)],
                    reads=[K("grow"), "cst"], writes=[pkg])
            s.op("act", lambda a, ptg=ptg: a.activation(out=B.gt[:], in_=ptg[:, 0:2 * NSB].rearrange("p (a b) -> p a b", a=2),
                                                        func=AF.Copy), reads=[pkg], writes=[K("gt")])
        s.op("act", lambda a: a.activation(out=B.beta_tm[:], in_=B.gt[:, 0, :], func=AF.Sigmoid),
             reads=[K("gt")], writes=[K("beta_tm")])
        s.op("dve", lambda v: v.tensor_scalar(out=B.nbeta_tm[:], in0=B.beta_tm[:], scalar1=-1.0, scalar2=None,
                                              op0=ALU.mult), reads=[K("beta_tm")], writes=[K("nbeta_tm")])
        s.op("act", lambda a: a.activation(out=B.tsm[:], in_=B.gt[:, 1, :], func=AF.Exp, bias=hp[:, 18 + d:19 + d]),
             reads=[K("gt"), "hp"], writes=[K("tsm")])
        s.op("dve", lambda v: v.tensor_scalar(out=B.tsm[:], in0=B.tsm[:], scalar1=1.0, scalar2=None, op0=ALU.add),
             reads=[K("tsm")], writes=[K("tsm")])
        s.op("act", lambda a: a.activation(out=B.tsm[:], in_=B.tsm[:], func=AF.Ln), reads=[K("tsm")], writes=[K("tsm")])
        s.op("dve", lambda v: v.tensor_scalar(out=B.g_tm[:], in0=B.tsm[:], scalar1=hq[:, d:d + 1], scalar2=None,
                                              op0=ALU.mult), reads=[K("tsm"), "hq"], writes=[K("g_tm")])
        pt, pk = ps_.next()
        mm(pt[:, 0:NSB], U, B.g_tm[:], ["cst", K("g_tm")], pk)
        s.op("act", lambda a: a.activation(out=B.gc_tm[:], in_=pt[:, 0:NSB], func=AF.Copy), reads=[pk], writes=[K("gc_tm")])
        pt2, pk2 = ps_.next()
        mm(pt2[:, 0:NSB], cst[:, C_BD, :], B.g_tm[:], ["cst", K("g_tm")], pk2)
        s.op("dve", lambda v: v.tensor_tensor(out=B.tsm[:], in0=pt2[:, 0:NSB], in1=B.gc_tm[:], op=ALU.subtract),
             reads=[pk2, K("gc_tm")], writes=[K("tsm")])
        s.op("act", lambda a: a.activation(out=B.tsm[:], in_=B.tsm[:], func=AF.Exp), reads=[K("tsm")], writes=[K("tsm")])
        for hf in range(2):
            s.op("dve", lambda v, hf=hf: v.tensor_scalar(out=B.ekd_tm[:, hf, :], in0=B.tsm[:],
                                                         scalar1=cst[:, C_MISC, hf:hf + 1], scalar2=None,
                                                         op0=ALU.mult), reads=[K("tsm"), "cst"], writes=[K("ekd_tm")])
        s.op("act", lambda a: a.activation(out=B.bexp_tm[:], in_=B.gc_tm[:], func=AF.Exp),
             reads=[K("gc_tm")], writes=[K("bexp_tm")])
        s.op("dve", lambda v: v.tensor_tensor(out=B.bexp_tm[:], in0=B.bexp_tm[:], in1=B.beta_tm[:], op=ALU.mult),
             reads=[K("bexp_tm"), K("beta_tm")], writes=[K("bexp_tm")])
        for hf in range(2):
            pt3, pk3 = ps_.next()
            mm(pt3[:, 0:NSB], cst[:, C_H0 + hf, :], B.g_tm[:], ["cst", K("g_tm")], pk3)
            s.op("act", lambda a, hf=hf, pt3=pt3: a.activation(out=B.glast[:, hf, :], in_=pt3[:, 0:NSB], func=AF.Exp),
                 reads=[pk3], writes=[K("glast")])
        s.op("dve", lambda v: v.memset(B.S[:], 0.0), writes=[K("S")])

    def dir_pre(d, sg, par):
        B = DB[d]
        K = lambda n: "%s_%d" % (n, d)
        U = cst[:, C_UF + d, :]
        NT = cst[:, C_NTF + d, :]
        NS = cst[:, C_NSF + d, :]
        ps_ = B.pre_ps
        for si in range(SEG):
            sb = sg * SEG + si
            tsl = slice(sb * 128, (sb + 1) * 128)
            kx = "_%d_%d_%d" % (d, par, si)
            s.op("dve", lambda v, sb=sb: v.tensor_scalar(out=B.gbc[:], in0=ones, scalar1=B.g_tm[:, sb:sb + 1],
                                                         scalar2=None, op0=ALU.mult),
                 reads=["cst", K("g_tm")], writes=[K("gbc")])
            pg, kg = ps_.next()
            mm(pg[:, 0:128], B.gbc[:], U, [K("gbc"), "cst"], kg)
            s.op("act", lambda a, pg=pg: a.activation(out=B.erow[:], in_=pg[:, 0:128], func=AF.Exp),
                 reads=[kg], writes=[K("erow")])
            s.op("dve", lambda v, pg=pg, sb=sb: v.scalar_tensor_tensor(
                out=B.dT[:], in0=pg[:, 0:128], scalar=B.gc_tm[:, sb:sb + 1], in1=NT, op0=ALU.subtract, op1=ALU.add),
                 reads=[kg, K("gc_tm"), "cst"], writes=[K("dT")])
            s.op("act", lambda a: a.activation(out=B.dT[:], in_=B.dT[:], func=AF.Exp), reads=[K("dT")], writes=[K("dT")])
            s.op("dve", lambda v, pg=pg: v.scalar_tensor_tensor(
                out=B.dS[:], in0=pg[:, 0:128], scalar=-1.0, in1=NS, op0=ALU.mult, op1=ALU.add),
                 reads=[kg, "cst"], writes=[K("dS")])
            s.op("act", lambda a, sb=sb: a.activation(out=B.dS[:], in_=B.dS[:], func=AF.Exp, bias=B.gc_tm[:, sb:sb + 1]),
                 reads=[K("dS"), K("gc_tm")], writes=[K("dS")])
            s.op("dve", lambda v, si=si, tsl=tsl: v.tensor_tensor(out=B.qd_sg[par][:, si, :], in0=qT[:, tsl], in1=B.erow[:],
                                                                 op=ALU.mult),
                 reads=["qT", K("erow")], writes=["qd" + kx])
            pk_, kk_ = ps_.next()
            mm(pk_[:, 0:128], kT[:, tsl], kT[:, tsl], ["kT"], kk_)
            s.op("dve", lambda v, pk_=pk_, sb=sb: v.scalar_tensor_tensor(
                out=B.PmT[0][:], in0=pk_[:, 0:128], scalar=B.nbeta_tm[:, sb:sb + 1], in1=B.dS[:],
                op0=ALU.mult, op1=ALU.mult), reads=[kk_, K("nbeta_tm"), K("dS")], writes=[K("PmT0")])
            pr, kr = ps_.next()
            s.op("pe", lambda pe, pr=pr: pe.transpose(pr[:, 0:128], B.PmT[0][:], ident), reads=[K("PmT0"), "cst"],
                 writes=[kr])
            s.op("act", lambda a, pr=pr: a.activation(out=B.Pm[0][:], in_=pr[:, 0:128], func=AF.Copy),
                 reads=[kr], writes=[K("Pm0")])
            s.op("dve", lambda v, pr=pr: v.tensor_tensor(out=B.X[:], in0=pr[:, 0:128], in1=ident, op=ALU.add),
                 reads=[kr, "cst"], writes=[K("X")])
            pq_, kq_ = ps_.next()
            mm(pq_[:, 0:128], kT[:, tsl], qT[:, tsl], ["kT", "qT"], kq_)
            s.op("dve", lambda v, pq_=pq_, si=si: v.tensor_tensor(out=B.qk_sg[par][:, si, :], in0=pq_[:, 0:128], in1=B.dT[:],
                                                                 op=ALU.mult),
                 reads=[kq_, K("dT")], writes=["qk" + kx])
            cur = 0
            for lvl in range(1, 6):
                nxt = 1 - cur
                pa, ka = ps_.next()
                mm(pa[:, 0:128], B.Pm[cur][:], B.PmT[cur][:], [K("Pm%d" % cur), K("PmT%d" % cur)], ka)
                s.op("act", lambda a, pa=pa, nxt=nxt: a.activation(out=B.PmT[nxt][:], in_=pa[:, 0:128], func=AF.Copy),
                     reads=[ka], writes=[K("PmT%d" % nxt)])
                if lvl < 5:
                    pb, kb = ps_.next()
                    mm(pb[:, 0:128], B.PmT[cur][:], B.Pm[cur][:], [K("Pm%d" % cur), K("PmT%d" % cur)], kb)
                    s.op("dve", lambda v, pb=pb, nxt=nxt: v.tensor_copy(out=B.Pm[nxt][:], in_=pb[:, 0:128]),
                         reads=[kb], writes=[K("Pm%d" % nxt)])
                px, kxp = ps_.next()
                mm(px[:, 0:128], B.PmT[nxt][:], B.X[:], [K("PmT%d" % nxt), K("X")], kxp)
                s.op("dve", lambda v, px=px: v.tensor_tensor(out=B.X[:], in0=B.X[:], in1=px[:, 0:128], op=ALU.add),
                     reads=[kxp, K("X")], writes=[K("X")])
                cur = nxt
            s.op("dve", lambda v, sb=sb: v.tensor_scalar(out=B.vb[:], in0=v_tm[:, sb, :], scalar1=B.beta_tm[:, sb:sb + 1],
                                                         scalar2=None, op0=ALU.mult),
                 reads=["v_tm", K("beta_tm")], writes=[K("vb")])
            s.op("dve", lambda v, sb=sb: v.tensor_scalar(out=B.kbg[:], in0=k_tm[:, sb, :], scalar1=B.bexp_tm[:, sb:sb + 1],
                                                         scalar2=None, op0=ALU.mult),
                 reads=["k_tm", K("bexp_tm")], writes=[K("kbg")])
            pu, ku = ps_.next()
            mm(pu[:, 0:128], B.X[:], B.vb[:], [K("X"), K("vb")], ku)
            s.op("act", lambda a, pu=pu, si=si: a.activation(out=B.u_sg[par][:, si, :], in_=pu[:, 0:128], func=AF.Copy),
                 reads=[ku], writes=["u" + kx])
            pw, kw = ps_.next()
            mm(pw[:, 0:128], B.kbg[:], B.X[:], [K("X"), K("kbg")], kw)
            s.op("act", lambda a, pw=pw, si=si: a.activation(out=B.wT_sg[par][:, si, :], in_=pw[:, 0:128], func=AF.Copy),
                 reads=[kw], writes=["wT" + kx])
            for hf in range(2):
                s.op("dve", lambda v, sb=sb, si=si, hf=hf: v.tensor_scalar(
                    out=B.kd_sg[par][:, si, hf, :], in0=k_tm[:, sb, :], scalar1=B.ekd_tm[:, hf, sb:sb + 1], scalar2=None,
                    op0=ALU.mult), reads=["k_tm", K("ekd_tm")], writes=["kd" + kx])

    def dir_scan(d, sg, par):
        B = DB[d]
        K = lambda n: "%s_%d" % (n, d)
        ps_ = B.scan_ps
        oT = oTd[d]
        ok = "oT%d" % d
        si_order = range(SEG) if d == 0 else range(SEG - 1, -1, -1)
        for si in si_order:
            sb = sg * SEG + si
            kx = "_%d_%d_%d" % (d, par, si)
            for hf in ((0, 1) if d == 0 else (1, 0)):
                r = slice(hf * 64, (hf + 1) * 64)
                tok = slice(sb * 128 + hf * 64, sb * 128 + (hf + 1) * 64)
                p1, k1 = ps_.next()
                mm(p1[:, 0:128], B.wT_sg[par][:, si, :], B.S[:], ["wT" + kx, K("S")], k1)
                s.op("dve", lambda v, p1=p1, r=r, si=si: v.tensor_tensor(out=B.vnew[r, :], in0=B.u_sg[par][r, si, :],
                                                                        in1=p1[r, 0:128], op=ALU.subtract),
                     reads=[k1, "u" + kx], writes=[K("vnew")])
                po, ko = ps_.next()
                s.group("pe", [
                    lambda pe, po=po, si=si, r=r: pe.matmul(po[:, 0:64], lhsT=B.S[:], rhs=B.qd_sg[par][:, si, r],
                                                            start=True, stop=False),
                    lambda pe, po=po, si=si, r=r: pe.matmul(po[:, 0:64], lhsT=B.vnew[:], rhs=B.qk_sg[par][:, si, r],
                                                            start=False, stop=True),
                    lambda pe, po=po, si=si, hf=hf: pe.matmul(po[:, 128:256], lhsT=B.kd_sg[par][:, si, hf, :], rhs=B.vnew[:],
                                                              start=True, stop=True)],
                    reads=[K("S"), "qd" + kx, K("vnew"), "qk" + kx, "kd" + kx], writes=[ko])
                s.op("dve", lambda v, po=po, hf=hf, sb=sb: v.scalar_tensor_tensor(
                    out=B.S[:], in0=B.S[:], scalar=B.glast[:, hf, sb:sb + 1], in1=po[:, 128:256],
                    op0=ALU.mult, op1=ALU.add), reads=[ko, K("S"), K("glast")], writes=[K("S")])
                s.op("act", lambda a, po=po, tok=tok: a.activation(out=oT[:, tok], in_=po[:, 0:64], func=AF.Copy),
                     reads=[ko], writes=[ok])

    def deferred(fn, *a):
        ch = []
        s.chain = ch
        try:
            fn(*a)
        finally:
            s.chain = None
        return ch

    nseg = NSB // SEG
    for h in range(NH):
        s.dma("sp", st_hp, hp[:], hp_d[h], writes=["hp"])
        s.op("act", lambda a: a.activation(out=hq[:, 0:2], in_=hp[:, 16:18], func=AF.Exp), reads=["hp"], writes=["hq"])
        s.op("dve", lambda v: v.tensor_scalar(out=hq[:, 0:2], in0=hq[:, 0:2], scalar1=-1.0, scalar2=None,
                                              op0=ALU.mult), reads=["hq"], writes=["hq"])
        conv_silu(qkv_d[h, 0], 0, qT, "qT")
        l2norm_inplace(qT, "qT", 128.0 ** -0.5)
        conv_silu(qkv_d[h, 1], 5, kT, "kT")
        l2norm_inplace(kT, "kT", 1.0)
        to_tm(kT, "kT", k_tm, "k_tm")
        conv_silu(qkv_d[h, 2], 10, oTd[0], "oT0")
        to_tm(oTd[0], "oT0", v_tm, "v_tm")
        s.run_chains([deferred(dir_setup, h, 0), deferred(dir_setup, h, 1)])
        order = [list(range(nseg)), list(range(nseg - 1, -1, -1))]
        for ph in range(nseg + 1):
            chains = []
            for d in range(2):
                if ph >= 1:
                    chains.append(deferred(dir_scan, d, order[d][ph - 1], (ph - 1) % 2))
                if ph < nseg:
                    chains.append(deferred(dir_pre, d, order[d][ph], ph % 2))
            s.run_chains(chains)
        s.dma("sp", st_in, upad[:, 2:SEQ + 2], z_d[h], writes=["upad"])
        s.op("act", lambda a: a.activation(out=upad[:, 2:SEQ + 2], in_=upad[:, 2:SEQ + 2], func=AF.Silu),
             reads=["upad"], writes=["upad"])
        for tb in range(NTB):
            sl = slice(tb * 512, (tb + 1) * 512)
            sl2 = slice(tb * 512 + 2, (tb + 1) * 512 + 2)
            s.op("dve", lambda v, sl=sl: v.tensor_tensor(out=oTd[0][:, sl], in0=oTd[0][:, sl], in1=oTd[1][:, sl], op=ALU.add),
                 reads=["oT0", "oT1"], writes=["oT0"])
            s.op("act", lambda a, sl=sl: a.activation(out=sm[:], in_=oTd[0][:, sl], func=AF.Square), reads=["oT0"], writes=["sm"])
            pt, pk = psr.next()
            mm(pt[:], ones, sm[:], ["sm", "cst"], pk)
            s.op("dve", lambda v, pt=pt: v.tensor_scalar(out=sm2[:], in0=pt[:], scalar1=1.0 / 128.0, scalar2=float(RMS_EPS),
                                                         op0=ALU.mult, op1=ALU.add), reads=[pk], writes=["sm2"])
            s.op("act", lambda a: a.activation(out=sm2[:], in_=sm2[:], func=AF.Ln), reads=["sm2"], writes=["sm2"])
            s.op("act", lambda a: a.activation(out=sm2[:], in_=sm2[:], func=AF.Exp, scale=-0.5), reads=["sm2"], writes=["sm2"])
            s.op("dve", lambda v, sl=sl: v.tensor_tensor(out=sm2[:], in0=sm2[:], in1=oTd[0][:, sl], op=ALU.mult),
                 reads=["sm2", "oT0"], writes=["sm2"])
            s.op("dve", lambda v, sl=sl, sl2=sl2: v.scalar_tensor_tensor(out=oTd[0][:, sl], in0=sm2[:], scalar=hp[:, 15:16],
                                                                        in1=upad[:, sl2], op0=ALU.mult, op1=ALU.mult),
                 reads=["sm2", "hp", "upad"], writes=["oT0"])
        s.dma("sp", st_out, out_d[h], oTd[0][:], reads=["oT0"])
    return c.close()
```

```python
import contextlib
import numpy as np
import concourse.bass as bass
import concourse.mybir as mybir
from concourse.bass_utils import run_bass_kernel_spmd

F32 = mybir.dt.float32
BF16 = mybir.dt.bfloat16
AF = mybir.ActivationFunctionType
ALU = mybir.AluOpType

D = 2048
KT = 16
NCORES = 8
IN_W = 13376
LN_EPS = 1e-5


class Stream:
    def __init__(self, key, sem):
        self.key = key
        self.sem = sem
        self.cnt = 0


class Sched:
    def __init__(self, nc, stack, same_sync=True):
        self.nc = nc
        self.stack = stack
        self.same_sync = same_sync
        self.eng = {"pe": nc.tensor, "act": nc.scalar, "dve": nc.vector, "pool": nc.gpsimd, "sp": nc.sync}
        self.semh = {}
        self.cnt = {}
        self.waited = {}
        for e in self.eng:
            self.semh[e] = stack.enter_context(nc.semaphore("sem_" + e))
            self.cnt[e] = 0
            self.waited[e] = {}
        self.keys = {}
        self.chain = None
        self.excl = set()
        self.streams = []
        self.stream_by_name = {}
        self.nstream = 0

    def stream(self, name=None):
        if name is not None and name in self.stream_by_name:
            return self.stream_by_name[name]
        st = self._new_stream(name)
        if name is not None:
            self.stream_by_name[name] = st
        return st

    def barrier(self):
        for e in self.eng:
            for e2 in self.eng:
                if e2 != e and self.cnt[e2] and self.waited[e].get(e2, 0) < self.cnt[e2]:
                    self.eng[e].wait_ge(self.semh[e2], self.cnt[e2])
                    self.waited[e][e2] = self.cnt[e2]
            for st in self.streams:
                if st.cnt and self.waited[e].get(st.key, 0) < st.cnt:
                    self.eng[e].wait_ge(st.sem, st.cnt)
                    self.waited[e][st.key] = st.cnt
        self.keys = {}

    def _new_stream(self, name=None):
        self.nstream += 1
        key = "dma%d_%s" % (self.nstream, name or "")
        sem = self.stack.enter_context(self.nc.semaphore(key))
        st = Stream(key, sem)
        self.semh[key] = sem
        self.streams.append(st)
        return st

    def _deps(self, e, reads, writes):
        deps = {}

        def add(ev):
            if ev is None:
                return
            k, v, pe = ev
            if pe == e and not self.same_sync:
                return
            if deps.get(k, 0) < v:
                deps[k] = v

        for k in reads:
            st = self.keys.get(k)
            if st:
                add(st["w"])
                if k in self.excl:
                    for ev in st["r"].values():
                        if ev[2] != e:
                            add(ev)
        for k in writes:
            st = self.keys.get(k)
            if st:
                add(st["w"])
                for ev in st["r"].values():
                    add(ev)
        for k, v in deps.items():
            if self.waited[e].get(k, 0) >= v:
                continue
            self.eng[e].wait_ge(self.semh[k], v)
            self.waited[e][k] = v

    def _record(self, ev, reads, writes):
        for k in reads:
            st = self.keys.setdefault(k, {"w": None, "r": {}})
            st["r"][ev[0]] = ev
        for k in writes:
            self.keys[k] = {"w": ev, "r": {}}

    EST_DUR = {"pe": 0.28, "act": 0.40, "dve": 0.33, "pool": 0.50, "sp": 2.0}

    def run_chains(self, chains):
        chs = [list(ch) for ch in chains if ch]
        pos = [0] * len(chs)
        free = {}
        wr = {}
        rd = {}
        last = [0.0] * len(chs)
        LAT = 0.15

        def est(item):
            kind, args = item
            if kind == "dma":
                e, reads, writes, dur = args[0], args[4], args[5], self.EST_DUR["sp"]
            else:
                e, reads, writes = args[0], args[2], args[3]
                dur = self.EST_DUR[e] * (len(args[1]) if kind == "group" else 1)
            t = free.get(e, 0.0)
            for k in reads:
                w = wr.get(k)
                if w:
                    t = max(t, w[0] + (LAT if w[1] != e else 0.0))
            for k in writes:
                w = wr.get(k)
                if w:
                    t = max(t, w[0] + (LAT if w[1] != e else 0.0))
                r = rd.get(k)
                if r:
                    t = max(t, r[0] + (LAT if r[1] != e else 0.0))
            return t, dur, e, reads, writes

        while True:
            best = None
            for ci in range(len(chs)):
                if pos[ci] >= len(chs[ci]):
                    continue
                info = est(chs[ci][pos[ci]])
                key = (info[0], last[ci], ci)
                if best is None or key < best[0]:
                    best = (key, ci, info)
            if best is None:
                break
            _, ci, (t, dur, e, reads, writes) = best
            kind, args = chs[ci][pos[ci]]
            pos[ci] += 1
            getattr(self, kind)(*args)
            fin = t + dur
            free[e] = fin
            last[ci] = fin
            for k in reads:
                r = rd.get(k)
                if not r or r[0] < fin:
                    rd[k] = (fin, e)
            for k in writes:
                wr[k] = (fin, e)
                rd.pop(k, None)

    def op(self, e, fn, reads=(), writes=()):
        if self.chain is not None:
            self.chain.append(("op", (e, fn, tuple(reads), tuple(writes))))
            return
        self._deps(e, reads, writes)
        ins = fn(self.eng[e])
        self.cnt[e] += 1
        ins.then_inc(self.semh[e], 1)
        self._record((e, self.cnt[e], e), reads, writes)

    def group(self, e, fns, reads=(), writes=()):
        if self.chain is not None:
            self.chain.append(("group", (e, list(fns), tuple(reads), tuple(writes))))
            return
        self._deps(e, reads, writes)
        ins = None
        for fn in fns:
            ins = fn(self.eng[e])
        self.cnt[e] += 1
        ins.then_inc(self.semh[e], 1)
        self._record((e, self.cnt[e], e), reads, writes)

    def dma(self, q, stream, out, in_, reads=(), writes=(), **kw):
        if self.chain is not None:
            assert not kw
            self.chain.append(("dma", (q, stream, out, in_, tuple(reads), tuple(writes))))
            return
        if not hasattr(stream, "subs"):
            stream.subs = {}
            stream.q0 = q
        if q != stream.q0:
            if q not in stream.subs:
                stream.subs[q] = self._new_stream((stream.key.split("_", 1)[1] or "x") + "_" + q)
            stream = stream.subs[q]
        kset = frozenset(reads) | frozenset(writes)
        if stream.cnt and getattr(stream, "last_keys", None) != kset and self.waited[q].get(stream.key, 0) < stream.cnt:
            self.eng[q].wait_ge(stream.sem, stream.cnt)
            self.waited[q][stream.key] = stream.cnt
        stream.last_keys = kset
        self._deps(q, reads, writes)
        ins = self.eng[q].dma_start(out=out, in_=in_, **kw)
        stream.cnt += 16
        ins.then_inc(stream.sem, 16)
        self._record((stream.key, stream.cnt, None), reads, writes)

    def merge_into(self, dst, srcs, reset=False):
        if reset or dst not in self.keys:
            self.keys[dst] = {"w": None, "r": {}}
        d = self.keys[dst]
        for k in srcs:
            st = self.keys.get(k)
            if not st:
                continue
            for ev in [st["w"]] + list(st["r"].values()):
                if ev is None:
                    continue
                cur = d["r"].get(ev[0])
                if cur is None or cur[1] < ev[1]:
                    d["r"][ev[0]] = ev

    def finish(self, q="sp"):
        for st in self.streams:
            if st.cnt and self.waited[q].get(st.key, 0) < st.cnt:
                self.eng[q].wait_ge(st.sem, st.cnt)
                self.waited[q][st.key] = st.cnt


class Ctx:
    def __init__(self):
        self.nc = bass.Bass("TRN2", target_bir_lowering=False)
        self.stack = contextlib.ExitStack()
        self.s = Sched(self.nc, self.stack)
        self.io = {}
        self.fused = False
        self.stage_stack = None
        self.stage_id = 0
        self._psr = None

    def begin_stage(self, io):
        self.fused = True
        self.io = dict(io)
        self.stage_id += 1
        self.stage_stack = contextlib.ExitStack()

    def sb(self, name, shape, dt):
        st = self.stage_stack if self.stage_stack is not None else self.stack
        return st.enter_context(self.nc.sbuf_tensor("s%d_%s" % (self.stage_id, name), shape, dt))

    def ps(self, name, shape=(128, 512), dt=F32):
        return self.stack.enter_context(self.nc.psum_tensor(name, list(shape), dt))

    def psum_ring(self):
        if self._psr is None:
            self._psr = PsumRing(self, 8)
        return self._psr

    def din(self, name, shape, dt=F32):
        if name in self.io:
            ap = self.io[name]
            assert list(ap.shape) == list(shape), (name, list(ap.shape), list(shape))
            return ap
        assert not self.fused, name
        return self.nc.dram_tensor(name, list(shape), dt, kind="ExternalInput").ap()

    def dout(self, name, shape, dt=F32):
        if name in self.io:
            ap = self.io[name]
            assert list(ap.shape) == list(shape), (name, list(ap.shape), list(shape))
            return ap
        assert not self.fused, name
        return self.nc.dram_tensor(name, list(shape), dt, kind="ExternalOutput").ap()

    def close(self):
        if self.fused:
            self.s.barrier()
            self.stage_stack.close()
            self.stage_stack = None
            return None
        self.s.finish("sp")
        self.stack.close()
        return self.nc

    def finish_all(self):
        self.s.finish("sp")
        self.stack.close()
        return self.nc


class PsumRing:
    def __init__(self, c, n, prefix="ps"):
        self.t = [c.ps("%s%d" % (prefix, i)) for i in range(n)]
        self.keys = ["%s%d" % (prefix, i) for i in range(n)]
        c.s.excl.update(self.keys)
        self.i = 0

    def next(self):
        i = self.i % len(self.t)
        self.i += 1
        return self.t[i], self.keys[i]

    def sub(self, idxs):
        r = object.__new__(PsumRing)
        r.t = [self.t[i] for i in idxs]
        r.keys = [self.keys[i] for i in idxs]
        r.i = 0
        return r


def load_small(c, q, stream, name, dram_ap, shape, dt=F32):
    t = c.sb(name, list(shape), dt)
    c.s.dma(q, stream, t[:], dram_ap, writes=[name])
    return t


def gemm_fm(c, W, n0, n1, xTb, xkey, kt_n, TT, wslots, wstreams, psr, epilogue, chunk=512, wq="pool"):
    s = c.s
    Wv = W.rearrange("(kt p) n -> p kt n", p=128)
    chunks = []
    a = n0
    while a < n1:
        cw = min(chunk, n1 - a)
        chunks.append((a, cw))
        a += cw
    nsl = len(wslots)

    def issue(ci):
        a, cw = chunks[ci]
        sl = ci % nsl
        s.dma(wq, wstreams[sl], wslots[sl][0][:, :kt_n, :cw], Wv[:, :, a:a + cw], writes=[wslots[sl][1]])

    for ci in range(min(nsl, len(chunks))):
        issue(ci)
    for ci, (a, cw) in enumerate(chunks):
        sl = ci % nsl
        wt, wkey = wslots[sl]
        j = 0
        while j < cw:
            rows = min(128, cw - j)
            for tb in range(TT // 512):
                pt, pkey = psr.next()
                fns = []
                for kt in range(kt_n):
                    fns.append(lambda pe, kt=kt, pt=pt, j=j, rows=rows, tb=tb, wt=wt: pe.matmul(
                        pt[:rows, :], lhsT=wt[:, kt, j:j + rows], rhs=xTb[:, kt, tb * 512:(tb + 1) * 512],
                        start=(kt == 0), stop=(kt == kt_n - 1)))
                s.group("pe", fns, reads=[wkey, xkey], writes=[pkey])
                epilogue(a + j, rows, tb, pt, pkey)
            j += rows
        if ci + nsl < len(chunks):
            issue(ci + nsl)


def ln_block(c, srcf, skey, kt_n, gT, bT, ones, psr, scr, out_bf=None, out_f32=None, okeys=(None, None), eps=LN_EPS):
    s = c.s
    sq, mean, rstd, tmp = scr
    p1, k1 = psr.next()
    p2, k2 = psr.next()
    s.group("pe", [lambda pe, kt=kt: pe.matmul(p1[:], lhsT=ones[:], rhs=srcf(kt), start=(kt == 0),
                                               stop=(kt == kt_n - 1)) for kt in range(kt_n)],
            reads=[skey, "ones"], writes=[k1])
    for kt in range(kt_n):
        q = sq[kt % 2]
        qk = "lnsq%d" % (kt % 2)
        s.op("act", lambda a, kt=kt, q=q: a.activation(out=q[:], in_=srcf(kt), func=AF.Square),
             reads=[skey], writes=[qk])
        s.op("pe", lambda pe, kt=kt, q=q: pe.matmul(p2[:], lhsT=ones[:], rhs=q[:], start=(kt == 0),
                                                    stop=(kt == kt_n - 1)),
             reads=[qk, "ones"], writes=[k2])
    s.op("act", lambda a: a.activation(out=mean[:], in_=p1[:], func=AF.Copy), reads=[k1], writes=["lnmean"])
    s.op("dve", lambda v: v.tensor_tensor(out=tmp[:], in0=mean[:], in1=mean[:], op=ALU.mult),
         reads=["lnmean"], writes=["lntmp"])
    s.op("dve", lambda v: v.tensor_tensor(out=rstd[:], in0=p2[:], in1=tmp[:], op=ALU.subtract),
         reads=[k2, "lntmp"], writes=["lnrstd"])
    s.op("dve", lambda v: v.tensor_scalar(out=rstd[:], in0=rstd[:], scalar1=float(eps), scalar2=None,
                                          op0=ALU.add), reads=["lnrstd"], writes=["lnrstd"])
    s.op("act", lambda a: a.activation(out=rstd[:], in_=rstd[:], func=AF.Ln), reads=["lnrstd"], writes=["lnrstd"])
    s.op("act", lambda a: a.activation(out=rstd[:], in_=rstd[:], func=AF.Exp, scale=-0.5),
         reads=["lnrstd"], writes=["lnrstd"])
    for kt in range(kt_n):
        s.op("dve", lambda v, kt=kt: v.tensor_tensor(out=tmp[:], in0=srcf(kt), in1=mean[:], op=ALU.subtract),
             reads=[skey, "lnmean"], writes=["lntmp"])
        s.op("dve", lambda v: v.tensor_tensor(out=tmp[:], in0=tmp[:], in1=rstd[:], op=ALU.mult),
             reads=["lntmp", "lnrstd"], writes=["lntmp"])
        if out_f32 is not None:
            s.op("act", lambda a, kt=kt: a.activation(out=out_f32(kt), in_=tmp[:], func=AF.Identity,
                                                      bias=bT[:, kt:kt + 1], scale=gT[:, kt:kt + 1]),
                 reads=["lntmp", "lngb"], writes=[okeys[1]])
        if out_bf is not None:
            s.op("act", lambda a, kt=kt: a.activation(out=out_bf(kt), in_=tmp[:], func=AF.Identity,
                                                      bias=bT[:, kt:kt + 1], scale=gT[:, kt:kt + 1]),
                 reads=["lntmp", "lngb"], writes=[okeys[0]])


def ln_scratch(c):
    return ([c.sb("lnsq0", [128, 512], F32), c.sb("lnsq1", [128, 512], F32)], c.sb("lnmean", [128, 512], F32),
            c.sb("lnrstd", [128, 512], F32), c.sb("lntmp", [128, 512], F32))


def build_k1(TT, do_ln, c=None, ranges=None):
    c = c or Ctx()
    nc, s = c.nc, c.s
    xT_d = c.din("xT", [D, TT])
    w_d = c.din("w", [D, IN_W])
    g_d = c.din("g", [128, KT])
    b_d = c.din("b", [128, KT])
    out_d = c.dout("projT", [IN_W, TT])
    xn_d = c.dout("xnT", [D, TT]) if do_ln else None

    st_misc = s.stream("misc")
    st_x = s.stream("x")
    st_xn = s.stream("xn")
    xf = c.sb("xf", [128, KT, 512], F32)
    xb = c.sb("xb", [128, KT, TT], BF16)
    gT = load_small(c, "sp", st_misc, "gT", g_d, [128, KT])
    bT = load_small(c, "sp", st_misc, "bT", b_d, [128, KT])
    s.keys["lngb"] = {"w": (st_misc.key, st_misc.cnt, None), "r": {}}
    ones = c.sb("ones", [128, 128], F32)
    s.op("dve", lambda v: v.memset(ones[:], 1.0 / D), writes=["ones"])
    psr = c.psum_ring()
    scr = ln_scratch(c) if do_ln else None
    xTv = xT_d.rearrange("(kt p) t -> p kt t", p=128)
    for tb in range(TT // 512):
        sl = slice(tb * 512, (tb + 1) * 512)
        s.dma("sp", st_x, xf[:], xTv[:, :, sl], writes=["xf"])
        if do_ln:
            ln_block(c, lambda kt: xf[:, kt, :], "xf", KT, gT, bT, ones, psr, scr,
                     out_bf=lambda kt, sl=sl: xb[:, kt, sl], out_f32=lambda kt: xf[:, kt, :], okeys=("xb", "xf"))
            s.dma("sp", st_xn, xn_d.rearrange("(kt p) t -> p kt t", p=128)[:, :, sl], xf[:], reads=["xf"])
        else:
            for kt in range(KT):
                s.op("dve", lambda v, kt=kt, sl=sl: v.tensor_copy(out=xb[:, kt, sl], in_=xf[:, kt, :]),
                     reads=["xf"], writes=["xb"])

    NSL = 3
    wslots = [(c.sb("w%d" % i, [128, KT, 512], BF16), "w%d" % i) for i in range(NSL)]
    wstreams = [s.stream("w%d" % i) for i in range(NSL)]
    NST = 2
    stg = [c.sb("stg%d" % i, [128, TT], F32) for i in range(NST)]
    ststreams = [s.stream("st%d" % i) for i in range(NST)]
    state = {"n": 0}
    ntb = TT // 512

    def epi(row0, rows, tb, pt, pkey):
        i = state["n"] % NST
        sk = "stg%d" % i
        s.op("act", lambda a: a.activation(out=stg[i][:rows, tb * 512:(tb + 1) * 512], in_=pt[:rows, :], func=AF.Copy),
             reads=[pkey], writes=[sk])
        if tb == ntb - 1:
            s.dma("sp", ststreams[i], out_d[row0:row0 + rows, :], stg[i][:rows, :], reads=[sk])
            state["n"] += 1

    for (n0, n1) in (ranges or [(0, IN_W)]):
        gemm_fm(c, w_d, n0, n1, xb, "xb", KT, TT, wslots, wstreams, psr, epi)
    return c.close()


def _vecT(v):
    return np.ascontiguousarray(v.reshape(-1, 128).T)


def run_k1(xT_cores, w, g, b, do_ln):
    TT = xT_cores[0].shape[1]
    nc = build_k1(TT, do_ln)
    in_maps = [{"xT": xT_cores[i], "w": w, "g": _vecT(g), "b": _vecT(b)} for i in range(NCORES)]
    res = run_bass_kernel_spmd(nc, in_maps, core_ids=list(range(NCORES)))
    return [r["projT"] for r in res.results], ([r["xnT"] for r in res.results] if do_ln else None)


S_LEN = 4096
FG = 256


def build_k2(NP=2, SBLK=256, c=None, S_LEN=S_LEN):
    c = c or Ctx()
    nc, s = c.nc, c.s
    u_d = c.din("uT", [NP, FG, S_LEN])
    cc_d = c.din("ccsc", [FG, 2 * FG], BF16)
    cs_d = c.din("csm", [S_LEN, S_LEN], BF16)
    ns_d = c.din("nsm", [S_LEN, S_LEN], BF16)
    out_d = c.dout("aT", [NP, FG, S_LEN])
    NST = S_LEN // 128
    st_misc = s.stream("misc")
    ccsc = c.sb("ccsc", [128, 2, 2 * FG], BF16)
    s.dma("sp", st_misc, ccsc[:], cc_d.rearrange("(ct p) n -> p ct n", p=128), writes=["ccsc"])
    psr = c.psum_ring()
    ub = [c.sb("ub%d" % i, [128, 2, S_LEN], BF16) for i in range(NP)]
    pq = [c.sb("pq%d" % i, [128, NST, 2 * FG], BF16) for i in range(NP)]
    for pi in range(NP):
        s.dma("pool", st_misc, ub[pi][:], u_d[pi].rearrange("(ct p) t -> p ct t", p=128), writes=["ub%d" % pi])
        for st in range(NST):
            pt, pk = psr.next()
            s.group("pe", [lambda pe, ct=ct, pt=pt, st=st, pi=pi: pe.matmul(
                pt[:], lhsT=ub[pi][:, ct, st * 128:(st + 1) * 128], rhs=ccsc[:, ct, :], start=(ct == 0), stop=(ct == 1))
                for ct in range(2)], reads=["ub%d" % pi, "ccsc"], writes=[pk])
            eng = "act" if st % 2 == 0 else "dve"
            if eng == "act":
                s.op("act", lambda a, pt=pt, st=st, pi=pi: a.activation(out=pq[pi][:, st, :], in_=pt[:], func=AF.Copy),
                     reads=[pk], writes=["pq%d" % pi])
            else:
                s.op("dve", lambda v, pt=pt, st=st, pi=pi: v.tensor_copy(out=pq[pi][:, st, :], in_=pt[:]),
                     reads=[pk], writes=["pq%d" % pi])
    NSL = 2
    csl = [(c.sb("cs%d" % i, [128, NST, SBLK], BF16), c.sb("ns%d" % i, [128, NST, SBLK], BF16)) for i in range(NSL)]
    cstr = [s.stream("cs%d" % i) for i in range(NSL)]
    nblk = S_LEN // SBLK
    csv = cs_d.rearrange("(st p) s -> p st s", p=128)
    nsv = ns_d.rearrange("(st p) s -> p st s", p=128)

    def issue(bi):
        sl = bi % NSL
        s.dma("sp", cstr[sl], csl[sl][0][:], csv[:, :, bi * SBLK:(bi + 1) * SBLK], writes=["csl%d" % sl])
        s.dma("sp", cstr[sl], csl[sl][1][:], nsv[:, :, bi * SBLK:(bi + 1) * SBLK], writes=["csl%d" % sl])

    NSTG = 2
    stg = [c.sb("stg%d" % i, [128, SBLK], F32) for i in range(NSTG)]
    sstr = [s.stream("st%d" % i) for i in range(NSTG)]
    n = 0
    for bi in range(min(NSL, nblk)):
        issue(bi)
    for bi in range(nblk):
        sl = bi % NSL
        for pi in range(NP):
            for ct in range(2):
                pt, pk = psr.next()
                fns = []
                for half in range(2):
                    for st in range(NST):
                        fns.append(lambda pe, half=half, st=st, pt=pt, pi=pi, ct=ct, sl=sl: pe.matmul(
                            pt[:, :SBLK], lhsT=pq[pi][:, st, half * FG + ct * 128: half * FG + (ct + 1) * 128],
                            rhs=csl[sl][half][:, st, :], start=(half == 0 and st == 0),
                            stop=(half == 1 and st == NST - 1)))
                s.group("pe", fns, reads=["pq%d" % pi, "csl%d" % sl], writes=[pk])
                i = n % NSTG
                n += 1
                s.op("act", lambda a, pt=pt, i=i: a.activation(out=stg[i][:], in_=pt[:, :SBLK], func=AF.Copy),
                     reads=[pk], writes=["stg%d" % i])
                s.dma("sp", sstr[i], out_d[pi, ct * 128:(ct + 1) * 128, bi * SBLK:(bi + 1) * SBLK], stg[i][:],
                      reads=["stg%d" % i])
        if bi + NSL < nblk:
            issue(bi + NSL)
    return c.close()


def dft_consts(S_LEN=S_LEN, flip=False):
    import ml_dtypes
    n = np.arange(S_LEN, dtype=np.int64)
    if flip:
        n = n[::-1].copy()
    ang = 2.0 * np.pi * ((n[:, None] * n[None, :]) % S_LEN).astype(np.float64) / S_LEN
    csm = (np.cos(ang) / 64.0).astype(np.float32).astype(ml_dtypes.bfloat16)
    nsm = (-np.sin(ang) / 64.0).astype(np.float32).astype(ml_dtypes.bfloat16)
    m = np.arange(FG, dtype=np.int64)
    angc = 2.0 * np.pi * ((m[:, None] * m[None, :]) % FG).astype(np.float64) / FG
    ccsc = np.concatenate([np.cos(angc) / 16.0, np.sin(angc) / 16.0], axis=1).astype(np.float32).astype(ml_dtypes.bfloat16)
    return ccsc, csm, nsm


def run_k2(uT_cores):
    ccsc, csm, nsm = dft_consts()
    nc = build_k2(uT_cores[0].shape[0])
    in_maps = [{"uT": uT_cores[i], "ccsc": ccsc, "csm": csm, "nsm": nsm} for i in range(NCORES)]
    res = run_bass_kernel_spmd(nc, in_maps, core_ids=list(range(NCORES)))
    return [r["aT"] for r in res.results]


NEG = -30000.0
C_ID, C_UF, C_UB, C_BD, C_H0, C_H1, C_NTF, C_NTB, C_NSF, C_NSB, C_ONE, C_MISC = range(12)
NCONST = 12
RMS_EPS = 1e-6
L2_EPS = 1e-6


def delta_consts():
    p = np.arange(128)
    same = (p[:, None] // 64) == (p[None, :] // 64)
    cst = np.zeros((128, NCONST, 128), np.float32)
    cst[:, C_ID] = np.eye(128)
    cst[:, C_UF] = same & (p[:, None] <= p[None, :])
    cst[:, C_UB] = same & (p[:, None] >= p[None, :])
    cst[:, C_BD] = same
    cst[:, C_H0] = (p[:, None] < 64) & np.ones((1, 128), bool)
    cst[:, C_H1] = (p[:, None] >= 64) & np.ones((1, 128), bool)
    cst[:, C_NTF] = np.where(same & (p[None, :] >= p[:, None]), 0.0, NEG)
    cst[:, C_NTB] = np.where(same & (p[None, :] <= p[:, None]), 0.0, NEG)
    cst[:, C_NSF] = np.where(same & (p[:, None] > p[None, :]), 0.0, NEG)
    cst[:, C_NSB] = np.where(same & (p[:, None] < p[None, :]), 0.0, NEG)
    cst[:, C_ONE] = 1.0
    cst[:, C_MISC, 0] = (p < 64)
    cst[:, C_MISC, 1] = (p >= 64)
    return cst


def build_k3(NH=8, SEQ=4096, SEG=8, c=None):
    c = c or Ctx()
    nc, s = c.nc, c.s
    NSB = SEQ // 128
    NTB = SEQ // 512
    qkv_d = c.din("qkvT", [NH, 3, 128, SEQ])
    z_d = c.din("zT", [NH, 128, SEQ])
    gr_d = c.io.get("gate_rows")
    gates_d = None if gr_d is not None else c.din("gates", [NH, 2, 2, 128, NSB])
    hp_d = c.din("hp", [NH, 128, 24])
    cst_d = c.din("consts", [128, NCONST, 128])
    out_d = c.dout("oT", [NH, 128, SEQ])

    st_c = s.stream("const")
    cst = c.sb("cst", [128, NCONST, 128], F32)
    s.dma("sp", st_c, cst[:], cst_d, writes=["cst"])
    ident = cst[:, C_ID, :]
    ones = cst[:, C_ONE, :]
    psr = c.psum_ring()

    st_in = s.stream("in")
    st_hp = s.stream("hp")
    st_out = s.stream("out")
    upad = c.sb("upad", [128, SEQ + 4], F32)
    ybuf = c.sb("ybuf", [128, SEQ], F32)
    qT = c.sb("qT", [128, SEQ], F32)
    kT = c.sb("kT", [128, SEQ], F32)
    k_tm = c.sb("k_tm", [128, NSB, 128], F32)
    v_tm = c.sb("v_tm", [128, NSB, 128], F32)
    oT = c.sb("oT", [128, SEQ], F32)
    hp = c.sb("hp", [128, 24], F32)
    hq = c.sb("hq", [128, 8], F32)
    gt = c.sb("gt", [128, 2, NSB], F32)
    grow = c.sb("grow", [NSB, 2, 128], F32)
    g_tm = c.sb("g_tm", [128, NSB], F32)
    beta_tm = c.sb("beta_tm", [128, NSB], F32)
    nbeta_tm = c.sb("nbeta_tm", [128, NSB], F32)
    gc_tm = c.sb("gc_tm", [128, NSB], F32)
    ngc_tm = c.sb("ngc_tm", [128, NSB], F32)
    bexp_tm = c.sb("bexp_tm", [128, NSB], F32)
    ekd_tm = c.sb("ekd_tm", [128, 2, NSB], F32)
    glast = c.sb("glast", [128, 2, NSB], F32)
    tsm = c.sb("tsm", [128, NSB], F32)
    S = c.sb("S", [128, 128], F32)
    vnew = c.sb("vnew", [128, 128], F32)
    sm = c.sb("sm", [128, 512], F32)
    sm2 = c.sb("sm2", [128, 512], F32)
    gbc = c.sb("gbc", [128, 128], F32)
    erow = c.sb("erow", [128, 128], F32)
    dT = c.sb("dT", [128, 128], F32)
    dS = c.sb("dS", [128, 128], F32)
    Pm = [c.sb("Pm%d" % i, [128, 128], F32) for i in range(2)]
    PmT = [c.sb("PmT%d" % i, [128, 128], F32) for i in range(2)]
    X = c.sb("X", [128, 128], F32)
    vb = c.sb("vb", [128, 128], F32)
    kbg = c.sb("kbg", [128, 128], F32)
    u_sg = c.sb("u_sg", [128, SEG, 128], F32)
    wT_sg = c.sb("wT_sg", [128, SEG, 128], F32)
    qd_sg = c.sb("qd_sg", [128, SEG, 128], F32)
    qk_sg = c.sb("qk_sg", [128, SEG, 128], F32)
    kd_sg = c.sb("kd_sg", [128, SEG, 2, 128], F32)

    s.op("dve", lambda v: v.memset(upad[:, 0:2], 0.0), writes=["upad"])
    s.op("dve", lambda v: v.memset(upad[:, SEQ + 2:SEQ + 4], 0.0), writes=["upad"])
    s.op("dve", lambda v: v.memset(vnew[:], 0.0), writes=["vnew"])

    def l2norm_inplace(buf, key, scale):
        for tb in range(NTB):
            sl = slice(tb * 512, (tb + 1) * 512)
            s.op("act", lambda a: a.activation(out=sm[:], in_=buf[:, sl], func=AF.Square), reads=[key], writes=["sm"])
            pt, pk = psr.next()
            s.op("pe", lambda pe: pe.matmul(pt[:], lhsT=ones, rhs=sm[:], start=True, stop=True),
                 reads=["sm", "cst"], writes=[pk])
            s.op("dve", lambda v: v.tensor_scalar(out=sm2[:], in0=pt[:], scalar1=float(L2_EPS), scalar2=None,
                                                  op0=ALU.add), reads=[pk], writes=["sm2"])
            s.op("act", lambda a: a.activation(out=sm2[:], in_=sm2[:], func=AF.Ln), reads=["sm2"], writes=["sm2"])
            s.op("act", lambda a: a.activation(out=sm2[:], in_=sm2[:], func=AF.Exp, scale=-0.5),
                 reads=["sm2"], writes=["sm2"])
            s.op("dve", lambda v: v.scalar_tensor_tensor(out=buf[:, sl], in0=buf[:, sl], scalar=float(scale),
                                                        in1=sm2[:], op0=ALU.mult, op1=ALU.mult),
                 reads=[key, "sm2"], writes=[key])

    def conv_silu(src_ap, col0, dst, dkey):
        s.dma("sp", st_in, upad[:, 2:SEQ + 2], src_ap, writes=["upad"])
        s.op("dve", lambda v: v.tensor_scalar(out=ybuf[:], in0=upad[:, 0:SEQ], scalar1=hp[:, col0:col0 + 1],
                                              scalar2=None, op0=ALU.mult), reads=["upad", "hp"], writes=["ybuf"])
        for j in range(1, 5):
            s.op("dve", lambda v, j=j: v.scalar_tensor_tensor(out=ybuf[:], in0=upad[:, j:SEQ + j],
                                                             scalar=hp[:, col0 + j:col0 + j + 1], in1=ybuf[:],
                                                             op0=ALU.mult, op1=ALU.add),
                 reads=["upad", "hp", "ybuf"], writes=["ybuf"])
        s.op("act", lambda a: a.activation(out=dst[:], in_=ybuf[:], func=AF.Silu), reads=["ybuf"], writes=[dkey])

    def to_tm(src, skey, dst, dkey):
        for g4 in range(NSB // 4):
            pt, pk = psr.next()
            fns = [lambda pe, i=i: pe.transpose(pt[:, i * 128:(i + 1) * 128],
                                                src[:, (g4 * 4 + i) * 128:(g4 * 4 + i + 1) * 128], ident)
                   for i in range(4)]
            s.group("pe", fns, reads=[skey, "cst"], writes=[pk])
            s.op("act", lambda a: a.activation(out=dst[:, g4 * 4:(g4 + 1) * 4, :],
                                               in_=pt[:].rearrange("p (a b) -> p a b", a=4), func=AF.Copy),
                 reads=[pk], writes=[dkey])

    def mm(out_pt, lhsT, rhs, reads, pk):
        s.op("pe", lambda pe: pe.matmul(out_pt, lhsT=lhsT, rhs=rhs, start=True, stop=True), reads=reads, writes=[pk])

    for h in range(NH):
        s.dma("sp", st_hp, hp[:], hp_d[h], writes=["hp"])
        s.op("act", lambda a: a.activation(out=hq[:, 0:2], in_=hp[:, 16:18], func=AF.Exp), reads=["hp"], writes=["hq"])
        s.op("dve", lambda v: v.tensor_scalar(out=hq[:, 0:2], in0=hq[:, 0:2], scalar1=-1.0, scalar2=None,
                                              op0=ALU.mult), reads=["hq"], writes=["hq"])
        conv_silu(qkv_d[h, 0], 0, qT, "qT")
        l2norm_inplace(qT, "qT", 128.0 ** -0.5)
        conv_silu(qkv_d[h, 1], 5, kT, "kT")
        l2norm_inplace(kT, "kT", 1.0)
        to_tm(kT, "kT", k_tm, "k_tm")
        conv_silu(qkv_d[h, 2], 10, oT, "oT")
        to_tm(oT, "oT", v_tm, "v_tm")

        for d in range(2):
            U = cst[:, C_UF + d, :]
            NT = cst[:, C_NTF + d, :]
            NS = cst[:, C_NSF + d, :]
            if gates_d is not None:
                s.dma("sp", st_hp, gt[:], gates_d[h, d].rearrange("g p n -> p g n"), writes=["gt"])
            else:
                for gi in range(2):
                    s.dma("sp", st_hp, grow[:, gi, :], gr_d[gi * 32 + d * 16 + h].rearrange("(n p) -> n p", p=128),
                          writes=["grow"])
                ptg, pkg = psr.next()
                s.group("pe", [lambda pe, gi=gi, ptg=ptg: pe.transpose(ptg[:, gi * NSB:(gi + 1) * NSB], grow[:, gi, :],
                                                                       cst[0:NSB, C_ID, 0:NSB]) for gi in range(2)],
                        reads=["grow", "cst"], writes=[pkg])
                s.op("act", lambda a, ptg=ptg: a.activation(out=gt[:], in_=ptg[:, 0:2 * NSB].rearrange("p (a b) -> p a b", a=2),
                                                            func=AF.Copy), reads=[pkg], writes=["gt"])
            s.op("act", lambda a: a.activation(out=beta_tm[:], in_=gt[:, 0, :], func=AF.Sigmoid),
                 reads=["gt"], writes=["beta_tm"])
            s.op("dve", lambda v: v.tensor_scalar(out=nbeta_tm[:], in0=beta_tm[:], scalar1=-1.0, scalar2=None,
                                                  op0=ALU.mult), reads=["beta_tm"], writes=["nbeta_tm"])
            s.op("act", lambda a, d=d: a.activation(out=tsm[:], in_=gt[:, 1, :], func=AF.Exp,
                                                    bias=hp[:, 18 + d:19 + d]), reads=["gt", "hp"], writes=["tsm"])
            s.op("dve", lambda v: v.tensor_scalar(out=tsm[:], in0=tsm[:], scalar1=1.0, scalar2=None, op0=ALU.add),
                 reads=["tsm"], writes=["tsm"])
            s.op("act", lambda a: a.activation(out=tsm[:], in_=tsm[:], func=AF.Ln), reads=["tsm"], writes=["tsm"])
            s.op("dve", lambda v, d=d: v.tensor_scalar(out=g_tm[:], in0=tsm[:], scalar1=hq[:, d:d + 1], scalar2=None,
                                                       op0=ALU.mult), reads=["tsm", "hq"], writes=["g_tm"])
            pt, pk = psr.next()
            mm(pt[:, 0:NSB], U, g_tm[:], ["cst", "g_tm"], pk)
            s.op("act", lambda a: a.activation(out=gc_tm[:], in_=pt[:, 0:NSB], func=AF.Copy), reads=[pk], writes=["gc_tm"])
            s.op("dve", lambda v: v.tensor_scalar(out=ngc_tm[:], in0=pt[:, 0:NSB], scalar1=-1.0, scalar2=None,
                                                  op0=ALU.mult), reads=[pk], writes=["ngc_tm"])
            pt2, pk2 = psr.next()
            mm(pt2[:, 0:NSB], cst[:, C_BD, :], g_tm[:], ["cst", "g_tm"], pk2)
            s.op("dve", lambda v: v.tensor_tensor(out=tsm[:], in0=pt2[:, 0:NSB], in1=gc_tm[:], op=ALU.subtract),
                 reads=[pk2, "gc_tm"], writes=["tsm"])
            s.op("act", lambda a: a.activation(out=tsm[:], in_=tsm[:], func=AF.Exp), reads=["tsm"], writes=["tsm"])
            for hf in range(2):
                s.op("dve", lambda v, hf=hf: v.tensor_scalar(out=ekd_tm[:, hf, :], in0=tsm[:],
                                                             scalar1=cst[:, C_MISC, hf:hf + 1], scalar2=None,
                                                             op0=ALU.mult), reads=["tsm", "cst"], writes=["ekd_tm"])
            s.op("act", lambda a: a.activation(out=bexp_tm[:], in_=gc_tm[:], func=AF.Exp), reads=["gc_tm"], writes=["bexp_tm"])
            s.op("dve", lambda v: v.tensor_tensor(out=bexp_tm[:], in0=bexp_tm[:], in1=beta_tm[:], op=ALU.mult),
                 reads=["bexp_tm", "beta_tm"], writes=["bexp_tm"])
            for hf in range(2):
                pt3, pk3 = psr.next()
                mm(pt3[:, 0:NSB], cst[:, C_H0 + hf, :], g_tm[:], ["cst", "g_tm"], pk3)
                s.op("act", lambda a, hf=hf, pt3=pt3: a.activation(out=glast[:, hf, :], in_=pt3[:, 0:NSB], func=AF.Exp),
                     reads=[pk3], writes=["glast"])
            s.op("dve", lambda v: v.memset(S[:], 0.0), writes=["S"])

            nseg = NSB // SEG
            seg_order = range(nseg) if d == 0 else range(nseg - 1, -1, -1)
            for sg in seg_order:
                for si in range(SEG):
                    sb = sg * SEG + si
                    tsl = slice(sb * 128, (sb + 1) * 128)
                    kx = "_%d" % si
                    s.op("dve", lambda v, sb=sb: v.tensor_scalar(out=gbc[:], in0=ones, scalar1=g_tm[:, sb:sb + 1],
                                                                 scalar2=None, op0=ALU.mult),
                         reads=["cst", "g_tm"], writes=["gbc"])
                    pg, kg = psr.next()
                    mm(pg[:, 0:128], gbc[:], U, ["gbc", "cst"], kg)
                    s.op("act", lambda a, pg=pg: a.activation(out=erow[:], in_=pg[:, 0:128], func=AF.Exp),
                         reads=[kg], writes=["erow"])
                    s.op("dve", lambda v, pg=pg, sb=sb: v.scalar_tensor_tensor(
                        out=dT[:], in0=pg[:, 0:128], scalar=gc_tm[:, sb:sb + 1], in1=NT, op0=ALU.subtract, op1=ALU.add),
                         reads=[kg, "gc_tm", "cst"], writes=["dT"])
                    s.op("act", lambda a: a.activation(out=dT[:], in_=dT[:], func=AF.Exp), reads=["dT"], writes=["dT"])
                    s.op("dve", lambda v, pg=pg: v.scalar_tensor_tensor(
                        out=dS[:], in0=pg[:, 0:128], scalar=-1.0, in1=NS, op0=ALU.mult, op1=ALU.add),
                         reads=[kg, "cst"], writes=["dS"])
                    s.op("act", lambda a, sb=sb: a.activation(out=dS[:], in_=dS[:], func=AF.Exp,
                                                              bias=gc_tm[:, sb:sb + 1]),
                         reads=["dS", "gc_tm"], writes=["dS"])
                    s.op("dve", lambda v, si=si, tsl=tsl: v.tensor_tensor(out=qd_sg[:, si, :], in0=qT[:, tsl], in1=erow[:],
                                                                         op=ALU.mult),
                         reads=["qT", "erow"], writes=["qd" + kx])
                    pk_, kk_ = psr.next()
                    mm(pk_[:, 0:128], kT[:, tsl], kT[:, tsl], ["kT"], kk_)
                    s.op("dve", lambda v, pk_=pk_, sb=sb: v.scalar_tensor_tensor(
                        out=PmT[0][:], in0=pk_[:, 0:128], scalar=nbeta_tm[:, sb:sb + 1], in1=dS[:],
                        op0=ALU.mult, op1=ALU.mult), reads=[kk_, "nbeta_tm", "dS"], writes=["PmT0"])
                    pr, kr = psr.next()
                    s.op("pe", lambda pe, pr=pr: pe.transpose(pr[:, 0:128], PmT[0][:], ident), reads=["PmT0", "cst"],
                         writes=[kr])
                    s.op("act", lambda a, pr=pr: a.activation(out=Pm[0][:], in_=pr[:, 0:128], func=AF.Copy),
                         reads=[kr], writes=["Pm0"])
                    s.op("dve", lambda v, pr=pr: v.tensor_tensor(out=X[:], in0=pr[:, 0:128], in1=ident, op=ALU.add),
                         reads=[kr, "cst"], writes=["X"])
                    pq_, kq_ = psr.next()
                    mm(pq_[:, 0:128], kT[:, tsl], qT[:, tsl], ["kT", "qT"], kq_)
                    s.op("dve", lambda v, pq_=pq_, si=si: v.tensor_tensor(out=qk_sg[:, si, :], in0=pq_[:, 0:128], in1=dT[:],
                                                                         op=ALU.mult),
                         reads=[kq_, "dT"], writes=["qk" + kx])
                    cur = 0
                    for lvl in range(1, 6):
                        nxt = 1 - cur
                        pa, ka = psr.next()
                        mm(pa[:, 0:128], Pm[cur][:], PmT[cur][:], ["Pm%d" % cur, "PmT%d" % cur], ka)
                        if lvl < 5:
                            pb, kb = psr.next()
                            mm(pb[:, 0:128], PmT[cur][:], Pm[cur][:], ["Pm%d" % cur, "PmT%d" % cur], kb)
                        s.op("act", lambda a, pa=pa, nxt=nxt: a.activation(out=PmT[nxt][:], in_=pa[:, 0:128], func=AF.Copy),
                             reads=[ka], writes=["PmT%d" % nxt])
                        if lvl < 5:
                            s.op("dve", lambda v, pb=pb, nxt=nxt: v.tensor_copy(out=Pm[nxt][:], in_=pb[:, 0:128]),
                                 reads=[kb], writes=["Pm%d" % nxt])
                        px, kxp = psr.next()
                        mm(px[:, 0:128], PmT[nxt][:], X[:], ["PmT%d" % nxt, "X"], kxp)
                        s.op("dve", lambda v, px=px: v.tensor_tensor(out=X[:], in0=X[:], in1=px[:, 0:128], op=ALU.add),
                             reads=[kxp, "X"], writes=["X"])
                        cur = nxt
                    s.op("dve", lambda v, sb=sb: v.tensor_scalar(out=vb[:], in0=v_tm[:, sb, :], scalar1=beta_tm[:, sb:sb + 1],
                                                                 scalar2=None, op0=ALU.mult),
                         reads=["v_tm", "beta_tm"], writes=["vb"])
                    s.op("dve", lambda v, sb=sb: v.tensor_scalar(out=kbg[:], in0=k_tm[:, sb, :], scalar1=bexp_tm[:, sb:sb + 1],
                                                                 scalar2=None, op0=ALU.mult),
                         reads=["k_tm", "bexp_tm"], writes=["kbg"])
                    pu, ku = psr.next()
                    mm(pu[:, 0:128], X[:], vb[:], ["X", "vb"], ku)
                    s.op("act", lambda a, pu=pu, si=si: a.activation(out=u_sg[:, si, :], in_=pu[:, 0:128], func=AF.Copy),
                         reads=[ku], writes=["u" + kx])
                    pw, kw = psr.next()
                    mm(pw[:, 0:128], kbg[:], X[:], ["X", "kbg"], kw)
                    s.op("act", lambda a, pw=pw, si=si: a.activation(out=wT_sg[:, si, :], in_=pw[:, 0:128], func=AF.Copy),
                         reads=[kw], writes=["wT" + kx])
                    for hf in range(2):
                        s.op("dve", lambda v, sb=sb, si=si, hf=hf: v.tensor_scalar(
                            out=kd_sg[:, si, hf, :], in0=k_tm[:, sb, :], scalar1=ekd_tm[:, hf, sb:sb + 1], scalar2=None,
                            op0=ALU.mult), reads=["k_tm", "ekd_tm"], writes=["kd" + kx])
                si_order = range(SEG) if d == 0 else range(SEG - 1, -1, -1)
                for si in si_order:
                    sb = sg * SEG + si
                    kx = "_%d" % si
                    for hf in ((0, 1) if d == 0 else (1, 0)):
                        r = slice(hf * 64, (hf + 1) * 64)
                        tok = slice(sb * 128 + hf * 64, sb * 128 + (hf + 1) * 64)
                        p1, k1 = psr.next()
                        mm(p1[:, 0:128], wT_sg[:, si, :], S[:], ["wT" + kx, "S"], k1)
                        s.op("dve", lambda v, p1=p1, r=r, si=si: v.tensor_tensor(out=vnew[r, :], in0=u_sg[r, si, :],
                                                                                in1=p1[r, 0:128], op=ALU.subtract),
                             reads=[k1, "u" + kx], writes=["vnew"])
                        po, ko = psr.next()
                        s.group("pe", [
                            lambda pe, po=po, si=si, r=r: pe.matmul(po[:, 0:64], lhsT=S[:], rhs=qd_sg[:, si, r],
                                                                    start=True, stop=False),
                            lambda pe, po=po, si=si, r=r: pe.matmul(po[:, 0:64], lhsT=vnew[:], rhs=qk_sg[:, si, r],
                                                                    start=False, stop=True)],
                            reads=["S", "qd" + kx, "vnew", "qk" + kx], writes=[ko])
                        p2, k2 = psr.next()
                        mm(p2[:, 0:128], kd_sg[:, si, hf, :], vnew[:], ["kd" + kx, "vnew"], k2)
                        s.op("dve", lambda v, p2=p2, hf=hf, sb=sb: v.scalar_tensor_tensor(
                            out=S[:], in0=S[:], scalar=glast[:, hf, sb:sb + 1], in1=p2[:, 0:128],
                            op0=ALU.mult, op1=ALU.add), reads=[k2, "S", "glast"], writes=["S"])
                        if d == 0:
                            s.op("act", lambda a, po=po, tok=tok: a.activation(out=oT[:, tok], in_=po[:, 0:64], func=AF.Copy),
                                 reads=[ko], writes=["oT"])
                        else:
                            s.op("dve", lambda v, po=po, tok=tok: v.tensor_tensor(out=oT[:, tok], in0=oT[:, tok],
                                                                                in1=po[:, 0:64], op=ALU.add),
                                 reads=[ko, "oT"], writes=["oT"])
        s.dma("sp", st_in, upad[:, 2:SEQ + 2], z_d[h], writes=["upad"])
        s.op("act", lambda a: a.activation(out=ybuf[:], in_=upad[:, 2:SEQ + 2], func=AF.Silu), reads=["upad"], writes=["ybuf"])
        for tb in range(NTB):
            sl = slice(tb * 512, (tb + 1) * 512)
            s.op("act", lambda a, sl=sl: a.activation(out=sm[:], in_=oT[:, sl], func=AF.Square), reads=["oT"], writes=["sm"])
            pt, pk = psr.next()
            mm(pt[:], ones, sm[:], ["sm", "cst"], pk)
            s.op("dve", lambda v, pt=pt: v.tensor_scalar(out=sm2[:], in0=pt[:], scalar1=1.0 / 128.0, scalar2=float(RMS_EPS),
                                                         op0=ALU.mult, op1=ALU.add), reads=[pk], writes=["sm2"])
            s.op("act", lambda a: a.activation(out=sm2[:], in_=sm2[:], func=AF.Ln), reads=["sm2"], writes=["sm2"])
            s.op("act", lambda a: a.activation(out=sm2[:], in_=sm2[:], func=AF.Exp, scale=-0.5), reads=["sm2"], writes=["sm2"])
            s.op("dve", lambda v, sl=sl: v.tensor_tensor(out=sm2[:], in0=sm2[:], in1=oT[:, sl], op=ALU.mult),
                 reads=["sm2", "oT"], writes=["sm2"])
            s.op("dve", lambda v, sl=sl: v.scalar_tensor_tensor(out=ybuf[:, sl], in0=sm2[:], scalar=hp[:, 15:16],
                                                               in1=ybuf[:, sl], op0=ALU.mult, op1=ALU.mult),
                 reads=["sm2", "hp", "ybuf"], writes=["ybuf"])
        s.dma("sp", st_out, out_d[h], ybuf[:], reads=["ybuf"])
    return c.close()


def run_k3(ins_cores):
    cst = delta_consts()
    NH = ins_cores[0]["qkvT"].shape[0]
    SEQ = ins_cores[0]["qkvT"].shape[3]
    nc = build_k3(NH, SEQ)
    in_maps = [dict(m, consts=cst) for m in ins_cores]
    res = run_bass_kernel_spmd(nc, in_maps, core_ids=list(range(len(ins_cores))))
    return [r["oT"] for r in res.results]


def pack_k3(qkv, z, beta_raw, a_raw, conv_w, a_log, dt_bias, onw, heads, n_heads_total):
    S_ = qkv.shape[0]
    W = n_heads_total * 128
    NSB = S_ // 128
    NH = len(heads)
    qkvT = np.empty((NH, 3, 128, S_), np.float32)
    zT = np.empty((NH, 128, S_), np.float32)
    gates = np.empty((NH, 2, 2, 128, NSB), np.float32)
    hp = np.zeros((NH, 128, 24), np.float32)
    for i, h in enumerate(heads):
        for j in range(3):
            cols = slice(j * W + h * 128, j * W + (h + 1) * 128)
            qkvT[i, j] = qkv[:, cols].T
            hp[i, :, 5 * j:5 * j + 5] = conv_w[:, cols].T
        zT[i] = z[:, h * 128:(h + 1) * 128].T
        for d in range(2):
            gates[i, d, 0] = beta_raw[:, d * n_heads_total + h].reshape(NSB, 128).T
            gates[i, d, 1] = a_raw[:, d * n_heads_total + h].reshape(NSB, 128).T
            hp[i, :, 16 + d] = a_log[d, h]
            hp[i, :, 18 + d] = dt_bias[d, h]
        hp[i, :, 15] = onw
    return {"qkvT": qkvT, "zT": zT, "gates": gates, "hp": hp}


ALPHA = 4.0 ** 0.25
NE = 8


def build_k4(TT, moe, NHALF=1, c=None):
    c = c or Ctx()
    nc, s = c.nc, c.s
    NTB = TT // 512
    NTT = TT // 128
    FF = 7168 if moe else 5632
    xT_d = c.din("xT", [D, TT * NHALF])
    aT_d = c.din("aT", [1024, TT * NHALF])
    bT_d = c.din("bT", [D, TT * NHALF])
    gfT_d = c.din("gfT", [D, TT * NHALF])
    gdT_d = c.din("gdT", [D, TT * NHALF])
    pT_d = c.din("pT", [256, TT * NHALF])
    wf_d = c.din("wf", [1024, D])
    wdl_d = c.din("wdl", [D, D])
    wo_d = c.din("wo", [D, D])
    pg_d = c.din("pg", [D, D])
    pp_d = c.din("pp", [256, D])
    lnp_d = c.din("lnp", [128, 4, KT])
    id_d = c.din("ident", [128, 128])
    if moe:
        rw_d = c.din("rw", [128, KT, NE])
        gu_d = c.din("egu", [NE, D, 2 * FF])
        dn_d = c.din("edn", [NE, FF, D])
        experts = [(gu_d[e], dn_d[e]) for e in range(NE)]
    else:
        gu_d = c.din("gu", [D, 2 * FF])
        dn_d = c.din("dn", [FF, D])
        experts = [(gu_d, dn_d)]
    out_d = c.dout("x2T", [D, TT * NHALF])

    st_misc = s.stream("misc")
    lnp = load_small(c, "sp", st_misc, "lnp", lnp_d, [128, 4, KT])
    ident = load_small(c, "sp", st_misc, "ident", id_d, [128, 128])
    s.keys["lngb"] = {"w": (st_misc.key, st_misc.cnt, None), "r": {}}
    ones = c.sb("ones", [128, 128], F32)
    s.op("dve", lambda v: v.memset(ones[:], 1.0 / D), writes=["ones"])
    ones1 = c.sb("ones1", [128, 128], F32)
    s.op("dve", lambda v: v.memset(ones1[:], 1.0), writes=["ones1"])
    psr = c.psum_ring()
    scr = ln_scratch(c)

    acc = c.sb("acc", [128, KT, TT], F32)
    bufA = c.sb("bufA", [128, KT, TT], BF16)
    bufB = c.sb("bufB", [128, max(KT * TT, 8192 + 4 * TT)], BF16)
    mb = bufB[:, 0:KT * TT].rearrange("p (a b) -> p a b", a=KT)
    ab = bufB[:, 0:8 * TT].rearrange("p (a b) -> p a b", a=8)
    NSL = 2
    wslots = [(c.sb("w%d" % i, [128, KT, 512], BF16), "w%d" % i) for i in range(NSL)]
    wstreams = [s.stream("w%d" % i) for i in range(NSL)]
    gts = [c.sb("gt%d" % i, [128, TT], F32) for i in range(2)]
    gstr = [s.stream("gt%d" % i) for i in range(2)]
    sgb = [c.sb("sg%d" % i, [128, 512], F32) for i in range(2)]
    tmpb = c.sb("tmpb", [128, 512], F32)
    pb = c.sb("pb", [128, 2, TT], BF16)
    wpp = c.sb("wpp", [128, 2, D], BF16)
    st_act = s.stream("actin")
    cnt = {"gt": 0, "sg": 0}

    wdstr = [s.stream("wd%d" % i) for i in range(2)]
    st_out = s.stream("out")
    if moe:
        rw = load_small(c, "sp", st_misc, "rw", rw_d, [128, KT, NE])
        comb_tm = c.sb("comb_tm", [128, NTT, NE], F32)
        rs = [c.sb("rs%d" % i, [128, NE], F32) for i in range(4)]
        r1 = c.sb("r1", [128, 8], F32)
        comb_e = c.sb("comb_e", [128, TT], F32)
        lbs = [c.sb("lb%d" % i, [128, 128], F32) for i in range(2)]
    s.dma("pool", st_act, wpp[:], pp_d.rearrange("(kt p) n -> p kt n", p=128), writes=["wpp"])
    for hf in range(NHALF):
        _k4_half(locals(), hf)
    return c.close()


def _k4_half(L, hf):
    g = globals()
    (c, s, TT, NTB, NTT, FF, moe, experts, psr, scr, acc, bufA, bufB, mb, ab, NSL, wslots, wstreams, gts, gstr, sgb, tmpb, pb,
     wpp, st_act, cnt, lnp, ident, ones, ones1, wdstr, st_out) = [L[k] for k in (
        "c", "s", "TT", "NTB", "NTT", "FF", "moe", "experts", "psr", "scr", "acc", "bufA", "bufB", "mb", "ab", "NSL", "wslots",
        "wstreams", "gts", "gstr", "sgb", "tmpb", "pb", "wpp", "st_act", "cnt", "lnp", "ident", "ones", "ones1", "wdstr", "st_out")]
    xT_d, aT_d, bT_d, gfT_d, gdT_d, pT_d, wf_d, wdl_d, wo_d, pg_d, out_d = [L[k] for k in (
        "xT_d", "aT_d", "bT_d", "gfT_d", "gdT_d", "pT_d", "wf_d", "wdl_d", "wo_d", "pg_d", "out_d")]
    if moe:
        rw, comb_tm, rs, r1, comb_e, lbs = [L[k] for k in ("rw", "comb_tm", "rs", "r1", "comb_e", "lbs")]
    c0 = hf * TT
    cs = slice(c0, c0 + TT)
    s.merge_into("bufB", ["wd0", "wd1", "hT0", "hT1"])
    s.dma("pool", st_act, ab, aT_d.rearrange("(kt p) t -> p kt t", p=128)[:, :, cs], writes=["bufB"])
    s.dma("pool", st_act, bufA[:], bT_d.rearrange("(kt p) t -> p kt t", p=128)[:, :, cs], writes=["bufA"])

    def load_tile(src_d, j, func):
        i = cnt["gt"] % 2
        cnt["gt"] += 1
        s.dma("sp", gstr[i], gts[i][:], src_d[j * 128:(j + 1) * 128, cs], writes=["gt%d" % i])
        if func is not None:
            s.op("act", lambda a: a.activation(out=gts[i][:], in_=gts[i][:], func=func), reads=["gt%d" % i],
                 writes=["gt%d" % i])
        return gts[i], "gt%d" % i

    cur = {}

    def epi1(row0, rows, tb, pt, pkey):
        j = row0 // 128
        sl = slice(tb * 512, (tb + 1) * 512)
        if tb == 0:
            cur["t"] = load_tile(gfT_d, j, AF.Sigmoid)
        g, gk = cur["t"]
        s.op("dve", lambda v: v.tensor_tensor(out=acc[:, j, sl], in0=g[:, sl], in1=pt[:], op=ALU.mult),
             reads=[gk, pkey], writes=["acc"])

    gemm_fm(c, wf_d, 0, D, ab, "bufB", 8, TT, wslots, wstreams, psr, epi1)

    def epi2(row0, rows, tb, pt, pkey):
        j = row0 // 128
        sl = slice(tb * 512, (tb + 1) * 512)
        if tb == 0:
            cur["t"] = load_tile(gdT_d, j, AF.Sigmoid)
        g, gk = cur["t"]
        s.op("dve", lambda v: v.tensor_tensor(out=tmpb[:], in0=g[:, sl], in1=pt[:], op=ALU.mult),
             reads=[gk, pkey], writes=["tmpb"])
        s.op("dve", lambda v: v.tensor_tensor(out=mb[:, j, sl], in0=tmpb[:], in1=acc[:, j, sl], op=ALU.add),
             reads=["tmpb", "acc"], writes=["bufB"])

    gemm_fm(c, wdl_d, 0, D, bufA, "bufA", KT, TT, wslots, wstreams, psr, epi2)

    def epi3(row0, rows, tb, pt, pkey):
        j = row0 // 128
        sl = slice(tb * 512, (tb + 1) * 512)
        if tb == 0:
            cur["t"] = load_tile(xT_d, j, None)
        g, gk = cur["t"]
        s.op("dve", lambda v: v.scalar_tensor_tensor(out=acc[:, j, sl], in0=g[:, sl], scalar=float(ALPHA), in1=pt[:],
                                                    op0=ALU.mult, op1=ALU.add), reads=[gk, pkey], writes=["acc"])

    gemm_fm(c, wo_d, 0, D, mb, "bufB", KT, TT, wslots, wstreams, psr, epi3)

    for tb in range(NTB):
        sl = slice(tb * 512, (tb + 1) * 512)
        ln_block(c, lambda kt, sl=sl: acc[:, kt, sl], "acc", KT, lnp[:, 0, :], lnp[:, 1, :], ones, psr, scr,
                 out_bf=lambda kt, sl=sl: bufA[:, kt, sl], out_f32=lambda kt, sl=sl: acc[:, kt, sl],
                 okeys=("bufA", "acc"))

    if moe:
        for tt in range(NTT):
            pt, pk = psr.next()
            s.group("pe", [lambda pe, kt=kt, tt=tt, pt=pt: pe.matmul(pt[:, 0:NE], lhsT=acc[:, kt, tt * 128:(tt + 1) * 128],
                                                                    rhs=rw[:, kt, :], start=(kt == 0), stop=(kt == KT - 1))
                           for kt in range(KT)], reads=["acc", "rw"], writes=[pk])
            s.op("act", lambda a, pt=pt: a.activation(out=rs[0][:], in_=pt[:, 0:NE], func=AF.Copy), reads=[pk], writes=["rs0"])
            s.op("dve", lambda v: v.reduce_max(out=r1[:, 0:1], in_=rs[0][:], axis=mybir.AxisListType.X),
                 reads=["rs0"], writes=["r1"])
            s.op("dve", lambda v: v.tensor_scalar(out=rs[1][:], in0=rs[0][:], scalar1=r1[:, 0:1], scalar2=None,
                                                  op0=ALU.is_equal), reads=["rs0", "r1"], writes=["rs1"])
            s.op("dve", lambda v: v.scalar_tensor_tensor(out=rs[2][:], in0=rs[1][:], scalar=-1e30, in1=rs[0][:],
                                                        op0=ALU.mult, op1=ALU.add), reads=["rs1", "rs0"], writes=["rs2"])
            s.op("dve", lambda v: v.reduce_max(out=r1[:, 1:2], in_=rs[2][:], axis=mybir.AxisListType.X),
                 reads=["rs2"], writes=["r1"])
            s.op("dve", lambda v: v.tensor_scalar(out=rs[3][:], in0=rs[2][:], scalar1=r1[:, 1:2], scalar2=None,
                                                  op0=ALU.is_equal), reads=["rs2", "r1"], writes=["rs3"])
            s.op("dve", lambda v: v.tensor_tensor(out=r1[:, 2:3], in0=r1[:, 1:2], in1=r1[:, 0:1], op=ALU.subtract),
                 reads=["r1"], writes=["r1"])
            s.op("act", lambda a: a.activation(out=r1[:, 3:4], in_=r1[:, 2:3], func=AF.Exp), reads=["r1"], writes=["r1"])
            s.op("dve", lambda v: v.tensor_scalar(out=r1[:, 4:5], in0=r1[:, 3:4], scalar1=1.0, scalar2=None, op0=ALU.add),
                 reads=["r1"], writes=["r1"])
            s.op("dve", lambda v: v.reciprocal(out=r1[:, 5:6], in_=r1[:, 4:5]), reads=["r1"], writes=["r1"])
            s.op("dve", lambda v: v.tensor_tensor(out=r1[:, 6:7], in0=r1[:, 3:4], in1=r1[:, 5:6], op=ALU.mult),
                 reads=["r1"], writes=["r1"])
            s.op("dve", lambda v: v.tensor_scalar(out=rs[0][:], in0=rs[1][:], scalar1=r1[:, 5:6], scalar2=None,
                                                  op0=ALU.mult), reads=["rs1", "r1"], writes=["rs0"])
            s.op("dve", lambda v, tt=tt: v.scalar_tensor_tensor(out=comb_tm[:, tt, :], in0=rs[3][:], scalar=r1[:, 6:7],
                                                               in1=rs[0][:], op0=ALU.mult, op1=ALU.add),
                 reads=["rs3", "rs0", "r1"], writes=["comb_tm"])
    for kt in range(KT):
        s.op("dve", lambda v, kt=kt: v.tensor_scalar(out=acc[:, kt, :], in0=acc[:, kt, :], scalar1=float(ALPHA), scalar2=None,
                                                     op0=ALU.mult), reads=["acc"], writes=["acc"])
    wds = [bufB[:, i * 4096:(i + 1) * 4096].rearrange("p (a b) -> p a b", a=2) for i in range(2)]
    hTs = [bufB[:, 8192 + i * 2 * TT: 8192 + (i + 1) * 2 * TT].rearrange("p (a b) -> p a b", a=2) for i in range(2)]
    for k in ("wd0", "wd1", "hT0", "hT1"):
        s.merge_into(k, ["bufB"], reset=True)
    nch = FF // 256
    work = [(e, ch) for e in range(len(experts)) for ch in range(nch)]

    def issue(wi):
        e, ch = work[wi]
        gu, dn = experts[e]
        guv = gu.rearrange("(kt p) n -> p kt n", p=128)
        c0 = ch * 256
        sl = wi % NSL
        s.dma("pool", wstreams[sl], wslots[sl][0][:, :, 0:256], guv[:, :, c0:c0 + 256], writes=[wslots[sl][1]])
        s.dma("pool", wstreams[sl], wslots[sl][0][:, :, 256:512], guv[:, :, FF + c0:FF + c0 + 256], writes=[wslots[sl][1]])
        s.dma("pool", wdstr[wi % 2], wds[wi % 2], dn[c0:c0 + 256, :].rearrange("(kt p) n -> p kt n", p=128),
              writes=["wd%d" % (wi % 2)])

    for wi in range(min(NSL, len(work))):
        issue(wi)
    for wi, (e, ch) in enumerate(work):
        if moe and ch == 0:
            for tt in range(NTT):
                lb = lbs[tt % 2]
                lk = "lb%d" % (tt % 2)
                s.op("dve", lambda v, tt=tt, lb=lb, e=e: v.tensor_scalar(out=lb[:], in0=ones1[:], scalar1=comb_tm[:, tt, e:e + 1],
                                                                       scalar2=None, op0=ALU.mult),
                     reads=["ones1", "comb_tm"], writes=[lk])
                if tt % 4 == 0:
                    pc, pck = psr.next()
                s.op("pe", lambda pe, pc=pc, tt=tt, lb=lb: pe.matmul(pc[:, (tt % 4) * 128:(tt % 4 + 1) * 128], lhsT=lb[:],
                                                                   rhs=ident[:], start=True, stop=True),
                     reads=[lk, "ident"], writes=[pck])
                if tt % 4 == 3:
                    s.op("act", lambda a, pc=pc, tt=tt: a.activation(out=comb_e[:, (tt // 4) * 512:(tt // 4 + 1) * 512],
                                                                     in_=pc[:], func=AF.Copy), reads=[pck], writes=["comb_e"])
        sl = wi % NSL
        wt, wkey = wslots[sl]
        hT = hTs[wi % 2]
        hk = "hT%d" % (wi % 2)
        wd = wds[wi % 2]
        wdk = "wd%d" % (wi % 2)
        for jt in range(2):
            for tb in range(NTB):
                tsl = slice(tb * 512, (tb + 1) * 512)
                pg_, pgk = psr.next()
                pu_, puk = psr.next()
                s.group("pe", [lambda pe, kt=kt, pg_=pg_, jt=jt, tsl=tsl, wt=wt: pe.matmul(
                    pg_[:], lhsT=wt[:, kt, jt * 128:(jt + 1) * 128], rhs=bufA[:, kt, tsl], start=(kt == 0), stop=(kt == KT - 1))
                    for kt in range(KT)], reads=[wkey, "bufA"], writes=[pgk])
                s.group("pe", [lambda pe, kt=kt, pu_=pu_, jt=jt, tsl=tsl, wt=wt: pe.matmul(
                    pu_[:], lhsT=wt[:, kt, 256 + jt * 128:256 + (jt + 1) * 128], rhs=bufA[:, kt, tsl], start=(kt == 0),
                    stop=(kt == KT - 1)) for kt in range(KT)], reads=[wkey, "bufA"], writes=[puk])
                i = cnt["sg"] % 2
                cnt["sg"] += 1
                s.op("act", lambda a, i=i, pg_=pg_: a.activation(out=sgb[i][:], in_=pg_[:], func=AF.Silu),
                     reads=[pgk], writes=["sg%d" % i])
                if moe:
                    s.op("dve", lambda v, i=i, pu_=pu_: v.tensor_tensor(out=tmpb[:], in0=sgb[i][:], in1=pu_[:], op=ALU.mult),
                         reads=["sg%d" % i, puk], writes=["tmpb"])
                    s.op("dve", lambda v, jt=jt, tsl=tsl, hT=hT: v.tensor_tensor(out=hT[:, jt, tsl], in0=tmpb[:],
                                                                                in1=comb_e[:, tsl], op=ALU.mult),
                         reads=["tmpb", "comb_e"], writes=[hk])
                else:
                    s.op("dve", lambda v, i=i, pu_=pu_, jt=jt, tsl=tsl, hT=hT: v.tensor_tensor(
                        out=hT[:, jt, tsl], in0=sgb[i][:], in1=pu_[:], op=ALU.mult),
                         reads=["sg%d" % i, puk], writes=[hk])
        for j in range(KT):
            for tb in range(NTB):
                tsl = slice(tb * 512, (tb + 1) * 512)
                pd_, pdk = psr.next()
                s.group("pe", [lambda pe, jt=jt, pd_=pd_, j=j, tsl=tsl, wd=wd, hT=hT: pe.matmul(
                    pd_[:], lhsT=wd[:, jt, j * 128:(j + 1) * 128], rhs=hT[:, jt, tsl], start=(jt == 0), stop=(jt == 1))
                    for jt in range(2)], reads=[wdk, hk], writes=[pdk])
                s.op("dve", lambda v, j=j, tsl=tsl, pd_=pd_: v.tensor_tensor(out=acc[:, j, tsl], in0=acc[:, j, tsl],
                                                                            in1=pd_[:], op=ALU.add),
                     reads=[pdk, "acc"], writes=["acc"])
        if wi + NSL < len(work):
            issue(wi + NSL)

    s.dma("pool", st_act, pb[:], pT_d.rearrange("(kt p) t -> p kt t", p=128)[:, :, cs], writes=["pb"])

    def epi6(row0, rows, tb, pt, pkey):
        j = row0 // 128
        sl = slice(tb * 512, (tb + 1) * 512)
        p2, p2k = psr.next()
        s.group("pe", [lambda pe, kt=kt: pe.matmul(p2[:], lhsT=wpp[:, kt, j * 128:(j + 1) * 128], rhs=pb[:, kt, sl],
                                                   start=(kt == 0), stop=(kt == 1)) for kt in range(2)],
                reads=["wpp", "pb"], writes=[p2k])
        i = cnt["sg"] % 2
        cnt["sg"] += 1
        s.op("act", lambda a: a.activation(out=sgb[i][:], in_=pt[:], func=AF.Sigmoid), reads=[pkey], writes=["sg%d" % i])
        s.op("dve", lambda v: v.tensor_tensor(out=tmpb[:], in0=sgb[i][:], in1=p2[:], op=ALU.mult),
             reads=["sg%d" % i, p2k], writes=["tmpb"])
        s.op("dve", lambda v: v.tensor_tensor(out=acc[:, j, sl], in0=acc[:, j, sl], in1=tmpb[:], op=ALU.add),
             reads=["tmpb", "acc"], writes=["acc"])

    gemm_fm(c, pg_d, 0, D, bufA, "bufA", KT, TT, wslots, wstreams, psr, epi6)

    for tb in range(NTB):
        sl = slice(tb * 512, (tb + 1) * 512)
        ln_block(c, lambda kt, sl=sl: acc[:, kt, sl], "acc", KT, lnp[:, 2, :], lnp[:, 3, :], ones, psr, scr,
                 out_f32=lambda kt, sl=sl: acc[:, kt, sl], okeys=(None, "acc"))
    s.dma("sp", st_out, out_d.rearrange("(kt p) t -> p kt t", p=128)[:, :, cs], acc[:], reads=["acc"])


def k4_weights(inp, layer):
    moe = (layer % 2 == 1)
    w = {
        "wf": inp["w_fourier"][layer], "wdl": inp["w_delta"][layer], "wo": inp["w_out"][layer],
        "pg": inp["ple_gate"][layer], "pp": inp["ple_proj"][layer],
        "lnp": np.ascontiguousarray(np.stack([_vecT(inp["ln1_g"][layer]), _vecT(inp["ln1_b"][layer]),
                                              _vecT(inp["ln2_g"][layer]), _vecT(inp["ln2_b"][layer])], axis=1)),
        "ident": np.eye(128, dtype=np.float32),
    }
    if moe:
        w["rw"] = np.ascontiguousarray(inp["router_w"][layer // 2].reshape(KT, 128, NE).transpose(1, 0, 2))
        w["egu"] = inp["exp_gate_up"][layer // 2]
        w["edn"] = inp["exp_down"][layer // 2]
    else:
        w["gu"] = inp["ffn_gate_up"][layer // 2]
        w["dn"] = inp["ffn_down"][layer // 2]
    return w


def run_k4(acts_cores, weights, moe, NHALF=1):
    TT = acts_cores[0]["xT"].shape[1] // NHALF
    nc = build_k4(TT, moe, NHALF)
    in_maps = [dict(a, **weights) for a in acts_cores]
    res = run_bass_kernel_spmd(nc, in_maps, core_ids=list(range(len(acts_cores))))
    return [r["x2T"] for r in res.results]


def pack_k3_fm(P, b, heads, conv_w, a_log, dt_bias, onw):
    cols = slice(b * S_LEN, (b + 1) * S_LEN)
    NSB = S_LEN // 128
    NH = len(heads)
    qkvT = np.empty((NH, 3, 128, S_LEN), np.float32)
    zT = np.empty((NH, 128, S_LEN), np.float32)
    gates = np.empty((NH, 2, 2, 128, NSB), np.float32)
    hp = np.zeros((NH, 128, 24), np.float32)
    for i, h in enumerate(heads):
        for j in range(3):
            r0 = 1024 + j * 2048 + h * 128
            qkvT[i, j] = P[r0:r0 + 128, cols]
            hp[i, :, 5 * j:5 * j + 5] = conv_w[:, j * 2048 + h * 128: j * 2048 + (h + 1) * 128].T
        zT[i] = P[7168 + h * 128: 7168 + (h + 1) * 128, cols]
        for d in range(2):
            gates[i, d, 0] = P[9216 + d * 16 + h, cols].reshape(NSB, 128).T
            gates[i, d, 1] = P[9248 + d * 16 + h, cols].reshape(NSB, 128).T
            hp[i, :, 16 + d] = a_log[d, h]
            hp[i, :, 18 + d] = dt_bias[d, h]
        hp[i, :, 15] = onw
    return {"qkvT": qkvT, "zT": zT, "gates": gates, "hp": hp}


def build_fused(S=4096):
    c = Ctx()
    nc = c.nc
    HALF = S // 2
    ein = lambda name, shape, dt=F32: nc.dram_tensor(name, list(shape), dt, kind="ExternalInput").ap()
    xT = ein("xT", [D, S])
    p0T = ein("p0T", [256, S])
    p1T = ein("p1T", [256, HALF])
    embg = ein("embg", [128, KT])
    embb = ein("embb", [128, KT])
    w_in = ein("w_in", [2, D, IN_W])
    hp = ein("hp", [2, 16, 128, 24])
    dcst = ein("dconsts", [128, NCONST, 128])
    ccsc = ein("ccsc", [FG, 2 * FG], BF16)
    csm = ein("csm", [S, S], BF16)
    nsm = ein("nsm", [S, S], BF16)
    wf = ein("wf", [2, 1024, D])
    wdl = ein("wdl", [2, D, D])
    wo = ein("wo", [2, D, D])
    pg = ein("pg", [2, D, D])
    pp = ein("pp", [2, 256, D])
    lnp = ein("lnp", [2, 128, 4, KT])
    ident = ein("ident", [128, 128])
    gu = ein("gu", [D, 2 * 5632])
    dn = ein("dn", [5632, D])
    rw = ein("rw", [128, KT, NE])
    egu = ein("egu", [NE, D, 2 * 7168])
    edn = ein("edn", [NE, 7168, D])
    out = nc.dram_tensor("x2T", [D, HALF], F32, kind="ExternalOutput").ap()
    scr = lambda name, shape: nc.dram_tensor(name, list(shape), F32).ap()
    projT = scr("projT_s", [IN_W, S])
    xn = scr("xn_s", [D, S])
    A_T = scr("A_s", [1024, S])
    B_T = scr("B_s", [D, S])
    x1s = scr("x1_s", [D, S])
    TT1 = min(2048, S)
    TT4 = min(1024, HALF)
    for layer in range(2):
        xin = xT if layer == 0 else x1s
        for blk in range(S // TT1):
            cols = slice(blk * TT1, (blk + 1) * TT1)
            io = {"xT": xin[:, cols], "w": w_in[layer], "g": embg, "b": embb, "projT": projT[:, cols]}
            if layer == 0:
                io["xnT"] = xn[:, cols]
            c.begin_stage(io)
            trim = (layer == 1 and blk * TT1 >= HALF)
            build_k1(TT1, layer == 0, c=c, ranges=([(0, 7168), (9216, 9280)] if trim else None))
        uview = projT[0:1024, :].rearrange("(g c) t -> g c t", g=4)
        aview = A_T.rearrange("(g c) t -> g c t", g=4)
        for gp in range(2):
            c.begin_stage({"uT": uview[2 * gp:2 * gp + 2], "ccsc": ccsc, "csm": csm, "nsm": nsm,
                           "aT": aview[2 * gp:2 * gp + 2]})
            build_k2(2, 256, c=c, S_LEN=S)
        c.begin_stage({"qkvT": projT[1024:7168, :].rearrange("(j h d) t -> h j d t", j=3, h=16),
                       "zT": projT[7168:9216, :].rearrange("(h d) t -> h d t", h=16),
                       "gate_rows": projT[9216:9280, :], "hp": hp[layer], "consts": dcst,
                       "oT": B_T.rearrange("(h d) t -> h d t", h=16)})
        build_k3v2(16, S, 4, c=c, own_half=(layer == 1))
        ntok = S if layer == 0 else HALF
        xres = xn if layer == 0 else x1s
        io = {"xT": xres[:, 0:ntok], "aT": A_T[:, 0:ntok], "bT": B_T[:, 0:ntok], "gfT": projT[9280:11328, 0:ntok],
              "gdT": projT[11328:13376, 0:ntok], "pT": (p0T if layer == 0 else p1T),
              "wf": wf[layer], "wdl": wdl[layer], "wo": wo[layer], "pg": pg[layer], "pp": pp[layer], "lnp": lnp[layer],
              "ident": ident, "x2T": (x1s if layer == 0 else out)}
        if layer == 0:
            io.update({"gu": gu, "dn": dn})
        else:
            io.update({"rw": rw, "egu": egu, "edn": edn})
        c.begin_stage(io)
        build_k4(TT4, layer == 1, ntok // TT4, c=c)
    return c.finish_all()


def fused_inputs(inputs, ncores, S):
    f32 = lambda v: np.asarray(v, np.float32)
    x = f32(inputs["x"])
    p = f32(inputs["p"])
    HALF = S // 2
    w_in = f32(inputs["w_in"])
    w_in_sw = w_in.copy()
    w_in_sw[:, :, 9216:9232], w_in_sw[:, :, 9232:9248] = w_in[:, :, 9232:9248], w_in[:, :, 9216:9232]
    w_in_sw[:, :, 9248:9264], w_in_sw[:, :, 9264:9280] = w_in[:, :, 9264:9280], w_in[:, :, 9248:9264]
    conv_w, a_log, dt_bias, onw = f32(inputs["conv_w"]), f32(inputs["a_log"]), f32(inputs["dt_bias"]), f32(inputs["o_norm_w"])
    hps = []
    for r in range(2):
        hp = np.zeros((2, 16, 128, 24), np.float32)
        for layer in range(2):
            cw = conv_w[layer][::-1] if r else conv_w[layer]
            for h in range(16):
                for j in range(3):
                    hp[layer, h, :, 5 * j:5 * j + 5] = cw[:, j * 2048 + h * 128: j * 2048 + (h + 1) * 128].T
                hp[layer, h, :, 15] = onw[layer]
                for d in range(2):
                    ds = 1 - d if r else d
                    hp[layer, h, :, 16 + d] = a_log[layer, ds, h]
                    hp[layer, h, :, 18 + d] = dt_bias[layer, ds, h]
        hps.append(hp)
    dfts = [dft_consts(S, flip=False), dft_consts(S, flip=True)]
    shared = {
        "embg": _vecT(f32(inputs["emb_ln_g"])), "embb": _vecT(f32(inputs["emb_ln_b"])),
        "dconsts": delta_consts(),
        "wf": f32(inputs["w_fourier"]), "wdl": f32(inputs["w_delta"]), "wo": f32(inputs["w_out"]),
        "pg": f32(inputs["ple_gate"]), "pp": f32(inputs["ple_proj"]),
        "lnp": np.ascontiguousarray(np.stack([np.stack([_vecT(f32(inputs[k])[layer]) for k in ("ln1_g", "ln1_b", "ln2_g", "ln2_b")],
                                                       axis=1) for layer in range(2)])),
        "ident": np.eye(128, dtype=np.float32),
        "gu": f32(inputs["ffn_gate_up"])[0], "dn": f32(inputs["ffn_down"])[0],
        "rw": np.ascontiguousarray(f32(inputs["router_w"])[0].reshape(KT, 128, NE).transpose(1, 0, 2)),
        "egu": f32(inputs["exp_gate_up"])[0], "edn": f32(inputs["exp_down"])[0],
    }
    in_maps = []
    for cidx in range(ncores):
        b, r = cidx // 2, cidx % 2
        xb = x[b][::-1] if r else x[b]
        p0 = p[0, b][::-1] if r else p[0, b]
        p1 = p[1, b][::-1] if r else p[1, b]
        m = dict(shared)
        m.update({"xT": np.ascontiguousarray(xb.T), "p0T": np.ascontiguousarray(p0.T),
                  "p1T": np.ascontiguousarray(p1[:HALF].T), "w_in": (w_in_sw if r else w_in), "hp": hps[r],
                  "ccsc": dfts[r][0], "csm": dfts[r][1], "nsm": dfts[r][2]})
        in_maps.append(m)
    return in_maps


def fused_gather(results, B_, S):
    HALF = S // 2
    out = np.empty((B_, S, D), np.float32)
    for cidx, r_ in enumerate(results):
        b, r = cidx // 2, cidx % 2
        y = r_["x2T"].T
        if r:
            out[b, S - 1 - np.arange(HALF)] = y
        else:
            out[b, 0:HALF] = y
    return out


def kernel(**inputs):
    B_, S, _ = np.asarray(inputs["x"]).shape
    ncores = 2 * B_
    nc = build_fused(S)
    in_maps = fused_inputs(inputs, ncores, S)
    res = run_bass_kernel_spmd(nc, in_maps, core_ids=list(range(ncores)))
    return fused_gather(res.results, B_, S)


def build_k3v2(NH=8, SEQ=4096, SEG=4, c=None, own_half=False):
    c = c or Ctx()
    nc, s = c.nc, c.s
    NSB = SEQ // 128
    NTB = SEQ // 512
    qkv_d = c.din("qkvT", [NH, 3, 128, SEQ])
    z_d = c.din("zT", [NH, 128, SEQ])
    gr_d = c.io.get("gate_rows")
    gates_d = None if gr_d is not None else c.din("gates", [NH, 2, 2, 128, NSB])
    hp_d = c.din("hp", [NH, 128, 24])
    cst_d = c.din("consts", [128, NCONST, 128])
    out_d = c.dout("oT", [NH, 128, SEQ])

    st_c = s.stream("const")
    cst = c.sb("cst", [128, NCONST, 128], F32)
    s.dma("sp", st_c, cst[:], cst_d, writes=["cst"])
    ident = cst[:, C_ID, :]
    ones = cst[:, C_ONE, :]
    psr = c.psum_ring()
    st_in = s.stream("in")
    st_hp = s.stream("hp")
    st_g = [s.stream("g0"), s.stream("g1")]
    st_out = s.stream("out")
    upad = c.sb("upad", [128, SEQ + 4], F32)
    qT = c.sb("qT", [128, SEQ], F32)
    kT = c.sb("kT", [128, SEQ], F32)
    k_tm = c.sb("k_tm", [128, NSB, 128], F32)
    v_tm = c.sb("v_tm", [128, NSB, 128], F32)
    oTd = [c.sb("oT%d" % d, [128, SEQ], F32) for d in range(2)]
    ybuf = oTd[1]
    hp = c.sb("hp", [128, 24], F32)
    hq = c.sb("hq", [128, 8], F32)
    sm = c.sb("sm", [128, 512], F32)
    sm2 = c.sb("sm2", [128, 512], F32)
    qTb = c.sb("qTb", [128, SEQ], BF16)
    kTb = c.sb("kTb", [128, SEQ], BF16)
    identb = c.sb("identb", [128, 128], BF16)
    s.op("act", lambda a: a.activation(out=identb[:], in_=ident, func=AF.Copy), reads=["cst"], writes=["identb"])

    class DirBuf:
        pass

    NPRE = 2

    DB = []
    for d in range(2):
        b = DirBuf()
        t = lambda name, shape: c.sb("%s_%d" % (name, d), shape, F32)
        tb = lambda name, shape: c.sb("%s_%d" % (name, d), shape, BF16)
        b.gt = t("gt", [128, 2, NSB])
        b.grow = t("grow", [NSB, 2, 128])
        b.g_tm = t("g_tm", [128, NSB])
        b.beta_tm = t("beta_tm", [128, NSB])
        b.nbeta_tm = t("nbeta_tm", [128, NSB])
        b.gc_tm = t("gc_tm", [128, NSB])
        b.bexp_tm = t("bexp_tm", [128, NSB])
        b.ekd_tm = t("ekd_tm", [128, 2, NSB])
        b.glast = t("glast", [128, 2, NSB])
        b.tsm = t("tsm", [128, NSB])
        b.S = t("S", [128, 128])
        b.Sb = tb("Sb", [128, 128])
        b.vnew = tb("vnew", [128, 128])
        b.tmp = []
        for q in range(NPRE):
            T = DirBuf()
            T.sfx = "%d_%d" % (q, d)
            T.gbc = t("gbc%d" % q, [128, 128])
            T.erow = t("erow%d" % q, [128, 128])
            T.dT = t("dT%d" % q, [128, 128])
            T.dS = t("dS%d" % q, [128, 128])
            T.Pm = [tb("Pm%d_%d" % (i, q), [128, 128]) for i in range(2)]
            T.PmT = [tb("PmT%d_%d" % (i, q), [128, 128]) for i in range(2)]
            T.X = tb("X%d" % q, [128, 128])
            T.vb = tb("vb%d" % q, [128, 128])
            T.kbg = tb("kbg%d" % q, [128, 128])
            T.ps = psr.sub([4 * d + q])
            b.tmp.append(T)
        b.u_sg = [t("u_sg%d" % p, [128, SEG, 128]) for p in range(2)]
        b.wT_sg = [tb("wT_sg%d" % p, [128, SEG, 128]) for p in range(2)]
        b.qd_sg = [tb("qd_sg%d" % p, [128, SEG, 128]) for p in range(2)]
        b.qk_sg = [tb("qk_sg%d" % p, [128, SEG, 128]) for p in range(2)]
        b.kd_sg = [tb("kd_sg%d" % p, [128, SEG, 2, 128]) for p in range(2)]
        b.vb_sg = [tb("vb_sg%d" % p, [128, SEG, 128]) for p in range(2)]
        b.kbg_sg = [tb("kbg_sg%d" % p, [128, SEG, 128]) for p in range(2)]
        b.pre_ps = psr.sub([4 * d, 4 * d + 1])
        b.scan_ps = psr.sub([4 * d + 2, 4 * d + 3])
        DB.append(b)

    s.op("dve", lambda v: v.memset(upad[:, 0:2], 0.0), writes=["upad"])
    s.op("dve", lambda v: v.memset(upad[:, SEQ + 2:SEQ + 4], 0.0), writes=["upad"])
    for d in range(2):
        s.op("dve", lambda v, d=d: v.memset(DB[d].vnew[:], 0.0), writes=["vnew_%d" % d])

    def l2norm_inplace(buf, key, scale):
        for tb in range(NTB):
            sl = slice(tb * 512, (tb + 1) * 512)
            s.op("act", lambda a, sl=sl: a.activation(out=sm[:], in_=buf[:, sl], func=AF.Square), reads=[key], writes=["sm"])
            pt, pk = psr.next()
            s.op("pe", lambda pe, pt=pt: pe.matmul(pt[:], lhsT=ones, rhs=sm[:], start=True, stop=True),
                 reads=["sm", "cst"], writes=[pk])
            s.op("dve", lambda v, pt=pt: v.tensor_scalar(out=sm2[:], in0=pt[:], scalar1=float(L2_EPS), scalar2=None,
                                                         op0=ALU.add), reads=[pk], writes=["sm2"])
            s.op("act", lambda a: a.activation(out=sm2[:], in_=sm2[:], func=AF.Ln), reads=["sm2"], writes=["sm2"])
            s.op("act", lambda a: a.activation(out=sm2[:], in_=sm2[:], func=AF.Exp, scale=-0.5),
                 reads=["sm2"], writes=["sm2"])
            s.op("dve", lambda v, sl=sl: v.scalar_tensor_tensor(out=buf[:, sl], in0=buf[:, sl], scalar=float(scale),
                                                               in1=sm2[:], op0=ALU.mult, op1=ALU.mult),
                 reads=[key, "sm2"], writes=[key])

    def conv_silu(src_ap, col0, dst, dkey):
        s.dma("sp", st_in, upad[:, 2:SEQ + 2], src_ap, writes=["upad"])
        s.op("dve", lambda v: v.tensor_scalar(out=ybuf[:], in0=upad[:, 0:SEQ], scalar1=hp[:, col0:col0 + 1],
                                              scalar2=None, op0=ALU.mult), reads=["upad", "hp"], writes=["oT1"])
        for j in range(1, 5):
            s.op("dve", lambda v, j=j: v.scalar_tensor_tensor(out=ybuf[:], in0=upad[:, j:SEQ + j],
                                                             scalar=hp[:, col0 + j:col0 + j + 1], in1=ybuf[:],
                                                             op0=ALU.mult, op1=ALU.add),
                 reads=["upad", "hp", "oT1"], writes=["oT1"])
        s.op("act", lambda a: a.activation(out=dst[:], in_=ybuf[:], func=AF.Silu), reads=["oT1"], writes=[dkey])

    def to_tm(src, skey, dst, dkey):
        for g4 in range(NSB // 4):
            pt, pk = psr.next()
            fns = [lambda pe, i=i, pt=pt, g4=g4: pe.transpose(pt[:, i * 128:(i + 1) * 128],
                                                              src[:, (g4 * 4 + i) * 128:(g4 * 4 + i + 1) * 128], ident)
                   for i in range(4)]
            s.group("pe", fns, reads=[skey, "cst"], writes=[pk])
            s.op("act", lambda a, pt=pt, g4=g4: a.activation(out=dst[:, g4 * 4:(g4 + 1) * 4, :],
                                                             in_=pt[:].rearrange("p (a b) -> p a b", a=4), func=AF.Copy),
                 reads=[pk], writes=[dkey])

    def mm(out_pt, lhsT, rhs, reads, pk):
        s.op("pe", lambda pe: pe.matmul(out_pt, lhsT=lhsT, rhs=rhs, start=True, stop=True), reads=reads, writes=[pk])

    def dir_setup(h, d):
        B = DB[d]
        K = lambda n: "%s_%d" % (n, d)
        U = cst[:, C_UF + d, :]
        ps_ = B.pre_ps
        if gates_d is not None:
            s.dma("sp", st_g[d], B.gt[:], gates_d[h, d].rearrange("g p n -> p g n"), writes=[K("gt")])
        else:
            for gi in range(2):
                s.dma("sp", st_g[d], B.grow[:, gi, :], gr_d[gi * 32 + d * 16 + h].rearrange("(n p) -> n p", p=128),
                      writes=[K("grow")])
            ptg, pkg = ps_.next()
            s.group("pe", [lambda pe, gi=gi, ptg=ptg: pe.transpose(ptg[:, gi * NSB:(gi + 1) * NSB], B.grow[:, gi, :],
                                                                   cst[0:NSB, C_ID, 0:NSB]) for gi in range(2)],
                    reads=[K("grow"), "cst"], writes=[pkg])
            s.op("act", lambda a, ptg=ptg: a.activation(out=B.gt[:], in_=ptg[:, 0:2 * NSB].rearrange("p (a b) -> p a b", a=2),
                                                        func=AF.Copy), reads=[pkg], writes=[K("gt")])
        s.op("act", lambda a: a.activation(out=B.beta_tm[:], in_=B.gt[:, 0, :], func=AF.Sigmoid),
             reads=[K("gt")], writes=[K("beta_tm")])
        s.op("dve", lambda v: v.tensor_scalar(out=B.nbeta_tm[:], in0=B.beta_tm[:], scalar1=-1.0, scalar2=None,
                                              op0=ALU.mult), reads=[K("beta_tm")], writes=[K("nbeta_tm")])
        s.op("act", lambda a: a.activation(out=B.tsm[:], in_=B.gt[:, 1, :], func=AF.Exp, bias=hp[:, 18 + d:19 + d]),
             reads=[K("gt"), "hp"], writes=[K("tsm")])
        s.op("dve", lambda v: v.tensor_scalar(out=B.tsm[:], in0=B.tsm[:], scalar1=1.0, scalar2=None, op0=ALU.add),
             reads=[K("tsm")], writes=[K("tsm")])
        s.op("act", lambda a: a.activation(out=B.tsm[:], in_=B.tsm[:], func=AF.Ln), reads=[K("tsm")], writes=[K("tsm")])
        s.op("dve", lambda v: v.tensor_scalar(out=B.g_tm[:], in0=B.tsm[:], scalar1=hq[:, d:d + 1], scalar2=None,
                                              op0=ALU.mult), reads=[K("tsm"), "hq"], writes=[K("g_tm")])
        pt, pk = ps_.next()
        mm(pt[:, 0:NSB], U, B.g_tm[:], ["cst", K("g_tm")], pk)
        s.op("act", lambda a: a.activation(out=B.gc_tm[:], in_=pt[:, 0:NSB], func=AF.Copy), reads=[pk], writes=[K("gc_tm")])
        pt2, pk2 = ps_.next()
        mm(pt2[:, 0:NSB], cst[:, C_BD, :], B.g_tm[:], ["cst", K("g_tm")], pk2)
        s.op("dve", lambda v: v.tensor_tensor(out=B.tsm[:], in0=pt2[:, 0:NSB], in1=B.gc_tm[:], op=ALU.subtract),
             reads=[pk2, K("gc_tm")], writes=[K("tsm")])
        s.op("act", lambda a: a.activation(out=B.tsm[:], in_=B.tsm[:], func=AF.Exp), reads=[K("tsm")], writes=[K("tsm")])
        for hf in range(2):
            s.op("dve", lambda v, hf=hf: v.tensor_scalar(out=B.ekd_tm[:, hf, :], in0=B.tsm[:],
                                                         scalar1=cst[:, C_MISC, hf:hf + 1], scalar2=None,
                                                         op0=ALU.mult), reads=[K("tsm"), "cst"], writes=[K("ekd_tm")])
        s.op("act", lambda a: a.activation(out=B.bexp_tm[:], in_=B.gc_tm[:], func=AF.Exp),
             reads=[K("gc_tm")], writes=[K("bexp_tm")])
        s.op("dve", lambda v: v.tensor_tensor(out=B.bexp_tm[:], in0=B.bexp_tm[:], in1=B.beta_tm[:], op=ALU.mult),
             reads=[K("bexp_tm"), K("beta_tm")], writes=[K("bexp_tm")])
        for hf in range(2):
            pt3, pk3 = ps_.next()
            mm(pt3[:, 0:NSB], cst[:, C_H0 + hf, :], B.g_tm[:], ["cst", K("g_tm")], pk3)
            s.op("act", lambda a, hf=hf, pt3=pt3: a.activation(out=B.glast[:, hf, :], in_=pt3[:, 0:NSB], func=AF.Exp),
                 reads=[pk3], writes=[K("glast")])
        s.op("dve", lambda v: v.memset(B.S[:], 0.0), writes=[K("S")])
        s.op("dve", lambda v: v.memset(B.Sb[:], 0.0), writes=[K("Sb")])

    def dir_pre(d, sg, par, q):
        B = DB[d]
        T = B.tmp[q]
        K = lambda n: "%s_%d" % (n, d)
        KT_ = lambda n: "%s_%s" % (n, T.sfx)
        U = cst[:, C_UF + d, :]
        NT = cst[:, C_NTF + d, :]
        NS = cst[:, C_NSF + d, :]
        ps_ = T.ps
        sgk = "_%d_%d" % (d, par)
        sbs = slice(sg * SEG, (sg + 1) * SEG)
        if q == 0:
            s.op("dve", lambda v: v.tensor_tensor(
                out=B.vb_sg[par][:], in0=v_tm[:, sbs, :], in1=B.beta_tm[:, sbs, None].broadcast_to([128, SEG, 128]),
                op=ALU.mult), reads=["v_tm", K("beta_tm")], writes=["vbs" + sgk])
            s.op("dve", lambda v: v.tensor_tensor(
                out=B.kbg_sg[par][:], in0=k_tm[:, sbs, :], in1=B.bexp_tm[:, sbs, None].broadcast_to([128, SEG, 128]),
                op=ALU.mult), reads=["k_tm", K("bexp_tm")], writes=["kbgs" + sgk])
        if q == NPRE - 1:
            for hf in range(2):
                s.op("dve", lambda v, hf=hf: v.tensor_tensor(
                    out=B.kd_sg[par][:, :, hf, :], in0=k_tm[:, sbs, :],
                    in1=B.ekd_tm[:, hf, sbs, None].broadcast_to([128, SEG, 128]), op=ALU.mult),
                     reads=["k_tm", K("ekd_tm")], writes=["kds" + sgk])
        for si in range(q, SEG, NPRE):
            sb = sg * SEG + si
            tsl = slice(sb * 128, (sb + 1) * 128)
            kx = "_%d_%d_%d" % (d, par, si)
            s.op("dve", lambda v, sb=sb: v.tensor_scalar(out=T.gbc[:], in0=ones, scalar1=B.g_tm[:, sb:sb + 1],
                                                          scalar2=None, op0=ALU.mult),
                 reads=["cst", K("g_tm")], writes=[KT_("gbc")])
            pg, kg = ps_.next()
            mm(pg[:, 0:128], T.gbc[:], U, [KT_("gbc"), "cst"], kg)
            s.op("act", lambda a, pg=pg: a.activation(out=T.erow[:], in_=pg[:, 0:128], func=AF.Exp),
                 reads=[kg], writes=[KT_("erow")])
            s.op("dve", lambda v, pg=pg, sb=sb: v.scalar_tensor_tensor(
                out=T.dT[:], in0=pg[:, 0:128], scalar=B.gc_tm[:, sb:sb + 1], in1=NT, op0=ALU.subtract, op1=ALU.add),
                 reads=[kg, K("gc_tm"), "cst"], writes=[KT_("dT")])
            s.op("dve", lambda v, pg=pg: v.scalar_tensor_tensor(
                out=T.dS[:], in0=pg[:, 0:128], scalar=-1.0, in1=NS, op0=ALU.mult, op1=ALU.add),
                 reads=[kg, "cst"], writes=[KT_("dS")])
            s.op("act", lambda a: a.activation(out=T.dT[:], in_=T.dT[:], func=AF.Exp), reads=[KT_("dT")], writes=[KT_("dT")])
            s.op("act", lambda a, sb=sb: a.activation(out=T.dS[:], in_=T.dS[:], func=AF.Exp, bias=B.gc_tm[:, sb:sb + 1]),
                 reads=[KT_("dS"), K("gc_tm")], writes=[KT_("dS")])
            s.op("pool", lambda v, si=si, tsl=tsl: v.tensor_tensor(out=B.qd_sg[par][:, si, :], in0=qT[:, tsl], in1=T.erow[:],
                                                                  op=ALU.mult),
                 reads=["qT", KT_("erow")], writes=["qd" + kx])
            pk_, kk_ = ps_.next()
            mm(pk_[:, 0:128], kTb[:, tsl], kTb[:, tsl], ["kTb"], kk_)
            s.op("dve", lambda v, pk_=pk_, sb=sb: v.scalar_tensor_tensor(
                out=T.PmT[0][:], in0=pk_[:, 0:128], scalar=B.nbeta_tm[:, sb:sb + 1], in1=T.dS[:],
                op0=ALU.mult, op1=ALU.mult), reads=[kk_, K("nbeta_tm"), KT_("dS")], writes=[KT_("PmT0")])
            pr, kr = ps_.next()
            mm(pr[:, 0:128], T.PmT[0][:], identb[:], [KT_("PmT0"), "identb"], kr)
            s.op("act", lambda a, pr=pr: a.activation(out=T.Pm[0][:], in_=pr[:, 0:128], func=AF.Copy),
                 reads=[kr], writes=[KT_("Pm0")])
            s.op("dve", lambda v, pr=pr: v.tensor_tensor(out=T.X[:], in0=pr[:, 0:128], in1=ident, op=ALU.add),
                 reads=[kr, "cst"], writes=[KT_("X")])
            pq_, kq_ = ps_.next()
            mm(pq_[:, 0:128], kTb[:, tsl], qTb[:, tsl], ["kTb", "qTb"], kq_)
            s.op("dve", lambda v, pq_=pq_, si=si: v.tensor_tensor(out=B.qk_sg[par][:, si, :], in0=pq_[:, 0:128], in1=T.dT[:],
                                                                 op=ALU.mult),
                 reads=[kq_, KT_("dT")], writes=["qk" + kx])
            cur = 0
            for lvl in range(1, 6):
                nxt = 1 - cur
                pa, ka = ps_.next()
                mm(pa[:, 0:128], T.Pm[cur][:], T.PmT[cur][:], [KT_("Pm%d" % cur), KT_("PmT%d" % cur)], ka)
                s.op("act", lambda a, pa=pa, nxt=nxt: a.activation(out=T.PmT[nxt][:], in_=pa[:, 0:128], func=AF.Copy),
                     reads=[ka], writes=[KT_("PmT%d" % nxt)])
                if lvl < 5:
                    pb, kb = ps_.next()
                    mm(pb[:, 0:128], T.PmT[cur][:], T.Pm[cur][:], [KT_("Pm%d" % cur), KT_("PmT%d" % cur)], kb)
                    if lvl % 2 == 0:
                        s.op("act", lambda a, pb=pb, nxt=nxt: a.activation(out=T.Pm[nxt][:], in_=pb[:, 0:128], func=AF.Copy),
                             reads=[kb], writes=[KT_("Pm%d" % nxt)])
                    else:
                        s.op("dve", lambda v, pb=pb, nxt=nxt: v.tensor_copy(out=T.Pm[nxt][:], in_=pb[:, 0:128]),
                             reads=[kb], writes=[KT_("Pm%d" % nxt)])
                px, kxp = ps_.next()
                mm(px[:, 0:128], T.PmT[nxt][:], T.X[:], [KT_("PmT%d" % nxt), KT_("X")], kxp)
                s.op("dve", lambda v, px=px: v.tensor_tensor(out=T.X[:], in0=T.X[:], in1=px[:, 0:128], op=ALU.add),
                     reads=[kxp, KT_("X")], writes=[KT_("X")])
                cur = nxt
            pu, ku = ps_.next()
            mm(pu[:, 0:128], T.X[:], B.vb_sg[par][:, si, :], [KT_("X"), "vbs" + sgk], ku)
            s.op("act", lambda a, pu=pu, si=si: a.activation(out=B.u_sg[par][:, si, :], in_=pu[:, 0:128], func=AF.Copy),
                 reads=[ku], writes=["u" + kx])
            pw, kw = ps_.next()
            mm(pw[:, 0:128], B.kbg_sg[par][:, si, :], T.X[:], [KT_("X"), "kbgs" + sgk], kw)
            s.op("act", lambda a, pw=pw, si=si: a.activation(out=B.wT_sg[par][:, si, :], in_=pw[:, 0:128], func=AF.Copy),
                 reads=[kw], writes=["wT" + kx])

    def dir_scan(d, sg, par):
        B = DB[d]
        K = lambda n: "%s_%d" % (n, d)
        ps_ = B.scan_ps
        oT = oTd[d]
        ok = "oT%d" % d
        si_order = range(SEG) if d == 0 else range(SEG - 1, -1, -1)
        for si in si_order:
            sb = sg * SEG + si
            kx = "_%d_%d_%d" % (d, par, si)
            for hf in ((0, 1) if d == 0 else (1, 0)):
                r = slice(hf * 64, (hf + 1) * 64)
                tok = slice(sb * 128 + hf * 64, sb * 128 + (hf + 1) * 64)
                p1, k1 = ps_.next()
                mm(p1[:, 0:128], B.wT_sg[par][:, si, :], B.Sb[:], ["wT" + kx, K("Sb")], k1)
                s.op("dve", lambda v, p1=p1, r=r, si=si: v.tensor_tensor(out=B.vnew[r, :], in0=B.u_sg[par][r, si, :],
                                                                        in1=p1[r, 0:128], op=ALU.subtract),
                     reads=[k1, "u" + kx], writes=[K("vnew")])
                po, ko = ps_.next()
                s.group("pe", [
                    lambda pe, po=po, si=si, r=r: pe.matmul(po[:, 0:64], lhsT=B.Sb[:], rhs=B.qd_sg[par][:, si, r],
                                                            start=True, stop=False),
                    lambda pe, po=po, si=si, r=r: pe.matmul(po[:, 0:64], lhsT=B.vnew[:], rhs=B.qk_sg[par][:, si, r],
                                                            start=False, stop=True),
                    lambda pe, po=po, si=si, hf=hf: pe.matmul(po[:, 128:256], lhsT=B.kd_sg[par][:, si, hf, :], rhs=B.vnew[:],
                                                              start=True, stop=True)],
                    reads=[K("Sb"), "qd" + kx, K("vnew"), "qk" + kx, "kds_%d_%d" % (d, par)], writes=[ko])
                s.op("dve", lambda v, po=po, hf=hf, sb=sb: v.scalar_tensor_tensor(
                    out=B.S[:], in0=B.S[:], scalar=B.glast[:, hf, sb:sb + 1], in1=po[:, 128:256],
                    op0=ALU.mult, op1=ALU.add), reads=[ko, K("S"), K("glast")], writes=[K("S")])
                s.op("act", lambda a: a.activation(out=B.Sb[:], in_=B.S[:], func=AF.Copy), reads=[K("S")], writes=[K("Sb")])
                s.op("act", lambda a, po=po, tok=tok: a.activation(out=oT[:, tok], in_=po[:, 0:64], func=AF.Copy),
                     reads=[ko], writes=[ok])

    def deferred(fn, *a):
        ch = []
        s.chain = ch
        try:
            fn(*a)
        finally:
            s.chain = None
        return ch

    nseg = NSB // SEG
    for h in range(NH):
        s.dma("sp", st_hp, hp[:], hp_d[h], writes=["hp"])
        s.op("act", lambda a: a.activation(out=hq[:, 0:2], in_=hp[:, 16:18], func=AF.Exp), reads=["hp"], writes=["hq"])
        s.op("dve", lambda v: v.tensor_scalar(out=hq[:, 0:2], in0=hq[:, 0:2], scalar1=-1.0, scalar2=None,
                                              op0=ALU.mult), reads=["hq"], writes=["hq"])
        conv_silu(qkv_d[h, 0], 0, qT, "qT")
        l2norm_inplace(qT, "qT", 128.0 ** -0.5)
        s.op("act", lambda a: a.activation(out=qTb[:], in_=qT[:], func=AF.Copy), reads=["qT"], writes=["qTb"])
        conv_silu(qkv_d[h, 1], 5, kT, "kT")
        l2norm_inplace(kT, "kT", 1.0)
        s.op("act", lambda a: a.activation(out=kTb[:], in_=kT[:], func=AF.Copy), reads=["kT"], writes=["kTb"])
        to_tm(kT, "kT", k_tm, "k_tm")
        conv_silu(qkv_d[h, 2], 10, oTd[0], "oT0")
        to_tm(oTd[0], "oT0", v_tm, "v_tm")
        s.run_chains([deferred(dir_setup, h, 0), deferred(dir_setup, h, 1)])
        order = [list(range(nseg // 2 if own_half else nseg)), list(range(nseg - 1, -1, -1))]
        for ph in range(nseg + 1):
            chains = []
            for d in range(2):
                if ph >= 1 and ph - 1 < len(order[d]):
                    chains.append(deferred(dir_scan, d, order[d][ph - 1], (ph - 1) % 2))
                if ph < len(order[d]):
                    for q in range(NPRE):
                        chains.append(deferred(dir_pre, d, order[d][ph], ph % 2, q))
            s.run_chains(chains)
        NOUT = SEQ // 2 if own_half else SEQ
        s.dma("sp", st_in, upad[:, 2:NOUT + 2], z_d[h][:, 0:NOUT], writes=["upad"])
        s.op("act", lambda a: a.activation(out=upad[:, 2:NOUT + 2], in_=upad[:, 2:NOUT + 2], func=AF.Silu),
             reads=["upad"], writes=["upad"])
        for tb in range(NOUT // 512):
            sl = slice(tb * 512, (tb + 1) * 512)
            sl2 = slice(tb * 512 + 2, (tb + 1) * 512 + 2)
            s.op("dve", lambda v, sl=sl: v.tensor_tensor(out=oTd[0][:, sl], in0=oTd[0][:, sl], in1=oTd[1][:, sl], op=ALU.add),
                 reads=["oT0", "oT1"], writes=["oT0"])
            s.op("act", lambda a, sl=sl: a.activation(out=sm[:], in_=oTd[0][:, sl], func=AF.Square), reads=["oT0"], writes=["sm"])
            pt, pk = psr.next()
            mm(pt[:], ones, sm[:], ["sm", "cst"], pk)
            s.op("dve", lambda v, pt=pt: v.tensor_scalar(out=sm2[:], in0=pt[:], scalar1=1.0 / 128.0, scalar2=float(RMS_EPS),
                                                         op0=ALU.mult, op1=ALU.add), reads=[pk], writes=["sm2"])
            s.op("act", lambda a: a.activation(out=sm2[:], in_=sm2[:], func=AF.Ln), reads=["sm2"], writes=["sm2"])
            s.op("act", lambda a: a.activation(out=sm2[:], in_=sm2[:], func=AF.Exp, scale=-0.5), reads=["sm2"], writes=["sm2"])
            s.op("dve", lambda v, sl=sl: v.tensor_tensor(out=sm2[:], in0=sm2[:], in1=oTd[0][:, sl], op=ALU.mult),
                 reads=["sm2", "oT0"], writes=["sm2"])
            s.op("dve", lambda v, sl=sl, sl2=sl2: v.scalar_tensor_tensor(out=oTd[0][:, sl], in0=sm2[:], scalar=hp[:, 15:16],
                                                                        in1=upad[:, sl2], op0=ALU.mult, op1=ALU.mult),
                 reads=["sm2", "hp", "upad"], writes=["oT0"])
        s.dma("sp", st_out, out_d[h][:, 0:NOUT], oTd[0][:, 0:NOUT], reads=["oT0"])
    return c.close()
```

```python
import contextlib
import numpy as np
import concourse.bass as bass
import concourse.mybir as mybir
from concourse.bass_utils import run_bass_kernel_spmd

F32 = mybir.dt.float32
BF16 = mybir.dt.bfloat16
AF = mybir.ActivationFunctionType
ALU = mybir.AluOpType

D = 2048
KT = 16
NCORES = 8
IN_W = 13376
LN_EPS = 1e-5


class Stream:
    def __init__(self, key, sem):
        self.key = key
        self.sem = sem
        self.cnt = 0


class Sched:
    def __init__(self, nc, stack, same_sync=True):
        self.nc = nc
        self.stack = stack
        self.same_sync = same_sync
        self.eng = {"pe": nc.tensor, "act": nc.scalar, "dve": nc.vector, "pool": nc.gpsimd, "sp": nc.sync}
        self.semh = {}
        self.cnt = {}
        self.waited = {}
        for e in self.eng:
            self.semh[e] = stack.enter_context(nc.semaphore("sem_" + e))
            self.cnt[e] = 0
            self.waited[e] = {}
        self.keys = {}
        self.chain = None
        self.excl = set()
        self.streams = []
        self.stream_by_name = {}
        self.nstream = 0

    def stream(self, name=None):
        if name is not None and name in self.stream_by_name:
            return self.stream_by_name[name]
        st = self._new_stream(name)
        if name is not None:
            self.stream_by_name[name] = st
        return st

    def barrier(self):
        for e in self.eng:
            for e2 in self.eng:
                if e2 != e and self.cnt[e2] and self.waited[e].get(e2, 0) < self.cnt[e2]:
                    self.eng[e].wait_ge(self.semh[e2], self.cnt[e2])
                    self.waited[e][e2] = self.cnt[e2]
            for st in self.streams:
                if st.cnt and self.waited[e].get(st.key, 0) < st.cnt:
                    self.eng[e].wait_ge(st.sem, st.cnt)
                    self.waited[e][st.key] = st.cnt
        self.keys = {}

    def _new_stream(self, name=None):
        self.nstream += 1
        key = "dma%d_%s" % (self.nstream, name or "")
        sem = self.stack.enter_context(self.nc.semaphore(key))
        st = Stream(key, sem)
        self.semh[key] = sem
        self.streams.append(st)
        return st

    def _deps(self, e, reads, writes):
        deps = {}

        def add(ev):
            if ev is None:
                return
            k, v, pe = ev
            if pe == e and not self.same_sync:
                return
            if deps.get(k, 0) < v:
                deps[k] = v

        for k in reads:
            st = self.keys.get(k)
            if st:
                add(st["w"])
                if k in self.excl:
                    for ev in st["r"].values():
                        if ev[2] != e:
                            add(ev)
        for k in writes:
            st = self.keys.get(k)
            if st:
                add(st["w"])
                for ev in st["r"].values():
                    add(ev)
        for k, v in deps.items():
            if self.waited[e].get(k, 0) >= v:
                continue
            self.eng[e].wait_ge(self.semh[k], v)
            self.waited[e][k] = v

    def _record(self, ev, reads, writes):
        for k in reads:
            st = self.keys.setdefault(k, {"w": None, "r": {}})
            st["r"][ev[0]] = ev
        for k in writes:
            self.keys[k] = {"w": ev, "r": {}}

    EST_DUR = {"pe": 0.28, "act": 0.40, "dve": 0.33, "pool": 0.50, "sp": 2.0}

    def run_chains(self, chains):
        chs = [list(ch) for ch in chains if ch]
        pos = [0] * len(chs)
        free = {}
        wr = {}
        rd = {}
        last = [0.0] * len(chs)
        LAT = 0.15

        def est(item):
            kind, args = item
            if kind == "dma":
                e, reads, writes, dur = args[0], args[4], args[5], self.EST_DUR["sp"]
            else:
                e, reads, writes = args[0], args[2], args[3]
                dur = self.EST_DUR[e] * (len(args[1]) if kind == "group" else 1)
            t = free.get(e, 0.0)
            for k in reads:
                w = wr.get(k)
                if w:
                    t = max(t, w[0] + (LAT if w[1] != e else 0.0))
            for k in writes:
                w = wr.get(k)
                if w:
                    t = max(t, w[0] + (LAT if w[1] != e else 0.0))
                r = rd.get(k)
                if r:
                    t = max(t, r[0] + (LAT if r[1] != e else 0.0))
            return t, dur, e, reads, writes

        while True:
            best = None
            for ci in range(len(chs)):
                if pos[ci] >= len(chs[ci]):
                    continue
                info = est(chs[ci][pos[ci]])
                key = (info[0], last[ci], ci)
                if best is None or key < best[0]:
                    best = (key, ci, info)
            if best is None:
                break
            _, ci, (t, dur, e, reads, writes) = best
            kind, args = chs[ci][pos[ci]]
            pos[ci] += 1
            getattr(self, kind)(*args)
            fin = t + dur
            free[e] = fin
            last[ci] = fin
            for k in reads:
                r = rd.get(k)
                if not r or r[0] < fin:
                    rd[k] = (fin, e)
            for k in writes:
                wr[k] = (fin, e)
                rd.pop(k, None)

    def op(self, e, fn, reads=(), writes=()):
        if self.chain is not None:
            self.chain.append(("op", (e, fn, tuple(reads), tuple(writes))))
            return
        self._deps(e, reads, writes)
        ins = fn(self.eng[e])
        self.cnt[e] += 1
        ins.then_inc(self.semh[e], 1)
        self._record((e, self.cnt[e], e), reads, writes)

    def group(self, e, fns, reads=(), writes=()):
        if self.chain is not None:
            self.chain.append(("group", (e, list(fns), tuple(reads), tuple(writes))))
            return
        self._deps(e, reads, writes)
        ins = None
        for fn in fns:
            ins = fn(self.eng[e])
        self.cnt[e] += 1
        ins.then_inc(self.semh[e], 1)
        self._record((e, self.cnt[e], e), reads, writes)

    def dma(self, q, stream, out, in_, reads=(), writes=(), **kw):
        if self.chain is not None:
            assert not kw
            self.chain.append(("dma", (q, stream, out, in_, tuple(reads), tuple(writes))))
            return
        if not hasattr(stream, "subs"):
            stream.subs = {}
            stream.q0 = q
        if q != stream.q0:
            if q not in stream.subs:
                stream.subs[q] = self._new_stream((stream.key.split("_", 1)[1] or "x") + "_" + q)
            stream = stream.subs[q]
        kset = frozenset(reads) | frozenset(writes)
        if stream.cnt and getattr(stream, "last_keys", None) != kset and self.waited[q].get(stream.key, 0) < stream.cnt:
            self.eng[q].wait_ge(stream.sem, stream.cnt)
            self.waited[q][stream.key] = stream.cnt
        stream.last_keys = kset
        self._deps(q, reads, writes)
        ins = self.eng[q].dma_start(out=out, in_=in_, **kw)
        stream.cnt += 16
        ins.then_inc(stream.sem, 16)
        self._record((stream.key, stream.cnt, None), reads, writes)

    def merge_into(self, dst, srcs, reset=False):
        if reset or dst not in self.keys:
            self.keys[dst] = {"w": None, "r": {}}
        d = self.keys[dst]
        for k in srcs:
            st = self.keys.get(k)
            if not st:
                continue
            for ev in [st["w"]] + list(st["r"].values()):
                if ev is None:
                    continue
                cur = d["r"].get(ev[0])
                if cur is None or cur[1] < ev[1]:
                    d["r"][ev[0]] = ev

    def finish(self, q="sp"):
        for st in self.streams:
            if st.cnt and self.waited[q].get(st.key, 0) < st.cnt:
                self.eng[q].wait_ge(st.sem, st.cnt)
                self.waited[q][st.key] = st.cnt


class Ctx:
    def __init__(self):
        self.nc = bass.Bass("TRN2", target_bir_lowering=False)
        self.stack = contextlib.ExitStack()
        self.s = Sched(self.nc, self.stack)
        self.io = {}
        self.fused = False
        self.stage_stack = None
        self.stage_id = 0
        self._psr = None

    def begin_stage(self, io):
        self.fused = True
        self.io = dict(io)
        self.stage_id += 1
        self.stage_stack = contextlib.ExitStack()

    def sb(self, name, shape, dt):
        st = self.stage_stack if self.stage_stack is not None else self.stack
        return st.enter_context(self.nc.sbuf_tensor("s%d_%s" % (self.stage_id, name), shape, dt))

    def ps(self, name, shape=(128, 512), dt=F32):
        return self.stack.enter_context(self.nc.psum_tensor(name, list(shape), dt))

    def psum_ring(self):
        if self._psr is None:
            self._psr = PsumRing(self, 8)
        return self._psr

    def din(self, name, shape, dt=F32):
        if name in self.io:
            ap = self.io[name]
            assert list(ap.shape) == list(shape), (name, list(ap.shape), list(shape))
            return ap
        assert not self.fused, name
        return self.nc.dram_tensor(name, list(shape), dt, kind="ExternalInput").ap()

    def dout(self, name, shape, dt=F32):
        if name in self.io:
            ap = self.io[name]
            assert list(ap.shape) == list(shape), (name, list(ap.shape), list(shape))
            return ap
        assert not self.fused, name
        return self.nc.dram_tensor(name, list(shape), dt, kind="ExternalOutput").ap()

    def close(self):
        if self.fused:
            self.s.barrier()
            self.stage_stack.close()
            self.stage_stack = None
            return None
        self.s.finish("sp")
        self.stack.close()
        return self.nc

    def finish_all(self):
        self.s.finish("sp")
        self.stack.close()
        return self.nc


class PsumRing:
    def __init__(self, c, n, prefix="ps"):
        self.t = [c.ps("%s%d" % (prefix, i)) for i in range(n)]
        self.keys = ["%s%d" % (prefix, i) for i in range(n)]
        c.s.excl.update(self.keys)
        self.i = 0

    def next(self):
        i = self.i % len(self.t)
        self.i += 1
        return self.t[i], self.keys[i]

    def sub(self, idxs):
        r = object.__new__(PsumRing)
        r.t = [self.t[i] for i in idxs]
        r.keys = [self.keys[i] for i in idxs]
        r.i = 0
        return r


def load_small(c, q, stream, name, dram_ap, shape, dt=F32):
    t = c.sb(name, list(shape), dt)
    c.s.dma(q, stream, t[:], dram_ap, writes=[name])
    return t


def gemm_fm(c, W, n0, n1, xTb, xkey, kt_n, TT, wslots, wstreams, psr, epilogue, chunk=512, wq="pool"):
    s = c.s
    Wv = W.rearrange("(kt p) n -> p kt n", p=128)
    chunks = []
    a = n0
    while a < n1:
        cw = min(chunk, n1 - a)
        chunks.append((a, cw))
        a += cw
    nsl = len(wslots)

    def issue(ci):
        a, cw = chunks[ci]
        sl = ci % nsl
        s.dma(wq, wstreams[sl], wslots[sl][0][:, :kt_n, :cw], Wv[:, :, a:a + cw], writes=[wslots[sl][1]])

    for ci in range(min(nsl, len(chunks))):
        issue(ci)
    for ci, (a, cw) in enumerate(chunks):
        sl = ci % nsl
        wt, wkey = wslots[sl]
        j = 0
        while j < cw:
            rows = min(128, cw - j)
            for tb in range(TT // 512):
                pt, pkey = psr.next()
                fns = []
                for kt in range(kt_n):
                    fns.append(lambda pe, kt=kt, pt=pt, j=j, rows=rows, tb=tb, wt=wt: pe.matmul(
                        pt[:rows, :], lhsT=wt[:, kt, j:j + rows], rhs=xTb[:, kt, tb * 512:(tb + 1) * 512],
                        start=(kt == 0), stop=(kt == kt_n - 1)))
                s.group("pe", fns, reads=[wkey, xkey], writes=[pkey])
                epilogue(a + j, rows, tb, pt, pkey)
            j += rows
        if ci + nsl < len(chunks):
            issue(ci + nsl)


def ln_block(c, srcf, skey, kt_n, gT, bT, ones, psr, scr, out_bf=None, out_f32=None, okeys=(None, None), eps=LN_EPS):
    s = c.s
    sq, mean, rstd, tmp = scr
    p1, k1 = psr.next()
    p2, k2 = psr.next()
    s.group("pe", [lambda pe, kt=kt: pe.matmul(p1[:], lhsT=ones[:], rhs=srcf(kt), start=(kt == 0),
                                               stop=(kt == kt_n - 1)) for kt in range(kt_n)],
            reads=[skey, "ones"], writes=[k1])
    for kt in range(kt_n):
        q = sq[kt % 2]
        qk = "lnsq%d" % (kt % 2)
        s.op("act", lambda a, kt=kt, q=q: a.activation(out=q[:], in_=srcf(kt), func=AF.Square),
             reads=[skey], writes=[qk])
        s.op("pe", lambda pe, kt=kt, q=q: pe.matmul(p2[:], lhsT=ones[:], rhs=q[:], start=(kt == 0),
                                                    stop=(kt == kt_n - 1)),
             reads=[qk, "ones"], writes=[k2])
    s.op("act", lambda a: a.activation(out=mean[:], in_=p1[:], func=AF.Copy), reads=[k1], writes=["lnmean"])
    s.op("dve", lambda v: v.tensor_tensor(out=tmp[:], in0=mean[:], in1=mean[:], op=ALU.mult),
         reads=["lnmean"], writes=["lntmp"])
    s.op("dve", lambda v: v.tensor_tensor(out=rstd[:], in0=p2[:], in1=tmp[:], op=ALU.subtract),
         reads=[k2, "lntmp"], writes=["lnrstd"])
    s.op("dve", lambda v: v.tensor_scalar(out=rstd[:], in0=rstd[:], scalar1=float(eps), scalar2=None,
                                          op0=ALU.add), reads=["lnrstd"], writes=["lnrstd"])
    s.op("act", lambda a: a.activation(out=rstd[:], in_=rstd[:], func=AF.Ln), reads=["lnrstd"], writes=["lnrstd"])
    s.op("act", lambda a: a.activation(out=rstd[:], in_=rstd[:], func=AF.Exp, scale=-0.5),
         reads=["lnrstd"], writes=["lnrstd"])
    for kt in range(kt_n):
        s.op("dve", lambda v, kt=kt: v.tensor_tensor(out=tmp[:], in0=srcf(kt), in1=mean[:], op=ALU.subtract),
             reads=[skey, "lnmean"], writes=["lntmp"])
        s.op("dve", lambda v: v.tensor_tensor(out=tmp[:], in0=tmp[:], in1=rstd[:], op=ALU.mult),
             reads=["lntmp", "lnrstd"], writes=["lntmp"])
        if out_f32 is not None:
            s.op("act", lambda a, kt=kt: a.activation(out=out_f32(kt), in_=tmp[:], func=AF.Identity,
                                                      bias=bT[:, kt:kt + 1], scale=gT[:, kt:kt + 1]),
                 reads=["lntmp", "lngb"], writes=[okeys[1]])
        if out_bf is not None:
            s.op("act", lambda a, kt=kt: a.activation(out=out_bf(kt), in_=tmp[:], func=AF.Identity,
                                                      bias=bT[:, kt:kt + 1], scale=gT[:, kt:kt + 1]),
                 reads=["lntmp", "lngb"], writes=[okeys[0]])


def ln_scratch(c):
    return ([c.sb("lnsq0", [128, 512], F32), c.sb("lnsq1", [128, 512], F32)], c.sb("lnmean", [128, 512], F32),
            c.sb("lnrstd", [128, 512], F32), c.sb("lntmp", [128, 512], F32))


def build_k1(TT, do_ln, c=None, ranges=None):
    c = c or Ctx()
    nc, s = c.nc, c.s
    xT_d = c.din("xT", [D, TT])
    w_d = c.din("w", [D, IN_W])
    g_d = c.din("g", [128, KT])
    b_d = c.din("b", [128, KT])
    out_d = c.dout("projT", [IN_W, TT])
    xn_d = c.dout("xnT", [D, TT]) if do_ln else None

    st_misc = s.stream("misc")
    st_x = s.stream("x")
    st_xn = s.stream("xn")
    xf = c.sb("xf", [128, KT, 512], F32)
    xb = c.sb("xb", [128, KT, TT], BF16)
    gT = load_small(c, "sp", st_misc, "gT", g_d, [128, KT])
    bT = load_small(c, "sp", st_misc, "bT", b_d, [128, KT])
    s.keys["lngb"] = {"w": (st_misc.key, st_misc.cnt, None), "r": {}}
    ones = c.sb("ones", [128, 128], F32)
    s.op("dve", lambda v: v.memset(ones[:], 1.0 / D), writes=["ones"])
    psr = c.psum_ring()
    scr = ln_scratch(c) if do_ln else None
    xTv = xT_d.rearrange("(kt p) t -> p kt t", p=128)
    for tb in range(TT // 512):
        sl = slice(tb * 512, (tb + 1) * 512)
        s.dma("sp", st_x, xf[:], xTv[:, :, sl], writes=["xf"])
        if do_ln:
            ln_block(c, lambda kt: xf[:, kt, :], "xf", KT, gT, bT, ones, psr, scr,
                     out_bf=lambda kt, sl=sl: xb[:, kt, sl], out_f32=lambda kt: xf[:, kt, :], okeys=("xb", "xf"))
            s.dma("sp", st_xn, xn_d.rearrange("(kt p) t -> p kt t", p=128)[:, :, sl], xf[:], reads=["xf"])
        else:
            for kt in range(KT):
                s.op("dve", lambda v, kt=kt, sl=sl: v.tensor_copy(out=xb[:, kt, sl], in_=xf[:, kt, :]),
                     reads=["xf"], writes=["xb"])

    NSL = 3
    wslots = [(c.sb("w%d" % i, [128, KT, 512], BF16), "w%d" % i) for i in range(NSL)]
    wstreams = [s.stream("w%d" % i) for i in range(NSL)]
    NST = 2
    stg = [c.sb("stg%d" % i, [128, TT], F32) for i in range(NST)]
    ststreams = [s.stream("st%d" % i) for i in range(NST)]
    state = {"n": 0}
    ntb = TT // 512

    def epi(row0, rows, tb, pt, pkey):
        i = state["n"] % NST
        sk = "stg%d" % i
        s.op("act", lambda a: a.activation(out=stg[i][:rows, tb * 512:(tb + 1) * 512], in_=pt[:rows, :], func=AF.Copy),
             reads=[pkey], writes=[sk])
        if tb == ntb - 1:
            s.dma("sp", ststreams[i], out_d[row0:row0 + rows, :], stg[i][:rows, :], reads=[sk])
            state["n"] += 1

    for (n0, n1) in (ranges or [(0, IN_W)]):
        gemm_fm(c, w_d, n0, n1, xb, "xb", KT, TT, wslots, wstreams, psr, epi)
    return c.close()


def _vecT(v):
    return np.ascontiguousarray(v.reshape(-1, 128).T)


def run_k1(xT_cores, w, g, b, do_ln):
    TT = xT_cores[0].shape[1]
    nc = build_k1(TT, do_ln)
    in_maps = [{"xT": xT_cores[i], "w": w, "g": _vecT(g), "b": _vecT(b)} for i in range(NCORES)]
    res = run_bass_kernel_spmd(nc, in_maps, core_ids=list(range(NCORES)))
    return [r["projT"] for r in res.results], ([r["xnT"] for r in res.results] if do_ln else None)


S_LEN = 4096
FG = 256


def build_k2(NP=2, SBLK=256, c=None, S_LEN=S_LEN):
    c = c or Ctx()
    nc, s = c.nc, c.s
    u_d = c.din("uT", [NP, FG, S_LEN])
    cc_d = c.din("ccsc", [FG, 2 * FG], BF16)
    cs_d = c.din("csm", [S_LEN, S_LEN], BF16)
    ns_d = c.din("nsm", [S_LEN, S_LEN], BF16)
    out_d = c.dout("aT", [NP, FG, S_LEN])
    NST = S_LEN // 128
    st_misc = s.stream("misc")
    ccsc = c.sb("ccsc", [128, 2, 2 * FG], BF16)
    s.dma("sp", st_misc, ccsc[:], cc_d.rearrange("(ct p) n -> p ct n", p=128), writes=["ccsc"])
    psr = c.psum_ring()
    ub = [c.sb("ub%d" % i, [128, 2, S_LEN], BF16) for i in range(NP)]
    pq = [c.sb("pq%d" % i, [128, NST, 2 * FG], BF16) for i in range(NP)]
    for pi in range(NP):
        s.dma("pool", st_misc, ub[pi][:], u_d[pi].rearrange("(ct p) t -> p ct t", p=128), writes=["ub%d" % pi])
        for st in range(NST):
            pt, pk = psr.next()
            s.group("pe", [lambda pe, ct=ct, pt=pt, st=st, pi=pi: pe.matmul(
                pt[:], lhsT=ub[pi][:, ct, st * 128:(st + 1) * 128], rhs=ccsc[:, ct, :], start=(ct == 0), stop=(ct == 1))
                for ct in range(2)], reads=["ub%d" % pi, "ccsc"], writes=[pk])
            eng = "act" if st % 2 == 0 else "dve"
            if eng == "act":
                s.op("act", lambda a, pt=pt, st=st, pi=pi: a.activation(out=pq[pi][:, st, :], in_=pt[:], func=AF.Copy),
                     reads=[pk], writes=["pq%d" % pi])
            else:
                s.op("dve", lambda v, pt=pt, st=st, pi=pi: v.tensor_copy(out=pq[pi][:, st, :], in_=pt[:]),
                     reads=[pk], writes=["pq%d" % pi])
    NSL = 2
    csl = [(c.sb("cs%d" % i, [128, NST, SBLK], BF16), c.sb("ns%d" % i, [128, NST, SBLK], BF16)) for i in range(NSL)]
    cstr = [s.stream("cs%d" % i) for i in range(NSL)]
    nblk = S_LEN // SBLK
    csv = cs_d.rearrange("(st p) s -> p st s", p=128)
    nsv = ns_d.rearrange("(st p) s -> p st s", p=128)

    def issue(bi):
        sl = bi % NSL
        s.dma("sp", cstr[sl], csl[sl][0][:], csv[:, :, bi * SBLK:(bi + 1) * SBLK], writes=["csl%d" % sl])
        s.dma("sp", cstr[sl], csl[sl][1][:], nsv[:, :, bi * SBLK:(bi + 1) * SBLK], writes=["csl%d" % sl])

    NSTG = 2
    stg = [c.sb("stg%d" % i, [128, SBLK], F32) for i in range(NSTG)]
    sstr = [s.stream("st%d" % i) for i in range(NSTG)]
    n = 0
    for bi in range(min(NSL, nblk)):
        issue(bi)
    for bi in range(nblk):
        sl = bi % NSL
        for pi in range(NP):
            for ct in range(2):
                pt, pk = psr.next()
                fns = []
                for half in range(2):
                    for st in range(NST):
                        fns.append(lambda pe, half=half, st=st, pt=pt, pi=pi, ct=ct, sl=sl: pe.matmul(
                            pt[:, :SBLK], lhsT=pq[pi][:, st, half * FG + ct * 128: half * FG + (ct + 1) * 128],
                            rhs=csl[sl][half][:, st, :], start=(half == 0 and st == 0),
                            stop=(half == 1 and st == NST - 1)))
                s.group("pe", fns, reads=["pq%d" % pi, "csl%d" % sl], writes=[pk])
                i = n % NSTG
                n += 1
                s.op("act", lambda a, pt=pt, i=i: a.activation(out=stg[i][:], in_=pt[:, :SBLK], func=AF.Copy),
                     reads=[pk], writes=["stg%d" % i])
                s.dma("sp", sstr[i], out_d[pi, ct * 128:(ct + 1) * 128, bi * SBLK:(bi + 1) * SBLK], stg[i][:],
                      reads=["stg%d" % i])
        if bi + NSL < nblk:
            issue(bi + NSL)
    return c.close()


def dft_consts(S_LEN=S_LEN, flip=False):
    import ml_dtypes
    n = np.arange(S_LEN, dtype=np.int64)
    if flip:
        n = n[::-1].copy()
    ang = 2.0 * np.pi * ((n[:, None] * n[None, :]) % S_LEN).astype(np.float64) / S_LEN
    csm = (np.cos(ang) / 64.0).astype(np.float32).astype(ml_dtypes.bfloat16)
    nsm = (-np.sin(ang) / 64.0).astype(np.float32).astype(ml_dtypes.bfloat16)
    m = np.arange(FG, dtype=np.int64)
    angc = 2.0 * np.pi * ((m[:, None] * m[None, :]) % FG).astype(np.float64) / FG
    ccsc = np.concatenate([np.cos(angc) / 16.0, np.sin(angc) / 16.0], axis=1).astype(np.float32).astype(ml_dtypes.bfloat16)
    return ccsc, csm, nsm


def run_k2(uT_cores):
    ccsc, csm, nsm = dft_consts()
    nc = build_k2(uT_cores[0].shape[0])
    in_maps = [{"uT": uT_cores[i], "ccsc": ccsc, "csm": csm, "nsm": nsm} for i in range(NCORES)]
    res = run_bass_kernel_spmd(nc, in_maps, core_ids=list(range(NCORES)))
    return [r["aT"] for r in res.results]


NEG = -30000.0
C_ID, C_UF, C_UB, C_BD, C_H0, C_H1, C_NTF, C_NTB, C_NSF, C_NSB, C_ONE, C_MISC = range(12)
NCONST = 12
RMS_EPS = 1e-6
L2_EPS = 1e-6


def delta_consts():
    p = np.arange(128)
    same = (p[:, None] // 64) == (p[None, :] // 64)
    cst = np.zeros((128, NCONST, 128), np.float32)
    cst[:, C_ID] = np.eye(128)
    cst[:, C_UF] = same & (p[:, None] <= p[None, :])
    cst[:, C_UB] = same & (p[:, None] >= p[None, :])
    cst[:, C_BD] = same
    cst[:, C_H0] = (p[:, None] < 64) & np.ones((1, 128), bool)
    cst[:, C_H1] = (p[:, None] >= 64) & np.ones((1, 128), bool)
    cst[:, C_NTF] = np.where(same & (p[None, :] >= p[:, None]), 0.0, NEG)
    cst[:, C_NTB] = np.where(same & (p[None, :] <= p[:, None]), 0.0, NEG)
    cst[:, C_NSF] = np.where(same & (p[:, None] > p[None, :]), 0.0, NEG)
    cst[:, C_NSB] = np.where(same & (p[:, None] < p[None, :]), 0.0, NEG)
    cst[:, C_ONE] = 1.0
    cst[:, C_MISC, 0] = (p < 64)
    cst[:, C_MISC, 1] = (p >= 64)
    return cst


def build_k3(NH=8, SEQ=4096, SEG=8, c=None):
    c = c or Ctx()
    nc, s = c.nc, c.s
    NSB = SEQ // 128
    NTB = SEQ // 512
    qkv_d = c.din("qkvT", [NH, 3, 128, SEQ])
    z_d = c.din("zT", [NH, 128, SEQ])
    gr_d = c.io.get("gate_rows")
    gates_d = None if gr_d is not None else c.din("gates", [NH, 2, 2, 128, NSB])
    hp_d = c.din("hp", [NH, 128, 24])
    cst_d = c.din("consts", [128, NCONST, 128])
    out_d = c.dout("oT", [NH, 128, SEQ])

    st_c = s.stream("const")
    cst = c.sb("cst", [128, NCONST, 128], F32)
    s.dma("sp", st_c, cst[:], cst_d, writes=["cst"])
    ident = cst[:, C_ID, :]
    ones = cst[:, C_ONE, :]
    psr = c.psum_ring()

    st_in = s.stream("in")
    st_hp = s.stream("hp")
    st_out = s.stream("out")
    upad = c.sb("upad", [128, SEQ + 4], F32)
    ybuf = c.sb("ybuf", [128, SEQ], F32)
    qT = c.sb("qT", [128, SEQ], F32)
    kT = c.sb("kT", [128, SEQ], F32)
    k_tm = c.sb("k_tm", [128, NSB, 128], F32)
    v_tm = c.sb("v_tm", [128, NSB, 128], F32)
    oT = c.sb("oT", [128, SEQ], F32)
    hp = c.sb("hp", [128, 24], F32)
    hq = c.sb("hq", [128, 8], F32)
    gt = c.sb("gt", [128, 2, NSB], F32)
    grow = c.sb("grow", [NSB, 2, 128], F32)
    g_tm = c.sb("g_tm", [128, NSB], F32)
    beta_tm = c.sb("beta_tm", [128, NSB], F32)
    nbeta_tm = c.sb("nbeta_tm", [128, NSB], F32)
    gc_tm = c.sb("gc_tm", [128, NSB], F32)
    ngc_tm = c.sb("ngc_tm", [128, NSB], F32)
    bexp_tm = c.sb("bexp_tm", [128, NSB], F32)
    ekd_tm = c.sb("ekd_tm", [128, 2, NSB], F32)
    glast = c.sb("glast", [128, 2, NSB], F32)
    tsm = c.sb("tsm", [128, NSB], F32)
    S = c.sb("S", [128, 128], F32)
    vnew = c.sb("vnew", [128, 128], F32)
    sm = c.sb("sm", [128, 512], F32)
    sm2 = c.sb("sm2", [128, 512], F32)
    gbc = c.sb("gbc", [128, 128], F32)
    erow = c.sb("erow", [128, 128], F32)
    dT = c.sb("dT", [128, 128], F32)
    dS = c.sb("dS", [128, 128], F32)
    Pm = [c.sb("Pm%d" % i, [128, 128], F32) for i in range(2)]
    PmT = [c.sb("PmT%d" % i, [128, 128], F32) for i in range(2)]
    X = c.sb("X", [128, 128], F32)
    vb = c.sb("vb", [128, 128], F32)
    kbg = c.sb("kbg", [128, 128], F32)
    u_sg = c.sb("u_sg", [128, SEG, 128], F32)
    wT_sg = c.sb("wT_sg", [128, SEG, 128], F32)
    qd_sg = c.sb("qd_sg", [128, SEG, 128], F32)
    qk_sg = c.sb("qk_sg", [128, SEG, 128], F32)
    kd_sg = c.sb("kd_sg", [128, SEG, 2, 128], F32)

    s.op("dve", lambda v: v.memset(upad[:, 0:2], 0.0), writes=["upad"])
    s.op("dve", lambda v: v.memset(upad[:, SEQ + 2:SEQ + 4], 0.0), writes=["upad"])
    s.op("dve", lambda v: v.memset(vnew[:], 0.0), writes=["vnew"])

    def l2norm_inplace(buf, key, scale):
        for tb in range(NTB):
            sl = slice(tb * 512, (tb + 1) * 512)
            s.op("act", lambda a: a.activation(out=sm[:], in_=buf[:, sl], func=AF.Square), reads=[key], writes=["sm"])
            pt, pk = psr.next()
            s.op("pe", lambda pe: pe.matmul(pt[:], lhsT=ones, rhs=sm[:], start=True, stop=True),
                 reads=["sm", "cst"], writes=[pk])
            s.op("dve", lambda v: v.tensor_scalar(out=sm2[:], in0=pt[:], scalar1=float(L2_EPS), scalar2=None,
                                                  op0=ALU.add), reads=[pk], writes=["sm2"])
            s.op("act", lambda a: a.activation(out=sm2[:], in_=sm2[:], func=AF.Ln), reads=["sm2"], writes=["sm2"])
            s.op("act", lambda a: a.activation(out=sm2[:], in_=sm2[:], func=AF.Exp, scale=-0.5),
                 reads=["sm2"], writes=["sm2"])
            s.op("dve", lambda v: v.scalar_tensor_tensor(out=buf[:, sl], in0=buf[:, sl], scalar=float(scale),
                                                        in1=sm2[:], op0=ALU.mult, op1=ALU.mult),
                 reads=[key, "sm2"], writes=[key])

    def conv_silu(src_ap, col0, dst, dkey):
        s.dma("sp", st_in, upad[:, 2:SEQ + 2], src_ap, writes=["upad"])
        s.op("dve", lambda v: v.tensor_scalar(out=ybuf[:], in0=upad[:, 0:SEQ], scalar1=hp[:, col0:col0 + 1],
                                              scalar2=None, op0=ALU.mult), reads=["upad", "hp"], writes=["ybuf"])
        for j in range(1, 5):
            s.op("dve", lambda v, j=j: v.scalar_tensor_tensor(out=ybuf[:], in0=upad[:, j:SEQ + j],
                                                             scalar=hp[:, col0 + j:col0 + j + 1], in1=ybuf[:],
                                                             op0=ALU.mult, op1=ALU.add),
                 reads=["upad", "hp", "ybuf"], writes=["ybuf"])
        s.op("act", lambda a: a.activation(out=dst[:], in_=ybuf[:], func=AF.Silu), reads=["ybuf"], writes=[dkey])

    def to_tm(src, skey, dst, dkey):
        for g4 in range(NSB // 4):
            pt, pk = psr.next()
            fns = [lambda pe, i=i: pe.transpose(pt[:, i * 128:(i + 1) * 128],
                                                src[:, (g4 * 4 + i) * 128:(g4 * 4 + i + 1) * 128], ident)
                   for i in range(4)]
            s.group("pe", fns, reads=[skey, "cst"], writes=[pk])
            s.op("act", lambda a: a.activation(out=dst[:, g4 * 4:(g4 + 1) * 4, :],
                                               in_=pt[:].rearrange("p (a b) -> p a b", a=4), func=AF.Copy),
                 reads=[pk], writes=[dkey])

    def mm(out_pt, lhsT, rhs, reads, pk):
        s.op("pe", lambda pe: pe.matmul(out_pt, lhsT=lhsT, rhs=rhs, start=True, stop=True), reads=reads, writes=[pk])

    for h in range(NH):
        s.dma("sp", st_hp, hp[:], hp_d[h], writes=["hp"])
        s.op("act", lambda a: a.activation(out=hq[:, 0:2], in_=hp[:, 16:18], func=AF.Exp), reads=["hp"], writes=["hq"])
        s.op("dve", lambda v: v.tensor_scalar(out=hq[:, 0:2], in0=hq[:, 0:2], scalar1=-1.0, scalar2=None,
                                              op0=ALU.mult), reads=["hq"], writes=["hq"])
        conv_silu(qkv_d[h, 0], 0, qT, "qT")
        l2norm_inplace(qT, "qT", 128.0 ** -0.5)
        conv_silu(qkv_d[h, 1], 5, kT, "kT")
        l2norm_inplace(kT, "kT", 1.0)
        to_tm(kT, "kT", k_tm, "k_tm")
        conv_silu(qkv_d[h, 2], 10, oT, "oT")
        to_tm(oT, "oT", v_tm, "v_tm")

        for d in range(2):
            U = cst[:, C_UF + d, :]
            NT = cst[:, C_NTF + d, :]
            NS = cst[:, C_NSF + d, :]
            if gates_d is not None:
                s.dma("sp", st_hp, gt[:], gates_d[h, d].rearrange("g p n -> p g n"), writes=["gt"])
            else:
                for gi in range(2):
                    s.dma("sp", st_hp, grow[:, gi, :], gr_d[gi * 32 + d * 16 + h].rearrange("(n p) -> n p", p=128),
                          writes=["grow"])
                ptg, pkg = psr.next()
                s.group("pe", [lambda pe, gi=gi, ptg=ptg: pe.transpose(ptg[:, gi * NSB:(gi + 1) * NSB], grow[:, gi, :],
                                                                       cst[0:NSB, C_ID, 0:NSB]) for gi in range(2)],
                        reads=["grow", "cst"], writes=[pkg])
                s.op("act", lambda a, ptg=ptg: a.activation(out=gt[:], in_=ptg[:, 0:2 * NSB].rearrange("p (a b) -> p a b", a=2),
                                                            func=AF.Copy), reads=[pkg], writes=["gt"])
            s.op("act", lambda a: a.activation(out=beta_tm[:], in_=gt[:, 0, :], func=AF.Sigmoid),
                 reads=["gt"], writes=["beta_tm"])
            s.op("dve", lambda v: v.tensor_scalar(out=nbeta_tm[:], in0=beta_tm[:], scalar1=-1.0, scalar2=None,
                                                  op0=ALU.mult), reads=["beta_tm"], writes=["nbeta_tm"])
            s.op("act", lambda a, d=d: a.activation(out=tsm[:], in_=gt[:, 1, :], func=AF.Exp,
                                                    bias=hp[:, 18 + d:19 + d]), reads=["gt", "hp"], writes=["tsm"])
            s.op("dve", lambda v: v.tensor_scalar(out=tsm[:], in0=tsm[:], scalar1=1.0, scalar2=None, op0=ALU.add),
                 reads=["tsm"], writes=["tsm"])
            s.op("act", lambda a: a.activation(out=tsm[:], in_=tsm[:], func=AF.Ln), reads=["tsm"], writes=["tsm"])
            s.op("dve", lambda v, d=d: v.tensor_scalar(out=g_tm[:], in0=tsm[:], scalar1=hq[:, d:d + 1], scalar2=None,
                                                       op0=ALU.mult), reads=["tsm", "hq"], writes=["g_tm"])
            pt, pk = psr.next()
            mm(pt[:, 0:NSB], U, g_tm[:], ["cst", "g_tm"], pk)
            s.op("act", lambda a: a.activation(out=gc_tm[:], in_=pt[:, 0:NSB], func=AF.Copy), reads=[pk], writes=["gc_tm"])
            s.op("dve", lambda v: v.tensor_scalar(out=ngc_tm[:], in0=pt[:, 0:NSB], scalar1=-1.0, scalar2=None,
                                                  op0=ALU.mult), reads=[pk], writes=["ngc_tm"])
            pt2, pk2 = psr.next()
            mm(pt2[:, 0:NSB], cst[:, C_BD, :], g_tm[:], ["cst", "g_tm"], pk2)
            s.op("dve", lambda v: v.tensor_tensor(out=tsm[:], in0=pt2[:, 0:NSB], in1=gc_tm[:], op=ALU.subtract),
                 reads=[pk2, "gc_tm"], writes=["tsm"])
            s.op("act", lambda a: a.activation(out=tsm[:], in_=tsm[:], func=AF.Exp), reads=["tsm"], writes=["tsm"])
            for hf in range(2):
                s.op("dve", lambda v, hf=hf: v.tensor_scalar(out=ekd_tm[:, hf, :], in0=tsm[:],
                                                             scalar1=cst[:, C_MISC, hf:hf + 1], scalar2=None,
                                                             op0=ALU.mult), reads=["tsm", "cst"], writes=["ekd_tm"])
            s.op("act", lambda a: a.activation(out=bexp_tm[:], in_=gc_tm[:], func=AF.Exp), reads=["gc_tm"], writes=["bexp_tm"])
            s.op("dve", lambda v: v.tensor_tensor(out=bexp_tm[:], in0=bexp_tm[:], in1=beta_tm[:], op=ALU.mult),
                 reads=["bexp_tm", "beta_tm"], writes=["bexp_tm"])
            for hf in range(2):
                pt3, pk3 = psr.next()
                mm(pt3[:, 0:NSB], cst[:, C_H0 + hf, :], g_tm[:], ["cst", "g_tm"], pk3)
                s.op("act", lambda a, hf=hf, pt3=pt3: a.activation(out=glast[:, hf, :], in_=pt3[:, 0:NSB], func=AF.Exp),
                     reads=[pk3], writes=["glast"])
            s.op("dve", lambda v: v.memset(S[:], 0.0), writes=["S"])

            nseg = NSB // SEG
            seg_order = range(nseg) if d == 0 else range(nseg - 1, -1, -1)
            for sg in seg_order:
                for si in range(SEG):
                    sb = sg * SEG + si
                    tsl = slice(sb * 128, (sb + 1) * 128)
                    kx = "_%d" % si
                    s.op("dve", lambda v, sb=sb: v.tensor_scalar(out=gbc[:], in0=ones, scalar1=g_tm[:, sb:sb + 1],
                                                                 scalar2=None, op0=ALU.mult),
                         reads=["cst", "g_tm"], writes=["gbc"])
                    pg, kg = psr.next()
                    mm(pg[:, 0:128], gbc[:], U, ["gbc", "cst"], kg)
                    s.op("act", lambda a, pg=pg: a.activation(out=erow[:], in_=pg[:, 0:128], func=AF.Exp),
                         reads=[kg], writes=["erow"])
                    s.op("dve", lambda v, pg=pg, sb=sb: v.scalar_tensor_tensor(
                        out=dT[:], in0=pg[:, 0:128], scalar=gc_tm[:, sb:sb + 1], in1=NT, op0=ALU.subtract, op1=ALU.add),
                         reads=[kg, "gc_tm", "cst"], writes=["dT"])
                    s.op("act", lambda a: a.activation(out=dT[:], in_=dT[:], func=AF.Exp), reads=["dT"], writes=["dT"])
                    s.op("dve", lambda v, pg=pg: v.scalar_tensor_tensor(
                        out=dS[:], in0=pg[:, 0:128], scalar=-1.0, in1=NS, op0=ALU.mult, op1=ALU.add),
                         reads=[kg, "cst"], writes=["dS"])
                    s.op("act", lambda a, sb=sb: a.activation(out=dS[:], in_=dS[:], func=AF.Exp,
                                                              bias=gc_tm[:, sb:sb + 1]),
                         reads=["dS", "gc_tm"], writes=["dS"])
                    s.op("dve", lambda v, si=si, tsl=tsl: v.tensor_tensor(out=qd_sg[:, si, :], in0=qT[:, tsl], in1=erow[:],
                                                                         op=ALU.mult),
                         reads=["qT", "erow"], writes=["qd" + kx])
                    pk_, kk_ = psr.next()
                    mm(pk_[:, 0:128], kT[:, tsl], kT[:, tsl], ["kT"], kk_)
                    s.op("dve", lambda v, pk_=pk_, sb=sb: v.scalar_tensor_tensor(
                        out=PmT[0][:], in0=pk_[:, 0:128], scalar=nbeta_tm[:, sb:sb + 1], in1=dS[:],
                        op0=ALU.mult, op1=ALU.mult), reads=[kk_, "nbeta_tm", "dS"], writes=["PmT0"])
                    pr, kr = psr.next()
                    s.op("pe", lambda pe, pr=pr: pe.transpose(pr[:, 0:128], PmT[0][:], ident), reads=["PmT0", "cst"],
                         writes=[kr])
                    s.op("act", lambda a, pr=pr: a.activation(out=Pm[0][:], in_=pr[:, 0:128], func=AF.Copy),
                         reads=[kr], writes=["Pm0"])
                    s.op("dve", lambda v, pr=pr: v.tensor_tensor(out=X[:], in0=pr[:, 0:128], in1=ident, op=ALU.add),
                         reads=[kr, "cst"], writes=["X"])
                    pq_, kq_ = psr.next()
                    mm(pq_[:, 0:128], kT[:, tsl], qT[:, tsl], ["kT", "qT"], kq_)
                    s.op("dve", lambda v, pq_=pq_, si=si: v.tensor_tensor(out=qk_sg[:, si, :], in0=pq_[:, 0:128], in1=dT[:],
                                                                         op=ALU.mult),
                         reads=[kq_, "dT"], writes=["qk" + kx])
                    cur = 0
                    for lvl in range(1, 6):
                        nxt = 1 - cur
                        pa, ka = psr.next()
                        mm(pa[:, 0:128], Pm[cur][:], PmT[cur][:], ["Pm%d" % cur, "PmT%d" % cur], ka)
                        if lvl < 5:
                            pb, kb = psr.next()
                            mm(pb[:, 0:128], PmT[cur][:], Pm[cur][:], ["Pm%d" % cur, "PmT%d" % cur], kb)
                        s.op("act", lambda a, pa=pa, nxt=nxt: a.activation(out=PmT[nxt][:], in_=pa[:, 0:128], func=AF.Copy),
                             reads=[ka], writes=["PmT%d" % nxt])
                        if lvl < 5:
                            s.op("dve", lambda v, pb=pb, nxt=nxt: v.tensor_copy(out=Pm[nxt][:], in_=pb[:, 0:128]),
                                 reads=[kb], writes=["Pm%d" % nxt])
                        px, kxp = psr.next()
                        mm(px[:, 0:128], PmT[nxt][:], X[:], ["PmT%d" % nxt, "X"], kxp)
                        s.op("dve", lambda v, px=px: v.tensor_tensor(out=X[:], in0=X[:], in1=px[:, 0:128], op=ALU.add),
                             reads=[kxp, "X"], writes=["X"])
                        cur = nxt
                    s.op("dve", lambda v, sb=sb: v.tensor_scalar(out=vb[:], in0=v_tm[:, sb, :], scalar1=beta_tm[:, sb:sb + 1],
                                                                 scalar2=None, op0=ALU.mult),
                         reads=["v_tm", "beta_tm"], writes=["vb"])
                    s.op("dve", lambda v, sb=sb: v.tensor_scalar(out=kbg[:], in0=k_tm[:, sb, :], scalar1=bexp_tm[:, sb:sb + 1],
                                                                 scalar2=None, op0=ALU.mult),
                         reads=["k_tm", "bexp_tm"], writes=["kbg"])
                    pu, ku = psr.next()
                    mm(pu[:, 0:128], X[:], vb[:], ["X", "vb"], ku)
                    s.op("act", lambda a, pu=pu, si=si: a.activation(out=u_sg[:, si, :], in_=pu[:, 0:128], func=AF.Copy),
                         reads=[ku], writes=["u" + kx])
                    pw, kw = psr.next()
                    mm(pw[:, 0:128], kbg[:], X[:], ["X", "kbg"], kw)
                    s.op("act", lambda a, pw=pw, si=si: a.activation(out=wT_sg[:, si, :], in_=pw[:, 0:128], func=AF.Copy),
                         reads=[kw], writes=["wT" + kx])
                    for hf in range(2):
                        s.op("dve", lambda v, sb=sb, si=si, hf=hf: v.tensor_scalar(
                            out=kd_sg[:, si, hf, :], in0=k_tm[:, sb, :], scalar1=ekd_tm[:, hf, sb:sb + 1], scalar2=None,
                            op0=ALU.mult), reads=["k_tm", "ekd_tm"], writes=["kd" + kx])
                si_order = range(SEG) if d == 0 else range(SEG - 1, -1, -1)
                for si in si_order:
                    sb = sg * SEG + si
                    kx = "_%d" % si
                    for hf in ((0, 1) if d == 0 else (1, 0)):
                        r = slice(hf * 64, (hf + 1) * 64)
                        tok = slice(sb * 128 + hf * 64, sb * 128 + (hf + 1) * 64)
                        p1, k1 = psr.next()
                        mm(p1[:, 0:128], wT_sg[:, si, :], S[:], ["wT" + kx, "S"], k1)
                        s.op("dve", lambda v, p1=p1, r=r, si=si: v.tensor_tensor(out=vnew[r, :], in0=u_sg[r, si, :],
                                                                                in1=p1[r, 0:128], op=ALU.subtract),
                             reads=[k1, "u" + kx], writes=["vnew"])
                        po, ko = psr.next()
                        s.group("pe", [
                            lambda pe, po=po, si=si, r=r: pe.matmul(po[:, 0:64], lhsT=S[:], rhs=qd_sg[:, si, r],
                                                                    start=True, stop=False),
                            lambda pe, po=po, si=si, r=r: pe.matmul(po[:, 0:64], lhsT=vnew[:], rhs=qk_sg[:, si, r],
                                                                    start=False, stop=True)],
                            reads=["S", "qd" + kx, "vnew", "qk" + kx], writes=[ko])
                        p2, k2 = psr.next()
                        mm(p2[:, 0:128], kd_sg[:, si, hf, :], vnew[:], ["kd" + kx, "vnew"], k2)
                        s.op("dve", lambda v, p2=p2, hf=hf, sb=sb: v.scalar_tensor_tensor(
                            out=S[:], in0=S[:], scalar=glast[:, hf, sb:sb + 1], in1=p2[:, 0:128],
                            op0=ALU.mult, op1=ALU.add), reads=[k2, "S", "glast"], writes=["S"])
                        if d == 0:
                            s.op("act", lambda a, po=po, tok=tok: a.activation(out=oT[:, tok], in_=po[:, 0:64], func=AF.Copy),
                                 reads=[ko], writes=["oT"])
                        else:
                            s.op("dve", lambda v, po=po, tok=tok: v.tensor_tensor(out=oT[:, tok], in0=oT[:, tok],
                                                                                in1=po[:, 0:64], op=ALU.add),
                                 reads=[ko, "oT"], writes=["oT"])
        s.dma("sp", st_in, upad[:, 2:SEQ + 2], z_d[h], writes=["upad"])
        s.op("act", lambda a: a.activation(out=ybuf[:], in_=upad[:, 2:SEQ + 2], func=AF.Silu), reads=["upad"], writes=["ybuf"])
        for tb in range(NTB):
            sl = slice(tb * 512, (tb + 1) * 512)
            s.op("act", lambda a, sl=sl: a.activation(out=sm[:], in_=oT[:, sl], func=AF.Square), reads=["oT"], writes=["sm"])
            pt, pk = psr.next()
            mm(pt[:], ones, sm[:], ["sm", "cst"], pk)
            s.op("dve", lambda v, pt=pt: v.tensor_scalar(out=sm2[:], in0=pt[:], scalar1=1.0 / 128.0, scalar2=float(RMS_EPS),
                                                         op0=ALU.mult, op1=ALU.add), reads=[pk], writes=["sm2"])
            s.op("act", lambda a: a.activation(out=sm2[:], in_=sm2[:], func=AF.Ln), reads=["sm2"], writes=["sm2"])
            s.op("act", lambda a: a.activation(out=sm2[:], in_=sm2[:], func=AF.Exp, scale=-0.5), reads=["sm2"], writes=["sm2"])
            s.op("dve", lambda v, sl=sl: v.tensor_tensor(out=sm2[:], in0=sm2[:], in1=oT[:, sl], op=ALU.mult),
                 reads=["sm2", "oT"], writes=["sm2"])
            s.op("dve", lambda v, sl=sl: v.scalar_tensor_tensor(out=ybuf[:, sl], in0=sm2[:], scalar=hp[:, 15:16],
                                                               in1=ybuf[:, sl], op0=ALU.mult, op1=ALU.mult),
                 reads=["sm2", "hp", "ybuf"], writes=["ybuf"])
        s.dma("sp", st_out, out_d[h], ybuf[:], reads=["ybuf"])
    return c.close()


def run_k3(ins_cores):
    cst = delta_consts()
    NH = ins_cores[0]["qkvT"].shape[0]
    SEQ = ins_cores[0]["qkvT"].shape[3]
    nc = build_k3(NH, SEQ)
    in_maps = [dict(m, consts=cst) for m in ins_cores]
    res = run_bass_kernel_spmd(nc, in_maps, core_ids=list(range(len(ins_cores))))
    return [r["oT"] for r in res.results]


def pack_k3(qkv, z, beta_raw, a_raw, conv_w, a_log, dt_bias, onw, heads, n_heads_total):
    S_ = qkv.shape[0]
    W = n_heads_total * 128
    NSB = S_ // 128
    NH = len(heads)
    qkvT = np.empty((NH, 3, 128, S_), np.float32)
    zT = np.empty((NH, 128, S_), np.float32)
    gates = np.empty((NH, 2, 2, 128, NSB), np.float32)
    hp = np.zeros((NH, 128, 24), np.float32)
    for i, h in enumerate(heads):
        for j in range(3):
            cols = slice(j * W + h * 128, j * W + (h + 1) * 128)
            qkvT[i, j] = qkv[:, cols].T
            hp[i, :, 5 * j:5 * j + 5] = conv_w[:, cols].T
        zT[i] = z[:, h * 128:(h + 1) * 128].T
        for d in range(2):
            gates[i, d, 0] = beta_raw[:, d * n_heads_total + h].reshape(NSB, 128).T
            gates[i, d, 1] = a_raw[:, d * n_heads_total + h].reshape(NSB, 128).T
            hp[i, :, 16 + d] = a_log[d, h]
            hp[i, :, 18 + d] = dt_bias[d, h]
        hp[i, :, 15] = onw
    return {"qkvT": qkvT, "zT": zT, "gates": gates, "hp": hp}


ALPHA = 4.0 ** 0.25
NE = 8


def build_k4(TT, moe, NHALF=1, c=None):
    c = c or Ctx()
    nc, s = c.nc, c.s
    NTB = TT // 512
    NTT = TT // 128
    FF = 7168 if moe else 5632
    xT_d = c.din("xT", [D, TT * NHALF])
    aT_d = c.din("aT", [1024, TT * NHALF])
    bT_d = c.din("bT", [D, TT * NHALF])
    gfT_d = c.din("gfT", [D, TT * NHALF])
    gdT_d = c.din("gdT", [D, TT * NHALF])
    pT_d = c.din("pT", [256, TT * NHALF])
    wf_d = c.din("wf", [1024, D])
    wdl_d = c.din("wdl", [D, D])
    wo_d = c.din("wo", [D, D])
    pg_d = c.din("pg", [D, D])
    pp_d = c.din("pp", [256, D])
    lnp_d = c.din("lnp", [128, 4, KT])
    id_d = c.din("ident", [128, 128])
    if moe:
        rw_d = c.din("rw", [128, KT, NE])
        gu_d = c.din("egu", [NE, D, 2 * FF])
        dn_d = c.din("edn", [NE, FF, D])
        experts = [(gu_d[e], dn_d[e]) for e in range(NE)]
    else:
        gu_d = c.din("gu", [D, 2 * FF])
        dn_d = c.din("dn", [FF, D])
        experts = [(gu_d, dn_d)]
    out_d = c.dout("x2T", [D, TT * NHALF])

    st_misc = s.stream("misc")
    lnp = load_small(c, "sp", st_misc, "lnp", lnp_d, [128, 4, KT])
    ident = load_small(c, "sp", st_misc, "ident", id_d, [128, 128])
    s.keys["lngb"] = {"w": (st_misc.key, st_misc.cnt, None), "r": {}}
    ones = c.sb("ones", [128, 128], F32)
    s.op("dve", lambda v: v.memset(ones[:], 1.0 / D), writes=["ones"])
    ones1 = c.sb("ones1", [128, 128], F32)
    s.op("dve", lambda v: v.memset(ones1[:], 1.0), writes=["ones1"])
    psr = c.psum_ring()
    scr = ln_scratch(c)

    acc = c.sb("acc", [128, KT, TT], F32)
    bufA = c.sb("bufA", [128, KT, TT], BF16)
    bufB = c.sb("bufB", [128, max(KT * TT, 8192 + 4 * TT)], BF16)
    mb = bufB[:, 0:KT * TT].rearrange("p (a b) -> p a b", a=KT)
    ab = bufB[:, 0:8 * TT].rearrange("p (a b) -> p a b", a=8)
    NSL = 2
    wslots = [(c.sb("w%d" % i, [128, KT, 512], BF16), "w%d" % i) for i in range(NSL)]
    wstreams = [s.stream("w%d" % i) for i in range(NSL)]
    gts = [c.sb("gt%d" % i, [128, TT], F32) for i in range(2)]
    gstr = [s.stream("gt%d" % i) for i in range(2)]
    sgb = [c.sb("sg%d" % i, [128, 512], F32) for i in range(2)]
    tmpb = c.sb("tmpb", [128, 512], F32)
    pb = c.sb("pb", [128, 2, TT], BF16)
    wpp = c.sb("wpp", [128, 2, D], BF16)
    st_act = s.stream("actin")
    cnt = {"gt": 0, "sg": 0}

    wdstr = [s.stream("wd%d" % i) for i in range(2)]
    st_out = s.stream("out")
    if moe:
        rw = load_small(c, "sp", st_misc, "rw", rw_d, [128, KT, NE])
        comb_tm = c.sb("comb_tm", [128, NTT, NE], F32)
        rs = [c.sb("rs%d" % i, [128, NE], F32) for i in range(4)]
        r1 = c.sb("r1", [128, 8], F32)
        comb_e = c.sb("comb_e", [128, TT], F32)
        lbs = [c.sb("lb%d" % i, [128, 128], F32) for i in range(2)]
    s.dma("pool", st_act, wpp[:], pp_d.rearrange("(kt p) n -> p kt n", p=128), writes=["wpp"])
    for hf in range(NHALF):
        _k4_half(locals(), hf)
    return c.close()


def _k4_half(L, hf):
    g = globals()
    (c, s, TT, NTB, NTT, FF, moe, experts, psr, scr, acc, bufA, bufB, mb, ab, NSL, wslots, wstreams, gts, gstr, sgb, tmpb, pb,
     wpp, st_act, cnt, lnp, ident, ones, ones1, wdstr, st_out) = [L[k] for k in (
        "c", "s", "TT", "NTB", "NTT", "FF", "moe", "experts", "psr", "scr", "acc", "bufA", "bufB", "mb", "ab", "NSL", "wslots",
        "wstreams", "gts", "gstr", "sgb", "tmpb", "pb", "wpp", "st_act", "cnt", "lnp", "ident", "ones", "ones1", "wdstr", "st_out")]
    xT_d, aT_d, bT_d, gfT_d, gdT_d, pT_d, wf_d, wdl_d, wo_d, pg_d, out_d = [L[k] for k in (
        "xT_d", "aT_d", "bT_d", "gfT_d", "gdT_d", "pT_d", "wf_d", "wdl_d", "wo_d", "pg_d", "out_d")]
    if moe:
        rw, comb_tm, rs, r1, comb_e, lbs = [L[k] for k in ("rw", "comb_tm", "rs", "r1", "comb_e", "lbs")]
    c0 = hf * TT
    cs = slice(c0, c0 + TT)
    s.merge_into("bufB", ["wd0", "wd1", "hT0", "hT1"])
    s.dma("pool", st_act, ab, aT_d.rearrange("(kt p) t -> p kt t", p=128)[:, :, cs], writes=["bufB"])
    s.dma("pool", st_act, bufA[:], bT_d.rearrange("(kt p) t -> p kt t", p=128)[:, :, cs], writes=["bufA"])

    def load_tile(src_d, j, func):
        i = cnt["gt"] % 2
        cnt["gt"] += 1
        s.dma("sp", gstr[i], gts[i][:], src_d[j * 128:(j + 1) * 128, cs], writes=["gt%d" % i])
        if func is not None:
            s.op("act", lambda a: a.activation(out=gts[i][:], in_=gts[i][:], func=func), reads=["gt%d" % i],
                 writes=["gt%d" % i])
        return gts[i], "gt%d" % i

    cur = {}

    def epi1(row0, rows, tb, pt, pkey):
        j = row0 // 128
        sl = slice(tb * 512, (tb + 1) * 512)
        if tb == 0:
            cur["t"] = load_tile(gfT_d, j, AF.Sigmoid)
        g, gk = cur["t"]
        s.op("dve", lambda v: v.tensor_tensor(out=acc[:, j, sl], in0=g[:, sl], in1=pt[:], op=ALU.mult),
             reads=[gk, pkey], writes=["acc"])

    gemm_fm(c, wf_d, 0, D, ab, "bufB", 8, TT, wslots, wstreams, psr, epi1)

    def epi2(row0, rows, tb, pt, pkey):
        j = row0 // 128
        sl = slice(tb * 512, (tb + 1) * 512)
        if tb == 0:
            cur["t"] = load_tile(gdT_d, j, AF.Sigmoid)
        g, gk = cur["t"]
        s.op("dve", lambda v: v.tensor_tensor(out=tmpb[:], in0=g[:, sl], in1=pt[:], op=ALU.mult),
             reads=[gk, pkey], writes=["tmpb"])
        s.op("dve", lambda v: v.tensor_tensor(out=mb[:, j, sl], in0=tmpb[:], in1=acc[:, j, sl], op=ALU.add),
             reads=["tmpb", "acc"], writes=["bufB"])

    gemm_fm(c, wdl_d, 0, D, bufA, "bufA", KT, TT, wslots, wstreams, psr, epi2)

    def epi3(row0, rows, tb, pt, pkey):
        j = row0 // 128
        sl = slice(tb * 512, (tb + 1) * 512)
        if tb == 0:
            cur["t"] = load_tile(xT_d, j, None)
        g, gk = cur["t"]
        s.op("dve", lambda v: v.scalar_tensor_tensor(out=acc[:, j, sl], in0=g[:, sl], scalar=float(ALPHA), in1=pt[:],
                                                    op0=ALU.mult, op1=ALU.add), reads=[gk, pkey], writes=["acc"])

    gemm_fm(c, wo_d, 0, D, mb, "bufB", KT, TT, wslots, wstreams, psr, epi3)

    for tb in range(NTB):
        sl = slice(tb * 512, (tb + 1) * 512)
        ln_block(c, lambda kt, sl=sl: acc[:, kt, sl], "acc", KT, lnp[:, 0, :], lnp[:, 1, :], ones, psr, scr,
                 out_bf=lambda kt, sl=sl: bufA[:, kt, sl], out_f32=lambda kt, sl=sl: acc[:, kt, sl],
                 okeys=("bufA", "acc"))

    if moe:
        for tt in range(NTT):
            pt, pk = psr.next()
            s.group("pe", [lambda pe, kt=kt, tt=tt, pt=pt: pe.matmul(pt[:, 0:NE], lhsT=acc[:, kt, tt * 128:(tt + 1) * 128],
                                                                    rhs=rw[:, kt, :], start=(kt == 0), stop=(kt == KT - 1))
                           for kt in range(KT)], reads=["acc", "rw"], writes=[pk])
            s.op("act", lambda a, pt=pt: a.activation(out=rs[0][:], in_=pt[:, 0:NE], func=AF.Copy), reads=[pk], writes=["rs0"])
            s.op("dve", lambda v: v.reduce_max(out=r1[:, 0:1], in_=rs[0][:], axis=mybir.AxisListType.X),
                 reads=["rs0"], writes=["r1"])
            s.op("dve", lambda v: v.tensor_scalar(out=rs[1][:], in0=rs[0][:], scalar1=r1[:, 0:1], scalar2=None,
                                                  op0=ALU.is_equal), reads=["rs0", "r1"], writes=["rs1"])
            s.op("dve", lambda v: v.scalar_tensor_tensor(out=rs[2][:], in0=rs[1][:], scalar=-1e30, in1=rs[0][:],
                                                        op0=ALU.mult, op1=ALU.add), reads=["rs1", "rs0"], writes=["rs2"])
            s.op("dve", lambda v: v.reduce_max(out=r1[:, 1:2], in_=rs[2][:], axis=mybir.AxisListType.X),
                 reads=["rs2"], writes=["r1"])
            s.op("dve", lambda v: v.tensor_scalar(out=rs[3][:], in0=rs[2][:], scalar1=r1[:, 1:2], scalar2=None,
                                                  op0=ALU.is_equal), reads=["rs2", "r1"], writes=["rs3"])
            s.op("dve", lambda v: v.tensor_tensor(out=r1[:, 2:3], in0=r1[:, 1:2], in1=r1[:, 0:1], op=ALU.subtract),
                 reads=["r1"], writes=["r1"])
            s.op("act", lambda a: a.activation(out=r1[:, 3:4], in_=r1[:, 2:3], func=AF.Exp), reads=["r1"], writes=["r1"])
            s.op("dve", lambda v: v.tensor_scalar(out=r1[:, 4:5], in0=r1[:, 3:4], scalar1=1.0, scalar2=None, op0=ALU.add),
                 reads=["r1"], writes=["r1"])
            s.op("dve", lambda v: v.reciprocal(out=r1[:, 5:6], in_=r1[:, 4:5]), reads=["r1"], writes=["r1"])
            s.op("dve", lambda v: v.tensor_tensor(out=r1[:, 6:7], in0=r1[:, 3:4], in1=r1[:, 5:6], op=ALU.mult),
                 reads=["r1"], writes=["r1"])
            s.op("dve", lambda v: v.tensor_scalar(out=rs[0][:], in0=rs[1][:], scalar1=r1[:, 5:6], scalar2=None,
                                                  op0=ALU.mult), reads=["rs1", "r1"], writes=["rs0"])
            s.op("dve", lambda v, tt=tt: v.scalar_tensor_tensor(out=comb_tm[:, tt, :], in0=rs[3][:], scalar=r1[:, 6:7],
                                                               in1=rs[0][:], op0=ALU.mult, op1=ALU.add),
                 reads=["rs3", "rs0", "r1"], writes=["comb_tm"])
    for kt in range(KT):
        s.op("dve", lambda v, kt=kt: v.tensor_scalar(out=acc[:, kt, :], in0=acc[:, kt, :], scalar1=float(ALPHA), scalar2=None,
                                                     op0=ALU.mult), reads=["acc"], writes=["acc"])
    wds = [bufB[:, i * 4096:(i + 1) * 4096].rearrange("p (a b) -> p a b", a=2) for i in range(2)]
    hTs = [bufB[:, 8192 + i * 2 * TT: 8192 + (i + 1) * 2 * TT].rearrange("p (a b) -> p a b", a=2) for i in range(2)]
    for k in ("wd0", "wd1", "hT0", "hT1"):
        s.merge_into(k, ["bufB"], reset=True)
    nch = FF // 256
    work = [(e, ch) for e in range(len(experts)) for ch in range(nch)]

    def issue(wi):
        e, ch = work[wi]
        gu, dn = experts[e]
        guv = gu.rearrange("(kt p) n -> p kt n", p=128)
        c0 = ch * 256
        sl = wi % NSL
        s.dma("pool", wstreams[sl], wslots[sl][0][:, :, 0:256], guv[:, :, c0:c0 + 256], writes=[wslots[sl][1]])
        s.dma("pool", wstreams[sl], wslots[sl][0][:, :, 256:512], guv[:, :, FF + c0:FF + c0 + 256], writes=[wslots[sl][1]])
        s.dma("pool", wdstr[wi % 2], wds[wi % 2], dn[c0:c0 + 256, :].rearrange("(kt p) n -> p kt n", p=128),
              writes=["wd%d" % (wi % 2)])

    for wi in range(min(NSL, len(work))):
        issue(wi)
    for wi, (e, ch) in enumerate(work):
        if moe and ch == 0:
            for tt in range(NTT):
                lb = lbs[tt % 2]
                lk = "lb%d" % (tt % 2)
                s.op("dve", lambda v, tt=tt, lb=lb, e=e: v.tensor_scalar(out=lb[:], in0=ones1[:], scalar1=comb_tm[:, tt, e:e + 1],
                                                                       scalar2=None, op0=ALU.mult),
                     reads=["ones1", "comb_tm"], writes=[lk])
                if tt % 4 == 0:
                    pc, pck = psr.next()
                s.op("pe", lambda pe, pc=pc, tt=tt, lb=lb: pe.matmul(pc[:, (tt % 4) * 128:(tt % 4 + 1) * 128], lhsT=lb[:],
                                                                   rhs=ident[:], start=True, stop=True),
                     reads=[lk, "ident"], writes=[pck])
                if tt % 4 == 3:
                    s.op("act", lambda a, pc=pc, tt=tt: a.activation(out=comb_e[:, (tt // 4) * 512:(tt // 4 + 1) * 512],
                                                                     in_=pc[:], func=AF.Copy), reads=[pck], writes=["comb_e"])
        sl = wi % NSL
        wt, wkey = wslots[sl]
        hT = hTs[wi % 2]
        hk = "hT%d" % (wi % 2)
        wd = wds[wi % 2]
        wdk = "wd%d" % (wi % 2)
        for jt in range(2):
            for tb in range(NTB):
                tsl = slice(tb * 512, (tb + 1) * 512)
                pg_, pgk = psr.next()
                pu_, puk = psr.next()
                s.group("pe", [lambda pe, kt=kt, pg_=pg_, jt=jt, tsl=tsl, wt=wt: pe.matmul(
                    pg_[:], lhsT=wt[:, kt, jt * 128:(jt + 1) * 128], rhs=bufA[:, kt, tsl], start=(kt == 0), stop=(kt == KT - 1))
                    for kt in range(KT)], reads=[wkey, "bufA"], writes=[pgk])
                s.group("pe", [lambda pe, kt=kt, pu_=pu_, jt=jt, tsl=tsl, wt=wt: pe.matmul(
                    pu_[:], lhsT=wt[:, kt, 256 + jt * 128:256 + (jt + 1) * 128], rhs=bufA[:, kt, tsl], start=(kt == 0),
                    stop=(kt == KT - 1)) for kt in range(KT)], reads=[wkey, "bufA"], writes=[puk])
                i = cnt["sg"] % 2
                cnt["sg"] += 1
                s.op("act", lambda a, i=i, pg_=pg_: a.activation(out=sgb[i][:], in_=pg_[:], func=AF.Silu),
                     reads=[pgk], writes=["sg%d" % i])
                if moe:
                    s.op("dve", lambda v, i=i, pu_=pu_: v.tensor_tensor(out=tmpb[:], in0=sgb[i][:], in1=pu_[:], op=ALU.mult),
                         reads=["sg%d" % i, puk], writes=["tmpb"])
                    s.op("dve", lambda v, jt=jt, tsl=tsl, hT=hT: v.tensor_tensor(out=hT[:, jt, tsl], in0=tmpb[:],
                                                                                in1=comb_e[:, tsl], op=ALU.mult),
                         reads=["tmpb", "comb_e"], writes=[hk])
                else:
                    s.op("dve", lambda v, i=i, pu_=pu_, jt=jt, tsl=tsl, hT=hT: v.tensor_tensor(
                        out=hT[:, jt, tsl], in0=sgb[i][:], in1=pu_[:], op=ALU.mult),
                         reads=["sg%d" % i, puk], writes=[hk])
        for j in range(KT):
            for tb in range(NTB):
                tsl = slice(tb * 512, (tb + 1) * 512)
                pd_, pdk = psr.next()
                s.group("pe", [lambda pe, jt=jt, pd_=pd_, j=j, tsl=tsl, wd=wd, hT=hT: pe.matmul(
                    pd_[:], lhsT=wd[:, jt, j * 128:(j + 1) * 128], rhs=hT[:, jt, tsl], start=(jt == 0), stop=(jt == 1))
                    for jt in range(2)], reads=[wdk, hk], writes=[pdk])
                s.op("dve", lambda v, j=j, tsl=tsl, pd_=pd_: v.tensor_tensor(out=acc[:, j, tsl], in0=acc[:, j, tsl],
                                                                            in1=pd_[:], op=ALU.add),
                     reads=[pdk, "acc"], writes=["acc"])
        if wi + NSL < len(work):
            issue(wi + NSL)

    s.dma("pool", st_act, pb[:], pT_d.rearrange("(kt p) t -> p kt t", p=128)[:, :, cs], writes=["pb"])

    def epi6(row0, rows, tb, pt, pkey):
        j = row0 // 128
        sl = slice(tb * 512, (tb + 1) * 512)
        p2, p2k = psr.next()
        s.group("pe", [lambda pe, kt=kt: pe.matmul(p2[:], lhsT=wpp[:, kt, j * 128:(j + 1) * 128], rhs=pb[:, kt, sl],
                                                   start=(kt == 0), stop=(kt == 1)) for kt in range(2)],
                reads=["wpp", "pb"], writes=[p2k])
        i = cnt["sg"] % 2
        cnt["sg"] += 1
        s.op("act", lambda a: a.activation(out=sgb[i][:], in_=pt[:], func=AF.Sigmoid), reads=[pkey], writes=["sg%d" % i])
        s.op("dve", lambda v: v.tensor_tensor(out=tmpb[:], in0=sgb[i][:], in1=p2[:], op=ALU.mult),
             reads=["sg%d" % i, p2k], writes=["tmpb"])
        s.op("dve", lambda v: v.tensor_tensor(out=acc[:, j, sl], in0=acc[:, j, sl], in1=tmpb[:], op=ALU.add),
             reads=["tmpb", "acc"], writes=["acc"])

    gemm_fm(c, pg_d, 0, D, bufA, "bufA", KT, TT, wslots, wstreams, psr, epi6)

    for tb in range(NTB):
        sl = slice(tb * 512, (tb + 1) * 512)
        ln_block(c, lambda kt, sl=sl: acc[:, kt, sl], "acc", KT, lnp[:, 2, :], lnp[:, 3, :], ones, psr, scr,
                 out_f32=lambda kt, sl=sl: acc[:, kt, sl], okeys=(None, "acc"))
    s.dma("sp", st_out, out_d.rearrange("(kt p) t -> p kt t", p=128)[:, :, cs], acc[:], reads=["acc"])


def k4_weights(inp, layer):
    moe = (layer % 2 == 1)
    w = {
        "wf": inp["w_fourier"][layer], "wdl": inp["w_delta"][layer], "wo": inp["w_out"][layer],
        "pg": inp["ple_gate"][layer], "pp": inp["ple_proj"][layer],
        "lnp": np.ascontiguousarray(np.stack([_vecT(inp["ln1_g"][layer]), _vecT(inp["ln1_b"][layer]),
                                              _vecT(inp["ln2_g"][layer]), _vecT(inp["ln2_b"][layer])], axis=1)),
        "ident": np.eye(128, dtype=np.float32),
    }
    if moe:
        w["rw"] = np.ascontiguousarray(inp["router_w"][layer // 2].reshape(KT, 128, NE).transpose(1, 0, 2))
        w["egu"] = inp["exp_gate_up"][layer // 2]
        w["edn"] = inp["exp_down"][layer // 2]
    else:
        w["gu"] = inp["ffn_gate_up"][layer // 2]
        w["dn"] = inp["ffn_down"][layer // 2]
    return w


def run_k4(acts_cores, weights, moe, NHALF=1):
    TT = acts_cores[0]["xT"].shape[1] // NHALF
    nc = build_k4(TT, moe, NHALF)
    in_maps = [dict(a, **weights) for a in acts_cores]
    res = run_bass_kernel_spmd(nc, in_maps, core_ids=list(range(len(acts_cores))))
    return [r["x2T"] for r in res.results]


def pack_k3_fm(P, b, heads, conv_w, a_log, dt_bias, onw):
    cols = slice(b * S_LEN, (b + 1) * S_LEN)
    NSB = S_LEN // 128
    NH = len(heads)
    qkvT = np.empty((NH, 3, 128, S_LEN), np.float32)
    zT = np.empty((NH, 128, S_LEN), np.float32)
    gates = np.empty((NH, 2, 2, 128, NSB), np.float32)
    hp = np.zeros((NH, 128, 24), np.float32)
    for i, h in enumerate(heads):
        for j in range(3):
            r0 = 1024 + j * 2048 + h * 128
            qkvT[i, j] = P[r0:r0 + 128, cols]
            hp[i, :, 5 * j:5 * j + 5] = conv_w[:, j * 2048 + h * 128: j * 2048 + (h + 1) * 128].T
        zT[i] = P[7168 + h * 128: 7168 + (h + 1) * 128, cols]
        for d in range(2):
            gates[i, d, 0] = P[9216 + d * 16 + h, cols].reshape(NSB, 128).T
            gates[i, d, 1] = P[9248 + d * 16 + h, cols].reshape(NSB, 128).T
            hp[i, :, 16 + d] = a_log[d, h]
            hp[i, :, 18 + d] = dt_bias[d, h]
        hp[i, :, 15] = onw
    return {"qkvT": qkvT, "zT": zT, "gates": gates, "hp": hp}


def build_fused(S=4096):
    c = Ctx()
    nc = c.nc
    HALF = S // 2
    ein = lambda name, shape, dt=F32: nc.dram_tensor(name, list(shape), dt, kind="ExternalInput").ap()
    xT = ein("xT", [D, S])
    p0T = ein("p0T", [256, S])
    p1T = ein("p1T", [256, HALF])
    embg = ein("embg", [128, KT])
    embb = ein("embb", [128, KT])
    w_in = ein("w_in", [2, D, IN_W])
    hp = ein("hp", [2, 16, 128, 24])
    dcst = ein("dconsts", [128, NCONST, 128])
    ccsc = ein("ccsc", [FG, 2 * FG], BF16)
    csm = ein("csm", [S, S], BF16)
    nsm = ein("nsm", [S, S], BF16)
    wf = ein("wf", [2, 1024, D])
    wdl = ein("wdl", [2, D, D])
    wo = ein("wo", [2, D, D])
    pg = ein("pg", [2, D, D])
    pp = ein("pp", [2, 256, D])
    lnp = ein("lnp", [2, 128, 4, KT])
    ident = ein("ident", [128, 128])
    gu = ein("gu", [D, 2 * 5632])
    dn = ein("dn", [5632, D])
    rw = ein("rw", [128, KT, NE])
    egu = ein("egu", [NE, D, 2 * 7168])
    edn = ein("edn", [NE, 7168, D])
    out = nc.dram_tensor("x2T", [D, HALF], F32, kind="ExternalOutput").ap()
    scr = lambda name, shape: nc.dram_tensor(name, list(shape), F32).ap()
    projT = scr("projT_s", [IN_W, S])
    xn = scr("xn_s", [D, S])
    A_T = scr("A_s", [1024, S])
    B_T = scr("B_s", [D, S])
    x1s = scr("x1_s", [D, S])
    TT1 = min(2048, S)
    TT4 = min(1024, HALF)
    for layer in range(2):
        xin = xT if layer == 0 else x1s
        for blk in range(S // TT1):
            cols = slice(blk * TT1, (blk + 1) * TT1)
            io = {"xT": xin[:, cols], "w": w_in[layer], "g": embg, "b": embb, "projT": projT[:, cols]}
            if layer == 0:
                io["xnT"] = xn[:, cols]
            c.begin_stage(io)
            trim = (layer == 1 and blk * TT1 >= HALF)
            build_k1(TT1, layer == 0, c=c, ranges=([(0, 7168), (9216, 9280)] if trim else None))
        uview = projT[0:1024, :].rearrange("(g c) t -> g c t", g=4)
        aview = A_T.rearrange("(g c) t -> g c t", g=4)
        for gp in range(2):
            c.begin_stage({"uT": uview[2 * gp:2 * gp + 2], "ccsc": ccsc, "csm": csm, "nsm": nsm,
                           "aT": aview[2 * gp:2 * gp + 2]})
            build_k2(2, 256, c=c, S_LEN=S)
        c.begin_stage({"qkvT": projT[1024:7168, :].rearrange("(j h d) t -> h j d t", j=3, h=16),
                       "zT": projT[7168:9216, :].rearrange("(h d) t -> h d t", h=16),
                       "gate_rows": projT[9216:9280, :], "hp": hp[layer], "consts": dcst,
                       "oT": B_T.rearrange("(h d) t -> h d t", h=16)})
        build_k3v2(16, S, 4, c=c, own_half=(layer == 1))
        ntok = S if layer == 0 else HALF
        xres = xn if layer == 0 else x1s
        io = {"xT": xres[:, 0:ntok], "aT": A_T[:, 0:ntok], "bT": B_T[:, 0:ntok], "gfT": projT[9280:11328, 0:ntok],
              "gdT": projT[11328:13376, 0:ntok], "pT": (p0T if layer == 0 else p1T),
              "wf": wf[layer], "wdl": wdl[layer], "wo": wo[layer], "pg": pg[layer], "pp": pp[layer], "lnp": lnp[layer],
              "ident": ident, "x2T": (x1s if layer == 0 else out)}
        if layer == 0:
            io.update({"gu": gu, "dn": dn})
        else:
            io.update({"rw": rw, "egu": egu, "edn": edn})
        c.begin_stage(io)
        build_k4(TT4, layer == 1, ntok // TT4, c=c)
    return c.finish_all()


def fused_inputs(inputs, ncores, S):
    f32 = lambda v: np.asarray(v, np.float32)
    x = f32(inputs["x"])
    p = f32(inputs["p"])
    HALF = S // 2
    w_in = f32(inputs["w_in"])
    w_in_sw = w_in.copy()
    w_in_sw[:, :, 9216:9232], w_in_sw[:, :, 9232:9248] = w_in[:, :, 9232:9248], w_in[:, :, 9216:9232]
    w_in_sw[:, :, 9248:9264], w_in_sw[:, :, 9264:9280] = w_in[:, :, 9264:9280], w_in[:, :, 9248:9264]
    conv_w, a_log, dt_bias, onw = f32(inputs["conv_w"]), f32(inputs["a_log"]), f32(inputs["dt_bias"]), f32(inputs["o_norm_w"])
    hps = []
    for r in range(2):
        hp = np.zeros((2, 16, 128, 24), np.float32)
        for layer in range(2):
            cw = conv_w[layer][::-1] if r else conv_w[layer]
            for h in range(16):
                for j in range(3):
                    hp[layer, h, :, 5 * j:5 * j + 5] = cw[:, j * 2048 + h * 128: j * 2048 + (h + 1) * 128].T
                hp[layer, h, :, 15] = onw[layer]
                for d in range(2):
                    ds = 1 - d if r else d
                    hp[layer, h, :, 16 + d] = a_log[layer, ds, h]
                    hp[layer, h, :, 18 + d] = dt_bias[layer, ds, h]
        hps.append(hp)
    dfts = [dft_consts(S, flip=False), dft_consts(S, flip=True)]
    shared = {
        "embg": _vecT(f32(inputs["emb_ln_g"])), "embb": _vecT(f32(inputs["emb_ln_b"])),
        "dconsts": delta_consts(),
        "wf": f32(inputs["w_fourier"]), "wdl": f32(inputs["w_delta"]), "wo": f32(inputs["w_out"]),
        "pg": f32(inputs["ple_gate"]), "pp": f32(inputs["ple_proj"]),
        "lnp": np.ascontiguousarray(np.stack([np.stack([_vecT(f32(inputs[k])[layer]) for k in ("ln1_g", "ln1_b", "ln2_g", "ln2_b")],
                                                       axis=1) for layer in range(2)])),
        "ident": np.eye(128, dtype=np.float32),
        "gu": f32(inputs["ffn_gate_up"])[0], "dn": f32(inputs["ffn_down"])[0],
        "rw": np.ascontiguousarray(f32(inputs["router_w"])[0].reshape(KT, 128, NE).transpose(1, 0, 2)),
        "egu": f32(inputs["exp_gate_up"])[0], "edn": f32(inputs["exp_down"])[0],
    }
    in_maps = []
    for cidx in range(ncores):
        b, r = cidx // 2, cidx % 2
        xb = x[b][::-1] if r else x[b]
        p0 = p[0, b][::-1] if r else p[0, b]
        p1 = p[1, b][::-1] if r else p[1, b]
        m = dict(shared)
        m.update({"xT": np.ascontiguousarray(xb.T), "p0T": np.ascontiguousarray(p0.T),
                  "p1T": np.ascontiguousarray(p1[:HALF].T), "w_in": (w_in_sw if r else w_in), "hp": hps[r],
                  "ccsc": dfts[r][0], "csm": dfts[r][1], "nsm": dfts[r][2]})
        in_maps.append(m)
    return in_maps


def fused_gather(results, B_, S):
    HALF = S // 2
    out = np.empty((B_, S, D), np.float32)
    for cidx, r_ in enumerate(results):
        b, r = cidx // 2, cidx % 2
        y = r_["x2T"].T
        if r:
            out[b, S - 1 - np.arange(HALF)] = y
        else:
            out[b, 0:HALF] = y
    return out


def kernel(**inputs):
    B_, S, _ = np.asarray(inputs["x"]).shape
    ncores = 2 * B_
    nc = build_fused(S)
    in_maps = fused_inputs(inputs, ncores, S)
    res = run_bass_kernel_spmd(nc, in_maps, core_ids=list(range(ncores)))
    return fused_gather(res.results, B_, S)


def build_k3v2(NH=8, SEQ=4096, SEG=4, c=None, own_half=False):
    c = c or Ctx()
    nc, s = c.nc, c.s
    NSB = SEQ // 128
    NTB = SEQ // 512
    qkv_d = c.din("qkvT", [NH, 3, 128, SEQ])
    z_d = c.din("zT", [NH, 128, SEQ])
    gr_d = c.io.get("gate_rows")
    gates_d = None if gr_d is not None else c.din("gates", [NH, 2, 2, 128, NSB])
    hp_d = c.din("hp", [NH, 128, 24])
    cst_d = c.din("consts", [128, NCONST, 128])
    out_d = c.dout("oT", [NH, 128, SEQ])

    st_c = s.stream("const")
    cst = c.sb("cst", [128, NCONST, 128], F32)
    s.dma("sp", st_c, cst[:], cst_d, writes=["cst"])
    ident = cst[:, C_ID, :]
    ones = cst[:, C_ONE, :]
    psr = c.psum_ring()
    st_in = s.stream("in")
    st_hp = s.stream("hp")
    st_g = [s.stream("g0"), s.stream("g1")]
    st_out = s.stream("out")
    upad = c.sb("upad", [128, SEQ + 4], F32)
    qT = c.sb("qT", [128, SEQ], F32)
    kT = c.sb("kT", [128, SEQ], F32)
    k_tm = c.sb("k_tm", [128, NSB, 128], F32)
    v_tm = c.sb("v_tm", [128, NSB, 128], F32)
    oTd = [c.sb("oT%d" % d, [128, SEQ], F32) for d in range(2)]
    ybuf = oTd[1]
    hp = c.sb("hp", [128, 24], F32)
    hq = c.sb("hq", [128, 8], F32)
    sm = c.sb("sm", [128, 512], F32)
    sm2 = c.sb("sm2", [128, 512], F32)
    qTb = c.sb("qTb", [128, SEQ], BF16)
    kTb = c.sb("kTb", [128, SEQ], BF16)
    identb = c.sb("identb", [128, 128], BF16)
    s.op("act", lambda a: a.activation(out=identb[:], in_=ident, func=AF.Copy), reads=["cst"], writes=["identb"])

    class DirBuf:
        pass

    NPRE = 2

    DB = []
    for d in range(2):
        b = DirBuf()
        t = lambda name, shape: c.sb("%s_%d" % (name, d), shape, F32)
        tb = lambda name, shape: c.sb("%s_%d" % (name, d), shape, BF16)
        b.gt = t("gt", [128, 2, NSB])
        b.grow = t("grow", [NSB, 2, 128])
        b.g_tm = t("g_tm", [128, NSB])
        b.beta_tm = t("beta_tm", [128, NSB])
        b.nbeta_tm = t("nbeta_tm", [128, NSB])
        b.gc_tm = t("gc_tm", [128, NSB])
        b.bexp_tm = t("bexp_tm", [128, NSB])
        b.ekd_tm = t("ekd_tm", [128, 2, NSB])
        b.glast = t("glast", [128, 2, NSB])
        b.tsm = t("tsm", [128, NSB])
        b.S = t("S", [128, 128])
        b.Sb = tb("Sb", [128, 128])
        b.vnew = tb("vnew", [128, 128])
        b.tmp = []
        for q in range(NPRE):
            T = DirBuf()
            T.sfx = "%d_%d" % (q, d)
            T.gbc = t("gbc%d" % q, [128, 256])
            T.erow = t("erow%d" % q, [128, 256])
            T.dT = t("dT%d" % q, [128, 256])
            T.dS = t("dS%d" % q, [128, 256])
            T.Pm = [tb("Pm%d_%d" % (i, q), [128, 256]) for i in range(2)]
            T.PmT = [tb("PmT%d_%d" % (i, q), [128, 256]) for i in range(2)]
            T.X = tb("X%d" % q, [128, 256])
            T.ps = psr.sub([4 * d + q])
            b.tmp.append(T)
        b.u_sg = [t("u_sg%d" % p, [128, SEG * 128]) for p in range(2)]
        b.wT_sg = [tb("wT_sg%d" % p, [128, SEG * 128]) for p in range(2)]
        b.qd_sg = [tb("qd_sg%d" % p, [128, SEG * 128]) for p in range(2)]
        b.qk_sg = [tb("qk_sg%d" % p, [128, SEG * 128]) for p in range(2)]
        b.kd_sg = [tb("kd_sg%d" % p, [128, SEG, 2, 128]) for p in range(2)]
        b.vb_sg = [tb("vb_sg%d" % p, [128, SEG, 128]) for p in range(2)]
        b.kbg_sg = [tb("kbg_sg%d" % p, [128, SEG, 128]) for p in range(2)]
        b.pre_ps = psr.sub([4 * d, 4 * d + 1])
        b.scan_ps = psr.sub([4 * d + 2, 4 * d + 3])
        DB.append(b)

    s.op("dve", lambda v: v.memset(upad[:, 0:2], 0.0), writes=["upad"])
    s.op("dve", lambda v: v.memset(upad[:, SEQ + 2:SEQ + 4], 0.0), writes=["upad"])
    for d in range(2):
        s.op("dve", lambda v, d=d: v.memset(DB[d].vnew[:], 0.0), writes=["vnew_%d" % d])

    def l2norm_inplace(buf, key, scale):
        for tb in range(NTB):
            sl = slice(tb * 512, (tb + 1) * 512)
            s.op("act", lambda a, sl=sl: a.activation(out=sm[:], in_=buf[:, sl], func=AF.Square), reads=[key], writes=["sm"])
            pt, pk = psr.next()
            s.op("pe", lambda pe, pt=pt: pe.matmul(pt[:], lhsT=ones, rhs=sm[:], start=True, stop=True),
                 reads=["sm", "cst"], writes=[pk])
            s.op("dve", lambda v, pt=pt: v.tensor_scalar(out=sm2[:], in0=pt[:], scalar1=float(L2_EPS), scalar2=None,
                                                         op0=ALU.add), reads=[pk], writes=["sm2"])
            s.op("act", lambda a: a.activation(out=sm2[:], in_=sm2[:], func=AF.Ln), reads=["sm2"], writes=["sm2"])
            s.op("act", lambda a: a.activation(out=sm2[:], in_=sm2[:], func=AF.Exp, scale=-0.5),
                 reads=["sm2"], writes=["sm2"])
            s.op("dve", lambda v, sl=sl: v.scalar_tensor_tensor(out=buf[:, sl], in0=buf[:, sl], scalar=float(scale),
                                                               in1=sm2[:], op0=ALU.mult, op1=ALU.mult),
                 reads=[key, "sm2"], writes=[key])

    def conv_silu(src_ap, col0, dst, dkey):
        s.dma("sp", st_in, upad[:, 2:SEQ + 2], src_ap, writes=["upad"])
        s.op("dve", lambda v: v.tensor_scalar(out=ybuf[:], in0=upad[:, 0:SEQ], scalar1=hp[:, col0:col0 + 1],
                                              scalar2=None, op0=ALU.mult), reads=["upad", "hp"], writes=["oT1"])
        for j in range(1, 5):
            s.op("dve", lambda v, j=j: v.scalar_tensor_tensor(out=ybuf[:], in0=upad[:, j:SEQ + j],
                                                             scalar=hp[:, col0 + j:col0 + j + 1], in1=ybuf[:],
                                                             op0=ALU.mult, op1=ALU.add),
                 reads=["upad", "hp", "oT1"], writes=["oT1"])
        s.op("act", lambda a: a.activation(out=dst[:], in_=ybuf[:], func=AF.Silu), reads=["oT1"], writes=[dkey])

    def to_tm(src, skey, dst, dkey):
        for g4 in range(NSB // 4):
            pt, pk = psr.next()
            fns = [lambda pe, i=i, pt=pt, g4=g4: pe.transpose(pt[:, i * 128:(i + 1) * 128],
                                                              src[:, (g4 * 4 + i) * 128:(g4 * 4 + i + 1) * 128], ident)
                   for i in range(4)]
            s.group("pe", fns, reads=[skey, "cst"], writes=[pk])
            s.op("act", lambda a, pt=pt, g4=g4: a.activation(out=dst[:, g4 * 4:(g4 + 1) * 4, :],
                                                             in_=pt[:].rearrange("p (a b) -> p a b", a=4), func=AF.Copy),
                 reads=[pk], writes=[dkey])

    def mm(out_pt, lhsT, rhs, reads, pk):
        s.op("pe", lambda pe: pe.matmul(out_pt, lhsT=lhsT, rhs=rhs, start=True, stop=True), reads=reads, writes=[pk])

    def dir_setup(h, d):
        B = DB[d]
        K = lambda n: "%s_%d" % (n, d)
        U = cst[:, C_UF + d, :]
        ps_ = B.pre_ps
        if gates_d is not None:
            s.dma("sp", st_g[d], B.gt[:], gates_d[h, d].rearrange("g p n -> p g n"), writes=[K("gt")])
        else:
            for gi in range(2):
                s.dma("sp", st_g[d], B.grow[:, gi, :], gr_d[gi * 32 + d * 16 + h].rearrange("(n p) -> n p", p=128),
                      writes=[K("grow")])
            ptg, pkg = ps_.next()
            s.group("pe", [lambda pe, gi=gi, ptg=ptg: pe.transpose(ptg[:, gi * NSB:(gi + 1) * NSB], B.grow[:, gi, :],
                                                                   cst[0:NSB, C_ID, 0:NSB]) for gi in range(2)],
                    reads=[K("grow"), "cst"], writes=[pkg])
            s.op("act", lambda a, ptg=ptg: a.activation(out=B.gt[:], in_=ptg[:, 0:2 * NSB].rearrange("p (a b) -> p a b", a=2),
                                                        func=AF.Copy), reads=[pkg], writes=[K("gt")])
        s.op("act", lambda a: a.activation(out=B.beta_tm[:], in_=B.gt[:, 0, :], func=AF.Sigmoid),
             reads=[K("gt")], writes=[K("beta_tm")])
        s.op("dve", lambda v: v.tensor_scalar(out=B.nbeta_tm[:], in0=B.beta_tm[:], scalar1=-1.0, scalar2=None,
                                              op0=ALU.mult), reads=[K("beta_tm")], writes=[K("nbeta_tm")])
        s.op("act", lambda a: a.activation(out=B.tsm[:], in_=B.gt[:, 1, :], func=AF.Exp, bias=hp[:, 18 + d:19 + d]),
             reads=[K("gt"), "hp"], writes=[K("tsm")])
        s.op("dve", lambda v: v.tensor_scalar(out=B.tsm[:], in0=B.tsm[:], scalar1=1.0, scalar2=None, op0=ALU.add),
             reads=[K("tsm")], writes=[K("tsm")])
        s.op("act", lambda a: a.activation(out=B.tsm[:], in_=B.tsm[:], func=AF.Ln), reads=[K("tsm")], writes=[K("tsm")])
        s.op("dve", lambda v: v.tensor_scalar(out=B.g_tm[:], in0=B.tsm[:], scalar1=hq[:, d:d + 1], scalar2=None,
                                              op0=ALU.mult), reads=[K("tsm"), "hq"], writes=[K("g_tm")])
        pt, pk = ps_.next()
        mm(pt[:, 0:NSB], U, B.g_tm[:], ["cst", K("g_tm")], pk)
        s.op("act", lambda a: a.activation(out=B.gc_tm[:], in_=pt[:, 0:NSB], func=AF.Copy), reads=[pk], writes=[K("gc_tm")])
        pt2, pk2 = ps_.next()
        mm(pt2[:, 0:NSB], cst[:, C_BD, :], B.g_tm[:], ["cst", K("g_tm")], pk2)
        s.op("dve", lambda v: v.tensor_tensor(out=B.tsm[:], in0=pt2[:, 0:NSB], in1=B.gc_tm[:], op=ALU.subtract),
             reads=[pk2, K("gc_tm")], writes=[K("tsm")])
        s.op("act", lambda a: a.activation(out=B.tsm[:], in_=B.tsm[:], func=AF.Exp), reads=[K("tsm")], writes=[K("tsm")])
        for hf in range(2):
            s.op("dve", lambda v, hf=hf: v.tensor_scalar(out=B.ekd_tm[:, hf, :], in0=B.tsm[:],
                                                         scalar1=cst[:, C_MISC, hf:hf + 1], scalar2=None,
                                                         op0=ALU.mult), reads=[K("tsm"), "cst"], writes=[K("ekd_tm")])
        s.op("act", lambda a: a.activation(out=B.bexp_tm[:], in_=B.gc_tm[:], func=AF.Exp),
             reads=[K("gc_tm")], writes=[K("bexp_tm")])
        s.op("dve", lambda v: v.tensor_tensor(out=B.bexp_tm[:], in0=B.bexp_tm[:], in1=B.beta_tm[:], op=ALU.mult),
             reads=[K("bexp_tm"), K("beta_tm")], writes=[K("bexp_tm")])
        for hf in range(2):
            pt3, pk3 = ps_.next()
            mm(pt3[:, 0:NSB], cst[:, C_H0 + hf, :], B.g_tm[:], ["cst", K("g_tm")], pk3)
            s.op("act", lambda a, hf=hf, pt3=pt3: a.activation(out=B.glast[:, hf, :], in_=pt3[:, 0:NSB], func=AF.Exp),
                 reads=[pk3], writes=[K("glast")])
        s.op("dve", lambda v: v.memset(B.S[:], 0.0), writes=[K("S")])
        s.op("dve", lambda v: v.memset(B.Sb[:], 0.0), writes=[K("Sb")])

    def dir_pre(d, sg, par, q):
        B = DB[d]
        T = B.tmp[q]
        K = lambda n: "%s_%d" % (n, d)
        KT_ = lambda n: "%s_%s" % (n, T.sfx)
        U = cst[:, C_UF + d, :]
        NT = cst[:, C_NTF + d, :]
        NS = cst[:, C_NSF + d, :]
        ps_ = T.ps
        sgk = "_%d_%d" % (d, par)
        sbs = slice(sg * SEG, (sg + 1) * SEG)
        if q == 0:
            s.op("dve", lambda v: v.tensor_tensor(
                out=B.vb_sg[par][:], in0=v_tm[:, sbs, :], in1=B.beta_tm[:, sbs, None].broadcast_to([128, SEG, 128]),
                op=ALU.mult), reads=["v_tm", K("beta_tm")], writes=["vbs" + sgk])
            s.op("dve", lambda v: v.tensor_tensor(
                out=B.kbg_sg[par][:], in0=k_tm[:, sbs, :], in1=B.bexp_tm[:, sbs, None].broadcast_to([128, SEG, 128]),
                op=ALU.mult), reads=["k_tm", K("bexp_tm")], writes=["kbgs" + sgk])
        if q == NPRE - 1:
            for hf in range(2):
                s.op("dve", lambda v, hf=hf: v.tensor_tensor(
                    out=B.kd_sg[par][:, :, hf, :], in0=k_tm[:, sbs, :],
                    in1=B.ekd_tm[:, hf, sbs, None].broadcast_to([128, SEG, 128]), op=ALU.mult),
                     reads=["k_tm", K("ekd_tm")], writes=["kds" + sgk])
        assert SEG == 2 * NPRE
        si0 = 2 * q
        sb0 = sg * SEG + si0
        sbp = slice(sb0, sb0 + 2)
        c2 = slice(si0 * 128, (si0 + 2) * 128)
        J = [slice(0, 128), slice(128, 256)]
        tsl = [slice((sb0 + j) * 128, (sb0 + j + 1) * 128) for j in range(2)]
        kxs = ["_%d_%d_%d" % (d, par, si0 + j) for j in range(2)]
        v3 = lambda ap: ap.rearrange("p (a b) -> p a b", a=2)
        bc = lambda ap: ap[:, None, :].broadcast_to([128, 2, 128])

        def mm2(lhs, rhs, reads):
            pt, pk = ps_.next()
            s.group("pe", [lambda pe, j=j, pt=pt: pe.matmul(pt[:, J[j]], lhsT=lhs(j), rhs=rhs(j), start=True, stop=True)
                           for j in range(2)], reads=reads, writes=[pk])
            return pt, pk

        s.op("dve", lambda v: v.tensor_copy(out=v3(T.gbc[:]), in_=B.g_tm[:, sbp, None].broadcast_to([128, 2, 128])),
             reads=[K("g_tm")], writes=[KT_("gbc")])
        pg, kg = mm2(lambda j: T.gbc[:, J[j]], lambda j: U, [KT_("gbc"), "cst"])
        s.op("act", lambda a: a.activation(out=T.erow[:], in_=pg[:, 0:256], func=AF.Exp), reads=[kg], writes=[KT_("erow")])
        for j in range(2):
            s.op("dve", lambda v, j=j: v.scalar_tensor_tensor(
                out=T.dT[:, J[j]], in0=pg[:, J[j]], scalar=B.gc_tm[:, sb0 + j:sb0 + j + 1], in1=NT,
                op0=ALU.subtract, op1=ALU.add), reads=[kg, K("gc_tm"), "cst"], writes=[KT_("dT")])
        s.op("dve", lambda v: v.scalar_tensor_tensor(
            out=v3(T.dS[:]), in0=v3(pg[:, 0:256]), scalar=-1.0, in1=bc(NS), op0=ALU.mult, op1=ALU.add),
             reads=[kg, "cst"], writes=[KT_("dS")])
        s.op("act", lambda a: a.activation(out=T.dT[:], in_=T.dT[:], func=AF.Exp), reads=[KT_("dT")], writes=[KT_("dT")])
        for j in range(2):
            s.op("act", lambda a, j=j: a.activation(out=T.dS[:, J[j]], in_=T.dS[:, J[j]], func=AF.Exp,
                                                    bias=B.gc_tm[:, sb0 + j:sb0 + j + 1]),
                 reads=[KT_("dS"), K("gc_tm")], writes=[KT_("dS")])
        s.op("pool", lambda v: v.tensor_tensor(out=B.qd_sg[par][:, c2], in0=qT[:, sb0 * 128:(sb0 + 2) * 128], in1=T.erow[:],
                                               op=ALU.mult),
             reads=["qT", KT_("erow")], writes=["qd" + kxs[0], "qd" + kxs[1]])
        pk_, kk_ = mm2(lambda j: kTb[:, tsl[j]], lambda j: kTb[:, tsl[j]], ["kTb"])
        for j in range(2):
            s.op("dve", lambda v, j=j: v.scalar_tensor_tensor(
                out=T.PmT[0][:, J[j]], in0=pk_[:, J[j]], scalar=B.nbeta_tm[:, sb0 + j:sb0 + j + 1], in1=T.dS[:, J[j]],
                op0=ALU.mult, op1=ALU.mult), reads=[kk_, K("nbeta_tm"), KT_("dS")], writes=[KT_("PmT0")])
        pr, kr = mm2(lambda j: T.PmT[0][:, J[j]], lambda j: identb[:], [KT_("PmT0"), "identb"])
        s.op("act", lambda a: a.activation(out=T.Pm[0][:], in_=pr[:, 0:256], func=AF.Copy), reads=[kr], writes=[KT_("Pm0")])
        s.op("dve", lambda v: v.tensor_tensor(out=v3(T.X[:]), in0=v3(pr[:, 0:256]), in1=bc(ident), op=ALU.add),
             reads=[kr, "cst"], writes=[KT_("X")])
        pq_, kq_ = mm2(lambda j: kTb[:, tsl[j]], lambda j: qTb[:, tsl[j]], ["kTb", "qTb"])
        s.op("dve", lambda v: v.tensor_tensor(out=B.qk_sg[par][:, c2], in0=pq_[:, 0:256], in1=T.dT[:], op=ALU.mult),
             reads=[kq_, KT_("dT")], writes=["qk" + kxs[0], "qk" + kxs[1]])
        cur = 0
        for lvl in range(1, 6):
            nxt = 1 - cur
            pa, ka = mm2(lambda j, cur=cur: T.Pm[cur][:, J[j]], lambda j, cur=cur: T.PmT[cur][:, J[j]],
                         [KT_("Pm%d" % cur), KT_("PmT%d" % cur)])
            s.op("act", lambda a, pa=pa, nxt=nxt: a.activation(out=T.PmT[nxt][:], in_=pa[:, 0:256], func=AF.Copy),
                 reads=[ka], writes=[KT_("PmT%d" % nxt)])
            if lvl < 5:
                pb, kb = mm2(lambda j, cur=cur: T.PmT[cur][:, J[j]], lambda j, cur=cur: T.Pm[cur][:, J[j]],
                             [KT_("Pm%d" % cur), KT_("PmT%d" % cur)])
                if lvl % 2 == 0:
                    s.op("act", lambda a, pb=pb, nxt=nxt: a.activation(out=T.Pm[nxt][:], in_=pb[:, 0:256], func=AF.Copy),
                         reads=[kb], writes=[KT_("Pm%d" % nxt)])
                else:
                    s.op("dve", lambda v, pb=pb, nxt=nxt: v.tensor_copy(out=T.Pm[nxt][:], in_=pb[:, 0:256]),
                         reads=[kb], writes=[KT_("Pm%d" % nxt)])
            px, kxp = mm2(lambda j, nxt=nxt: T.PmT[nxt][:, J[j]], lambda j: T.X[:, J[j]], [KT_("PmT%d" % nxt), KT_("X")])
            s.op("dve", lambda v, px=px: v.tensor_tensor(out=T.X[:], in0=T.X[:], in1=px[:, 0:256], op=ALU.add),
                 reads=[kxp, KT_("X")], writes=[KT_("X")])
            cur = nxt
        pu, ku = mm2(lambda j: T.X[:, J[j]], lambda j: B.vb_sg[par][:, si0 + j, :], [KT_("X"), "vbs" + sgk])
        s.op("act", lambda a: a.activation(out=B.u_sg[par][:, c2], in_=pu[:, 0:256], func=AF.Copy),
             reads=[ku], writes=["u" + kxs[0], "u" + kxs[1]])
        pw, kw = mm2(lambda j: B.kbg_sg[par][:, si0 + j, :], lambda j: T.X[:, J[j]], [KT_("X"), "kbgs" + sgk])
        s.op("act", lambda a: a.activation(out=B.wT_sg[par][:, c2], in_=pw[:, 0:256], func=AF.Copy),
             reads=[kw], writes=["wT" + kxs[0], "wT" + kxs[1]])

    def dir_scan(d, sg, par):
        B = DB[d]
        K = lambda n: "%s_%d" % (n, d)
        ps_ = B.scan_ps
        oT = oTd[d]
        ok = "oT%d" % d
        si_order = range(SEG) if d == 0 else range(SEG - 1, -1, -1)
        for si in si_order:
            sb = sg * SEG + si
            kx = "_%d_%d_%d" % (d, par, si)
            for hf in ((0, 1) if d == 0 else (1, 0)):
                r = slice(hf * 64, (hf + 1) * 64)
                tok = slice(sb * 128 + hf * 64, sb * 128 + (hf + 1) * 64)
                p1, k1 = ps_.next()
                mm(p1[:, 0:128], B.wT_sg[par][:, si * 128:(si + 1) * 128], B.Sb[:], ["wT" + kx, K("Sb")], k1)
                s.op("dve", lambda v, p1=p1, r=r, si=si: v.tensor_tensor(out=B.vnew[r, :], in0=B.u_sg[par][r, si * 128:(si + 1) * 128],
                                                                        in1=p1[r, 0:128], op=ALU.subtract),
                     reads=[k1, "u" + kx], writes=[K("vnew")])
                po, ko = ps_.next()
                s.group("pe", [
                    lambda pe, po=po, si=si, hf=hf: pe.matmul(po[:, 0:64], lhsT=B.Sb[:],
                                                              rhs=B.qd_sg[par][:, si * 128 + hf * 64:si * 128 + (hf + 1) * 64],
                                                            start=True, stop=False),
                    lambda pe, po=po, si=si, hf=hf: pe.matmul(po[:, 0:64], lhsT=B.vnew[:],
                                                              rhs=B.qk_sg[par][:, si * 128 + hf * 64:si * 128 + (hf + 1) * 64],
                                                            start=False, stop=True),
                    lambda pe, po=po, si=si, hf=hf: pe.matmul(po[:, 128:256], lhsT=B.kd_sg[par][:, si, hf, :], rhs=B.vnew[:],
                                                              start=True, stop=True)],
                    reads=[K("Sb"), "qd" + kx, K("vnew"), "qk" + kx, "kds_%d_%d" % (d, par)], writes=[ko])
                s.op("dve", lambda v, po=po, hf=hf, sb=sb: v.scalar_tensor_tensor(
                    out=B.S[:], in0=B.S[:], scalar=B.glast[:, hf, sb:sb + 1], in1=po[:, 128:256],
                    op0=ALU.mult, op1=ALU.add), reads=[ko, K("S"), K("glast")], writes=[K("S")])
                s.op("act", lambda a: a.activation(out=B.Sb[:], in_=B.S[:], func=AF.Copy), reads=[K("S")], writes=[K("Sb")])
                s.op("act", lambda a, po=po, tok=tok: a.activation(out=oT[:, tok], in_=po[:, 0:64], func=AF.Copy),
                     reads=[ko], writes=[ok])

    def deferred(fn, *a):
        ch = []
        s.chain = ch
        try:
            fn(*a)
        finally:
            s.chain = None
        return ch

    nseg = NSB // SEG
    for h in range(NH):
        s.dma("sp", st_hp, hp[:], hp_d[h], writes=["hp"])
        s.op("act", lambda a: a.activation(out=hq[:, 0:2], in_=hp[:, 16:18], func=AF.Exp), reads=["hp"], writes=["hq"])
        s.op("dve", lambda v: v.tensor_scalar(out=hq[:, 0:2], in0=hq[:, 0:2], scalar1=-1.0, scalar2=None,
                                              op0=ALU.mult), reads=["hq"], writes=["hq"])
        conv_silu(qkv_d[h, 0], 0, qT, "qT")
        l2norm_inplace(qT, "qT", 128.0 ** -0.5)
        s.op("act", lambda a: a.activation(out=qTb[:], in_=qT[:], func=AF.Copy), reads=["qT"], writes=["qTb"])
        conv_silu(qkv_d[h, 1], 5, kT, "kT")
        l2norm_inplace(kT, "kT", 1.0)
        s.op("act", lambda a: a.activation(out=kTb[:], in_=kT[:], func=AF.Copy), reads=["kT"], writes=["kTb"])
        to_tm(kT, "kT", k_tm, "k_tm")
        conv_silu(qkv_d[h, 2], 10, oTd[0], "oT0")
        to_tm(oTd[0], "oT0", v_tm, "v_tm")
        s.run_chains([deferred(dir_setup, h, 0), deferred(dir_setup, h, 1)])
        order = [list(range(nseg // 2 if own_half else nseg)), list(range(nseg - 1, -1, -1))]
        for ph in range(nseg + 1):
            chains = []
            for d in range(2):
                if ph >= 1 and ph - 1 < len(order[d]):
                    chains.append(deferred(dir_scan, d, order[d][ph - 1], (ph - 1) % 2))
                if ph < len(order[d]):
                    for q in range(NPRE):
                        chains.append(deferred(dir_pre, d, order[d][ph], ph % 2, q))
            s.run_chains(chains)
        NOUT = SEQ // 2 if own_half else SEQ
        s.dma("sp", st_in, upad[:, 2:NOUT + 2], z_d[h][:, 0:NOUT], writes=["upad"])
        s.op("act", lambda a: a.activation(out=upad[:, 2:NOUT + 2], in_=upad[:, 2:NOUT + 2], func=AF.Silu),
             reads=["upad"], writes=["upad"])
        for tb in range(NOUT // 512):
            sl = slice(tb * 512, (tb + 1) * 512)
            sl2 = slice(tb * 512 + 2, (tb + 1) * 512 + 2)
            s.op("dve", lambda v, sl=sl: v.tensor_tensor(out=oTd[0][:, sl], in0=oTd[0][:, sl], in1=oTd[1][:, sl], op=ALU.add),
                 reads=["oT0", "oT1"], writes=["oT0"])
            s.op("act", lambda a, sl=sl: a.activation(out=sm[:], in_=oTd[0][:, sl], func=AF.Square), reads=["oT0"], writes=["sm"])
            pt, pk = psr.next()
            mm(pt[:], ones, sm[:], ["sm", "cst"], pk)
            s.op("dve", lambda v, pt=pt: v.tensor_scalar(out=sm2[:], in0=pt[:], scalar1=1.0 / 128.0, scalar2=float(RMS_EPS),
                                                         op0=ALU.mult, op1=ALU.add), reads=[pk], writes=["sm2"])
            s.op("act", lambda a: a.activation(out=sm2[:], in_=sm2[:], func=AF.Ln), reads=["sm2"], writes=["sm2"])
            s.op("act", lambda a: a.activation(out=sm2[:], in_=sm2[:], func=AF.Exp, scale=-0.5), reads=["sm2"], writes=["sm2"])
            s.op("dve", lambda v, sl=sl: v.tensor_tensor(out=sm2[:], in0=sm2[:], in1=oTd[0][:, sl], op=ALU.mult),
                 reads=["sm2", "oT0"], writes=["sm2"])
            s.op("dve", lambda v, sl=sl, sl2=sl2: v.scalar_tensor_tensor(out=oTd[0][:, sl], in0=sm2[:], scalar=hp[:, 15:16],
                                                                        in1=upad[:, sl2], op0=ALU.mult, op1=ALU.mult),
                 reads=["sm2", "hp", "upad"], writes=["oT0"])
        s.dma("sp", st_out, out_d[h][:, 0:NOUT], oTd[0][:, 0:NOUT], reads=["oT0"])
    return c.close()
```
